# Optimizing a Trainium2 kernel written in Bass

```python
import jax
import jax.numpy as jnp
from jax import lax
import numpy as np


D_MODEL = 1024
BATCH = 2
SEQ = 8192
DEPTH = 2

GRID_W = 64
EPS = 1e-6
ROPE_THETA = 10000.0
BLOCK = 128
HEAD_DIM = 64
A_Q_HEADS = 8
A_KV_HEADS = 2
WINDOW = 128
B_Q_HEADS = 8
B_KV_HEADS = 2
AB_IN_WIDTHS = (A_Q_HEADS * HEAD_DIM, A_KV_HEADS * HEAD_DIM, A_KV_HEADS * HEAD_DIM,
                B_Q_HEADS * HEAD_DIM, B_KV_HEADS * HEAD_DIM, B_KV_HEADS * HEAD_DIM)
AB_IN = sum(AB_IN_WIDTHS)
AB_OUT = (A_Q_HEADS + B_Q_HEADS) * HEAD_DIM
C_HEADS = 8
C_DK = 128
C_DV = 128
C_CONV = 5
C_CHUNK = 64
C_QK_W = C_HEADS * C_DK
C_V_W = C_HEADS * C_DV
C_IN_WIDTHS = (C_QK_W, C_QK_W, C_V_W, C_V_W, C_HEADS, C_HEADS, C_HEADS, C_HEADS)
C_IN = sum(C_IN_WIDTHS)
C_CONV_W = 2 * C_QK_W + C_V_W
D_FF = 3584
N_EXPERTS = 8
TOP_K = 2
D_FF_EXPERT = 3584
N_EVEN = (DEPTH + 1) // 2
N_ODD = DEPTH // 2

kernel_name = "hybrid_swa_axial_gdn_moe_encoder"

F32 = jnp.float32


def split_cols(t, widths):
    out, start = [], 0
    for w in widths:
        out.append(t[..., start:start + w])
        start += w
    return out


def rmsnorm(x, g):
    xf = x.astype(F32)
    y = xf * lax.rsqrt(jnp.mean(xf * xf, axis=-1, keepdims=True) + EPS)
    return (y * g.astype(F32)).astype(x.dtype)


def l2norm(x):
    return x * lax.rsqrt(jnp.sum(x * x, axis=-1, keepdims=True) + EPS)


def rope_tables(pos, dim):
    inv = ROPE_THETA ** (-jnp.arange(0, dim, 2, dtype=F32) / dim)
    ang = pos.astype(F32)[:, None] * inv[None, :]
    return jnp.cos(ang), jnp.sin(ang)


def apply_rope(x, cos, sin):
    xf = x.astype(F32)
    x1, x2 = jnp.split(xf, 2, axis=-1)
    c = cos[None, :, None, :]
    s = sin[None, :, None, :]
    return jnp.concatenate([x1 * c - x2 * s, x2 * c + x1 * s], axis=-1).astype(x.dtype)


def window_attention(q, k, v, sink):
    Bn, S, Hq, D = q.shape
    Hkv = k.shape[2]
    G = Hq // Hkv
    nb = S // BLOCK
    qb = q.reshape(Bn, nb, BLOCK, Hkv, G, D)
    pad = ((0, 0), (BLOCK, BLOCK), (0, 0), (0, 0))
    kp = jnp.pad(k, pad).reshape(Bn, nb + 2, BLOCK, Hkv, D)
    vp = jnp.pad(v, pad).reshape(Bn, nb + 2, BLOCK, Hkv, D)
    kb = jnp.concatenate([kp[:, :-2], kp[:, 1:-1], kp[:, 2:]], axis=2)
    vb = jnp.concatenate([vp[:, :-2], vp[:, 1:-1], vp[:, 2:]], axis=2)
    s = jnp.einsum('bnqhgd,bnkhd->bnhgqk', qb, kb).astype(F32) * (D ** -0.5)
    qpos = jnp.arange(nb)[:, None] * BLOCK + jnp.arange(BLOCK)[None, :]
    kpos = jnp.arange(nb)[:, None] * BLOCK - BLOCK + jnp.arange(3 * BLOCK)[None, :]
    rel = qpos[:, :, None] - kpos[:, None, :]
    valid = (jnp.abs(rel) <= WINDOW) & (kpos[:, None, :] >= 0) & (kpos[:, None, :] < S)
    s = jnp.where(valid[None, :, None, None], s, -jnp.inf)
    sink_col = jnp.broadcast_to(sink.astype(F32).reshape(Hkv, G)[None, None, :, :, None, None],
                                s.shape[:-1] + (1,))
    p = jax.nn.softmax(jnp.concatenate([s, sink_col], axis=-1), axis=-1)[..., :-1]
    o = jnp.einsum('bnhgqk,bnkhd->bnqhgd', p.astype(v.dtype), vb)
    return o.reshape(Bn, S, Hq, D)


def global_attention(q, k, v):
    Bn, S, Hq, D = q.shape
    Hkv = k.shape[2]
    G = Hq // Hkv
    nb = S // BLOCK
    qb = jnp.moveaxis(q.reshape(Bn, nb, BLOCK, Hkv, G, D), 1, 0)
    scale = D ** -0.5

    def one_block(qi):
        s = jnp.einsum('bqhgd,bkhd->bhgqk', qi, k).astype(F32) * scale
        p = jax.nn.softmax(s, axis=-1)
        return jnp.einsum('bhgqk,bkhd->bqhgd', p.astype(v.dtype), v)

    o = lax.map(one_block, qb)
    return jnp.moveaxis(o, 0, 1).reshape(Bn, S, Hq, D)


def mixer_ab(h, w_in, qn_a, kn_a, sink_a, qn_b, kn_b, w_out, rope_1d, rope_row, rope_col):
    Bn, S, _ = h.shape
    qa, ka, va, qb, kb, vb = split_cols(h @ w_in, AB_IN_WIDTHS)
    qa = qa.reshape(Bn, S, A_Q_HEADS, HEAD_DIM)
    ka = ka.reshape(Bn, S, A_KV_HEADS, HEAD_DIM)
    va = va.reshape(Bn, S, A_KV_HEADS, HEAD_DIM)
    qb = qb.reshape(Bn, S, B_Q_HEADS, HEAD_DIM)
    kb = kb.reshape(Bn, S, B_KV_HEADS, HEAD_DIM)
    vb = vb.reshape(Bn, S, B_KV_HEADS, HEAD_DIM)
    qa = apply_rope(rmsnorm(qa, qn_a), *rope_1d)
    ka = apply_rope(rmsnorm(ka, kn_a), *rope_1d)
    half = HEAD_DIM // 2
    qb = rmsnorm(qb, qn_b)
    kb = rmsnorm(kb, kn_b)
    qb = jnp.concatenate([apply_rope(qb[..., :half], *rope_row), apply_rope(qb[..., half:], *rope_col)], axis=-1)
    kb = jnp.concatenate([apply_rope(kb[..., :half], *rope_row), apply_rope(kb[..., half:], *rope_col)], axis=-1)
    oa = window_attention(qa, ka, va, sink_a)
    ob = global_attention(qb, kb, vb)
    o = jnp.concatenate([oa, ob], axis=2).reshape(Bn, S, AB_OUT)
    return o @ w_out


def centered_conv(x, w):
    pad = (w.shape[0] - 1) // 2
    return lax.conv_general_dilated(x, w[:, None, :], window_strides=(1,), padding=[(pad, pad)],
                                    dimension_numbers=('NWC', 'WIO', 'NWC'),
                                    feature_group_count=x.shape[-1])


def gated_delta_chunked(q, k, v, g, beta):
    Bn, S, H, Dk = q.shape
    Dv = v.shape[-1]
    n = S // C_CHUNK

    def to_chunks(t):
        return t.reshape(Bn, n, C_CHUNK, H, -1).transpose(0, 3, 1, 2, 4)

    q, k, v = to_chunks(q), to_chunks(k), to_chunks(v)
    g = g.reshape(Bn, n, C_CHUNK, H).transpose(0, 3, 1, 2)
    beta = beta.reshape(Bn, n, C_CHUNK, H).transpose(0, 3, 1, 2)
    g = jnp.cumsum(g, axis=-1)
    kb = k * beta[..., None]
    vb = v * beta[..., None]
    incl = jnp.tril(jnp.ones((C_CHUNK, C_CHUNK), bool))
    strict = jnp.tril(jnp.ones((C_CHUNK, C_CHUNK), bool), -1)
    decay = jnp.exp(jnp.where(incl, g[..., :, None] - g[..., None, :], -jnp.inf))
    m = jnp.where(strict, jnp.einsum('bhncd,bhnkd->bhnck', kb, k) * decay, 0.0)
    a = m + jnp.eye(C_CHUNK, dtype=F32)
    rhs = jnp.concatenate([vb, kb * jnp.exp(g)[..., None]], axis=-1)
    sol = lax.linalg.triangular_solve(a, rhs, left_side=True, lower=True)
    u, w = sol[..., :Dv], sol[..., Dv:]
    qk = jnp.where(incl, jnp.einsum('bhncd,bhnkd->bhnck', q, k) * decay, 0.0)
    xs = tuple(jnp.moveaxis(t, 2, 0) for t in (q, k, u, w, g, qk))

    def step(state, inp):
        qi, ki, ui, wi, gi, qki = inp
        v_new = ui - jnp.matmul(wi, state)
        o = jnp.matmul(qi * jnp.exp(gi)[..., None], state) + jnp.matmul(qki, v_new)
        g_last = gi[..., -1]
        state = state * jnp.exp(g_last)[..., None, None] + jnp.einsum(
            'bhcd,bhce->bhde', ki * jnp.exp(g_last[..., None] - gi)[..., None], v_new)
        return state, o

    state0 = jnp.zeros((Bn, H, Dk, Dv), F32)
    _, o = lax.scan(step, state0, xs)
    return o.transpose(1, 0, 3, 2, 4).reshape(Bn, S, H, Dv)


def mixer_c(h, w_in, conv_w, a_log_f, dt_bias_f, a_log_b, dt_bias_b, out_norm, w_out):
    Bn, S, _ = h.shape
    proj = h @ w_in
    qkv = proj[..., :C_CONV_W]
    z, a_f, b_f, a_b, b_b = split_cols(proj[..., C_CONV_W:], C_IN_WIDTHS[3:])
    qkv = jax.nn.silu(centered_conv(qkv, conv_w)).astype(F32)
    q, k, v = split_cols(qkv, (C_QK_W, C_QK_W, C_V_W))
    q = l2norm(q.reshape(Bn, S, C_HEADS, C_DK)) * (C_DK ** -0.5)
    k = l2norm(k.reshape(Bn, S, C_HEADS, C_DK))
    v = v.reshape(Bn, S, C_HEADS, C_DV)
    g_f = -jnp.exp(a_log_f.astype(F32)) * jax.nn.softplus(a_f.astype(F32) + dt_bias_f.astype(F32))
    g_b = -jnp.exp(a_log_b.astype(F32)) * jax.nn.softplus(a_b.astype(F32) + dt_bias_b.astype(F32))
    beta_f = jax.nn.sigmoid(b_f.astype(F32))
    beta_b = jax.nn.sigmoid(b_b.astype(F32))
    o_f = gated_delta_chunked(q, k, v, g_f, beta_f)
    o_b = gated_delta_chunked(q[:, ::-1], k[:, ::-1], v[:, ::-1], g_b[:, ::-1], beta_b[:, ::-1])[:, ::-1]
    o = o_f + o_b
    o = rmsnorm(o, out_norm) * jax.nn.silu(z.astype(F32).reshape(Bn, S, C_HEADS, C_DV))
    return o.reshape(Bn, S, C_V_W).astype(h.dtype) @ w_out


def swiglu(h, w_gate, w_up, w_down):
    return (jax.nn.silu(h @ w_gate) * (h @ w_up)) @ w_down


def moe_swiglu(h, w_router, w_gate, w_up, w_down):
    logits = (h @ w_router).astype(F32)
    top_v, top_i = lax.top_k(logits, TOP_K)
    gates = jax.nn.softmax(top_v, axis=-1)
    combine = jnp.sum(jax.nn.one_hot(top_i, N_EXPERTS, dtype=F32) * gates[..., None], axis=-2)
    y = jnp.zeros_like(h)
    for e in range(N_EXPERTS):
        y = y + combine[..., e:e + 1].astype(h.dtype) * swiglu(h, w_gate[e], w_up[e], w_down[e])
    return y


def setup_inputs(seed: int = 0) -> dict:
    key = jax.random.key(seed)
    ks = iter(jax.random.split(key, 64))

    def nrm(shape, scale):
        return jax.random.normal(next(ks), shape, F32) * scale

    def gain(shape):
        return 1.0 + nrm(shape, 0.02)

    def dt_bias(shape):
        dt = jnp.exp(jax.random.uniform(next(ks), shape, F32, minval=np.log(1e-3), maxval=np.log(1e-1)))
        return dt + jnp.log(-jnp.expm1(-dt))

    def a_log(shape):
        return jnp.log(jax.random.uniform(next(ks), shape, F32, minval=1.0, maxval=16.0))

    d = D_MODEL
    return {
        "x": nrm((BATCH, SEQ, d), 1.0),
        "ab_norm": gain((N_EVEN, d)),
        "ab_w_in": nrm((N_EVEN, d, AB_IN), d ** -0.5),
        "ab_q_norm_a": gain((N_EVEN, HEAD_DIM)),
        "ab_k_norm_a": gain((N_EVEN, HEAD_DIM)),
        "ab_sink_a": nrm((N_EVEN, A_Q_HEADS), 0.5),
        "ab_q_norm_b": gain((N_EVEN, HEAD_DIM)),
        "ab_k_norm_b": gain((N_EVEN, HEAD_DIM)),
        "ab_w_out": nrm((N_EVEN, AB_OUT, d), AB_OUT ** -0.5),
        "ffn_norm": gain((N_EVEN, d)),
        "ffn_w_gate": nrm((N_EVEN, d, D_FF), d ** -0.5),
        "ffn_w_up": nrm((N_EVEN, d, D_FF), d ** -0.5),
        "ffn_w_down": nrm((N_EVEN, D_FF, d), D_FF ** -0.5),
        "c_norm": gain((N_ODD, d)),
        "c_w_in": nrm((N_ODD, d, C_IN), d ** -0.5),
        "c_conv": nrm((N_ODD, C_CONV, C_CONV_W), C_CONV ** -0.5),
        "c_a_log_fwd": a_log((N_ODD, C_HEADS)),
        "c_dt_bias_fwd": dt_bias((N_ODD, C_HEADS)),
        "c_a_log_bwd": a_log((N_ODD, C_HEADS)),
        "c_dt_bias_bwd": dt_bias((N_ODD, C_HEADS)),
        "c_out_norm": gain((N_ODD, C_DV)),
        "c_w_out": nrm((N_ODD, C_V_W, d), C_V_W ** -0.5),
        "moe_norm": gain((N_ODD, d)),
        "moe_w_router": nrm((N_ODD, d, N_EXPERTS), d ** -0.5),
        "moe_w_gate": nrm((N_ODD, N_EXPERTS, d, D_FF_EXPERT), d ** -0.5),
        "moe_w_up": nrm((N_ODD, N_EXPERTS, d, D_FF_EXPERT), d ** -0.5),
        "moe_w_down": nrm((N_ODD, N_EXPERTS, D_FF_EXPERT, d), D_FF_EXPERT ** -0.5),
    }


def reference(x, ab_norm, ab_w_in, ab_q_norm_a, ab_k_norm_a, ab_sink_a, ab_q_norm_b, ab_k_norm_b,
              ab_w_out, ffn_norm, ffn_w_gate, ffn_w_up, ffn_w_down, c_norm, c_w_in, c_conv,
              c_a_log_fwd, c_dt_bias_fwd, c_a_log_bwd, c_dt_bias_bwd, c_out_norm, c_w_out,
              moe_norm, moe_w_router, moe_w_gate, moe_w_up, moe_w_down):
    S = x.shape[1]
    ROWS = S // GRID_W
    t = jnp.arange(S)
    row = jnp.repeat(jnp.arange(ROWS), GRID_W)
    col = jnp.tile(jnp.arange(GRID_W), ROWS)
    rope_1d = rope_tables(t, HEAD_DIM)
    rope_row = rope_tables(row, HEAD_DIM // 2)
    rope_col = rope_tables(col, HEAD_DIM // 2)
    for layer in range(DEPTH):
        i = layer // 2
        if layer % 2 == 0:
            x = x + mixer_ab(rmsnorm(x, ab_norm[i]), ab_w_in[i], ab_q_norm_a[i], ab_k_norm_a[i],
                             ab_sink_a[i], ab_q_norm_b[i], ab_k_norm_b[i], ab_w_out[i],
                             rope_1d, rope_row, rope_col)
            x = x + swiglu(rmsnorm(x, ffn_norm[i]), ffn_w_gate[i], ffn_w_up[i], ffn_w_down[i])
        else:
            x = x + mixer_c(rmsnorm(x, c_norm[i]), c_w_in[i], c_conv[i], c_a_log_fwd[i], c_dt_bias_fwd[i],
                            c_a_log_bwd[i], c_dt_bias_bwd[i], c_out_norm[i], c_w_out[i])
            x = x + moe_swiglu(rmsnorm(x, moe_norm[i]), moe_w_router[i], moe_w_gate[i], moe_w_up[i],
                               moe_w_down[i])
    return x
```

```python
import numpy as np
import concourse.bass as bass
import concourse.mybir as mybir
from concourse.bass_utils import run_bass_kernel_spmd
from contextlib import ExitStack

F32 = mybir.dt.float32
BF16 = mybir.dt.bfloat16
I32 = mybir.dt.int32
ALU = mybir.AluOpType
AF = mybir.ActivationFunctionType
AX = mybir.AxisListType

ENGS = ["tensor", "vector", "scalar", "gpsimd", "sync"]
NCORES = 8


class _Op:
    __slots__ = ("eng", "seq", "fn", "deps", "signal", "count", "dma", "dsem", "dcum", "snap")

    def __init__(self, eng, seq, fn, dma):
        self.eng = eng
        self.seq = seq
        self.fn = fn
        self.deps = []
        self.signal = False
        self.count = 0
        self.dma = dma
        self.dsem = -1
        self.dcum = 0
        self.snap = None


class Lazy:
    def __init__(self, P):
        self.P = P
        self.q = []

    def __getattr__(self, name):
        f = getattr(self.P, name)

        def rec(*a, **k):
            self.q.append(lambda: f(*a, **k))
        return rec


def interleave(lazies):
    n = max(len(L.q) for L in lazies)
    for i in range(n):
        for L in lazies:
            if i < len(L.q):
                L.q[i]()


class Prog:
    DMA_RING = 12

    def __init__(self):
        self.nc = bass.Bass("TRN2", target_bir_lowering=False)
        self.es = ExitStack()
        self.ops = {e: [] for e in ENGS}
        self.lastw = {}
        self.readers = {}
        self.known = {e: {} for e in ENGS}
        self.knownd = {e: {} for e in ENGS}
        self.dmas = []
        self.ring_last = {}
        self.ring_cum = {}
        self.ring_next = {e: 0 for e in ENGS}
        self.n_sb = 0
        self.alias = {}
        self.scopes = []
        self.fence_t = self.es.enter_context(self.nc.sbuf_tensor("fence_t", [128, 64], BF16))
        self.nfence = 0

    def push_scope(self):
        self.scopes.append(self.es)
        self.es = ExitStack()

    def pop_scope(self):
        self.es.close()
        self.es = self.scopes.pop()
        self.alias = {}

    def fence(self):
        toks = []
        for e in ENGS:
            if self.ops[e] and not self.ops[e][-1].dma:
                toks.append(("e", e, self.ops[e][-1].seq))
        for key, idx in self.ring_last.items():
            toks.append(("d", idx))
        ft = self.fence_t
        n = self.nfence
        self.nfence += 1
        self.push_scope()
        pf = self.ps([128, 512], F32, f"fence_ps{n}")
        self.op("vector", lambda e: e.memset(ft[:, 0:16], 0.0), extra=toks)
        self.op("scalar", lambda e: e.copy(out=ft[:, 16:32], in_=ft[:, 16:32]), extra=toks)
        self.op("gpsimd", lambda e: e.memset(ft[:, 32:48], 0.0), extra=toks)
        self.op("tensor", lambda e: e.matmul(pf[0:1, 0:8], lhsT=ft[0:1, 48:49], rhs=ft[0:1, 48:56], start=True, stop=True),
                extra=toks)
        self.op("sync", lambda e: e.dma_start(out=ft[0:1, 56:60], in_=ft[0:1, 60:64]), extra=toks, dma=True)
        self.scopes_tmp = None
        self.es.close()
        self.es = self.scopes.pop()

    def bank(self, key, bank_id):
        self.alias[key] = ("BANK", bank_id)

    def sb(self, shape, dt, name=None):
        self.n_sb += 1
        return self.es.enter_context(self.nc.sbuf_tensor(f"sb{self.n_sb}_{name or ''}", list(shape), dt))

    def ps(self, shape, dt, name=None):
        self.n_sb += 1
        return self.es.enter_context(self.nc.psum_tensor(f"ps{self.n_sb}_{name or ''}", list(shape), dt))

    def dram(self, name, shape, dt, kind):
        return self.nc.dram_tensor(name, list(shape), dt, kind=kind)

    def _dep_tokens(self, reads, writes):
        deps = set()
        for k in reads:
            w = self.lastw.get(k)
            if w is not None:
                deps.add(w)
        for k in writes:
            w = self.lastw.get(k)
            if w is not None:
                deps.add(w)
            for r in self.readers.get(k, ()):
                deps.add(r)
        return deps

    def _need(self, eng, tok):
        if tok[0] == "e":
            _, se, sq = tok
            if se == eng and eng in ("tensor", "sync"):
                return False
            return self.known[eng].get(se, -1) < sq
        else:
            d = self.dmas[tok[1]]
            return self.knownd[eng].get(d.dsem, 0) < d.dcum

    def _learn(self, eng, tok):
        if tok[0] == "e":
            _, se, sq = tok
            src = self.ops[se][sq]
            src.signal = True
            k = self.known[eng]
            if k.get(se, -1) < sq:
                k[se] = sq
            if src.snap is not None:
                sk, sd = src.snap
                for a, b in sk.items():
                    if k.get(a, -1) < b:
                        k[a] = b
                kd = self.knownd[eng]
                for a, b in sd.items():
                    if kd.get(a, 0) < b:
                        kd[a] = b
        else:
            d = self.dmas[tok[1]]
            kd = self.knownd[eng]
            if kd.get(d.dsem, 0) < d.dcum:
                kd[d.dsem] = d.dcum

    def op(self, eng, fn, reads=(), writes=(), dma=False, extra=()):
        al = self.alias
        if al:
            excl = {al[k] for k in reads if k in al} | {al[k] for k in writes if k in al}
            reads = [k for k in reads if k not in al]
            writes = [k for k in writes if k not in al] + list(excl)
        deps = self._dep_tokens(reads, writes)
        deps.update(extra)
        seq = len(self.ops[eng])
        o = _Op(eng, seq, fn, dma)
        if dma:
            slot = self.ring_next[eng]
            self.ring_next[eng] = (slot + 1) % self.DMA_RING
            key = (eng, slot)
            prev = self.ring_last.get(key)
            if prev is not None:
                deps.add(("d", prev))
            o.dsem = key
            self.ring_cum[key] = self.ring_cum.get(key, 0) + 16
            o.dcum = self.ring_cum[key]
        for tok in sorted(deps, key=lambda t: (t[0], str(t[1]), t[2] if len(t) > 2 else 0)):
            if self._need(eng, tok):
                o.deps.append(tok)
                self._learn(eng, tok)
        o.snap = (dict(self.known[eng]), dict(self.knownd[eng]))
        self.ops[eng].append(o)
        if dma:
            idx = len(self.dmas)
            self.dmas.append(o)
            self.ring_last[o.dsem] = idx
            tok = ("d", idx)
        else:
            tok = ("e", eng, seq)
        for k in writes:
            self.lastw[k] = tok
            self.readers[k] = []
        for k in reads:
            if k not in writes:
                self.readers.setdefault(k, []).append(tok)
        return tok

    def pe(self, fn, reads=(), writes=()):
        return self.op("tensor", fn, reads, writes)

    def dve(self, fn, reads=(), writes=()):
        return self.op("vector", fn, reads, writes)

    def act(self, fn, reads=(), writes=()):
        return self.op("scalar", fn, reads, writes)

    def pool(self, fn, reads=(), writes=()):
        return self.op("gpsimd", fn, reads, writes)

    def dma(self, out, in_, reads=(), writes=(), q="sync", **kw):
        return self.op(q, lambda e: e.dma_start(out=out, in_=in_, **kw), reads, writes, dma=True)

    def build(self):
        nc = self.nc
        es = self.es
        esem = {e: es.enter_context(nc.semaphore(f"s_{e}")) for e in ENGS}
        dsem = {}
        for key in self.ring_cum:
            dsem[key] = es.enter_context(nc.semaphore(f"d_{key[0]}_{key[1]}"))
        for e in ENGS:
            c = 0
            for o in self.ops[e]:
                if o.signal and not o.dma:
                    c += 1
                o.count = c
        block = es.enter_context(nc.Block())
        ops = self.ops
        dmas = self.dmas
        ring_cum = self.ring_cum

        def emit(engname):
            def body(eng):
                for o in ops[engname]:
                    for tok in o.deps:
                        if tok[0] == "e":
                            eng.wait_ge(esem[tok[1]], ops[tok[1]][tok[2]].count)
                        else:
                            d = dmas[tok[1]]
                            eng.wait_ge(dsem[d.dsem], d.dcum)
                    ins = o.fn(eng)
                    if o.dma:
                        ins.then_inc(dsem[o.dsem], 16)
                    elif o.signal:
                        ins.then_inc(esem[engname], 1)
                for key, cum in ring_cum.items():
                    if key[0] == engname:
                        eng.wait_ge(dsem[key], cum)
            return body

        for e in ENGS:
            if not ops[e]:
                continue
            getattr(block, e)(emit(e))
        es.close()
        return nc


EPS = 1e-6


def bcast_rows(handle, ncols, nparts=128, offset=0):
    return bass.AP(handle, offset, [[0, nparts], [1, ncols]])


def emit_norm_T(P, x_ap, xkey, g_bc, dstT, dst_cols, dkey, W, uid):
    nc = P.nc
    xn = W["xn"][uid % 2]
    kxn = ("xn", uid % 2)
    psT = W["psT"]
    if g_bc is not None:
        junk = W["junk"]
        ss = W["ss"][uid % 2]
        kss = ("ss", uid % 2)
        P.act(lambda e: e.activation(out=junk[:], in_=x_ap, func=AF.Square, accum_out=ss[:, 0:1]),
              reads=[xkey], writes=[kss, "junk"])
        P.act(lambda e: e.activation(out=ss[:, 1:2], in_=ss[:, 0:1], func=AF.Sqrt,
                                     scale=1.0 / 1024.0, bias=W["epsc"][:, 0:1]),
              reads=[kss], writes=[kss])
        P.dve(lambda e: e.reciprocal(out=ss[:, 2:3], in_=ss[:, 1:2]), reads=[kss], writes=[kss])
        P.dve(lambda e: e.scalar_tensor_tensor(out=xn[:], in0=x_ap, scalar=ss[:, 2:3], in1=g_bc[:],
                                               op0=ALU.mult, op1=ALU.mult),
              reads=[xkey, kss, "gbc"], writes=[kxn])
    else:
        P.dve(lambda e: e.tensor_copy(out=xn[:], in_=x_ap), reads=[xkey], writes=[kxn])

    def tr(e):
        ins = None
        for kc in range(8):
            ins = e.transpose(out=psT[:, kc * 128:(kc + 1) * 128], in_=xn[:, kc * 128:(kc + 1) * 128],
                              identity=W["ident"][:])
        return ins
    P.pe(tr, reads=[kxn, "ident"], writes=["psT"])
    P.act(lambda e: e.copy(out=dstT[:, :, dst_cols], in_=psT[:].rearrange("p (k c) -> p k c", k=8)),
          reads=["psT"], writes=[dkey])


def alloc_norm_work(P):
    W = {}
    W["xn"] = [P.sb([128, 1024], BF16) for _ in range(2)]
    W["junk"] = P.sb([128, 1024], BF16)
    W["ss"] = [P.sb([128, 4], F32) for _ in range(2)]
    W["psT"] = P.ps([128, 1024], BF16)
    W["ident"] = P.sb([128, 128], BF16)
    W["epsc"] = P.sb([128, 1], F32)
    return W


NTOK = 2048
NT = NTOK // 128
DM = 1024
DFF = 3584
NF = DFF // 128
FR = 7


def ffn_handles(P, pf, n_exp, with_proj):
    H = {}
    H["g"] = P.dram(pf + "g", [1, DM], F32, "ExternalInput")
    H["wg"] = P.dram(pf + "wg", [n_exp, DM, DFF], F32, "ExternalInput")
    H["wu"] = P.dram(pf + "wu", [n_exp, DM, DFF], F32, "ExternalInput")
    H["wd"] = P.dram(pf + "wd", [n_exp, DFF, DM], F32, "ExternalInput")
    H["ident"] = P.dram(pf + "ident", [128, 128], BF16, "ExternalInput")
    if n_exp > 1:
        H["wr"] = P.dram(pf + "wr", [DM, n_exp], F32, "ExternalInput")
        H["identf"] = P.dram(pf + "identf", [128, 128], F32, "ExternalInput")
    if with_proj:
        H["wo"] = P.dram(pf + "wo", [DM, DM], F32, "ExternalInput")
    return H


def build_ffn(n_exp, with_proj, P=None, pf="", x_handle=None, o_handle=None, H=None, x_row0=0, y_handle=None, y_row0=0, dyn=None):
    standalone = P is None
    if standalone:
        P = Prog()
    nc = P.nc
    if not standalone:
        P.push_scope()
    x_d = x_handle if x_handle is not None else P.dram(pf + "x", [NTOK, DM], F32, "ExternalInput")
    if H is None:
        H = ffn_handles(P, pf, n_exp, with_proj)
    g_d, wg_d, wu_d, wd_d, id_d = H["g"], H["wg"], H["wu"], H["wd"], H["ident"]
    y_d = y_handle if y_handle is not None else P.dram(pf + "y", [NTOK, DM], F32, "ExternalOutput")
    if n_exp > 1:
        wr_d, idf_d = H["wr"], H["identf"]
    if with_proj:
        o_d = o_handle if o_handle is not None else P.dram(pf + "o", [NTOK, DM], F32, "ExternalInput")
        wo_d = H["wo"]
    W = alloc_norm_work(P)
    yacc = P.sb([128, NT, DM], F32, "yacc")
    xnT = P.sb([128, 8, NTOK], BF16, "xnT")
    gbc = P.sb([128, DM], F32, "gbc")
    actT = P.sb([128, FR, NTOK], BF16, "actT")
    wgc = [P.sb([128, 8, 128], BF16) for _ in range(3)]
    wuc = [P.sb([128, 8, 128], BF16) for _ in range(3)]
    wdc = [P.sb([128, DM], BF16) for _ in range(FR + 3)]
    sg = [P.sb([128, 512], F32) for _ in range(2)]
    psG = [P.ps([128, 512], F32) for _ in range(2)]
    psU = [P.ps([128, 512], F32) for _ in range(2)]
    psY = [P.ps([128, 512], F32) for _ in range(2)]
    W["psRL"] = P.ps([128, 512], F32)
    for i_, k_ in enumerate(["psT", "psRL", ("psG", 0), ("psG", 1), ("psU", 0), ("psU", 1), ("psY", 0), ("psY", 1)]):
        P.bank(k_, i_)

    P.dma(W["ident"][:], id_d.ap(), writes=["ident"])
    P.dma(gbc[:], bcast_rows(g_d, DM), writes=["gbc"])
    P.dve(lambda e: e.memset(W["epsc"][:], EPS), writes=["epsc"])
    if dyn is not None:
        sel = P.sb([128, 4], F32, "sel")
        actf = actT.bitcast(F32)
        selt = [actf[:, b_, :] for b_ in range(2)]
        seltk = [[("actT", b_, tg_) for tg_ in range(4)] for b_ in range(2)]
        P.dma(sel[:], bcast_rows(dyn, 4), writes=["sel"])
        seli = [0]

        def load_sel(handle, dst_ap, dkey, t):
            for q in range(4):
                b = seli[0] % 2
                seli[0] += 1
                P.dma(selt[b], handle.ap()[q * NTOK + t * 128:q * NTOK + (t + 1) * 128, :], writes=seltk[b])
                if q == 0:
                    P.dve(lambda e, b=b: e.tensor_scalar(out=dst_ap, in0=selt[b], scalar1=sel[:, 0:1], scalar2=None,
                                                        op0=ALU.mult), reads=seltk[b] + ["sel"], writes=[dkey])
                else:
                    P.dve(lambda e, b=b, q=q: e.scalar_tensor_tensor(out=dst_ap, in0=selt[b], scalar=sel[:, q:q + 1],
                                                                     in1=dst_ap, op0=ALU.mult, op1=ALU.add),
                          reads=seltk[b] + ["sel", dkey], writes=[dkey])
    for t in range(NT):
        if dyn is not None:
            load_sel(x_d, yacc[:, t, :], ("y", t), t)
        else:
            r0 = x_row0 + t * 128
            P.dma(yacc[:, t, :], x_d.ap()[r0:r0 + 128, :], reads=[("x1s", r0 // 128)], writes=[("y", t)])

    if with_proj:
        wo = P.sb([128, 8, DM], BF16, "wo")
        P.dma(wo[:], wo_d.ap().rearrange("(k p) c -> p k c", p=128), writes=["wo"], q="gpsimd")
        ot = [P.sb([128, DM], F32) for _ in range(2)]
        for t in range(NT):
            if dyn is not None:
                load_sel(o_d, ot[t % 2][:], ("ot", t % 2), t)
            else:
                r0 = x_row0 + t * 128
                P.dma(ot[t % 2][:], o_d.ap()[r0:r0 + 128, :], writes=[("ot", t % 2)])
            emit_norm_T(P, ot[t % 2][:], ("ot", t % 2), None, xnT, slice(t * 128, (t + 1) * 128), ("xnT", t), W, t)
            for h in range(2):
                def mm(e, t=t, h=h):
                    ins = None
                    for kc in range(8):
                        ins = e.matmul(psY[h][:], lhsT=xnT[:, kc, t * 128:(t + 1) * 128],
                                       rhs=wo[:, kc, h * 512:(h + 1) * 512], start=(kc == 0), stop=(kc == 7))
                    return ins
                P.pe(mm, reads=[("xnT", t), "wo"], writes=[("psY", h)])
                P.dve(lambda e, t=t, h=h: e.tensor_tensor(out=yacc[:, t, h * 512:(h + 1) * 512], in0=psY[h][:],
                                                          in1=yacc[:, t, h * 512:(h + 1) * 512], op=ALU.add),
                      reads=[("psY", h), ("y", t)], writes=[("y", t)])

    for t in range(NT):
        emit_norm_T(P, yacc[:, t, :], ("y", t), gbc, xnT, slice(t * 128, (t + 1) * 128), ("xnT", t), W, t)
    allx = [("xnT", t) for t in range(NT)]

    comb = None
    if n_exp > 1:
        W["psRA"], W["psRB"] = psG[0], psG[1]
        comb = emit_router_keys(P, yacc, gbc, W, wr_d, idf_d, n_exp)

    ci = 0
    di = 0
    for ex in range(n_exp):
        wgv = wg_d.ap()[ex].rearrange("(k p) c -> p k c", p=128)
        wuv = wu_d.ap()[ex].rearrange("(k p) c -> p k c", p=128)
        wdv = wd_d.ap()[ex].rearrange("(f p) c -> p f c", p=128)
        for r in range(NF // FR):
            dslots = []
            for fi in range(FR):
                f = r * FR + fi
                cs = ci % 3
                ci += 1
                P.dma(wgc[cs][:], wgv[:, :, f * 128:(f + 1) * 128], writes=[("wgc", cs)], q="gpsimd")
                P.dma(wuc[cs][:], wuv[:, :, f * 128:(f + 1) * 128], writes=[("wuc", cs)], q="gpsimd")
                ds = di % (FR + 3)
                di += 1
                dslots.append(ds)
                P.dma(wdc[ds][:], wdv[:, f, :], writes=[("wdc", ds)], q="gpsimd")
                for tg in range(4):
                    b = (fi * 4 + tg) % 2

                    def mmg(e, cs=cs, tg=tg, b=b):
                        ins = None
                        for kc in range(8):
                            ins = e.matmul(psG[b][:], lhsT=wgc[cs][:, kc, :], rhs=xnT[:, kc, tg * 512:(tg + 1) * 512],
                                           start=(kc == 0), stop=(kc == 7))
                        return ins

                    def mmu(e, cs=cs, tg=tg, b=b):
                        ins = None
                        for kc in range(8):
                            ins = e.matmul(psU[b][:], lhsT=wuc[cs][:, kc, :], rhs=xnT[:, kc, tg * 512:(tg + 1) * 512],
                                           start=(kc == 0), stop=(kc == 7))
                        return ins
                    xk = [("xnT", t) for t in range(tg * 4, tg * 4 + 4)]
                    P.pe(mmg, reads=[("wgc", cs)] + xk, writes=[("psG", b)])
                    P.pe(mmu, reads=[("wuc", cs)] + xk, writes=[("psU", b)])
                    P.act(lambda e, b=b: e.activation(out=sg[b][:], in_=psG[b][:], func=AF.Silu),
                          reads=[("psG", b)], writes=[("sg", b)])
                    P.dve(lambda e, b=b, fi=fi, tg=tg: e.tensor_tensor(out=actT[:, fi, tg * 512:(tg + 1) * 512],
                                                                       in0=sg[b][:], in1=psU[b][:], op=ALU.mult),
                          reads=[("sg", b), ("psU", b)], writes=[("actT", fi, tg)])
            for t in range(NT):
                for h in range(2):
                    def mmd(e, t=t, h=h, dslots=dslots):
                        ins = None
                        for fi in range(FR):
                            ins = e.matmul(psY[h][:], lhsT=actT[:, fi, t * 128:(t + 1) * 128],
                                           rhs=wdc[dslots[fi]][:, h * 512:(h + 1) * 512],
                                           start=(fi == 0), stop=(fi == FR - 1))
                        return ins
                    P.pe(mmd, reads=[("actT", fi, t // 4) for fi in range(FR)] + [("wdc", s) for s in dslots],
                         writes=[("psY", h)])
                    if comb is None:
                        P.dve(lambda e, t=t, h=h: e.tensor_tensor(out=yacc[:, t, h * 512:(h + 1) * 512], in0=psY[h][:],
                                                                  in1=yacc[:, t, h * 512:(h + 1) * 512], op=ALU.add),
                              reads=[("psY", h), ("y", t)], writes=[("y", t)])
                    else:
                        P.dve(lambda e, t=t, h=h, ex=ex: e.scalar_tensor_tensor(
                            out=yacc[:, t, h * 512:(h + 1) * 512], in0=psY[h][:],
                            scalar=comb[:, t, ex:ex + 1], in1=yacc[:, t, h * 512:(h + 1) * 512],
                            op0=ALU.mult, op1=ALU.add),
                            reads=[("psY", h), ("y", t), "comb"], writes=[("y", t)])
    yv = y_d.ap()[y_row0:y_row0 + NTOK, :].rearrange("(t p) d -> p t d", p=128)
    for t4 in range(4):
        P.dma(yv[:, t4 * 4:(t4 + 1) * 4, :], yacc[:, t4 * 4:(t4 + 1) * 4, :],
              reads=[("y", t) for t in range(t4 * 4, t4 * 4 + 4)],
              writes=[("yout", pf, (y_row0 // 128) + t) for t in range(t4 * 4, t4 * 4 + 4)])
    if standalone:
        return P.build()
    P.pop_scope()


def emit_router_keys(P, yacc, gbc, W, wr_d, idf_d, n_exp):
    identf = P.sb([128, 128], F32, "identf")
    wr = P.sb([128, 8, n_exp], F32, "wr")
    comb = P.sb([128, NT, n_exp], F32, "comb")
    x32 = P.sb([128, DM], F32, "rx32")
    xT32 = P.sb([128, 8, 128], F32, "rxT32")
    rs = P.sb([128, 16], F32, "rsmall")
    lg = P.sb([128, 8], F32, "rlg")
    mx = P.sb([128, 8], F32, "rmx")
    tmp = P.sb([128, 8], F32, "rtmp")
    psA = W["psRA"]
    psB = W["psRB"]
    psL = W["psRL"]
    P.dma(identf[:], idf_d.ap(), writes=["identf"])
    P.dma(wr[:], wr_d.ap().rearrange("(k p) e -> p k e", p=128), writes=["wr"])
    for t in range(NT):
        xt = yacc[:, t, :]
        P.act(lambda e, xt=xt: e.activation(out=W["junk"][:], in_=xt, func=AF.Square, accum_out=rs[:, 0:1]),
              reads=[("y", t)], writes=["junk", "rs"])
        P.act(lambda e: e.activation(out=rs[:, 1:2], in_=rs[:, 0:1], func=AF.Sqrt, scale=1.0 / 1024.0,
                                     bias=W["epsc"][:, 0:1]), reads=["rs"], writes=["rs"])
        P.dve(lambda e: e.reciprocal(out=rs[:, 2:3], in_=rs[:, 1:2]), reads=["rs"], writes=["rs"])
        P.dve(lambda e, xt=xt: e.scalar_tensor_tensor(out=x32[:], in0=xt, scalar=rs[:, 2:3], in1=gbc[:],
                                                      op0=ALU.mult, op1=ALU.mult),
              reads=[("y", t), "rs", "gbc"], writes=["rx32"])

        def tr(e):
            ins = None
            for kc in range(8):
                dst = (psA if kc < 4 else psB)[:, (kc % 4) * 128:(kc % 4 + 1) * 128]
                ins = e.transpose(out=dst, in_=x32[:, kc * 128:(kc + 1) * 128], identity=identf[:])
            return ins
        P.pe(tr, reads=["rx32", "identf"], writes=[("psG", 0), ("psG", 1)])
        P.act(lambda e: e.copy(out=xT32[:, 0:4, :], in_=psA[:].rearrange("p (k c) -> p k c", k=4)),
              reads=[("psG", 0)], writes=["rxTa"])
        P.act(lambda e: e.copy(out=xT32[:, 4:8, :], in_=psB[:].rearrange("p (k c) -> p k c", k=4)),
              reads=[("psG", 1)], writes=["rxTb"])

        def mm(e):
            ins = None
            for kc in range(8):
                ins = e.matmul(psL[:, 0:n_exp], lhsT=xT32[:, kc, :], rhs=wr[:, kc, :], start=(kc == 0), stop=(kc == 7))
            return ins
        P.pe(mm, reads=["rxTa", "rxTb", "wr"], writes=["psRL"])
        P.dve(lambda e: e.tensor_copy(out=lg[:], in_=psL[:, 0:n_exp]), reads=["psRL"], writes=["rlg"])
        P.dve(lambda e: e.max(out=mx[:], in_=lg[:]), reads=["rlg"], writes=["rmx"])
        P.dve(lambda e: e.tensor_tensor(out=rs[:, 4:5], in0=mx[:, 1:2], in1=mx[:, 0:1], op=ALU.subtract),
              reads=["rmx"], writes=["rs"])
        P.act(lambda e: e.activation(out=rs[:, 5:6], in_=rs[:, 4:5], func=AF.Exp), reads=["rs"], writes=["rs"])
        P.dve(lambda e: e.tensor_scalar(out=rs[:, 6:7], in0=rs[:, 5:6], scalar1=1.0, scalar2=None, op0=ALU.add),
              reads=["rs"], writes=["rs"])
        P.dve(lambda e: e.reciprocal(out=rs[:, 7:8], in_=rs[:, 6:7]), reads=["rs"], writes=["rs"])
        P.dve(lambda e: e.tensor_tensor(out=rs[:, 8:9], in0=rs[:, 5:6], in1=rs[:, 7:8], op=ALU.mult),
              reads=["rs"], writes=["rs"])
        P.dve(lambda e, t=t: e.tensor_scalar(out=comb[:, t, :], in0=lg[:], scalar1=mx[:, 0:1], scalar2=rs[:, 7:8],
                                             op0=ALU.is_equal, op1=ALU.mult),
              reads=["rlg", "rmx", "rs"], writes=["comb"])
        P.dve(lambda e: e.tensor_scalar(out=tmp[:], in0=lg[:], scalar1=mx[:, 1:2], scalar2=rs[:, 8:9],
                                        op0=ALU.is_equal, op1=ALU.mult),
              reads=["rlg", "rmx", "rs"], writes=["rtmp"])
        P.dve(lambda e, t=t: e.tensor_tensor(out=comb[:, t, :], in0=comb[:, t, :], in1=tmp[:], op=ALU.add),
              reads=["rtmp", "comb"], writes=["comb"])
    return comb


SEQ = 8192
NG = SEQ // 512
KA_GROUPS = {0: 0, 1: 1, 2: 2, 3: 3, 4: 4, 15: 5}


def build_attn(P=None, pf="", y_handle=None, nq=1):
    standalone = P is None
    if standalone:
        P = Prog()
    nc = P.nc
    if not standalone:
        P.push_scope()
    xr_d = P.dram(pf + "xr", [SEQ, DM], F32, "ExternalInput")
    g_d = P.dram(pf + "g", [1, DM], F32, "ExternalInput")
    wq_d = P.dram(pf + "wq", [DM, 1024], F32, "ExternalInput")
    wk_d = P.dram(pf + "wk", [DM, 256], F32, "ExternalInput")
    wv_d = P.dram(pf + "wv", [DM, 256], F32, "ExternalInput")
    wo_d = P.dram(pf + "wo", [64, 16, DM], F32, "ExternalInput")
    gains_d = P.dram(pf + "gains", [128, 4], F32, "ExternalInput")
    rope_d = P.dram(pf + "rope", [4, 128, SEQ], F32, "ExternalInput")
    rmat_d = P.dram(pf + "rmat", [3, 128, 128], BF16, "ExternalInput")
    masks_d = P.dram(pf + "masks", [4, 128, 512], BF16, "ExternalInput")
    sink_d = P.dram(pf + "sink", [1, 8], F32, "ExternalInput")
    id_d = P.dram(pf + "ident", [128, 128], BF16, "ExternalInput")
    y_d = y_handle if y_handle is not None else P.dram(pf + "y", [NTOK, DM], F32, "ExternalOutput")
    full = nq > 1
    ka_groups = {G_: G_ for G_ in range(NG)} if full else KA_GROUPS
    nka = 64 if full else 24

    W = alloc_norm_work(P)
    gbc = P.sb([128, DM], F32, "gbc")
    wq = P.sb([128, 8, 1024], BF16, "wq")
    wk = P.sb([128, 8, 256], BF16, "wk")
    wv = P.sb([128, 8, 256], BF16, "wv")
    wo = P.sb([64, 16, DM], BF16, "wo")
    gains = P.sb([128, 4], F32, "gains")
    rmat = P.sb([128, 3, 128], BF16, "rmat")
    masks = P.sb([128, 4, 512], BF16, "masks")
    KTa = P.sb([128, nka * 128], BF16, "KTa")
    KTb = P.sb([128, SEQ], BF16, "KTb")
    Va = P.sb([128, nka, 2, 65], BF16, "Va")
    Vb = P.sb([128, 64, 2, 65], BF16, "Vb")
    QTa = P.sb([128, 4, 4, 128], BF16, "QTa")
    QTb = P.sb([128, 4, 4, 128], BF16, "QTb")
    xt = [P.sb([128, DM], F32) for _ in range(2)]
    xng = [P.sb([128, 8, 512], BF16) for _ in range(1 if full else 2)]
    tab = [P.sb([128, 512], F32) for _ in range(4)]
    qg = P.sb([128, 512], BF16, "qg")
    sq = P.sb([128, 512], BF16, "sq")
    lnt = P.sb([128, 512], F32, "lnt")
    rstd = P.sb([128, 512], F32, "rstd")
    t1 = P.sb([128, 512], F32, "t1")
    t2 = P.sb([128, 512], F32, "t2")
    pT = [P.sb([128, 512], BF16) for _ in range(6)]
    den = P.sb([65, 512], F32, "den")
    nxng = len(xng)
    NTq = 16 * nq
    onesf = P.sb([65, 64], F32, "onesf")
    sinkrow = P.sb([64, 1024], F32, "sinkrow")
    sink8 = P.sb([64, 8], F32, "sink8")
    denb = P.sb([64, 512], F32, "denb")
    lnr = P.sb([64, 512], F32, "lnr") if not full else None
    rec = P.sb([64, 512], F32, "rec") if not full else None
    OT = [P.sb([64, 16, 128], BF16) for _ in range(2)]
    xres = P.sb([128, DM], F32, "xres") if not full else None
    ysb = P.sb([128, DM], F32, "ysb")
    if full:
        lnr_ap, lnr_k, rec_ap, rec_k = lnt[0:64, :], "lnt", rstd[0:64, :], "rstd"
    else:
        lnr_ap, lnr_k, rec_ap, rec_k = lnr[:], "lnr", rec[:], "rec"
    b0 = W["psT"]
    bk = [None] + [P.ps([128, 512], F32) for _ in range(7)]

    def BK(i):
        return ("bank", i)
    P.bank("psT", 0)
    for i_ in range(1, 8):
        P.bank(BK(i_), i_)

    P.dma(W["ident"][:], id_d.ap(), writes=["ident"])
    P.dma(gbc[:], bcast_rows(g_d, DM), writes=["gbc"])
    P.dve(lambda e: e.memset(W["epsc"][:], EPS), writes=["epsc"])
    P.dma(gains[:], gains_d.ap(), writes=["gains"])
    P.dma(rmat[:], rmat_d.ap().rearrange("r p c -> p r c"), writes=["rmat"])
    P.dma(masks[:], masks_d.ap().rearrange("r p c -> p r c"), writes=["masks"])
    P.dma(wq[:], wq_d.ap().rearrange("(k p) c -> p k c", p=128), writes=["wq"], q="gpsimd")
    P.dma(wk[:], wk_d.ap().rearrange("(k p) c -> p k c", p=128), writes=["wk"], q="gpsimd")
    P.dma(wv[:], wv_d.ap().rearrange("(k p) c -> p k c", p=128), writes=["wv"], q="gpsimd")
    P.dma(wo[:], wo_d.ap(), writes=["wo"], q="gpsimd")
    P.dve(lambda e: e.memset(Va[:, :, :, 64:65], 1.0), writes=["Va1"])
    P.dve(lambda e: e.memset(Vb[:, :, :, 64:65], 1.0), writes=["Vb1"])
    P.dve(lambda e: e.memset(onesf[:], 1.0), writes=["onesf"])
    P.dma(sink8[:], bcast_rows(sink_d, 8, 64), writes=["sink8"])
    P.act(lambda e: e.activation(out=sink8[:], in_=sink8[:], func=AF.Exp), reads=["sink8"], writes=["sink8"])
    P.dve(lambda e: e.memset(sinkrow[:], 0.0), writes=["sinkrow"])
    for h in range(8):
        P.dve(lambda e, h=h: e.tensor_scalar(out=sinkrow[:, h * 128:(h + 1) * 128], in0=sinkrow[:, h * 128:(h + 1) * 128],
                                             scalar1=sink8[:, h:h + 1], scalar2=None, op0=ALU.add),
              reads=["sinkrow", "sink8"], writes=["sinkrow"])

    xrv = xr_d.ap().rearrange("(t p) d -> p t d", p=128)

    def load_group(G):
        pb = G % nxng
        for tl in range(4):
            tg = G * 4 + tl
            P.dma(xt[tg % 2][:], xrv[:, tg, :], writes=[("xt", tg % 2)])
            emit_norm_T(P, xt[tg % 2][:], ("xt", tg % 2), gbc, xng[pb], slice(tl * 128, (tl + 1) * 128),
                        ("xng", pb, tl), W, tg)
        for i in range(4):
            P.dma(tab[i][:], rope_d.ap()[i][:, G * 512:(G + 1) * 512], writes=[("tab", i)])
        return pb

    qkc = [0]

    def qk_block(pb, lhs_fn, gcol, ti, ri, dest3, dkey):
        b = 1 + (qkc[0] % 2)
        qkc[0] += 1
        xk = [("xng", pb, tl) for tl in range(4)]

        def mm(e):
            ins = None
            for kc in range(8):
                ins = e.matmul(bk[b][:], lhsT=lhs_fn(kc), rhs=xng[pb][:, kc, :], start=(kc == 0), stop=(kc == 7))
            return ins
        P.pe(mm, reads=xk + ["wq", "wk"], writes=[BK(b)])
        P.act(lambda e: e.activation(out=qg[:], in_=bk[b][:], func=AF.Copy, scale=gains[:, gcol:gcol + 1]),
              reads=[BK(b), "gains"], writes=["qg"])
        P.act(lambda e: e.activation(out=sq[:], in_=bk[b][:], func=AF.Square), reads=[BK(b)], writes=["sq"])
        P.pe(lambda e: e.matmul(bk[3][:], lhsT=rmat[:, ri, :], rhs=qg[:], start=True, stop=True),
             reads=["qg", "rmat"], writes=[BK(3)])
        P.pe(lambda e: e.matmul(bk[4][:], lhsT=rmat[:, 2, :], rhs=sq[:], start=True, stop=True),
             reads=["sq", "rmat"], writes=[BK(4)])
        P.act(lambda e: e.activation(out=lnt[:], in_=bk[4][:], func=AF.Ln, scale=1.0 / 64.0, bias=W["epsc"][:, 0:1]),
              reads=[BK(4), "epsc"], writes=["lnt"])
        P.act(lambda e: e.activation(out=rstd[:], in_=lnt[:], func=AF.Exp, scale=-0.5), reads=["lnt"], writes=["rstd"])
        P.dve(lambda e: e.tensor_tensor(out=t1[:], in0=qg[:], in1=tab[ti][:], op=ALU.mult),
              reads=["qg", ("tab", ti)], writes=["t1"])
        P.dve(lambda e: e.tensor_tensor(out=t2[:], in0=bk[3][:], in1=tab[ti + 1][:], op=ALU.mult),
              reads=[BK(3), ("tab", ti + 1)], writes=["t2"])
        P.dve(lambda e: e.tensor_tensor(out=t1[:], in0=t1[:], in1=t2[:], op=ALU.add), reads=["t1", "t2"], writes=["t1"])
        P.dve(lambda e: e.tensor_tensor(out=dest3, in0=t1[:].rearrange("p (a b) -> p a b", a=4),
                                        in1=rstd[:].rearrange("p (a b) -> p a b", a=4), op=ALU.mult),
              reads=["t1", "rstd"], writes=[dkey])

    for G in range(NG):
        pb = load_group(G)
        qk_block(pb, lambda kc: wk[:, kc, 128:256], 3, 2, 1,
                 KTb[:, G * 512:(G + 1) * 512].rearrange("p (a b) -> p a b", a=4), ("KTb", G))
        sg = ka_groups.get(G)
        if sg is not None:
            qk_block(pb, lambda kc: wk[:, kc, 0:128], 1, 0, 0,
                     KTa[:, sg * 512:(sg + 1) * 512].rearrange("p (a b) -> p a b", a=4), ("KTa", sg))
        for tl in range(4):
            tg = G * 4 + tl

            def mmv(e, tl=tl, pb=pb):
                ins = None
                for kc in range(8):
                    ins = e.matmul(bk[5][:, 0:256], lhsT=xng[pb][:, kc, tl * 128:(tl + 1) * 128], rhs=wv[:, kc, :],
                                   start=(kc == 0), stop=(kc == 7))
                return ins
            P.pe(mmv, reads=[("xng", pb, tl), "wv"], writes=[BK(5)])
            P.act(lambda e, tg=tg: e.copy(out=Vb[:, tg, :, 0:64], in_=bk[5][:, 128:256].rearrange("p (h d) -> p h d", h=2)),
                  reads=[BK(5)], writes=[("Vb", tg)])
            if sg is not None:
                sl = sg * 4 + tl
                P.act(lambda e, sl=sl: e.copy(out=Va[:, sl, :, 0:64], in_=bk[5][:, 0:128].rearrange("p (h d) -> p h d", h=2)),
                      reads=[BK(5)], writes=[("Va", sl)])

    pcount = [0]
    ocount = [0]
    D = 2

    def attend(R, kind, tl, g, slots, mk, obuf, hbase):
        QT = QTa if kind == "a" else QTb
        KT = KTa if kind == "a" else KTb
        V = Va if kind == "a" else Vb
        ob = 4 + (ocount[0] % 2)
        ocount[0] += 1
        n = len(slots)
        hist = []
        for i in range(n + D):
            if i < n:
                s = slots[i]
                sb_ = 1 + (pcount[0] % 3)
                pb_ = pcount[0] % 6
                pcount[0] += 1
                hist.append(pb_)
                kkey = ("KTa", s // 4) if kind == "a" else ("KTb", s // 4)
                R.pe(lambda e, s=s, sb_=sb_: e.matmul(bk[sb_][:], lhsT=KT[g * 64:(g + 1) * 64, s * 128:(s + 1) * 128],
                                                       rhs=QT[g * 64:(g + 1) * 64, tl, :, :], start=True, stop=True),
                     reads=[kkey, ("QT", kind)], writes=[BK(sb_)])
                R.act(lambda e, sb_=sb_, pb_=pb_: e.activation(out=pT[pb_][:], in_=bk[sb_][:], func=AF.Exp, scale=0.125),
                      reads=[BK(sb_)], writes=[("pT", pb_)])
                if mk[i] is not None:
                    R.dve(lambda e, pb_=pb_, m=mk[i]: e.tensor_tensor(out=pT[pb_][:], in0=pT[pb_][:], in1=masks[:, m, :],
                                                                      op=ALU.mult),
                          reads=[("pT", pb_), "masks"], writes=[("pT", pb_)])
            if i >= D:
                j = i - D
                s = slots[j]
                pb_ = hist[j]
                vkey = ("Va", s) if kind == "a" else ("Vb", s)
                R.pe(lambda e, s=s, pb_=pb_, j=j: e.matmul(bk[ob][0:65, :], lhsT=V[:, s, g, :], rhs=pT[pb_][:],
                                                            start=(j == 0), stop=(j == n - 1)),
                     reads=[vkey, ("pT", pb_), "Va1", "Vb1"], writes=[BK(ob)])
        R.dve(lambda e: e.tensor_copy(out=den[64:65, :], in_=bk[ob][64:65, :]), reads=[BK(ob)], writes=["den"])
        R.pe(lambda e: e.matmul(bk[6][0:64, :], lhsT=onesf[64:65, :], rhs=den[64:65, :], start=True, stop=True),
             reads=["den", "onesf"], writes=[BK(6)])
        if kind == "a":
            R.dve(lambda e: e.tensor_tensor(out=denb[:], in0=bk[6][0:64, :], in1=sinkrow[:, g * 512:(g + 1) * 512],
                                            op=ALU.add), reads=[BK(6), "sinkrow"], writes=["denb"])
            R.act(lambda e: e.activation(out=lnr_ap, in_=denb[:], func=AF.Ln), reads=["denb"], writes=[lnr_k])
        else:
            R.act(lambda e: e.activation(out=lnr_ap, in_=bk[6][0:64, :], func=AF.Ln), reads=[BK(6)], writes=[lnr_k])
        R.act(lambda e: e.activation(out=rec_ap, in_=lnr_ap, func=AF.Exp, scale=-1.0), reads=[lnr_k], writes=[rec_k])
        h0 = hbase + 4 * g
        R.dve(lambda e: e.tensor_tensor(out=OT[obuf][:, h0:h0 + 4, :],
                                        in0=bk[ob][0:64, :].rearrange("p (a b) -> p a b", a=4),
                                        in1=rec_ap.rearrange("p (a b) -> p a b", a=4), op=ALU.mult),
              reads=[BK(ob), rec_k], writes=[("OT", obuf, kind, g)])

    yv = y_d.ap().rearrange("(t p) d -> p t d", p=128)
    for G in range(4 * nq):
        pb = load_group(G)
        for j in range(4):
            qk_block(pb, lambda kc, j=j: wq[:, kc, j * 128:(j + 1) * 128], 0, 0, 0, QTa[:, :, j, :], ("QT", "a"))
            qk_block(pb, lambda kc, j=j: wq[:, kc, 512 + j * 128:512 + (j + 1) * 128], 2, 2, 1, QTb[:, :, j, :], ("QT", "b"))
        for tl in range(4):
            t = G * 4 + tl
            obuf = t % 2
            if full:
                left = t - 1 if t > 0 else 0
                right = t + 1 if t < NTq - 1 else NTq - 1
            else:
                left = t - 1 if t > 0 else 23
                right = t + 1
            for g in range(2):
                attend(P, "a", tl, g, [left, t, right], [0 if t == 0 else 1, None, 3 if t == NTq - 1 else 2], obuf, 0)
            for g in range(2):
                attend(P, "b", tl, g, list(range(64)), [None] * 64, obuf, 8)
            if full:
                xres, xres_k = xt[t % 2], ("xt", t % 2)
            else:
                xres_k = "xres"
            P.dma(xres[:], xrv[:, t, :], writes=[xres_k])
            okeys = [("OT", obuf, k, g) for k in "ab" for g in range(2)]
            for h2 in range(2):
                def mmo(e, h2=h2, obuf=obuf):
                    ins = None
                    for h in range(16):
                        ins = e.matmul(bk[7][:], lhsT=OT[obuf][:, h, :], rhs=wo[:, h, h2 * 512:(h2 + 1) * 512],
                                       start=(h == 0), stop=(h == 15))
                    return ins
                P.pe(mmo, reads=okeys + ["wo"], writes=[BK(7)])
                P.dve(lambda e, h2=h2, xres=xres: e.tensor_tensor(out=ysb[:, h2 * 512:(h2 + 1) * 512], in0=bk[7][:],
                                                                  in1=xres[:, h2 * 512:(h2 + 1) * 512], op=ALU.add),
                      reads=[BK(7), xres_k], writes=[("ysb", h2)])
            P.dma(yv[:, t, :], ysb[:], reads=[("ysb", 0), ("ysb", 1)], writes=[("x1s", t)])
    if standalone:
        return P.build()
    P.pop_scope()


def rope_consts(r):
    pos = (np.arange(SEQ) + 2048 * r) % SEQ
    posf = pos.astype(np.float32)
    inv32 = (np.float32(10000.0) ** (-np.arange(0, 64, 2, dtype=np.float32) / np.float32(64))).astype(np.float32)
    inv16 = (np.float32(10000.0) ** (-np.arange(0, 32, 2, dtype=np.float32) / np.float32(32))).astype(np.float32)
    ang_a = posf[:, None] * inv32[None, :]
    row = (pos // 64).astype(np.float32)
    col = (pos % 64).astype(np.float32)
    ang_r = row[:, None] * inv16[None, :]
    ang_c = col[:, None] * inv16[None, :]
    d = np.arange(128) % 64
    tabs = np.zeros((4, 128, SEQ), np.float32)
    tabs[0] = np.cos(ang_a).astype(np.float32)[:, d % 32].T
    tabs[1] = np.sin(ang_a).astype(np.float32)[:, d % 32].T
    ang_b = np.where((d < 32)[None, :], ang_r[:, d % 16], ang_c[:, d % 16])
    tabs[2] = np.cos(ang_b).astype(np.float32).T
    tabs[3] = np.sin(ang_b).astype(np.float32).T
    return tabs


def rot_consts():
    import ml_dtypes
    Ra = np.zeros((64, 64), np.float32)
    for i in range(64):
        if i < 32:
            Ra[i, i + 32] = -1.0
        else:
            Ra[i, i - 32] = 1.0
    Rb = np.zeros((64, 64), np.float32)
    for i in range(64):
        if (i % 32) < 16:
            Rb[i, i + 16] = -1.0
        else:
            Rb[i, i - 16] = 1.0
    out = np.zeros((3, 128, 128), np.float32)
    for blk in range(2):
        s = slice(blk * 64, (blk + 1) * 64)
        out[0, s, s] = Ra.T
        out[1, s, s] = Rb.T
        out[2, s, s] = 1.0
    return out.astype(ml_dtypes.bfloat16)


def mask_consts(r):
    import ml_dtypes
    k = np.arange(128)[:, None]
    q = np.arange(128)[None, :]
    mL = (k >= q).astype(np.float32)
    mR = (q >= k).astype(np.float32)
    m = np.zeros((4, 128, 512), np.float32)
    m[0] = np.tile(mL if (r is not None and r > 0) else 0 * mL, (1, 4))
    m[1] = np.tile(mL, (1, 4))
    m[2] = np.tile(mR, (1, 4))
    m[3] = np.tile(mR if (r is not None and r < 3) else 0 * mR, (1, 4))
    return m.astype(ml_dtypes.bfloat16)


def attn_inputs(x, ab_norm, ab_w_in, qn_a, kn_a, sink_a, qn_b, kn_b, ab_w_out, fused=False):
    import ml_dtypes
    w = ab_w_in[0]
    qa, ka, va, qb, kb, vb = (w[:, 0:512], w[:, 512:640], w[:, 640:768], w[:, 768:1280], w[:, 1280:1408], w[:, 1408:1536])

    def pair(wq_):
        cols = []
        for j in range(4):
            cols.append(wq_[:, j * 64:(j + 1) * 64])
            cols.append(wq_[:, (j + 4) * 64:(j + 5) * 64])
        return np.concatenate(cols, axis=1)
    wq = np.ascontiguousarray(np.concatenate([pair(qa), pair(qb)], axis=1))
    wk = np.ascontiguousarray(np.concatenate([ka, kb], axis=1))
    wv = np.ascontiguousarray(np.concatenate([va, vb], axis=1))
    wo = np.ascontiguousarray(ab_w_out[0].reshape(16, 64, DM).transpose(1, 0, 2))
    gains = np.ascontiguousarray(np.stack([np.tile(qn_a[0], 2), np.tile(kn_a[0], 2), np.tile(qn_b[0], 2), np.tile(kn_b[0], 2)], axis=1))
    ident = np.eye(128, dtype=np.float32).astype(ml_dtypes.bfloat16)
    rmat = rot_consts()
    maps = []
    if fused:
        rope0, masks0 = rope_consts(0), mask_consts(None)
    for c in range(NCORES):
        b, r = c // 4, c % 4
        if fused:
            maps.append({"xr": x[b], "g": ab_norm[0:1], "wq": wq, "wk": wk, "wv": wv, "wo": wo, "gains": gains,
                         "rope": rope0, "rmat": rmat, "masks": masks0, "sink": sink_a[0:1], "ident": ident})
            continue
        xr = np.ascontiguousarray(np.roll(x[b], -2048 * r, axis=0))
        maps.append({"xr": xr, "g": ab_norm[0:1], "wq": wq, "wk": wk, "wv": wv, "wo": wo, "gains": gains,
                     "rope": rope_consts(r), "rmat": rmat, "masks": mask_consts(r), "sink": sink_a[0:1], "ident": ident})
    return maps


NTILE = SEQ // 128
DBG = {}


def build_gdn(phases=(1, 2, 3), nsteps=NTILE, P=None, pf="", x_handle=None, o_handle=None, nhp=1):
    standalone = P is None
    if standalone:
        P = Prog()
    else:
        P.push_scope()
    nc = P.nc
    fusedm = x_handle is not None
    xp_d = x_handle if fusedm else P.dram(pf + "xp", [SEQ + 128, DM], F32, "ExternalInput")
    g_d = P.dram(pf + "g", [1, DM], F32, "ExternalInput")
    wf_d = P.dram(pf + "wf", [nhp, DM, 768], F32, "ExternalInput")
    wt_d = P.dram(pf + "wt", [nhp, DM, 264], F32, "ExternalInput")
    cw_d = P.dram(pf + "cw", [nhp, 128, 6, 5], F32, "ExternalInput")
    gc_d = P.dram(pf + "gconst", [nhp, 8], F32, "ExternalInput")
    on_d = P.dram(pf + "onorm", [1, 128], F32, "ExternalInput")
    mk_d = P.dram(pf + "gmask", [5, 128, 128], F32, "ExternalInput")
    id_d = P.dram(pf + "ident", [128, 128], BF16, "ExternalInput")
    y_d = o_handle if fusedm else P.dram(pf + "y", [SEQ, 256], F32, "ExternalOutput")
    SK = "ExternalOutput" if DBG.get("dump") else "Internal"
    qT_s = P.dram(pf + "qT_s", [2, 128, SEQ], BF16, SK)
    kT_s = P.dram(pf + "kT_s", [2, 128, SEQ], BF16, SK)
    k_s = P.dram(pf + "k_s", [2, SEQ, 128], BF16, SK)
    v_s = P.dram(pf + "v_s", [2, SEQ, 128], BF16, SK)
    z_s = P.dram(pf + "z_s", [SEQ, 256], BF16, SK)
    o_s = P.dram(pf + "o_s", [2, 2, SEQ, 128], F32, SK)

    W = alloc_norm_work(P)
    gbc = P.sb([128, DM], F32, "gbc")
    wf = P.sb([128, 8, 768], BF16, "wf")
    wt = P.sb([128, 8, 264], BF16, "wt")
    cw = P.sb([128, 6, 5], F32, "cw")
    gconst = P.sb([128, 8], F32, "gconst")
    onorm = P.sb([128, 2, 128], F32, "onorm")
    gmask = P.sb([128, 5, 128], F32, "gmask")
    onesb = P.sb([128, 128], BF16, "onesb")
    onesf = P.sb([128, 128], F32, "onesf")
    onec = P.sb([128, 1], F32, "onec")
    gates = P.sb([128, NTILE, 8], F32, "gates")
    xt = [P.sb([128, DM], F32) for _ in range(2)]
    xng = [P.sb([128, 8, 516], BF16) for _ in range(2)]
    pre = [P.sb([128, 516], F32) for _ in range(2)]
    cacc = P.sb([128, 512], F32, "cacc")
    cblk = P.sb([128, 512], F32, "cblk")
    sqb = P.sb([128, 512], BF16, "sqb")
    lnt = P.sb([128, 512], F32, "lnt")
    rst = P.sb([128, 512], F32, "rst")
    nT = [P.sb([128, 512], BF16) for _ in range(2)]
    tm = [P.sb([128, 4, 128], BF16) for _ in range(2)]
    zsb = [P.sb([128, 256], BF16) for _ in range(2)]
    gtmp = P.sb([128, 8], F32, "gtmp")
    banks = [P.ps([128, 512], F32) for _ in range(7)]
    bankT = W["psT"]

    def BQ(b, q):
        return ("bq", b, q)

    def BK(b):
        return [BQ(b, q) for q in range(4)]

    KT_ALL = [BQ(7, q) for q in range(4)]
    for b_ in range(8):
        for q_ in range(4):
            P.bank(BQ(b_, q_), b_)

    P.dma(W["ident"][:], id_d.ap(), writes=["ident"])
    P.dma(gbc[:], bcast_rows(g_d, DM), writes=["gbc"])
    P.dve(lambda e: e.memset(W["epsc"][:], EPS), writes=["epsc"])
    P.dma(onorm[:, 0, :], bcast_rows(on_d, 128), writes=["onorm"])
    P.dma(onorm[:, 1, :], bcast_rows(on_d, 128), writes=["onorm"])
    P.dma(gmask[:], mk_d.ap().rearrange("r p c -> p r c"), writes=["gmask"])
    P.dve(lambda e: e.memset(onesb[:], 1.0), writes=["onesb"])
    P.dve(lambda e: e.memset(onesf[:], 1.0), writes=["onesf"])
    P.dve(lambda e: e.memset(onec[:], 1.0), writes=["onec"])
    def load_weights(hp):
        P.dma(wf[:], wf_d.ap()[hp].rearrange("(k p) c -> p k c", p=128), writes=["wf"], q="gpsimd")
        P.dma(wt[:], wt_d.ap()[hp].rearrange("(k p) c -> p k c", p=128), writes=["wt"], q="gpsimd")
        P.dma(cw[:], cw_d.ap()[hp], writes=["cw"])
        P.dma(gconst[:], bcast_rows(gc_d, 8, 128, hp * 8), writes=["gconst"])
        P.act(lambda e: e.activation(out=gconst[:, 4:8], in_=gconst[:, 4:8], func=AF.Exp), reads=["gconst"], writes=["gconst"])
        P.dve(lambda e: e.tensor_scalar(out=gconst[:, 4:8], in0=gconst[:, 4:8], scalar1=-1.0, scalar2=None, op0=ALU.mult),
              reads=["gconst"], writes=["gconst"])

    xpv = xp_d.ap()
    uid = [0]

    def norm_rows(x_ap, xkey, rows, dst, dkey):
        u = uid[0]
        uid[0] += 1
        xn = W["xn"][u % 2]
        kxn = ("xn", u % 2)
        ss = W["ss"][u % 2]
        kss = ("ss", u % 2)
        psT = bankT
        P.act(lambda e: e.activation(out=W["junk"][0:rows, :], in_=x_ap, func=AF.Square, accum_out=ss[0:rows, 0:1]),
              reads=[xkey], writes=[kss, "junk"])
        P.act(lambda e: e.activation(out=ss[0:rows, 1:2], in_=ss[0:rows, 0:1], func=AF.Sqrt, scale=1.0 / 1024.0,
                                     bias=W["epsc"][0:rows, 0:1]), reads=[kss, "epsc"], writes=[kss])
        P.dve(lambda e: e.reciprocal(out=ss[0:rows, 2:3], in_=ss[0:rows, 1:2]), reads=[kss], writes=[kss])
        P.dve(lambda e: e.scalar_tensor_tensor(out=xn[0:rows, :], in0=x_ap, scalar=ss[0:rows, 2:3], in1=gbc[0:rows, :],
                                               op0=ALU.mult, op1=ALU.mult), reads=[xkey, kss, "gbc"], writes=[kxn])

        def tr(e):
            ins = None
            for kc in range(8):
                ins = e.transpose(out=psT[:, kc * 128:kc * 128 + rows], in_=xn[0:rows, kc * 128:(kc + 1) * 128],
                                  identity=W["ident"][0:rows, 0:rows])
            return ins
        P.pe(tr, reads=[kxn, "ident"], writes=KT_ALL)
        P.act(lambda e: e.copy(out=dst, in_=psT[:].rearrange("p (k c) -> p k c", k=8)[:, :, 0:rows]),
              reads=KT_ALL, writes=[dkey])

    def phase1(hp):
        for G in (range(DBG.get('ng', NG)) if 1 in phases else []):
            pb = G % 2
            LV = DBG.get('lv', 9)
            for tl in range(5):
                rows = 128 if tl < 4 else 4
                u = uid[0]
                xo = 0 if fusedm else 2
                if tl < 4:
                    r0 = G * 512 + xo + tl * 128
                    P.dma(xt[u % 2][0:rows, :], xpv[r0:r0 + rows, :], reads=[("yout", "f_", r0 // 128)], writes=[("xt", u % 2)])
                else:
                    if fusedm and (G == 0 or G == NG - 1):
                        P.dve(lambda e, u=u: e.memset(xt[u % 2][0:4, :], 0.0), writes=[("xt", u % 2)])
                    if not (fusedm and G == 0):
                        r0 = G * 512 + xo - 2
                        P.dma(xt[u % 2][0:2, :], xpv[r0:r0 + 2, :], reads=[("yout", "f_", r0 // 128)], writes=[("xt", u % 2)])
                    if not (fusedm and G == NG - 1):
                        r0 = G * 512 + xo + 512
                        P.dma(xt[u % 2][2:4, :], xpv[r0:r0 + 2, :], reads=[("yout", "f_", r0 // 128)], writes=[("xt", u % 2)])
                norm_rows(xt[u % 2][0:rows, :], ("xt", u % 2), rows, xng[pb][:, :, tl * 128:tl * 128 + rows], ("xng", pb, tl))
            xk = [("xng", pb, tl) for tl in range(5)]
            for blk in (range(6) if LV >= 2 else []):
                pbk = 1 + (blk % 2)
                prb = blk % 2

                def mm_main(e, blk=blk, pbk=pbk, pb=pb):
                    ins = None
                    for kc in range(8):
                        ins = e.matmul(banks[pbk][:], lhsT=wf[:, kc, blk * 128:(blk + 1) * 128], rhs=xng[pb][:, kc, 0:512],
                                       start=(kc == 0), stop=(kc == 7))
                    return ins

                def mm_halo(e, blk=blk, pb=pb):
                    ins = None
                    for kc in range(8):
                        ins = e.matmul(banks[3][:, 0:4], lhsT=wf[:, kc, blk * 128:(blk + 1) * 128], rhs=xng[pb][:, kc, 512:516],
                                       start=(kc == 0), stop=(kc == 7))
                    return ins
                P.pe(mm_main, reads=xk + ["wf"], writes=BK(pbk))
                P.pe(mm_halo, reads=xk + ["wf"], writes=BK(3))
                P.act(lambda e, pbk=pbk, prb=prb: e.copy(out=pre[prb][:, 2:514], in_=banks[pbk][:]), reads=BK(pbk), writes=[("pre", prb)])
                P.act(lambda e, prb=prb: e.copy(out=pre[prb][:, 0:2], in_=banks[3][:, 0:2]), reads=BK(3), writes=[("pre", prb)])
                P.act(lambda e, prb=prb: e.copy(out=pre[prb][:, 514:516], in_=banks[3][:, 2:4]), reads=BK(3), writes=[("pre", prb)])
                P.dve(lambda e, blk=blk, prb=prb: e.tensor_scalar(out=cacc[:], in0=pre[prb][:, 0:512], scalar1=cw[:, blk, 0:1],
                                                                  scalar2=None, op0=ALU.mult),
                      reads=[("pre", prb), "cw"], writes=["cacc"])
                for tap in range(1, 5):
                    P.dve(lambda e, blk=blk, prb=prb, tap=tap: e.scalar_tensor_tensor(
                        out=cacc[:], in0=pre[prb][:, tap:tap + 512], scalar=cw[:, blk, tap:tap + 1], in1=cacc[:],
                        op0=ALU.mult, op1=ALU.add), reads=[("pre", prb), "cw", "cacc"], writes=["cacc"])
                nb = blk % 2
                h = blk % 2
                if LV < 3:
                    continue
                if blk < 4:
                    P.act(lambda e: e.activation(out=cblk[:], in_=cacc[:], func=AF.Silu), reads=["cacc"], writes=["cblk"])
                    P.act(lambda e: e.activation(out=sqb[:], in_=cblk[:], func=AF.Square), reads=["cblk"], writes=["sqb"])
                    P.pe(lambda e: e.matmul(banks[4][:], lhsT=onesb[:], rhs=sqb[:], start=True, stop=True),
                         reads=["sqb", "onesb"], writes=BK(4))
                    P.act(lambda e: e.activation(out=lnt[:], in_=banks[4][:], func=AF.Ln, bias=W["epsc"][:, 0:1]),
                          reads=BK(4) + ["epsc"], writes=["lnt"])
                    P.act(lambda e: e.activation(out=rst[:], in_=lnt[:], func=AF.Exp, scale=-0.5), reads=["lnt"], writes=["rst"])
                    qs = (128.0 ** -0.5) if blk < 2 else 1.0
                    P.dve(lambda e, nb=nb, qs=qs: e.scalar_tensor_tensor(out=nT[nb][:], in0=cblk[:], scalar=qs, in1=rst[:],
                                                                         op0=ALU.mult, op1=ALU.mult),
                          reads=["cblk", "rst"], writes=[("nT", nb)])
                    dst = qT_s if blk < 2 else kT_s
                    nm = "qT" if blk < 2 else "kT"
                    if LV >= 4:
                      P.dma(dst.ap()[h][:, G * 512:(G + 1) * 512], nT[nb][:], reads=[("nT", nb)],
                          writes=[(nm, h, G * 4 + tl) for tl in range(4)])
                else:
                    P.act(lambda e, nb=nb: e.activation(out=nT[nb][:], in_=cacc[:], func=AF.Silu), reads=["cacc"], writes=[("nT", nb)])
                if blk >= 2 and LV >= 5:
                    bT = bankT

                    def trk(e, nb=nb):
                        ins = None
                        for tl in range(4):
                            ins = e.transpose(out=bT[:, tl * 128:(tl + 1) * 128], in_=nT[nb][:, tl * 128:(tl + 1) * 128],
                                              identity=W["ident"][:])
                        return ins
                    P.pe(trk, reads=[("nT", nb), "ident"], writes=KT_ALL)
                    P.act(lambda e, nb=nb: e.copy(out=tm[nb][:], in_=bT[:, 0:512].rearrange("p (t c) -> p t c", t=4)),
                          reads=KT_ALL, writes=[("tm", nb)])
                    dst = k_s if blk < 4 else v_s
                    nm = "k" if blk < 4 else "v"
                    P.dma(dst.ap()[h].rearrange("(t p) d -> p t d", p=128)[:, G * 4:(G + 1) * 4, :], tm[nb][:],
                          reads=[("tm", nb)], writes=[(nm, h, G * 4 + tl) for tl in range(4)])
            for tl in (range(4) if LV >= 6 else []):
                t = G * 4 + tl
                zb = t % 2

                def mmt(e, tl=tl, pb=pb):
                    ins = None
                    for kc in range(8):
                        ins = e.matmul(banks[5][:, 0:264], lhsT=xng[pb][:, kc, tl * 128:(tl + 1) * 128], rhs=wt[:, kc, :],
                                       start=(kc == 0), stop=(kc == 7))
                    return ins
                P.pe(mmt, reads=xk + ["wt"], writes=BK(5))
                P.act(lambda e, zb=zb: e.activation(out=zsb[zb][:], in_=banks[5][:, 0:256], func=AF.Silu), reads=BK(5), writes=[("zsb", zb)])
                P.dma(z_s.ap()[t * 128:(t + 1) * 128, :], zsb[zb][:], reads=[("zsb", zb)], writes=[("z", t)])
                if LV < 7:
                    continue
                P.dve(lambda e: e.tensor_tensor(out=gtmp[:, 0:4], in0=banks[5][:, 256:260], in1=gconst[:, 0:4], op=ALU.add),
                      reads=BK(5) + ["gconst"], writes=["gtmp"])
                SUB = DBG.get('sub', 9)
                if SUB >= 2:
                    P.act(lambda e: e.activation(out=gtmp[:, 0:4], in_=gtmp[:, 0:4], func=AF.Exp), reads=["gtmp"], writes=["gtmp"])
                if SUB >= 3:
                    P.act(lambda e: e.activation(out=gtmp[:, 0:4], in_=gtmp[:, 0:4], func=AF.Ln, bias=onec[:, 0:1]),
                          reads=["gtmp", "onec"], writes=["gtmp"])
                if SUB >= 4:
                    P.dve(lambda e, t=t: e.tensor_tensor(out=gates[:, t, 0:4], in0=gtmp[:, 0:4], in1=gconst[:, 4:8], op=ALU.mult),
                          reads=["gtmp", "gconst"], writes=[("gates", t)])
                if LV < 8:
                    continue
                P.act(lambda e: e.activation(out=gtmp[:, 4:8], in_=banks[5][:, 260:264], func=AF.Exp, scale=-1.0),
                      reads=BK(5), writes=["gtmp"])
                P.dve(lambda e: e.tensor_scalar(out=gtmp[:, 4:8], in0=gtmp[:, 4:8], scalar1=1.0, scalar2=None, op0=ALU.add),
                      reads=["gtmp"], writes=["gtmp"])
                P.dve(lambda e, t=t: e.reciprocal(out=gates[:, t, 4:8], in_=gtmp[:, 4:8]), reads=["gtmp"], writes=[("gates", t)])

    allb = [P.ps([128, 512], F32)] if False else None
    chains = [(h, d) for d in range(2) for h in range(2)]
    bank_ap = banks + [None]
    pbank8 = P.ps

    def Q(ci, q):
        b = 2 * ci + q // 4
        qq = q % 4
        if b == 7:
            ap = bankT.bitcast(F32)[:, qq * 128:(qq + 1) * 128]
        else:
            ap = banks[b][:, qq * 128:(qq + 1) * 128]
        return ap, BQ(b, qq)

    def Qb(ci, q):
        b = 2 * ci + q // 4
        qq = q % 4
        if b == 7:
            ap = bankT[:, qq * 256:qq * 256 + 128]
        else:
            ap = banks[b].bitcast(BF16)[:, qq * 256:qq * 256 + 128]
        return ap, BQ(b, qq)

    st = {}
    for ci in range(4):
        d = {}
        d["S32"] = P.sb([128, 128], F32)
        d["Sbf"] = P.sb([128, 128], BF16)
        for nm in ["qT", "kT", "k", "v"]:
            d[nm] = [P.sb([128, 128], BF16) for _ in range(2)]
        for nm in ["gcol", "cols"]:
            d[nm] = P.sb([128, 8], F32)
        for nm in ["TG", "IB", "Dm", "E", "Bm", "Eb", "erow", "u"]:
            d[nm] = P.sb([128, 128], F32)
        for nm in ["X", "XT", "Pa", "PaT", "Pb", "PbT", "Za", "Zb", "vb", "kbg", "kd", "qgT", "nwT", "qkT", "vn"]:
            d[nm] = P.sb([128, 128], BF16)
        d["osb"] = [P.sb([128, 128], F32) for _ in range(2)]
        st[ci] = d

    def K(ci, nm):
        return (nm, "c", ci)

    def chain_tile(ci, s, R):
        h, dr = chains[ci]
        d = st[ci]
        t = s if dr == 0 else NTILE - 1 - s
        lb = s % 2
        incl = gmask[:, 0 + 2 * dr, :]
        strict = gmask[:, 1 + 2 * dr, :]
        identf = gmask[:, 4, :]
        gcolumn = gates[:, t, 2 * dr + h:2 * dr + h + 1]
        bcolumn = gates[:, t, 4 + 2 * dr + h:4 + 2 * dr + h + 1]
        for nm, src in (("qT", qT_s), ("kT", kT_s)):
            R.dma(d[nm][lb][:], src.ap()[h][:, t * 128:(t + 1) * 128], reads=[(nm, h, t)], writes=[K(ci, nm + str(lb))])
        for nm, src in (("k", k_s), ("v", v_s)):
            R.dma(d[nm][lb][:], src.ap()[h][t * 128:(t + 1) * 128, :], reads=[(nm, h, t)], writes=[K(ci, nm + str(lb))])
        qT, kT, kk, vv = d["qT"][lb], d["kT"][lb], d["k"][lb], d["v"][lb]
        kqT, kkT, kkk, kvv = K(ci, "qT" + str(lb)), K(ci, "kT" + str(lb)), K(ci, "k" + str(lb)), K(ci, "v" + str(lb))
        q0, k0 = Q(ci, 0)
        q1, k1 = Q(ci, 1)
        q2, k2 = Q(ci, 2)
        q3, k3 = Q(ci, 3)
        q4, k4 = Q(ci, 4)
        q5, k5 = Q(ci, 5)
        q6, k6 = Q(ci, 6)
        q7, k7 = Q(ci, 7)
        R.pe(lambda e: e.matmul(q0, lhsT=kT[:], rhs=kT[:], start=True, stop=True), reads=[kkT], writes=[k0])
        R.pe(lambda e: e.matmul(q1, lhsT=kT[:], rhs=qT[:], start=True, stop=True), reads=[kkT, kqT], writes=[k1])
        R.dve(lambda e: e.tensor_scalar(out=d["TG"][:], in0=incl, scalar1=gcolumn, scalar2=None, op0=ALU.mult),
              reads=["gmask", ("gates", t)], writes=[K(ci, "TG")])
        R.dve(lambda e: e.tensor_scalar(out=d["IB"][:], in0=identf, scalar1=bcolumn, scalar2=None, op0=ALU.mult),
              reads=["gmask", ("gates", t)], writes=[K(ci, "IB")])
        R.pe(lambda e: e.matmul(q2, lhsT=onesf[:], rhs=d["TG"][:], start=True, stop=True),
             reads=[K(ci, "TG"), "onesf"], writes=[k2])
        R.pe(lambda e: e.matmul(q3[:, 0:2], lhsT=d["TG"][:], rhs=onesf[:, 0:2], start=True, stop=True),
             reads=[K(ci, "TG"), "onesf"], writes=[k3])
        R.dve(lambda e: e.tensor_copy(out=d["gcol"][:, 0:1], in_=q3[:, 0:1]), reads=[k3], writes=[K(ci, "gcol")])
        R.pe(lambda e: e.matmul(q3, lhsT=onesf[:], rhs=d["IB"][:], start=True, stop=True),
             reads=[K(ci, "IB"), "onesf", K(ci, "gcol")], writes=[k3])
        R.dve(lambda e: e.tensor_scalar(out=d["Dm"][:], in0=q2, scalar1=d["gcol"][:, 0:1], scalar2=0.0,
                                        op0=ALU.subtract, op1=ALU.min), reads=[k2, K(ci, "gcol")], writes=[K(ci, "Dm")])
        R.act(lambda e: e.activation(out=d["E"][:], in_=d["Dm"][:], func=AF.Exp), reads=[K(ci, "Dm")], writes=[K(ci, "E")])
        R.act(lambda e: e.activation(out=d["erow"][:], in_=q2, func=AF.Exp), reads=[k2], writes=[K(ci, "erow")])
        R.dve(lambda e: e.tensor_tensor(out=d["E"][:], in0=d["E"][:], in1=incl, op=ALU.mult),
              reads=[K(ci, "E"), "gmask"], writes=[K(ci, "E")])
        R.dve(lambda e: e.tensor_tensor(out=d["Bm"][:], in0=q3, in1=strict, op=ALU.mult), reads=[k3, "gmask"], writes=[K(ci, "Bm")])
        R.dve(lambda e: e.tensor_tensor(out=d["Eb"][:], in0=d["E"][:], in1=d["Bm"][:], op=ALU.mult),
              reads=[K(ci, "E"), K(ci, "Bm")], writes=[K(ci, "Eb")])
        R.dve(lambda e: e.scalar_tensor_tensor(out=d["X"][:], in0=q0, scalar=-1.0, in1=d["Eb"][:], op0=ALU.mult, op1=ALU.mult),
              reads=[k0, K(ci, "Eb")], writes=[K(ci, "X")])
        R.dve(lambda e: e.tensor_tensor(out=d["qkT"][:], in0=q1, in1=d["E"][:], op=ALU.mult),
              reads=[k1, K(ci, "E")], writes=[K(ci, "qkT")])
        cols = d["cols"]
        R.act(lambda e: e.activation(out=cols[:, 0:1], in_=d["gcol"][:, 0:1], func=AF.Exp), reads=[K(ci, "gcol")], writes=[K(ci, "cols")])
        R.dve(lambda e: e.tensor_tensor(out=cols[:, 1:2], in0=cols[:, 0:1], in1=bcolumn, op=ALU.mult),
              reads=[K(ci, "cols"), ("gates", t)], writes=[K(ci, "cols")])
        for cc in range(2):
            col = (cc * 64 + 63) if dr == 0 else (cc * 64)
            R.dve(lambda e, cc=cc, col=col: e.tensor_copy(out=cols[cc * 64:(cc + 1) * 64, 3:4], in_=q2[cc * 64:(cc + 1) * 64, col:col + 1]),
                  reads=[k2], writes=[K(ci, "cols")])
        R.dve(lambda e: e.tensor_tensor(out=cols[:, 4:5], in0=cols[:, 3:4], in1=d["gcol"][:, 0:1], op=ALU.subtract),
              reads=[K(ci, "cols"), K(ci, "gcol")], writes=[K(ci, "cols")])
        R.act(lambda e: e.activation(out=cols[:, 2:3], in_=cols[:, 4:5], func=AF.Exp), reads=[K(ci, "cols")], writes=[K(ci, "cols")])
        R.dve(lambda e: e.tensor_scalar(out=d["vb"][:], in0=vv[:], scalar1=bcolumn, scalar2=None, op0=ALU.mult),
              reads=[kvv, ("gates", t)], writes=[K(ci, "vb")])
        R.dve(lambda e: e.tensor_scalar(out=d["kbg"][:], in0=kk[:], scalar1=cols[:, 1:2], scalar2=None, op0=ALU.mult),
              reads=[kkk, K(ci, "cols")], writes=[K(ci, "kbg")])
        R.dve(lambda e: e.tensor_scalar(out=d["kd"][:], in0=kk[:], scalar1=cols[:, 2:3], scalar2=None, op0=ALU.mult),
              reads=[kkk, K(ci, "cols")], writes=[K(ci, "kd")])
        R.dve(lambda e: e.tensor_tensor(out=d["qgT"][:], in0=qT[:], in1=d["erow"][:], op=ALU.mult),
              reads=[kqT, K(ci, "erow")], writes=[K(ci, "qgT")])
        qb4, _ = Qb(ci, 4)
        R.pe(lambda e: e.transpose(out=qb4, in_=d["X"][:], identity=W["ident"][:]), reads=[K(ci, "X"), "ident"], writes=[k4])
        R.act(lambda e: e.copy(out=d["XT"][:], in_=qb4), reads=[k4], writes=[K(ci, "XT")])
        R.dve(lambda e: e.tensor_tensor(out=d["Za"][:], in0=d["X"][:], in1=identf, op=ALU.add),
              reads=[K(ci, "X"), "gmask"], writes=[K(ci, "Za")])
        Pc, PcT, kPc, kPcT = d["X"], d["XT"], K(ci, "X"), K(ci, "XT")
        Zc, kZc = d["Za"], K(ci, "Za")
        for lvl in range(5):
            Pn, PnT = (d["Pa"], d["PaT"]) if lvl % 2 == 0 else (d["Pb"], d["PbT"])
            kPn, kPnT = (K(ci, "Pa"), K(ci, "PaT")) if lvl % 2 == 0 else (K(ci, "Pb"), K(ci, "PbT"))
            Zn, kZn = (d["Zb"], K(ci, "Zb")) if lvl % 2 == 0 else (d["Za"], K(ci, "Za"))
            R.pe(lambda e, Pc=Pc, PcT=PcT: e.matmul(q5, lhsT=Pc[:], rhs=PcT[:], start=True, stop=True),
                 reads=[kPc, kPcT], writes=[k5])
            R.act(lambda e, PnT=PnT: e.copy(out=PnT[:], in_=q5), reads=[k5], writes=[kPnT])
            if lvl < 4:
                R.pe(lambda e, Pc=Pc, PcT=PcT: e.matmul(q4, lhsT=PcT[:], rhs=Pc[:], start=True, stop=True),
                     reads=[kPc, kPcT], writes=[k4])
                R.act(lambda e, Pn=Pn: e.copy(out=Pn[:], in_=q4), reads=[k4], writes=[kPn])
            R.pe(lambda e, PnT=PnT, Zc=Zc: e.matmul(q6, lhsT=PnT[:], rhs=Zc[:], start=True, stop=True),
                 reads=[kPnT, kZc], writes=[k6])
            R.dve(lambda e, Zn=Zn, Zc=Zc: e.tensor_tensor(out=Zn[:], in0=q6, in1=Zc[:], op=ALU.add),
                  reads=[k6, kZc], writes=[kZn])
            Pc, PcT, kPc, kPcT = Pn, PnT, kPn, kPnT
            Zc, kZc = Zn, kZn
        R.pe(lambda e: e.matmul(q0, lhsT=Zc[:], rhs=d["vb"][:], start=True, stop=True), reads=[kZc, K(ci, "vb")], writes=[k0])
        R.act(lambda e: e.copy(out=d["u"][:], in_=q0), reads=[k0], writes=[K(ci, "u")])
        R.pe(lambda e: e.matmul(q1, lhsT=d["kbg"][:], rhs=Zc[:], start=True, stop=True), reads=[kZc, K(ci, "kbg")], writes=[k1])
        R.act(lambda e: e.activation(out=d["nwT"][:], in_=q1, func=AF.Copy, scale=-1.0), reads=[k1], writes=[K(ci, "nwT")])
        order = [0, 1] if dr == 0 else [1, 0]
        osb = d["osb"][lb]
        for cc in order:
            rs = slice(cc * 64, (cc + 1) * 64)
            R.pe(lambda e, rs=rs: e.matmul(q2[rs, :], lhsT=d["nwT"][:, rs], rhs=d["Sbf"][:], start=True, stop=True),
                 reads=[K(ci, "nwT"), ("Sbf", ci)], writes=[k2])
            R.dve(lambda e, rs=rs: e.tensor_tensor(out=d["vn"][rs, :], in0=q2[rs, :], in1=d["u"][rs, :], op=ALU.add),
                  reads=[k2, K(ci, "u")], writes=[K(ci, "vn")])

            def mmo(e, rs=rs):
                e.matmul(q3[rs, :], lhsT=d["qgT"][:, rs], rhs=d["Sbf"][:], start=True, stop=False)
                return e.matmul(q3[rs, :], lhsT=d["qkT"][rs, rs], rhs=d["vn"][rs, :], start=False, stop=True)
            R.pe(mmo, reads=[K(ci, "qgT"), K(ci, "qkT"), K(ci, "vn"), ("Sbf", ci)], writes=[k3])
            R.pe(lambda e, rs=rs: e.matmul(q7, lhsT=d["kd"][rs, :], rhs=d["vn"][rs, :], start=True, stop=True),
                 reads=[K(ci, "kd"), K(ci, "vn")], writes=[k7])
            dcol = (cc * 64 + 63) if dr == 0 else (cc * 64)
            R.dve(lambda e, dcol=dcol: e.scalar_tensor_tensor(out=d["S32"][:], in0=d["S32"][:], scalar=d["erow"][:, dcol:dcol + 1],
                                                              in1=q7, op0=ALU.mult, op1=ALU.add),
                  reads=[("S32", ci), K(ci, "erow"), k7], writes=[("S32", ci)])
            R.act(lambda e: e.copy(out=d["Sbf"][:], in_=d["S32"][:]), reads=[("S32", ci)], writes=[("Sbf", ci)])
            R.act(lambda e, rs=rs: e.copy(out=osb[rs, :], in_=q3[rs, :]), reads=[k3], writes=[K(ci, "osb" + str(lb))])
        R.dma(o_s.ap()[dr][h][t * 128:(t + 1) * 128, :], osb[:], reads=[K(ci, "osb" + str(lb))], writes=[("o", dr, h, t)])

    def phase2(hp):
        for ci in range(4):
            P.dve(lambda e, ci=ci: e.memset(st[ci]["S32"][:], 0.0), writes=[("S32", ci)])
            P.dve(lambda e, ci=ci: e.memset(st[ci]["Sbf"][:], 0.0), writes=[("Sbf", ci)])
        for s in (range(nsteps) if 2 in phases else []):
            Ls = [Lazy(P) for _ in range(4)]
            for ci in range(4):
                chain_tile(ci, s, Ls[ci])
            interleave(Ls)

    if DBG.get("dump"):
        gd_ = P.dram("gates_o", [128, NTILE, 8], F32, "ExternalOutput")
        P.dma(gd_.ap(), gates[:], reads=[("gates", t_) for t_ in range(NTILE)])
    of = [P.sb([128, 2, 128], F32) for _ in range(2)]
    obk = [P.sb([128, 2, 128], F32) for _ in range(2)]
    zt = [P.sb([128, 256], BF16) for _ in range(2)]
    gz = P.sb([128, 256], F32, "gz")
    osum = P.sb([128, 2, 128], F32, "osum")
    jk = P.sb([128, 128], F32, "jk")
    s3 = P.sb([128, 8], F32, "s3")
    yo = [P.sb([128, 256], F32) for _ in range(2)]
    def phase3(hp):
        for t in (range(NTILE) if 3 in phases else []):
            b = t % 2
            for h in range(2):
                P.dma(of[b][:, h, :], o_s.ap()[0][h][t * 128:(t + 1) * 128, :], reads=[("o", 0, h, t)], writes=[("of", b)])
                P.dma(obk[b][:, h, :], o_s.ap()[1][h][t * 128:(t + 1) * 128, :], reads=[("o", 1, h, t)], writes=[("obk", b)])
            P.dma(zt[b][:], z_s.ap()[t * 128:(t + 1) * 128, :], reads=[("z", t)], writes=[("zt", b)])
            P.dve(lambda e, b=b: e.tensor_tensor(out=osum[:], in0=of[b][:], in1=obk[b][:], op=ALU.add),
                  reads=[("of", b), ("obk", b)], writes=["osum"])
            P.dve(lambda e, b=b: e.tensor_tensor(out=gz[:], in0=zt[b][:], in1=onorm[:].rearrange("p h d -> p (h d)"), op=ALU.mult),
                  reads=[("zt", b), "onorm"], writes=["gz"])
            for h in range(2):
                P.act(lambda e, h=h: e.activation(out=jk[:], in_=osum[:, h, :], func=AF.Square, accum_out=s3[:, h:h + 1]),
                      reads=["osum"], writes=["jk", "s3"])
            P.act(lambda e: e.activation(out=s3[:, 2:4], in_=s3[:, 0:2], func=AF.Sqrt, scale=1.0 / 128.0, bias=W["epsc"][:, 0:1]),
                  reads=["s3", "epsc"], writes=["s3"])
            P.dve(lambda e: e.reciprocal(out=s3[:, 4:6], in_=s3[:, 2:4]), reads=["s3"], writes=["s3"])
            for h in range(2):
                P.dve(lambda e, h=h, b=b: e.scalar_tensor_tensor(out=yo[b][:, h * 128:(h + 1) * 128], in0=osum[:, h, :],
                                                                 scalar=s3[:, 4 + h:5 + h], in1=gz[:, h * 128:(h + 1) * 128],
                                                                 op0=ALU.mult, op1=ALU.mult),
                      reads=["osum", "s3", "gz"], writes=[("yo", b)])
            if fusedm:
                P.dma(y_d.ap()[t * 128:(t + 1) * 128, hp * 256:(hp + 1) * 256], yo[b][:], reads=[("yo", b)], writes=[("gdn_o", t)])
            else:
                P.dma(y_d.ap()[t * 128:(t + 1) * 128, :], yo[b][:], reads=[("yo", b)])
    for hp in range(nhp):
        load_weights(hp)
        phase1(hp)
        phase2(hp)
        phase3(hp)
    if standalone:
        return P.build()
    P.pop_scope()


def gdn_masks():
    m = np.zeros((5, 128, 128), np.float32)
    kp = np.arange(128)[:, None]
    c = np.arange(128)[None, :]
    same = (kp // 64) == (c // 64)
    m[0] = (same & (kp <= c))
    m[1] = (same & (kp < c))
    m[2] = (same & (kp >= c))
    m[3] = (same & (kp > c))
    m[4] = np.eye(128)
    return m


def gdn_weights(hp, c_w_in, c_conv, a_log_f, dtb_f, a_log_b, dtb_b):
    w = c_w_in[0]
    hs = [2 * hp, 2 * hp + 1]
    cols = []
    for base in (0, 1024, 2048):
        for h in hs:
            cols.append(w[:, base + h * 128: base + (h + 1) * 128])
    wf = np.ascontiguousarray(np.concatenate(cols, axis=1))
    zc = [w[:, 3072 + h * 128:3072 + (h + 1) * 128] for h in hs]
    gb = 4096
    gcols = [w[:, gb + 0 + h:gb + 0 + h + 1] for h in hs] + [w[:, gb + 16 + h:gb + 16 + h + 1] for h in hs] + \
            [w[:, gb + 8 + h:gb + 8 + h + 1] for h in hs] + [w[:, gb + 24 + h:gb + 24 + h + 1] for h in hs]
    wt = np.ascontiguousarray(np.concatenate(zc + gcols, axis=1))
    cw = np.zeros((128, 6, 5), np.float32)
    bi = 0
    for base in (0, 1024, 2048):
        for h in hs:
            cw[:, bi, :] = c_conv[0][:, base + h * 128: base + (h + 1) * 128].T
            bi += 1
    gconst = np.array([dtb_f[0][hs[0]], dtb_f[0][hs[1]], dtb_b[0][hs[0]], dtb_b[0][hs[1]],
                       a_log_f[0][hs[0]], a_log_f[0][hs[1]], a_log_b[0][hs[0]], a_log_b[0][hs[1]]], np.float32)
    return wf, wt, cw, gconst


def gdn_inputs(x2, c_norm, c_w_in, c_conv, a_log_f, dtb_f, a_log_b, dtb_b, out_norm):
    import ml_dtypes
    w = c_w_in[0]
    ident = np.eye(128, dtype=np.float32).astype(ml_dtypes.bfloat16)
    masks = gdn_masks()
    maps = []
    for c in range(NCORES):
        b, hp = c // 4, c % 4
        hs = [2 * hp, 2 * hp + 1]
        cols = []
        for base in (0, 1024, 2048):
            for h in hs:
                cols.append(w[:, base + h * 128: base + (h + 1) * 128])
        wf = np.ascontiguousarray(np.concatenate(cols, axis=1))
        zc = [w[:, 3072 + h * 128:3072 + (h + 1) * 128] for h in hs]
        gb = 4096
        gcols = [w[:, gb + 0 + h:gb + 0 + h + 1] for h in hs] + [w[:, gb + 16 + h:gb + 16 + h + 1] for h in hs] + \
                [w[:, gb + 8 + h:gb + 8 + h + 1] for h in hs] + [w[:, gb + 24 + h:gb + 24 + h + 1] for h in hs]
        wt = np.ascontiguousarray(np.concatenate(zc + gcols, axis=1))
        cw = np.zeros((128, 6, 5), np.float32)
        bi = 0
        for base in (0, 1024, 2048):
            for h in hs:
                cw[:, bi, :] = c_conv[0][:, base + h * 128: base + (h + 1) * 128].T
                bi += 1
        gconst = np.array([[dtb_f[0][hs[0]], dtb_f[0][hs[1]], dtb_b[0][hs[0]], dtb_b[0][hs[1]],
                            a_log_f[0][hs[0]], a_log_f[0][hs[1]], a_log_b[0][hs[0]], a_log_b[0][hs[1]]]], np.float32)
        xp = np.zeros((SEQ + 128, DM), np.float32)
        xp[2:2 + SEQ] = x2[b]
        maps.append({"xp": xp, "g": c_norm[0:1], "wf": wf[None], "wt": wt[None], "cw": cw[None], "gconst": gconst,
                     "onorm": out_norm[0:1], "gmask": masks, "ident": ident})
    return maps


def build_l0():
    P = Prog()
    x1_s = P.dram("x1_s", [NTOK, DM], F32, "Internal")
    build_attn(P, "a_", x1_s)
    P.fence()
    build_ffn(1, False, P, "f_", x1_s)
    return P.build()


def build_fused():
    P = Prog()
    x1_s = P.dram("x1_s", [SEQ, DM], F32, "Internal")
    x2_s = P.dram("x2_s", [SEQ, DM], F32, "Internal")
    og_s = P.dram("og_s", [SEQ, DM], F32, "Internal")
    qoff = P.dram("m_sel", [1, 4], F32, "ExternalInput")
    build_attn(P, "a_", x1_s, nq=4)
    P.fence()
    H = ffn_handles(P, "f_", 1, False)
    for q in range(4):
        build_ffn(1, False, P, "f_", x_handle=x1_s, H=H, x_row0=q * NTOK, y_handle=x2_s, y_row0=q * NTOK)
    P.fence()
    build_gdn(P=P, pf="g_", x_handle=x2_s, o_handle=og_s, nhp=4)
    P.fence()
    build_ffn(8, True, P, "m_", x_handle=x2_s, o_handle=og_s, dyn=qoff)
    return P.build()


def kernel(x, ab_norm, ab_w_in, ab_q_norm_a, ab_k_norm_a, ab_sink_a, ab_q_norm_b, ab_k_norm_b,
                 ab_w_out, ffn_norm, ffn_w_gate, ffn_w_up, ffn_w_down, c_norm, c_w_in, c_conv,
                 c_a_log_fwd, c_dt_bias_fwd, c_a_log_bwd, c_dt_bias_bwd, c_out_norm, c_w_out,
                 moe_norm, moe_w_router, moe_w_gate, moe_w_up, moe_w_down):
    f = lambda a: np.ascontiguousarray(np.asarray(a, dtype=np.float32))
    x = f(x)
    cores = list(range(NCORES))
    ident = _ident_bf16()
    identf = np.eye(128, dtype=np.float32)
    amaps = attn_inputs(x, f(ab_norm), f(ab_w_in), f(ab_q_norm_a), f(ab_k_norm_a), f(ab_sink_a), f(ab_q_norm_b),
                        f(ab_k_norm_b), f(ab_w_out), fused=True)
    gw = [gdn_weights(hp, f(c_w_in), f(c_conv), f(c_a_log_fwd), f(c_dt_bias_fwd), f(c_a_log_bwd), f(c_dt_bias_bwd))
          for hp in range(4)]
    g_wf = np.ascontiguousarray(np.stack([w[0] for w in gw]))
    g_wt = np.ascontiguousarray(np.stack([w[1] for w in gw]))
    g_cw = np.ascontiguousarray(np.stack([w[2] for w in gw]))
    g_gc = np.ascontiguousarray(np.stack([w[3] for w in gw]))
    gmask = gdn_masks()
    maps = []
    for c in cores:
        m = {"a_" + k: v for k, v in amaps[c].items()}
        m.update({"f_g": f(ffn_norm)[0:1], "f_wg": f(ffn_w_gate), "f_wu": f(ffn_w_up), "f_wd": f(ffn_w_down), "f_ident": ident})
        m.update({"g_g": f(c_norm)[0:1], "g_wf": g_wf, "g_wt": g_wt, "g_cw": g_cw, "g_gconst": g_gc,
                  "g_onorm": f(c_out_norm)[0:1], "g_gmask": gmask, "g_ident": ident})
        m.update({"m_wo": f(c_w_out)[0], "m_g": f(moe_norm)[0:1], "m_wg": f(moe_w_gate)[0], "m_wu": f(moe_w_up)[0],
                  "m_wd": f(moe_w_down)[0], "m_wr": f(moe_w_router)[0], "m_ident": ident, "m_identf": identf,
                  "m_sel": np.eye(4, dtype=np.float32)[c % 4][None, :]})
        maps.append(m)
    res = run_bass_kernel_spmd(_prog("fused", build_fused), maps, core_ids=cores)
    y = np.concatenate([r["m_y"] for r in res.results], 0)
    return y.reshape(2, SEQ, DM).astype(np.float32)


_CACHE = {}
STAGES = {}


def _prog(name, fn):
    if name not in _CACHE:
        _CACHE[name] = fn()
    return _CACHE[name]


def _ident_bf16():
    import ml_dtypes
    return np.eye(128, dtype=np.float32).astype(ml_dtypes.bfloat16)


def kernel_unfused(x, ab_norm, ab_w_in, ab_q_norm_a, ab_k_norm_a, ab_sink_a, ab_q_norm_b, ab_k_norm_b,
           ab_w_out, ffn_norm, ffn_w_gate, ffn_w_up, ffn_w_down, c_norm, c_w_in, c_conv,
           c_a_log_fwd, c_dt_bias_fwd, c_a_log_bwd, c_dt_bias_bwd, c_out_norm, c_w_out,
           moe_norm, moe_w_router, moe_w_gate, moe_w_up, moe_w_down):
    f = lambda a: np.ascontiguousarray(np.asarray(a, dtype=np.float32))
    x = f(x)
    cores = list(range(NCORES))
    ident = _ident_bf16()
    identf = np.eye(128, dtype=np.float32)
    amaps = attn_inputs(x, f(ab_norm), f(ab_w_in), f(ab_q_norm_a), f(ab_k_norm_a), f(ab_sink_a), f(ab_q_norm_b),
                        f(ab_k_norm_b), f(ab_w_out))
    maps = []
    for c in cores:
        m = {"a_" + k: v for k, v in amaps[c].items()}
        m.update({"f_g": f(ffn_norm)[0:1], "f_wg": f(ffn_w_gate), "f_wu": f(ffn_w_up), "f_wd": f(ffn_w_down), "f_ident": ident})
        maps.append(m)
    res = run_bass_kernel_spmd(_prog("l0", build_l0), maps, core_ids=cores)
    x2 = np.concatenate([r["f_y"] for r in res.results], 0)
    STAGES["x2"] = x2
    maps = gdn_inputs(x2.reshape(2, SEQ, DM), f(c_norm), f(c_w_in), f(c_conv), f(c_a_log_fwd), f(c_dt_bias_fwd),
                      f(c_a_log_bwd), f(c_dt_bias_bwd), f(c_out_norm))
    res = run_bass_kernel_spmd(_prog("gdn", build_gdn), maps, core_ids=cores)
    o = np.zeros((2, SEQ, DM), np.float32)
    for c in cores:
        b, hp = c // 4, c % 4
        o[b][:, hp * 256:(hp + 1) * 256] = res.results[c]["y"]
    o = o.reshape(-1, DM)
    STAGES["o"] = o
    maps = [{"x": np.ascontiguousarray(x2[c * NTOK:(c + 1) * NTOK]), "o": np.ascontiguousarray(o[c * NTOK:(c + 1) * NTOK]),
             "wo": f(c_w_out)[0], "g": f(moe_norm)[0:1], "wg": f(moe_w_gate)[0], "wu": f(moe_w_up)[0], "wd": f(moe_w_down)[0],
             "wr": f(moe_w_router)[0], "ident": ident, "identf": identf} for c in cores]
    res = run_bass_kernel_spmd(_prog("moe", lambda: build_ffn(8, True)), maps, core_ids=cores)
    y = np.concatenate([r["y"] for r in res.results], 0)
    return y.reshape(2, SEQ, DM).astype(np.float32)
```

```python
import numpy as np
import concourse.bass as bass
import concourse.mybir as mybir
from concourse.bass_utils import run_bass_kernel_spmd
from contextlib import ExitStack

F32 = mybir.dt.float32
BF16 = mybir.dt.bfloat16
I32 = mybir.dt.int32
ALU = mybir.AluOpType
AF = mybir.ActivationFunctionType
AX = mybir.AxisListType

ENGS = ["tensor", "vector", "scalar", "gpsimd", "sync"]
NCORES = 8


class _Op:
    __slots__ = ("eng", "seq", "fn", "deps", "signal", "count", "dma", "dsem", "dcum", "snap")

    def __init__(self, eng, seq, fn, dma):
        self.eng = eng
        self.seq = seq
        self.fn = fn
        self.deps = []
        self.signal = False
        self.count = 0
        self.dma = dma
        self.dsem = -1
        self.dcum = 0
        self.snap = None


class Lazy:
    def __init__(self, P):
        self.P = P
        self.q = []
        self.grp = None

    def begin(self):
        self.grp = []

    def end(self):
        g, self.grp = self.grp, None
        self.q.append(lambda: [t() for t in g])

    def __getattr__(self, name):
        f = getattr(self.P, name)

        def rec(*a, **k):
            (self.q if self.grp is None else self.grp).append(lambda: f(*a, **k))
        return rec


def interleave(lazies):
    n = max(len(L.q) for L in lazies)
    for i in range(n):
        for L in lazies:
            if i < len(L.q):
                L.q[i]()


class Prog:
    DMA_RING = 12

    def __init__(self):
        self.nc = bass.Bass("TRN2", target_bir_lowering=False)
        self.es = ExitStack()
        self.ops = {e: [] for e in ENGS}
        self.lastw = {}
        self.readers = {}
        self.known = {e: {} for e in ENGS}
        self.knownd = {e: {} for e in ENGS}
        self.dmas = []
        self.ring_last = {}
        self.ring_cum = {}
        self.ring_next = {e: 0 for e in ENGS}
        self.n_sb = 0
        self.alias = {}
        self.scopes = []
        self.fence_t = self.es.enter_context(self.nc.sbuf_tensor("fence_t", [128, 64], BF16))
        self.nfence = 0

    def push_scope(self):
        self.scopes.append(self.es)
        self.es = ExitStack()

    def pop_scope(self):
        self.es.close()
        self.es = self.scopes.pop()
        self.alias = {}

    def fence(self):
        toks = []
        for e in ENGS:
            if self.ops[e] and not self.ops[e][-1].dma:
                toks.append(("e", e, self.ops[e][-1].seq))
        for key, idx in self.ring_last.items():
            toks.append(("d", idx))
        ft = self.fence_t
        n = self.nfence
        self.nfence += 1
        self.push_scope()
        pf = self.ps([128, 512], F32, f"fence_ps{n}")
        self.op("vector", lambda e: e.memset(ft[:, 0:16], 0.0), extra=toks)
        self.op("scalar", lambda e: e.copy(out=ft[:, 16:32], in_=ft[:, 16:32]), extra=toks)
        self.op("gpsimd", lambda e: e.memset(ft[:, 32:48], 0.0), extra=toks)
        self.op("tensor", lambda e: e.matmul(pf[0:1, 0:8], lhsT=ft[0:1, 48:49], rhs=ft[0:1, 48:56], start=True, stop=True),
                extra=toks)
        self.op("sync", lambda e: e.dma_start(out=ft[0:1, 56:60], in_=ft[0:1, 60:64]), extra=toks, dma=True)
        self.scopes_tmp = None
        self.es.close()
        self.es = self.scopes.pop()

    def bank(self, key, bank_id):
        self.alias[key] = ("BANK", bank_id)

    def sb(self, shape, dt, name=None):
        self.n_sb += 1
        return self.es.enter_context(self.nc.sbuf_tensor(f"sb{self.n_sb}_{name or ''}", list(shape), dt))

    def ps(self, shape, dt, name=None):
        self.n_sb += 1
        return self.es.enter_context(self.nc.psum_tensor(f"ps{self.n_sb}_{name or ''}", list(shape), dt))

    def dram(self, name, shape, dt, kind):
        return self.nc.dram_tensor(name, list(shape), dt, kind=kind)

    def _dep_tokens(self, reads, writes):
        deps = set()
        for k in reads:
            w = self.lastw.get(k)
            if w is not None:
                deps.add(w)
        for k in writes:
            w = self.lastw.get(k)
            if w is not None:
                deps.add(w)
            for r in self.readers.get(k, ()):
                deps.add(r)
        return deps

    def _need(self, eng, tok):
        if tok[0] == "e":
            _, se, sq = tok
            if se == eng and eng in ("tensor", "sync"):
                return False
            return self.known[eng].get(se, -1) < sq
        else:
            d = self.dmas[tok[1]]
            return self.knownd[eng].get(d.dsem, 0) < d.dcum

    def _learn(self, eng, tok):
        if tok[0] == "e":
            _, se, sq = tok
            src = self.ops[se][sq]
            src.signal = True
            k = self.known[eng]
            if k.get(se, -1) < sq:
                k[se] = sq
            if src.snap is not None:
                sk, sd = src.snap
                for a, b in sk.items():
                    if k.get(a, -1) < b:
                        k[a] = b
                kd = self.knownd[eng]
                for a, b in sd.items():
                    if kd.get(a, 0) < b:
                        kd[a] = b
        else:
            d = self.dmas[tok[1]]
            kd = self.knownd[eng]
            if kd.get(d.dsem, 0) < d.dcum:
                kd[d.dsem] = d.dcum

    def op(self, eng, fn, reads=(), writes=(), dma=False, extra=()):
        al = self.alias
        if al:
            excl = {al[k] for k in reads if k in al} | {al[k] for k in writes if k in al}
            reads = [k for k in reads if k not in al]
            writes = [k for k in writes if k not in al] + list(excl)
        deps = self._dep_tokens(reads, writes)
        deps.update(extra)
        seq = len(self.ops[eng])
        o = _Op(eng, seq, fn, dma)
        if dma:
            slot = self.ring_next[eng]
            self.ring_next[eng] = (slot + 1) % self.DMA_RING
            key = (eng, slot)
            prev = self.ring_last.get(key)
            if prev is not None:
                deps.add(("d", prev))
            o.dsem = key
            self.ring_cum[key] = self.ring_cum.get(key, 0) + 16
            o.dcum = self.ring_cum[key]
        for tok in sorted(deps, key=lambda t: (t[0], str(t[1]), t[2] if len(t) > 2 else 0)):
            if self._need(eng, tok):
                o.deps.append(tok)
                self._learn(eng, tok)
        o.snap = (dict(self.known[eng]), dict(self.knownd[eng]))
        self.ops[eng].append(o)
        if dma:
            idx = len(self.dmas)
            self.dmas.append(o)
            self.ring_last[o.dsem] = idx
            tok = ("d", idx)
        else:
            tok = ("e", eng, seq)
        for k in writes:
            self.lastw[k] = tok
            self.readers[k] = []
        for k in reads:
            if k not in writes:
                self.readers.setdefault(k, []).append(tok)
        return tok

    def pe(self, fn, reads=(), writes=()):
        return self.op("tensor", fn, reads, writes)

    def dve(self, fn, reads=(), writes=()):
        return self.op("vector", fn, reads, writes)

    def act(self, fn, reads=(), writes=()):
        return self.op("scalar", fn, reads, writes)

    def pool(self, fn, reads=(), writes=()):
        return self.op("gpsimd", fn, reads, writes)

    def dma(self, out, in_, reads=(), writes=(), q="sync", **kw):
        return self.op(q, lambda e: e.dma_start(out=out, in_=in_, **kw), reads, writes, dma=True)

    def build(self):
        nc = self.nc
        es = self.es
        esem = {e: es.enter_context(nc.semaphore(f"s_{e}")) for e in ENGS}
        dsem = {}
        for key in self.ring_cum:
            dsem[key] = es.enter_context(nc.semaphore(f"d_{key[0]}_{key[1]}"))
        for e in ENGS:
            c = 0
            for o in self.ops[e]:
                if o.signal and not o.dma:
                    c += 1
                o.count = c
        block = es.enter_context(nc.Block())
        ops = self.ops
        dmas = self.dmas
        ring_cum = self.ring_cum

        def emit(engname):
            def body(eng):
                for o in ops[engname]:
                    for tok in o.deps:
                        if tok[0] == "e":
                            eng.wait_ge(esem[tok[1]], ops[tok[1]][tok[2]].count)
                        else:
                            d = dmas[tok[1]]
                            eng.wait_ge(dsem[d.dsem], d.dcum)
                    ins = o.fn(eng)
                    if o.dma:
                        ins.then_inc(dsem[o.dsem], 16)
                    elif o.signal:
                        ins.then_inc(esem[engname], 1)
                for key, cum in ring_cum.items():
                    if key[0] == engname:
                        eng.wait_ge(dsem[key], cum)
            return body

        for e in ENGS:
            if not ops[e]:
                continue
            getattr(block, e)(emit(e))
        es.close()
        return nc


EPS = 1e-6


def bcast_rows(handle, ncols, nparts=128, offset=0):
    return bass.AP(handle, offset, [[0, nparts], [1, ncols]])


def emit_norm_T(P, x_ap, xkey, g_bc, dstT, dst_cols, dkey, W, uid):
    nc = P.nc
    xn = W["xn"][uid % 2]
    kxn = ("xn", uid % 2)
    psT = W["psT"]
    if g_bc is not None:
        junk = W["junk"]
        ss = W["ss"][uid % 2]
        kss = ("ss", uid % 2)
        P.act(lambda e: e.activation(out=junk[:], in_=x_ap, func=AF.Square, accum_out=ss[:, 0:1]),
              reads=[xkey], writes=[kss, "junk"])
        P.act(lambda e: e.activation(out=ss[:, 1:2], in_=ss[:, 0:1], func=AF.Sqrt,
                                     scale=1.0 / 1024.0, bias=W["epsc"][:, 0:1]),
              reads=[kss], writes=[kss])
        P.dve(lambda e: e.reciprocal(out=ss[:, 2:3], in_=ss[:, 1:2]), reads=[kss], writes=[kss])
        P.dve(lambda e: e.scalar_tensor_tensor(out=xn[:], in0=x_ap, scalar=ss[:, 2:3], in1=g_bc[:],
                                               op0=ALU.mult, op1=ALU.mult),
              reads=[xkey, kss, "gbc"], writes=[kxn])
    else:
        P.dve(lambda e: e.tensor_copy(out=xn[:], in_=x_ap), reads=[xkey], writes=[kxn])

    def tr(e):
        ins = None
        for kc in range(8):
            ins = e.transpose(out=psT[:, kc * 128:(kc + 1) * 128], in_=xn[:, kc * 128:(kc + 1) * 128],
                              identity=W["ident"][:])
        return ins
    P.pe(tr, reads=[kxn, "ident"], writes=["psT"])
    P.act(lambda e: e.copy(out=dstT[:, :, dst_cols], in_=psT[:].rearrange("p (k c) -> p k c", k=8)),
          reads=["psT"], writes=[dkey])


def alloc_norm_work(P):
    W = {}
    W["xn"] = [P.sb([128, 1024], BF16) for _ in range(2)]
    W["junk"] = P.sb([128, 1024], BF16)
    W["ss"] = [P.sb([128, 4], F32) for _ in range(2)]
    W["psT"] = P.ps([128, 1024], BF16)
    W["ident"] = P.sb([128, 128], BF16)
    W["epsc"] = P.sb([128, 1], F32)
    return W


NTOK = 2048
NT = NTOK // 128
DM = 1024
DFF = 3584
NF = DFF // 128
FR = 7


def ffn_handles(P, pf, n_exp, with_proj):
    H = {}
    H["g"] = P.dram(pf + "g", [1, DM], F32, "ExternalInput")
    H["wg"] = P.dram(pf + "wg", [n_exp, DM, DFF], F32, "ExternalInput")
    H["wu"] = P.dram(pf + "wu", [n_exp, DM, DFF], F32, "ExternalInput")
    H["wd"] = P.dram(pf + "wd", [n_exp, DFF, DM], F32, "ExternalInput")
    H["ident"] = P.dram(pf + "ident", [128, 128], BF16, "ExternalInput")
    if n_exp > 1:
        H["wr"] = P.dram(pf + "wr", [DM, n_exp], F32, "ExternalInput")
        H["identf"] = P.dram(pf + "identf", [128, 128], F32, "ExternalInput")
    if with_proj:
        H["wo"] = P.dram(pf + "wo", [DM, DM], F32, "ExternalInput")
    return H


def build_ffn(n_exp, with_proj, P=None, pf="", x_handle=None, o_handle=None, H=None, x_row0=0, y_handle=None, y_row0=0, dyn=None):
    standalone = P is None
    if standalone:
        P = Prog()
    nc = P.nc
    if not standalone:
        P.push_scope()
    x_d = x_handle if x_handle is not None else P.dram(pf + "x", [NTOK, DM], F32, "ExternalInput")
    if H is None:
        H = ffn_handles(P, pf, n_exp, with_proj)
    g_d, wg_d, wu_d, wd_d, id_d = H["g"], H["wg"], H["wu"], H["wd"], H["ident"]
    y_d = y_handle if y_handle is not None else P.dram(pf + "y", [NTOK, DM], F32, "ExternalOutput")
    if n_exp > 1:
        wr_d, idf_d = H["wr"], H["identf"]
    if with_proj:
        o_d = o_handle if o_handle is not None else P.dram(pf + "o", [NTOK, DM], F32, "ExternalInput")
        wo_d = H["wo"]
    W = alloc_norm_work(P)
    yacc = P.sb([128, NT, DM], F32, "yacc")
    xnT = P.sb([128, 8, NTOK], BF16, "xnT")
    gbc = P.sb([128, DM], F32, "gbc")
    actT = P.sb([128, FR, NTOK], BF16, "actT")
    wgc = [P.sb([128, 8, 128], BF16) for _ in range(3)]
    wuc = [P.sb([128, 8, 128], BF16) for _ in range(3)]
    wdc = [P.sb([128, DM], BF16) for _ in range(FR + 3)]
    sg = [P.sb([128, 512], F32) for _ in range(2)]
    psG = [P.ps([128, 512], F32) for _ in range(2)]
    psU = [P.ps([128, 512], F32) for _ in range(2)]
    psY = [P.ps([128, 512], F32) for _ in range(2)]
    W["psRL"] = P.ps([128, 512], F32)
    for i_, k_ in enumerate(["psT", "psRL", ("psG", 0), ("psG", 1), ("psU", 0), ("psU", 1), ("psY", 0), ("psY", 1)]):
        P.bank(k_, i_)

    P.dma(W["ident"][:], id_d.ap(), writes=["ident"])
    P.dma(gbc[:], bcast_rows(g_d, DM), writes=["gbc"])
    P.dve(lambda e: e.memset(W["epsc"][:], EPS), writes=["epsc"])
    if dyn is not None:
        sel = P.sb([128, 4], F32, "sel")
        actf = actT.bitcast(F32)
        selt = [actf[:, b_, :] for b_ in range(2)]
        seltk = [[("actT", b_, tg_) for tg_ in range(4)] for b_ in range(2)]
        P.dma(sel[:], bcast_rows(dyn, 4), writes=["sel"])
        seli = [0]

        def load_sel(handle, dst_ap, dkey, t):
            for q in range(4):
                b = seli[0] % 2
                seli[0] += 1
                P.dma(selt[b], handle.ap()[q * NTOK + t * 128:q * NTOK + (t + 1) * 128, :], writes=seltk[b])
                if q == 0:
                    P.dve(lambda e, b=b: e.tensor_scalar(out=dst_ap, in0=selt[b], scalar1=sel[:, 0:1], scalar2=None,
                                                        op0=ALU.mult), reads=seltk[b] + ["sel"], writes=[dkey])
                else:
                    P.dve(lambda e, b=b, q=q: e.scalar_tensor_tensor(out=dst_ap, in0=selt[b], scalar=sel[:, q:q + 1],
                                                                     in1=dst_ap, op0=ALU.mult, op1=ALU.add),
                          reads=seltk[b] + ["sel", dkey], writes=[dkey])
    for t in range(NT):
        if dyn is not None:
            load_sel(x_d, yacc[:, t, :], ("y", t), t)
        else:
            r0 = x_row0 + t * 128
            P.dma(yacc[:, t, :], x_d.ap()[r0:r0 + 128, :], reads=[("x1s", r0 // 128)], writes=[("y", t)])

    if with_proj:
        wo = P.sb([128, 8, DM], BF16, "wo")
        P.dma(wo[:], wo_d.ap().rearrange("(k p) c -> p k c", p=128), writes=["wo"], q="gpsimd")
        ot = [P.sb([128, DM], F32) for _ in range(2)]
        for t in range(NT):
            if dyn is not None:
                load_sel(o_d, ot[t % 2][:], ("ot", t % 2), t)
            else:
                r0 = x_row0 + t * 128
                P.dma(ot[t % 2][:], o_d.ap()[r0:r0 + 128, :], writes=[("ot", t % 2)])
            emit_norm_T(P, ot[t % 2][:], ("ot", t % 2), None, xnT, slice(t * 128, (t + 1) * 128), ("xnT", t), W, t)
            for h in range(2):
                def mm(e, t=t, h=h):
                    ins = None
                    for kc in range(8):
                        ins = e.matmul(psY[h][:], lhsT=xnT[:, kc, t * 128:(t + 1) * 128],
                                       rhs=wo[:, kc, h * 512:(h + 1) * 512], start=(kc == 0), stop=(kc == 7))
                    return ins
                P.pe(mm, reads=[("xnT", t), "wo"], writes=[("psY", h)])
                P.dve(lambda e, t=t, h=h: e.tensor_tensor(out=yacc[:, t, h * 512:(h + 1) * 512], in0=psY[h][:],
                                                          in1=yacc[:, t, h * 512:(h + 1) * 512], op=ALU.add),
                      reads=[("psY", h), ("y", t)], writes=[("y", t)])

    for t in range(NT):
        emit_norm_T(P, yacc[:, t, :], ("y", t), gbc, xnT, slice(t * 128, (t + 1) * 128), ("xnT", t), W, t)
    allx = [("xnT", t) for t in range(NT)]

    comb = None
    if n_exp > 1:
        W["psRA"], W["psRB"] = psG[0], psG[1]
        comb = emit_router_keys(P, yacc, gbc, W, wr_d, idf_d, n_exp)

    ci = 0
    di = 0
    for ex in range(n_exp):
        wgv = wg_d.ap()[ex].rearrange("(k p) c -> p k c", p=128)
        wuv = wu_d.ap()[ex].rearrange("(k p) c -> p k c", p=128)
        wdv = wd_d.ap()[ex].rearrange("(f p) c -> p f c", p=128)
        for r in range(NF // FR):
            dslots = []
            for fi in range(FR):
                f = r * FR + fi
                cs = ci % 3
                ci += 1
                P.dma(wgc[cs][:], wgv[:, :, f * 128:(f + 1) * 128], writes=[("wgc", cs)], q="gpsimd")
                P.dma(wuc[cs][:], wuv[:, :, f * 128:(f + 1) * 128], writes=[("wuc", cs)], q="gpsimd")
                ds = di % (FR + 3)
                di += 1
                dslots.append(ds)
                P.dma(wdc[ds][:], wdv[:, f, :], writes=[("wdc", ds)], q="gpsimd")
                for tg in range(4):
                    b = (fi * 4 + tg) % 2

                    def mmg(e, cs=cs, tg=tg, b=b):
                        ins = None
                        for kc in range(8):
                            ins = e.matmul(psG[b][:], lhsT=wgc[cs][:, kc, :], rhs=xnT[:, kc, tg * 512:(tg + 1) * 512],
                                           start=(kc == 0), stop=(kc == 7))
                        return ins

                    def mmu(e, cs=cs, tg=tg, b=b):
                        ins = None
                        for kc in range(8):
                            ins = e.matmul(psU[b][:], lhsT=wuc[cs][:, kc, :], rhs=xnT[:, kc, tg * 512:(tg + 1) * 512],
                                           start=(kc == 0), stop=(kc == 7))
                        return ins
                    xk = [("xnT", t) for t in range(tg * 4, tg * 4 + 4)]
                    P.pe(mmg, reads=[("wgc", cs)] + xk, writes=[("psG", b)])
                    P.pe(mmu, reads=[("wuc", cs)] + xk, writes=[("psU", b)])
                    P.act(lambda e, b=b: e.activation(out=sg[b][:], in_=psG[b][:], func=AF.Silu),
                          reads=[("psG", b)], writes=[("sg", b)])
                    P.dve(lambda e, b=b, fi=fi, tg=tg: e.tensor_tensor(out=actT[:, fi, tg * 512:(tg + 1) * 512],
                                                                       in0=sg[b][:], in1=psU[b][:], op=ALU.mult),
                          reads=[("sg", b), ("psU", b)], writes=[("actT", fi, tg)])
            for t in range(NT):
                for h in range(2):
                    def mmd(e, t=t, h=h, dslots=dslots):
                        ins = None
                        for fi in range(FR):
                            ins = e.matmul(psY[h][:], lhsT=actT[:, fi, t * 128:(t + 1) * 128],
                                           rhs=wdc[dslots[fi]][:, h * 512:(h + 1) * 512],
                                           start=(fi == 0), stop=(fi == FR - 1))
                        return ins
                    P.pe(mmd, reads=[("actT", fi, t // 4) for fi in range(FR)] + [("wdc", s) for s in dslots],
                         writes=[("psY", h)])
                    if comb is None:
                        P.dve(lambda e, t=t, h=h: e.tensor_tensor(out=yacc[:, t, h * 512:(h + 1) * 512], in0=psY[h][:],
                                                                  in1=yacc[:, t, h * 512:(h + 1) * 512], op=ALU.add),
                              reads=[("psY", h), ("y", t)], writes=[("y", t)])
                    else:
                        P.dve(lambda e, t=t, h=h, ex=ex: e.scalar_tensor_tensor(
                            out=yacc[:, t, h * 512:(h + 1) * 512], in0=psY[h][:],
                            scalar=comb[:, t, ex:ex + 1], in1=yacc[:, t, h * 512:(h + 1) * 512],
                            op0=ALU.mult, op1=ALU.add),
                            reads=[("psY", h), ("y", t), "comb"], writes=[("y", t)])
    yv = y_d.ap()[y_row0:y_row0 + NTOK, :].rearrange("(t p) d -> p t d", p=128)
    for t4 in range(4):
        P.dma(yv[:, t4 * 4:(t4 + 1) * 4, :], yacc[:, t4 * 4:(t4 + 1) * 4, :],
              reads=[("y", t) for t in range(t4 * 4, t4 * 4 + 4)],
              writes=[("yout", pf, (y_row0 // 128) + t) for t in range(t4 * 4, t4 * 4 + 4)])
    if standalone:
        return P.build()
    P.pop_scope()


def emit_router_keys(P, yacc, gbc, W, wr_d, idf_d, n_exp):
    identf = P.sb([128, 128], F32, "identf")
    wr = P.sb([128, 8, n_exp], F32, "wr")
    comb = P.sb([128, NT, n_exp], F32, "comb")
    x32 = P.sb([128, DM], F32, "rx32")
    xT32 = P.sb([128, 8, 128], F32, "rxT32")
    rs = P.sb([128, 16], F32, "rsmall")
    lg = P.sb([128, 8], F32, "rlg")
    mx = P.sb([128, 8], F32, "rmx")
    tmp = P.sb([128, 8], F32, "rtmp")
    psA = W["psRA"]
    psB = W["psRB"]
    psL = W["psRL"]
    P.dma(identf[:], idf_d.ap(), writes=["identf"])
    P.dma(wr[:], wr_d.ap().rearrange("(k p) e -> p k e", p=128), writes=["wr"])
    for t in range(NT):
        xt = yacc[:, t, :]
        P.act(lambda e, xt=xt: e.activation(out=W["junk"][:], in_=xt, func=AF.Square, accum_out=rs[:, 0:1]),
              reads=[("y", t)], writes=["junk", "rs"])
        P.act(lambda e: e.activation(out=rs[:, 1:2], in_=rs[:, 0:1], func=AF.Sqrt, scale=1.0 / 1024.0,
                                     bias=W["epsc"][:, 0:1]), reads=["rs"], writes=["rs"])
        P.dve(lambda e: e.reciprocal(out=rs[:, 2:3], in_=rs[:, 1:2]), reads=["rs"], writes=["rs"])
        P.dve(lambda e, xt=xt: e.scalar_tensor_tensor(out=x32[:], in0=xt, scalar=rs[:, 2:3], in1=gbc[:],
                                                      op0=ALU.mult, op1=ALU.mult),
              reads=[("y", t), "rs", "gbc"], writes=["rx32"])

        def tr(e):
            ins = None
            for kc in range(8):
                dst = (psA if kc < 4 else psB)[:, (kc % 4) * 128:(kc % 4 + 1) * 128]
                ins = e.transpose(out=dst, in_=x32[:, kc * 128:(kc + 1) * 128], identity=identf[:])
            return ins
        P.pe(tr, reads=["rx32", "identf"], writes=[("psG", 0), ("psG", 1)])
        P.act(lambda e: e.copy(out=xT32[:, 0:4, :], in_=psA[:].rearrange("p (k c) -> p k c", k=4)),
              reads=[("psG", 0)], writes=["rxTa"])
        P.act(lambda e: e.copy(out=xT32[:, 4:8, :], in_=psB[:].rearrange("p (k c) -> p k c", k=4)),
              reads=[("psG", 1)], writes=["rxTb"])

        def mm(e):
            ins = None
            for kc in range(8):
                ins = e.matmul(psL[:, 0:n_exp], lhsT=xT32[:, kc, :], rhs=wr[:, kc, :], start=(kc == 0), stop=(kc == 7))
            return ins
        P.pe(mm, reads=["rxTa", "rxTb", "wr"], writes=["psRL"])
        P.dve(lambda e: e.tensor_copy(out=lg[:], in_=psL[:, 0:n_exp]), reads=["psRL"], writes=["rlg"])
        P.dve(lambda e: e.max(out=mx[:], in_=lg[:]), reads=["rlg"], writes=["rmx"])
        P.dve(lambda e: e.tensor_tensor(out=rs[:, 4:5], in0=mx[:, 1:2], in1=mx[:, 0:1], op=ALU.subtract),
              reads=["rmx"], writes=["rs"])
        P.act(lambda e: e.activation(out=rs[:, 5:6], in_=rs[:, 4:5], func=AF.Exp), reads=["rs"], writes=["rs"])
        P.dve(lambda e: e.tensor_scalar(out=rs[:, 6:7], in0=rs[:, 5:6], scalar1=1.0, scalar2=None, op0=ALU.add),
              reads=["rs"], writes=["rs"])
        P.dve(lambda e: e.reciprocal(out=rs[:, 7:8], in_=rs[:, 6:7]), reads=["rs"], writes=["rs"])
        P.dve(lambda e: e.tensor_tensor(out=rs[:, 8:9], in0=rs[:, 5:6], in1=rs[:, 7:8], op=ALU.mult),
              reads=["rs"], writes=["rs"])
        P.dve(lambda e, t=t: e.tensor_scalar(out=comb[:, t, :], in0=lg[:], scalar1=mx[:, 0:1], scalar2=rs[:, 7:8],
                                             op0=ALU.is_equal, op1=ALU.mult),
              reads=["rlg", "rmx", "rs"], writes=["comb"])
        P.dve(lambda e: e.tensor_scalar(out=tmp[:], in0=lg[:], scalar1=mx[:, 1:2], scalar2=rs[:, 8:9],
                                        op0=ALU.is_equal, op1=ALU.mult),
              reads=["rlg", "rmx", "rs"], writes=["rtmp"])
        P.dve(lambda e, t=t: e.tensor_tensor(out=comb[:, t, :], in0=comb[:, t, :], in1=tmp[:], op=ALU.add),
              reads=["rtmp", "comb"], writes=["comb"])
    return comb


SEQ = 8192
NG = SEQ // 512
KA_GROUPS = {0: 0, 1: 1, 2: 2, 3: 3, 4: 4, 15: 5}


def build_attn(P=None, pf="", y_handle=None, nq=1):
    standalone = P is None
    if standalone:
        P = Prog()
    nc = P.nc
    if not standalone:
        P.push_scope()
    xr_d = P.dram(pf + "xr", [SEQ, DM], F32, "ExternalInput")
    g_d = P.dram(pf + "g", [1, DM], F32, "ExternalInput")
    wq_d = P.dram(pf + "wq", [DM, 1024], F32, "ExternalInput")
    wk_d = P.dram(pf + "wk", [DM, 256], F32, "ExternalInput")
    wv_d = P.dram(pf + "wv", [DM, 256], F32, "ExternalInput")
    wo_d = P.dram(pf + "wo", [64, 16, DM], F32, "ExternalInput")
    gains_d = P.dram(pf + "gains", [128, 4], F32, "ExternalInput")
    rope_d = P.dram(pf + "rope", [4, 128, SEQ], F32, "ExternalInput")
    rmat_d = P.dram(pf + "rmat", [3, 128, 128], BF16, "ExternalInput")
    masks_d = P.dram(pf + "masks", [4, 128, 512], BF16, "ExternalInput")
    sink_d = P.dram(pf + "sink", [1, 8], F32, "ExternalInput")
    id_d = P.dram(pf + "ident", [128, 128], BF16, "ExternalInput")
    y_d = y_handle if y_handle is not None else P.dram(pf + "y", [NTOK, DM], F32, "ExternalOutput")
    full = nq > 1
    ka_groups = {G_: G_ for G_ in range(NG)} if full else KA_GROUPS
    nka = 64 if full else 24

    W = alloc_norm_work(P)
    gbc = P.sb([128, DM], F32, "gbc")
    wq = P.sb([128, 8, 1024], BF16, "wq")
    wk = P.sb([128, 8, 256], BF16, "wk")
    wv = P.sb([128, 8, 256], BF16, "wv")
    wo = P.sb([64, 16, DM], BF16, "wo")
    gains = P.sb([128, 4], F32, "gains")
    rmat = P.sb([128, 3, 128], BF16, "rmat")
    masks = P.sb([128, 4, 512], BF16, "masks")
    KTa = P.sb([128, nka * 128], BF16, "KTa")
    KTb = P.sb([128, SEQ], BF16, "KTb")
    Va = P.sb([128, nka, 2, 65], BF16, "Va")
    Vb = P.sb([128, 64, 2, 65], BF16, "Vb")
    QTa = P.sb([128, 4, 4, 128], BF16, "QTa")
    QTb = P.sb([128, 4, 4, 128], BF16, "QTb")
    xt = [P.sb([128, DM], F32) for _ in range(2)]
    xng = [P.sb([128, 8, 512], BF16) for _ in range(1 if full else 2)]
    tab = [P.sb([128, 512], F32) for _ in range(4)]
    qg = P.sb([128, 512], BF16, "qg")
    sq = P.sb([128, 512], BF16, "sq")
    lnt = P.sb([128, 512], F32, "lnt")
    rstd = P.sb([128, 512], F32, "rstd")
    t1 = P.sb([128, 512], F32, "t1")
    t2 = P.sb([128, 512], F32, "t2")
    pT = [P.sb([128, 512], BF16) for _ in range(6)]
    den = P.sb([65, 512], F32, "den")
    nxng = len(xng)
    NTq = 16 * nq
    onesf = P.sb([65, 64], F32, "onesf")
    sinkrow = P.sb([64, 1024], F32, "sinkrow")
    sink8 = P.sb([64, 8], F32, "sink8")
    denb = P.sb([64, 512], F32, "denb")
    lnr = P.sb([64, 512], F32, "lnr") if not full else None
    rec = P.sb([64, 512], F32, "rec") if not full else None
    OT = [P.sb([64, 16, 128], BF16) for _ in range(2)]
    xres = P.sb([128, DM], F32, "xres") if not full else None
    ysb = P.sb([128, DM], F32, "ysb")
    if full:
        lnr_ap, lnr_k, rec_ap, rec_k = lnt[0:64, :], "lnt", rstd[0:64, :], "rstd"
    else:
        lnr_ap, lnr_k, rec_ap, rec_k = lnr[:], "lnr", rec[:], "rec"
    b0 = W["psT"]
    bk = [None] + [P.ps([128, 512], F32) for _ in range(7)]

    def BK(i):
        return ("bank", i)
    P.bank("psT", 0)
    for i_ in range(1, 8):
        P.bank(BK(i_), i_)

    P.dma(W["ident"][:], id_d.ap(), writes=["ident"])
    P.dma(gbc[:], bcast_rows(g_d, DM), writes=["gbc"])
    P.dve(lambda e: e.memset(W["epsc"][:], EPS), writes=["epsc"])
    P.dma(gains[:], gains_d.ap(), writes=["gains"])
    P.dma(rmat[:], rmat_d.ap().rearrange("r p c -> p r c"), writes=["rmat"])
    P.dma(masks[:], masks_d.ap().rearrange("r p c -> p r c"), writes=["masks"])
    P.dma(wq[:], wq_d.ap().rearrange("(k p) c -> p k c", p=128), writes=["wq"], q="gpsimd")
    P.dma(wk[:], wk_d.ap().rearrange("(k p) c -> p k c", p=128), writes=["wk"], q="gpsimd")
    P.dma(wv[:], wv_d.ap().rearrange("(k p) c -> p k c", p=128), writes=["wv"], q="gpsimd")
    P.dma(wo[:], wo_d.ap(), writes=["wo"], q="gpsimd")
    P.dve(lambda e: e.memset(Va[:, :, :, 64:65], 1.0), writes=["Va1"])
    P.dve(lambda e: e.memset(Vb[:, :, :, 64:65], 1.0), writes=["Vb1"])
    P.dve(lambda e: e.memset(onesf[:], 1.0), writes=["onesf"])
    P.dma(sink8[:], bcast_rows(sink_d, 8, 64), writes=["sink8"])
    P.act(lambda e: e.activation(out=sink8[:], in_=sink8[:], func=AF.Exp), reads=["sink8"], writes=["sink8"])
    P.dve(lambda e: e.memset(sinkrow[:], 0.0), writes=["sinkrow"])
    for h in range(8):
        P.dve(lambda e, h=h: e.tensor_scalar(out=sinkrow[:, h * 128:(h + 1) * 128], in0=sinkrow[:, h * 128:(h + 1) * 128],
                                             scalar1=sink8[:, h:h + 1], scalar2=None, op0=ALU.add),
              reads=["sinkrow", "sink8"], writes=["sinkrow"])

    xrv = xr_d.ap().rearrange("(t p) d -> p t d", p=128)

    def load_group(G):
        pb = G % nxng
        for tl in range(4):
            tg = G * 4 + tl
            P.dma(xt[tg % 2][:], xrv[:, tg, :], writes=[("xt", tg % 2)])
            emit_norm_T(P, xt[tg % 2][:], ("xt", tg % 2), gbc, xng[pb], slice(tl * 128, (tl + 1) * 128),
                        ("xng", pb, tl), W, tg)
        for i in range(4):
            P.dma(tab[i][:], rope_d.ap()[i][:, G * 512:(G + 1) * 512], writes=[("tab", i)])
        return pb

    qkc = [0]

    def qk_block(pb, lhs_fn, gcol, ti, ri, dest3, dkey):
        b = 1 + (qkc[0] % 2)
        qkc[0] += 1
        xk = [("xng", pb, tl) for tl in range(4)]

        def mm(e):
            ins = None
            for kc in range(8):
                ins = e.matmul(bk[b][:], lhsT=lhs_fn(kc), rhs=xng[pb][:, kc, :], start=(kc == 0), stop=(kc == 7))
            return ins
        P.pe(mm, reads=xk + ["wq", "wk"], writes=[BK(b)])
        P.act(lambda e: e.activation(out=qg[:], in_=bk[b][:], func=AF.Copy, scale=gains[:, gcol:gcol + 1]),
              reads=[BK(b), "gains"], writes=["qg"])
        P.act(lambda e: e.activation(out=sq[:], in_=bk[b][:], func=AF.Square), reads=[BK(b)], writes=["sq"])
        P.pe(lambda e: e.matmul(bk[3][:], lhsT=rmat[:, ri, :], rhs=qg[:], start=True, stop=True),
             reads=["qg", "rmat"], writes=[BK(3)])
        P.pe(lambda e: e.matmul(bk[4][:], lhsT=rmat[:, 2, :], rhs=sq[:], start=True, stop=True),
             reads=["sq", "rmat"], writes=[BK(4)])
        P.act(lambda e: e.activation(out=lnt[:], in_=bk[4][:], func=AF.Ln, scale=1.0 / 64.0, bias=W["epsc"][:, 0:1]),
              reads=[BK(4), "epsc"], writes=["lnt"])
        P.act(lambda e: e.activation(out=rstd[:], in_=lnt[:], func=AF.Exp, scale=-0.5), reads=["lnt"], writes=["rstd"])
        P.dve(lambda e: e.tensor_tensor(out=t1[:], in0=qg[:], in1=tab[ti][:], op=ALU.mult),
              reads=["qg", ("tab", ti)], writes=["t1"])
        P.dve(lambda e: e.tensor_tensor(out=t2[:], in0=bk[3][:], in1=tab[ti + 1][:], op=ALU.mult),
              reads=[BK(3), ("tab", ti + 1)], writes=["t2"])
        P.dve(lambda e: e.tensor_tensor(out=t1[:], in0=t1[:], in1=t2[:], op=ALU.add), reads=["t1", "t2"], writes=["t1"])
        P.dve(lambda e: e.tensor_tensor(out=dest3, in0=t1[:].rearrange("p (a b) -> p a b", a=4),
                                        in1=rstd[:].rearrange("p (a b) -> p a b", a=4), op=ALU.mult),
              reads=["t1", "rstd"], writes=[dkey])

    for G in range(NG):
        pb = load_group(G)
        qk_block(pb, lambda kc: wk[:, kc, 128:256], 3, 2, 1,
                 KTb[:, G * 512:(G + 1) * 512].rearrange("p (a b) -> p a b", a=4), ("KTb", G))
        sg = ka_groups.get(G)
        if sg is not None:
            qk_block(pb, lambda kc: wk[:, kc, 0:128], 1, 0, 0,
                     KTa[:, sg * 512:(sg + 1) * 512].rearrange("p (a b) -> p a b", a=4), ("KTa", sg))
        for tl in range(4):
            tg = G * 4 + tl

            def mmv(e, tl=tl, pb=pb):
                ins = None
                for kc in range(8):
                    ins = e.matmul(bk[5][:, 0:256], lhsT=xng[pb][:, kc, tl * 128:(tl + 1) * 128], rhs=wv[:, kc, :],
                                   start=(kc == 0), stop=(kc == 7))
                return ins
            P.pe(mmv, reads=[("xng", pb, tl), "wv"], writes=[BK(5)])
            P.act(lambda e, tg=tg: e.copy(out=Vb[:, tg, :, 0:64], in_=bk[5][:, 128:256].rearrange("p (h d) -> p h d", h=2)),
                  reads=[BK(5)], writes=[("Vb", tg)])
            if sg is not None:
                sl = sg * 4 + tl
                P.act(lambda e, sl=sl: e.copy(out=Va[:, sl, :, 0:64], in_=bk[5][:, 0:128].rearrange("p (h d) -> p h d", h=2)),
                      reads=[BK(5)], writes=[("Va", sl)])

    pcount = [0]
    ocount = [0]
    D = 3

    def attend(R, kind, tl, g, slots, mk, obuf, hbase):
        QT = QTa if kind == "a" else QTb
        KT = KTa if kind == "a" else KTb
        V = Va if kind == "a" else Vb
        ob = 4 + (ocount[0] % 2)
        ocount[0] += 1
        n = len(slots)
        hist = []
        for i in range(n + D):
            if i < n:
                s = slots[i]
                sb_ = (1, 2, 3, 6)[pcount[0] % 4]
                pb_ = pcount[0] % 6
                pcount[0] += 1
                hist.append(pb_)
                kkey = ("KTa", s // 4) if kind == "a" else ("KTb", s // 4)
                R.pe(lambda e, s=s, sb_=sb_: e.matmul(bk[sb_][:], lhsT=KT[g * 64:(g + 1) * 64, s * 128:(s + 1) * 128],
                                                       rhs=QT[g * 64:(g + 1) * 64, tl, :, :], start=True, stop=True),
                     reads=[kkey, ("QT", kind)], writes=[BK(sb_)])
                R.act(lambda e, sb_=sb_, pb_=pb_: e.activation(out=pT[pb_][:], in_=bk[sb_][:], func=AF.Exp, scale=0.125),
                      reads=[BK(sb_)], writes=[("pT", pb_)])
                if mk[i] is not None:
                    R.dve(lambda e, pb_=pb_, m=mk[i]: e.tensor_tensor(out=pT[pb_][:], in0=pT[pb_][:], in1=masks[:, m, :],
                                                                      op=ALU.mult),
                          reads=[("pT", pb_), "masks"], writes=[("pT", pb_)])
            if i >= D:
                j = i - D
                s = slots[j]
                pb_ = hist[j]
                vkey = ("Va", s) if kind == "a" else ("Vb", s)
                R.pe(lambda e, s=s, pb_=pb_, j=j: e.matmul(bk[ob][0:65, :], lhsT=V[:, s, g, :], rhs=pT[pb_][:],
                                                            start=(j == 0), stop=(j == n - 1)),
                     reads=[vkey, ("pT", pb_), "Va1", "Vb1"], writes=[BK(ob)])
        R.dve(lambda e: e.tensor_copy(out=den[64:65, :], in_=bk[ob][64:65, :]), reads=[BK(ob)], writes=["den"])
        R.pe(lambda e: e.matmul(bk[6][0:64, :], lhsT=onesf[64:65, :], rhs=den[64:65, :], start=True, stop=True),
             reads=["den", "onesf"], writes=[BK(6)])
        if kind == "a":
            R.dve(lambda e: e.tensor_tensor(out=denb[:], in0=bk[6][0:64, :], in1=sinkrow[:, g * 512:(g + 1) * 512],
                                            op=ALU.add), reads=[BK(6), "sinkrow"], writes=["denb"])
            R.act(lambda e: e.activation(out=lnr_ap, in_=denb[:], func=AF.Ln), reads=["denb"], writes=[lnr_k])
        else:
            R.act(lambda e: e.activation(out=lnr_ap, in_=bk[6][0:64, :], func=AF.Ln), reads=[BK(6)], writes=[lnr_k])
        R.act(lambda e: e.activation(out=rec_ap, in_=lnr_ap, func=AF.Exp, scale=-1.0), reads=[lnr_k], writes=[rec_k])
        h0 = hbase + 4 * g
        R.dve(lambda e: e.tensor_tensor(out=OT[obuf][:, h0:h0 + 4, :],
                                        in0=bk[ob][0:64, :].rearrange("p (a b) -> p a b", a=4),
                                        in1=rec_ap.rearrange("p (a b) -> p a b", a=4), op=ALU.mult),
              reads=[BK(ob), rec_k], writes=[("OT", obuf, kind, g)])

    yv = y_d.ap().rearrange("(t p) d -> p t d", p=128)
    for G in range(4 * nq):
        pb = load_group(G)
        for j in range(4):
            qk_block(pb, lambda kc, j=j: wq[:, kc, j * 128:(j + 1) * 128], 0, 0, 0, QTa[:, :, j, :], ("QT", "a"))
            qk_block(pb, lambda kc, j=j: wq[:, kc, 512 + j * 128:512 + (j + 1) * 128], 2, 2, 1, QTb[:, :, j, :], ("QT", "b"))
        for tl in range(4):
            t = G * 4 + tl
            obuf = t % 2
            if full:
                left = t - 1 if t > 0 else 0
                right = t + 1 if t < NTq - 1 else NTq - 1
            else:
                left = t - 1 if t > 0 else 23
                right = t + 1
            for g in range(2):
                attend(P, "a", tl, g, [left, t, right], [0 if t == 0 else 1, None, 3 if t == NTq - 1 else 2], obuf, 0)
            for g in range(2):
                attend(P, "b", tl, g, list(range(64)), [None] * 64, obuf, 8)
            if full:
                xres, xres_k = xt[t % 2], ("xt", t % 2)
            else:
                xres_k = "xres"
            P.dma(xres[:], xrv[:, t, :], writes=[xres_k])
            okeys = [("OT", obuf, k, g) for k in "ab" for g in range(2)]
            for h2 in range(2):
                def mmo(e, h2=h2, obuf=obuf):
                    ins = None
                    for h in range(16):
                        ins = e.matmul(bk[7][:], lhsT=OT[obuf][:, h, :], rhs=wo[:, h, h2 * 512:(h2 + 1) * 512],
                                       start=(h == 0), stop=(h == 15))
                    return ins
                P.pe(mmo, reads=okeys + ["wo"], writes=[BK(7)])
                P.dve(lambda e, h2=h2, xres=xres: e.tensor_tensor(out=ysb[:, h2 * 512:(h2 + 1) * 512], in0=bk[7][:],
                                                                  in1=xres[:, h2 * 512:(h2 + 1) * 512], op=ALU.add),
                      reads=[BK(7), xres_k], writes=[("ysb", h2)])
            P.dma(yv[:, t, :], ysb[:], reads=[("ysb", 0), ("ysb", 1)], writes=[("x1s", t)])
    if standalone:
        return P.build()
    P.pop_scope()


def rope_consts(r):
    pos = (np.arange(SEQ) + 2048 * r) % SEQ
    posf = pos.astype(np.float32)
    inv32 = (np.float32(10000.0) ** (-np.arange(0, 64, 2, dtype=np.float32) / np.float32(64))).astype(np.float32)
    inv16 = (np.float32(10000.0) ** (-np.arange(0, 32, 2, dtype=np.float32) / np.float32(32))).astype(np.float32)
    ang_a = posf[:, None] * inv32[None, :]
    row = (pos // 64).astype(np.float32)
    col = (pos % 64).astype(np.float32)
    ang_r = row[:, None] * inv16[None, :]
    ang_c = col[:, None] * inv16[None, :]
    d = np.arange(128) % 64
    tabs = np.zeros((4, 128, SEQ), np.float32)
    tabs[0] = np.cos(ang_a).astype(np.float32)[:, d % 32].T
    tabs[1] = np.sin(ang_a).astype(np.float32)[:, d % 32].T
    ang_b = np.where((d < 32)[None, :], ang_r[:, d % 16], ang_c[:, d % 16])
    tabs[2] = np.cos(ang_b).astype(np.float32).T
    tabs[3] = np.sin(ang_b).astype(np.float32).T
    return tabs


def rot_consts():
    import ml_dtypes
    Ra = np.zeros((64, 64), np.float32)
    for i in range(64):
        if i < 32:
            Ra[i, i + 32] = -1.0
        else:
            Ra[i, i - 32] = 1.0
    Rb = np.zeros((64, 64), np.float32)
    for i in range(64):
        if (i % 32) < 16:
            Rb[i, i + 16] = -1.0
        else:
            Rb[i, i - 16] = 1.0
    out = np.zeros((3, 128, 128), np.float32)
    for blk in range(2):
        s = slice(blk * 64, (blk + 1) * 64)
        out[0, s, s] = Ra.T
        out[1, s, s] = Rb.T
        out[2, s, s] = 1.0
    return out.astype(ml_dtypes.bfloat16)


def mask_consts(r):
    import ml_dtypes
    k = np.arange(128)[:, None]
    q = np.arange(128)[None, :]
    mL = (k >= q).astype(np.float32)
    mR = (q >= k).astype(np.float32)
    m = np.zeros((4, 128, 512), np.float32)
    m[0] = np.tile(mL if (r is not None and r > 0) else 0 * mL, (1, 4))
    m[1] = np.tile(mL, (1, 4))
    m[2] = np.tile(mR, (1, 4))
    m[3] = np.tile(mR if (r is not None and r < 3) else 0 * mR, (1, 4))
    return m.astype(ml_dtypes.bfloat16)


def attn_inputs(x, ab_norm, ab_w_in, qn_a, kn_a, sink_a, qn_b, kn_b, ab_w_out, fused=False):
    import ml_dtypes
    w = ab_w_in[0]
    qa, ka, va, qb, kb, vb = (w[:, 0:512], w[:, 512:640], w[:, 640:768], w[:, 768:1280], w[:, 1280:1408], w[:, 1408:1536])

    def pair(wq_):
        cols = []
        for j in range(4):
            cols.append(wq_[:, j * 64:(j + 1) * 64])
            cols.append(wq_[:, (j + 4) * 64:(j + 5) * 64])
        return np.concatenate(cols, axis=1)
    wq = np.ascontiguousarray(np.concatenate([pair(qa), pair(qb)], axis=1))
    wk = np.ascontiguousarray(np.concatenate([ka, kb], axis=1))
    wv = np.ascontiguousarray(np.concatenate([va, vb], axis=1))
    wo = np.ascontiguousarray(ab_w_out[0].reshape(16, 64, DM).transpose(1, 0, 2))
    gains = np.ascontiguousarray(np.stack([np.tile(qn_a[0], 2), np.tile(kn_a[0], 2), np.tile(qn_b[0], 2), np.tile(kn_b[0], 2)], axis=1))
    ident = np.eye(128, dtype=np.float32).astype(ml_dtypes.bfloat16)
    rmat = rot_consts()
    maps = []
    if fused:
        rope0, masks0 = rope_consts(0), mask_consts(None)
    for c in range(NCORES):
        b, r = c // 4, c % 4
        if fused:
            maps.append({"xr": x[b], "g": ab_norm[0:1], "wq": wq, "wk": wk, "wv": wv, "wo": wo, "gains": gains,
                         "rope": rope0, "rmat": rmat, "masks": masks0, "sink": sink_a[0:1], "ident": ident})
            continue
        xr = np.ascontiguousarray(np.roll(x[b], -2048 * r, axis=0))
        maps.append({"xr": xr, "g": ab_norm[0:1], "wq": wq, "wk": wk, "wv": wv, "wo": wo, "gains": gains,
                     "rope": rope_consts(r), "rmat": rmat, "masks": mask_consts(r), "sink": sink_a[0:1], "ident": ident})
    return maps


NTILE = SEQ // 128
DBG = {}


def build_gdn(phases=(1, 2, 3), nsteps=NTILE, P=None, pf="", x_handle=None, o_handle=None, nhp=1):
    standalone = P is None
    if standalone:
        P = Prog()
    else:
        P.push_scope()
    nc = P.nc
    fusedm = x_handle is not None
    xp_d = x_handle if fusedm else P.dram(pf + "xp", [SEQ + 128, DM], F32, "ExternalInput")
    g_d = P.dram(pf + "g", [1, DM], F32, "ExternalInput")
    wf_d = P.dram(pf + "wf", [nhp, DM, 768], F32, "ExternalInput")
    wt_d = P.dram(pf + "wt", [nhp, DM, 264], F32, "ExternalInput")
    cw_d = P.dram(pf + "cw", [nhp, 128, 6, 5], F32, "ExternalInput")
    gc_d = P.dram(pf + "gconst", [nhp, 8], F32, "ExternalInput")
    on_d = P.dram(pf + "onorm", [1, 128], F32, "ExternalInput")
    mk_d = P.dram(pf + "gmask", [5, 128, 128], F32, "ExternalInput")
    id_d = P.dram(pf + "ident", [128, 128], BF16, "ExternalInput")
    y_d = o_handle if fusedm else P.dram(pf + "y", [SEQ, 256], F32, "ExternalOutput")
    SK = "ExternalOutput" if DBG.get("dump") else "Internal"
    qT_s = P.dram(pf + "qT_s", [2, 128, SEQ], BF16, SK)
    kT_s = P.dram(pf + "kT_s", [2, 128, SEQ], BF16, SK)
    k_s = P.dram(pf + "k_s", [2, SEQ, 128], BF16, SK)
    v_s = P.dram(pf + "v_s", [2, SEQ, 128], BF16, SK)
    z_s = P.dram(pf + "z_s", [SEQ, 256], BF16, SK)
    o_s = P.dram(pf + "o_s", [2, 2, SEQ, 128], F32, SK)

    W = alloc_norm_work(P)
    gbc = P.sb([128, DM], F32, "gbc")
    wf = P.sb([128, 8, 768], BF16, "wf")
    wt = P.sb([128, 8, 264], BF16, "wt")
    cw = P.sb([128, 6, 5], F32, "cw")
    gconst = P.sb([128, 8], F32, "gconst")
    onorm = P.sb([128, 2, 128], F32, "onorm")
    gmask = P.sb([128, 5, 128], F32, "gmask")
    onesb = P.sb([128, 128], BF16, "onesb")
    onesf = P.sb([128, 128], F32, "onesf")
    onec = P.sb([128, 1], F32, "onec")
    gates = P.sb([128, NTILE, 8], F32, "gates")
    xt = [P.sb([128, DM], F32) for _ in range(2)]
    xng = [P.sb([128, 8, 516], BF16) for _ in range(2)]
    pre = [P.sb([128, 516], F32) for _ in range(2)]
    cacc = [P.sb([128, 512], F32) for _ in range(2)]
    cblk = [P.sb([128, 512], F32) for _ in range(2)]
    sqb = [P.sb([128, 512], BF16) for _ in range(2)]
    lnt = [P.sb([128, 512], F32) for _ in range(2)]
    rst = [P.sb([128, 512], F32) for _ in range(2)]
    nT = [P.sb([128, 512], BF16) for _ in range(2)]
    tm = [P.sb([128, 4, 128], BF16) for _ in range(2)]
    zsb = [P.sb([128, 256], BF16) for _ in range(2)]
    gtmp = P.sb([128, 8], F32, "gtmp")
    banks = [P.ps([128, 512], F32) for _ in range(7)]
    bankT = W["psT"]

    def BQ(b, q):
        return ("bq", b, q)

    def BK(b):
        return [BQ(b, q) for q in range(4)]

    KT_ALL = [BQ(7, q) for q in range(4)]
    for b_ in range(8):
        for q_ in range(4):
            P.bank(BQ(b_, q_), b_)

    P.dma(W["ident"][:], id_d.ap(), writes=["ident"])
    P.dma(gbc[:], bcast_rows(g_d, DM), writes=["gbc"])
    P.dve(lambda e: e.memset(W["epsc"][:], EPS), writes=["epsc"])
    P.dma(onorm[:, 0, :], bcast_rows(on_d, 128), writes=["onorm"])
    P.dma(onorm[:, 1, :], bcast_rows(on_d, 128), writes=["onorm"])
    P.dma(gmask[:], mk_d.ap().rearrange("r p c -> p r c"), writes=["gmask"])
    P.dve(lambda e: e.memset(onesb[:], 1.0), writes=["onesb"])
    P.dve(lambda e: e.memset(onesf[:], 1.0), writes=["onesf"])
    P.dve(lambda e: e.memset(onec[:], 1.0), writes=["onec"])
    def load_weights(hp):
        P.dma(wf[:], wf_d.ap()[hp].rearrange("(k p) c -> p k c", p=128), writes=["wf"], q="gpsimd")
        P.dma(wt[:], wt_d.ap()[hp].rearrange("(k p) c -> p k c", p=128), writes=["wt"], q="gpsimd")
        P.dma(cw[:], cw_d.ap()[hp], writes=["cw"])
        P.dma(gconst[:], bcast_rows(gc_d, 8, 128, hp * 8), writes=["gconst"])
        P.act(lambda e: e.activation(out=gconst[:, 4:8], in_=gconst[:, 4:8], func=AF.Exp), reads=["gconst"], writes=["gconst"])
        P.dve(lambda e: e.tensor_scalar(out=gconst[:, 4:8], in0=gconst[:, 4:8], scalar1=-1.0, scalar2=None, op0=ALU.mult),
              reads=["gconst"], writes=["gconst"])

    xpv = xp_d.ap()
    uid = [0]

    def norm_rows(x_ap, xkey, rows, dst, dkey):
        u = uid[0]
        uid[0] += 1
        xn = W["xn"][u % 2]
        kxn = ("xn", u % 2)
        ss = W["ss"][u % 2]
        kss = ("ss", u % 2)
        psT = bankT
        P.act(lambda e: e.activation(out=W["junk"][0:rows, :], in_=x_ap, func=AF.Square, accum_out=ss[0:rows, 0:1]),
              reads=[xkey], writes=[kss, "junk"])
        P.act(lambda e: e.activation(out=ss[0:rows, 1:2], in_=ss[0:rows, 0:1], func=AF.Sqrt, scale=1.0 / 1024.0,
                                     bias=W["epsc"][0:rows, 0:1]), reads=[kss, "epsc"], writes=[kss])
        P.dve(lambda e: e.reciprocal(out=ss[0:rows, 2:3], in_=ss[0:rows, 1:2]), reads=[kss], writes=[kss])
        P.dve(lambda e: e.scalar_tensor_tensor(out=xn[0:rows, :], in0=x_ap, scalar=ss[0:rows, 2:3], in1=gbc[0:rows, :],
                                               op0=ALU.mult, op1=ALU.mult), reads=[xkey, kss, "gbc"], writes=[kxn])

        def tr(e):
            ins = None
            for kc in range(8):
                ins = e.transpose(out=psT[:, kc * 128:kc * 128 + rows], in_=xn[0:rows, kc * 128:(kc + 1) * 128],
                                  identity=W["ident"][0:rows, 0:rows])
            return ins
        P.pe(tr, reads=[kxn, "ident"], writes=KT_ALL)
        P.act(lambda e: e.copy(out=dst, in_=psT[:].rearrange("p (k c) -> p k c", k=8)[:, :, 0:rows]),
              reads=KT_ALL, writes=[dkey])

    def phase1(hp):
        for G in (range(DBG.get('ng', NG)) if 1 in phases else []):
            pb = G % 2
            LV = DBG.get('lv', 9)
            for tl in range(5):
                rows = 128 if tl < 4 else 4
                u = uid[0]
                xo = 0 if fusedm else 2
                if tl < 4:
                    r0 = G * 512 + xo + tl * 128
                    P.dma(xt[u % 2][0:rows, :], xpv[r0:r0 + rows, :], reads=[("yout", "f_", r0 // 128)], writes=[("xt", u % 2)])
                else:
                    if fusedm and (G == 0 or G == NG - 1):
                        P.dve(lambda e, u=u: e.memset(xt[u % 2][0:4, :], 0.0), writes=[("xt", u % 2)])
                    if not (fusedm and G == 0):
                        r0 = G * 512 + xo - 2
                        P.dma(xt[u % 2][0:2, :], xpv[r0:r0 + 2, :], reads=[("yout", "f_", r0 // 128)], writes=[("xt", u % 2)])
                    if not (fusedm and G == NG - 1):
                        r0 = G * 512 + xo + 512
                        P.dma(xt[u % 2][2:4, :], xpv[r0:r0 + 2, :], reads=[("yout", "f_", r0 // 128)], writes=[("xt", u % 2)])
                norm_rows(xt[u % 2][0:rows, :], ("xt", u % 2), rows, xng[pb][:, :, tl * 128:tl * 128 + rows], ("xng", pb, tl))
            xk = [("xng", pb, tl) for tl in range(5)]
            Ls = [Lazy(P), Lazy(P), Lazy(P)]
            for blk in (range(6) if LV >= 2 else []):
                R = Ls[blk % 2]
                pbk = 1 + (blk % 2)
                prb = blk % 2

                def mm_main(e, blk=blk, pbk=pbk, pb=pb):
                    ins = None
                    for kc in range(8):
                        ins = e.matmul(banks[pbk][:], lhsT=wf[:, kc, blk * 128:(blk + 1) * 128], rhs=xng[pb][:, kc, 0:512],
                                       start=(kc == 0), stop=(kc == 7))
                    return ins

                def mm_halo(e, blk=blk, pb=pb):
                    ins = None
                    for kc in range(8):
                        ins = e.matmul(banks[3][:, 0:4], lhsT=wf[:, kc, blk * 128:(blk + 1) * 128], rhs=xng[pb][:, kc, 512:516],
                                       start=(kc == 0), stop=(kc == 7))
                    return ins
                R.pe(mm_main, reads=xk + ["wf"], writes=BK(pbk))
                R.begin()
                R.pe(mm_halo, reads=xk + ["wf"], writes=BK(3))
                R.act(lambda e, prb=prb: e.copy(out=pre[prb][:, 0:2], in_=banks[3][:, 0:2]), reads=BK(3), writes=[("pre", prb)])
                R.act(lambda e, prb=prb: e.copy(out=pre[prb][:, 514:516], in_=banks[3][:, 2:4]), reads=BK(3), writes=[("pre", prb)])
                R.end()
                R.act(lambda e, pbk=pbk, prb=prb: e.copy(out=pre[prb][:, 2:514], in_=banks[pbk][:]), reads=BK(pbk), writes=[("pre", prb)])
                R.dve(lambda e, blk=blk, prb=prb: e.tensor_scalar(out=cacc[prb][:], in0=pre[prb][:, 0:512], scalar1=cw[:, blk, 0:1],
                                                                  scalar2=None, op0=ALU.mult),
                      reads=[("pre", prb), "cw"], writes=[("cacc", prb)])
                for tap in range(1, 5):
                    R.dve(lambda e, blk=blk, prb=prb, tap=tap: e.scalar_tensor_tensor(
                        out=cacc[prb][:], in0=pre[prb][:, tap:tap + 512], scalar=cw[:, blk, tap:tap + 1], in1=cacc[prb][:],
                        op0=ALU.mult, op1=ALU.add), reads=[("pre", prb), "cw", ("cacc", prb)], writes=[("cacc", prb)])
                nb = blk % 2
                h = blk % 2
                if LV < 3:
                    continue
                if blk < 4:
                    R.act(lambda e, prb=prb: e.activation(out=cblk[prb][:], in_=cacc[prb][:], func=AF.Silu), reads=[("cacc", prb)], writes=[("cblk", prb)])
                    R.act(lambda e, prb=prb: e.activation(out=sqb[prb][:], in_=cblk[prb][:], func=AF.Square), reads=[("cblk", prb)], writes=[("sqb", prb)])
                    R.begin()
                    R.pe(lambda e, prb=prb: e.matmul(banks[4][:], lhsT=onesb[:], rhs=sqb[prb][:], start=True, stop=True),
                         reads=[("sqb", prb), "onesb"], writes=BK(4))
                    R.act(lambda e, prb=prb: e.activation(out=lnt[prb][:], in_=banks[4][:], func=AF.Ln, bias=W["epsc"][:, 0:1]),
                          reads=BK(4) + ["epsc"], writes=[("lnt", prb)])
                    R.end()
                    R.act(lambda e, prb=prb: e.activation(out=rst[prb][:], in_=lnt[prb][:], func=AF.Exp, scale=-0.5), reads=[("lnt", prb)], writes=[("rst", prb)])
                    qs = (128.0 ** -0.5) if blk < 2 else 1.0
                    R.dve(lambda e, nb=nb, qs=qs, prb=prb: e.scalar_tensor_tensor(out=nT[nb][:], in0=cblk[prb][:], scalar=qs, in1=rst[prb][:],
                                                                         op0=ALU.mult, op1=ALU.mult),
                          reads=[("cblk", prb), ("rst", prb)], writes=[("nT", nb)])
                    dst = qT_s if blk < 2 else kT_s
                    nm = "qT" if blk < 2 else "kT"
                    if LV >= 4:
                      R.dma(dst.ap()[h][:, G * 512:(G + 1) * 512], nT[nb][:], reads=[("nT", nb)],
                          writes=[(nm, h, G * 4 + tl) for tl in range(4)])
                else:
                    R.act(lambda e, nb=nb, prb=prb: e.activation(out=nT[nb][:], in_=cacc[prb][:], func=AF.Silu), reads=[("cacc", prb)], writes=[("nT", nb)])
                if blk >= 2 and LV >= 5:
                    bT = bankT

                    def trk(e, nb=nb):
                        ins = None
                        for tl in range(4):
                            ins = e.transpose(out=bT[:, tl * 128:(tl + 1) * 128], in_=nT[nb][:, tl * 128:(tl + 1) * 128],
                                              identity=W["ident"][:])
                        return ins
                    R.begin()
                    R.pe(trk, reads=[("nT", nb), "ident"], writes=KT_ALL)
                    R.act(lambda e, nb=nb: e.copy(out=tm[nb][:], in_=bT[:, 0:512].rearrange("p (t c) -> p t c", t=4)),
                          reads=KT_ALL, writes=[("tm", nb)])
                    R.end()
                    dst = k_s if blk < 4 else v_s
                    nm = "k" if blk < 4 else "v"
                    R.dma(dst.ap()[h].rearrange("(t p) d -> p t d", p=128)[:, G * 4:(G + 1) * 4, :], tm[nb][:],
                          reads=[("tm", nb)], writes=[(nm, h, G * 4 + tl) for tl in range(4)])
            R = Ls[2]
            for tl in (range(4) if LV >= 6 else []):
                t = G * 4 + tl
                zb = t % 2

                def mmt(e, tl=tl, pb=pb):
                    ins = None
                    for kc in range(8):
                        ins = e.matmul(banks[5][:, 0:264], lhsT=xng[pb][:, kc, tl * 128:(tl + 1) * 128], rhs=wt[:, kc, :],
                                       start=(kc == 0), stop=(kc == 7))
                    return ins
                R.pe(mmt, reads=xk + ["wt"], writes=BK(5))
                R.act(lambda e, zb=zb: e.activation(out=zsb[zb][:], in_=banks[5][:, 0:256], func=AF.Silu), reads=BK(5), writes=[("zsb", zb)])
                R.dma(z_s.ap()[t * 128:(t + 1) * 128, :], zsb[zb][:], reads=[("zsb", zb)], writes=[("z", t)])
                if LV < 7:
                    continue
                R.dve(lambda e: e.tensor_tensor(out=gtmp[:, 0:4], in0=banks[5][:, 256:260], in1=gconst[:, 0:4], op=ALU.add),
                      reads=BK(5) + ["gconst"], writes=["gtmp"])
                SUB = DBG.get('sub', 9)
                if SUB >= 2:
                    R.act(lambda e: e.activation(out=gtmp[:, 0:4], in_=gtmp[:, 0:4], func=AF.Exp), reads=["gtmp"], writes=["gtmp"])
                if SUB >= 3:
                    R.act(lambda e: e.activation(out=gtmp[:, 0:4], in_=gtmp[:, 0:4], func=AF.Ln, bias=onec[:, 0:1]),
                          reads=["gtmp", "onec"], writes=["gtmp"])
                if SUB >= 4:
                    R.dve(lambda e, t=t: e.tensor_tensor(out=gates[:, t, 0:4], in0=gtmp[:, 0:4], in1=gconst[:, 4:8], op=ALU.mult),
                          reads=["gtmp", "gconst"], writes=[("gates", t)])
                if LV < 8:
                    continue
                R.act(lambda e: e.activation(out=gtmp[:, 4:8], in_=banks[5][:, 260:264], func=AF.Exp, scale=-1.0),
                      reads=BK(5), writes=["gtmp"])
                R.dve(lambda e: e.tensor_scalar(out=gtmp[:, 4:8], in0=gtmp[:, 4:8], scalar1=1.0, scalar2=None, op0=ALU.add),
                      reads=["gtmp"], writes=["gtmp"])
                R.dve(lambda e, t=t: e.reciprocal(out=gates[:, t, 4:8], in_=gtmp[:, 4:8]), reads=["gtmp"], writes=[("gates", t)])
            interleave(Ls)

    allb = [P.ps([128, 512], F32)] if False else None
    chains = [(h, d) for d in range(2) for h in range(2)]
    bank_ap = banks + [None]
    pbank8 = P.ps

    def Q(ci, q):
        b = 2 * ci + q // 4
        qq = q % 4
        if b == 7:
            ap = bankT.bitcast(F32)[:, qq * 128:(qq + 1) * 128]
        else:
            ap = banks[b][:, qq * 128:(qq + 1) * 128]
        return ap, BQ(b, qq)

    def Qb(ci, q):
        b = 2 * ci + q // 4
        qq = q % 4
        if b == 7:
            ap = bankT[:, qq * 256:qq * 256 + 128]
        else:
            ap = banks[b].bitcast(BF16)[:, qq * 256:qq * 256 + 128]
        return ap, BQ(b, qq)

    st = {}
    for ci in range(4):
        d = {}
        d["S32"] = P.sb([128, 128], F32)
        d["Sbf"] = P.sb([128, 128], BF16)
        for nm in ["qT", "kT", "k", "v"]:
            d[nm] = [P.sb([128, 128], BF16) for _ in range(2)]
        for nm in ["gcol", "cols"]:
            d[nm] = P.sb([128, 8], F32)
        for nm in ["TG", "IB", "Dm", "E", "Bm", "Eb"]:
            d[nm] = P.sb([128, 128], F32)
        for nm in ["erow", "u"]:
            d[nm] = [P.sb([128, 128], F32) for _ in range(2)]
        for nm in ["X", "XT", "Pa", "PaT", "Pb", "PbT", "Za", "Zb", "vb", "kbg", "vn"]:
            d[nm] = P.sb([128, 128], BF16)
        for nm in ["kd", "qgT", "nwT", "qkT"]:
            d[nm] = [P.sb([128, 128], BF16) for _ in range(2)]
        d["osb"] = [P.sb([128, 128], F32) for _ in range(2)]
        st[ci] = d

    def K(ci, nm):
        return (nm, "c", ci)

    def chain_tile(ci, s, Rp, Rs):
        h, dr = chains[ci]
        d = st[ci]
        t = s if dr == 0 else NTILE - 1 - s
        lb = s % 2
        incl = gmask[:, 0 + 2 * dr, :]
        strict = gmask[:, 1 + 2 * dr, :]
        identf = gmask[:, 4, :]
        gcolumn = gates[:, t, 2 * dr + h:2 * dr + h + 1]
        bcolumn = gates[:, t, 4 + 2 * dr + h:4 + 2 * dr + h + 1]
        for nm, src in (("qT", qT_s), ("kT", kT_s)):
            Rp.dma(d[nm][lb][:], src.ap()[h][:, t * 128:(t + 1) * 128], reads=[(nm, h, t)], writes=[K(ci, nm + str(lb))])
        for nm, src in (("k", k_s), ("v", v_s)):
            Rp.dma(d[nm][lb][:], src.ap()[h][t * 128:(t + 1) * 128, :], reads=[(nm, h, t)], writes=[K(ci, nm + str(lb))])
        qT, kT, kk, vv = d["qT"][lb], d["kT"][lb], d["k"][lb], d["v"][lb]
        kqT, kkT, kkk, kvv = K(ci, "qT" + str(lb)), K(ci, "kT" + str(lb)), K(ci, "k" + str(lb)), K(ci, "v" + str(lb))
        q0, k0 = Q(ci, 0)
        q1, k1 = Q(ci, 1)
        q2, k2 = Q(ci, 2)
        q3, k3 = Q(ci, 3)
        q4, k4 = Q(ci, 4)
        q5, k5 = Q(ci, 5)
        q6, k6 = Q(ci, 6)
        q7, k7 = Q(ci, 7)
        Rp.pe(lambda e: e.matmul(q0, lhsT=kT[:], rhs=kT[:], start=True, stop=True), reads=[kkT], writes=[k0])
        Rp.pe(lambda e: e.matmul(q1, lhsT=kT[:], rhs=qT[:], start=True, stop=True), reads=[kkT, kqT], writes=[k1])
        Rp.dve(lambda e: e.tensor_scalar(out=d["TG"][:], in0=incl, scalar1=gcolumn, scalar2=None, op0=ALU.mult),
              reads=["gmask", ("gates", t)], writes=[K(ci, "TG")])
        Rp.dve(lambda e: e.tensor_scalar(out=d["IB"][:], in0=identf, scalar1=bcolumn, scalar2=None, op0=ALU.mult),
              reads=["gmask", ("gates", t)], writes=[K(ci, "IB")])
        Rp.pe(lambda e: e.matmul(q2, lhsT=onesf[:], rhs=d["TG"][:], start=True, stop=True),
             reads=[K(ci, "TG"), "onesf"], writes=[k2])
        Rp.pe(lambda e: e.matmul(q3[:, 0:2], lhsT=d["TG"][:], rhs=onesf[:, 0:2], start=True, stop=True),
             reads=[K(ci, "TG"), "onesf"], writes=[k3])
        Rp.dve(lambda e: e.tensor_copy(out=d["gcol"][:, 0:1], in_=q3[:, 0:1]), reads=[k3], writes=[K(ci, "gcol")])
        Rp.pe(lambda e: e.matmul(q3, lhsT=onesf[:], rhs=d["IB"][:], start=True, stop=True),
             reads=[K(ci, "IB"), "onesf", K(ci, "gcol")], writes=[k3])
        Rp.dve(lambda e: e.tensor_scalar(out=d["Dm"][:], in0=q2, scalar1=d["gcol"][:, 0:1], scalar2=0.0,
                                        op0=ALU.subtract, op1=ALU.min), reads=[k2, K(ci, "gcol")], writes=[K(ci, "Dm")])
        Rp.act(lambda e: e.activation(out=d["E"][:], in_=d["Dm"][:], func=AF.Exp), reads=[K(ci, "Dm")], writes=[K(ci, "E")])
        Rp.act(lambda e: e.activation(out=d["erow"][lb][:], in_=q2, func=AF.Exp), reads=[k2], writes=[K(ci, "erow" + str(lb))])
        Rp.dve(lambda e: e.tensor_tensor(out=d["E"][:], in0=d["E"][:], in1=incl, op=ALU.mult),
              reads=[K(ci, "E"), "gmask"], writes=[K(ci, "E")])
        Rp.dve(lambda e: e.tensor_tensor(out=d["Bm"][:], in0=q3, in1=strict, op=ALU.mult), reads=[k3, "gmask"], writes=[K(ci, "Bm")])
        Rp.dve(lambda e: e.tensor_tensor(out=d["Eb"][:], in0=d["E"][:], in1=d["Bm"][:], op=ALU.mult),
              reads=[K(ci, "E"), K(ci, "Bm")], writes=[K(ci, "Eb")])
        Rp.dve(lambda e: e.scalar_tensor_tensor(out=d["X"][:], in0=q0, scalar=-1.0, in1=d["Eb"][:], op0=ALU.mult, op1=ALU.mult),
              reads=[k0, K(ci, "Eb")], writes=[K(ci, "X")])
        Rp.dve(lambda e: e.tensor_tensor(out=d["qkT"][lb][:], in0=q1, in1=d["E"][:], op=ALU.mult),
              reads=[k1, K(ci, "E")], writes=[K(ci, "qkT" + str(lb))])
        cols = d["cols"]
        Rp.act(lambda e: e.activation(out=cols[:, 0:1], in_=d["gcol"][:, 0:1], func=AF.Exp), reads=[K(ci, "gcol")], writes=[K(ci, "cols")])
        Rp.dve(lambda e: e.tensor_tensor(out=cols[:, 1:2], in0=cols[:, 0:1], in1=bcolumn, op=ALU.mult),
              reads=[K(ci, "cols"), ("gates", t)], writes=[K(ci, "cols")])
        for cc in range(2):
            col = (cc * 64 + 63) if dr == 0 else (cc * 64)
            Rp.dve(lambda e, cc=cc, col=col: e.tensor_copy(out=cols[cc * 64:(cc + 1) * 64, 3:4], in_=q2[cc * 64:(cc + 1) * 64, col:col + 1]),
                  reads=[k2], writes=[K(ci, "cols")])
        Rp.dve(lambda e: e.tensor_tensor(out=cols[:, 4:5], in0=cols[:, 3:4], in1=d["gcol"][:, 0:1], op=ALU.subtract),
              reads=[K(ci, "cols"), K(ci, "gcol")], writes=[K(ci, "cols")])
        Rp.act(lambda e: e.activation(out=cols[:, 2:3], in_=cols[:, 4:5], func=AF.Exp), reads=[K(ci, "cols")], writes=[K(ci, "cols")])
        Rp.dve(lambda e: e.tensor_scalar(out=d["vb"][:], in0=vv[:], scalar1=bcolumn, scalar2=None, op0=ALU.mult),
              reads=[kvv, ("gates", t)], writes=[K(ci, "vb")])
        Rp.dve(lambda e: e.tensor_scalar(out=d["kbg"][:], in0=kk[:], scalar1=cols[:, 1:2], scalar2=None, op0=ALU.mult),
              reads=[kkk, K(ci, "cols")], writes=[K(ci, "kbg")])
        Rp.dve(lambda e: e.tensor_scalar(out=d["kd"][lb][:], in0=kk[:], scalar1=cols[:, 2:3], scalar2=None, op0=ALU.mult),
              reads=[kkk, K(ci, "cols")], writes=[K(ci, "kd" + str(lb))])
        Rp.dve(lambda e: e.tensor_tensor(out=d["qgT"][lb][:], in0=qT[:], in1=d["erow"][lb][:], op=ALU.mult),
              reads=[kqT, K(ci, "erow" + str(lb))], writes=[K(ci, "qgT" + str(lb))])
        qb4, _ = Qb(ci, 0)
        Rp.pe(lambda e: e.transpose(out=qb4, in_=d["X"][:], identity=W["ident"][:]), reads=[K(ci, "X"), "ident"], writes=[k0])
        Rp.act(lambda e: e.copy(out=d["XT"][:], in_=qb4), reads=[k0], writes=[K(ci, "XT")])
        Rp.dve(lambda e: e.tensor_tensor(out=d["Za"][:], in0=d["X"][:], in1=identf, op=ALU.add),
              reads=[K(ci, "X"), "gmask"], writes=[K(ci, "Za")])
        Pc, PcT, kPc, kPcT = d["X"], d["XT"], K(ci, "X"), K(ci, "XT")
        Zc, kZc = d["Za"], K(ci, "Za")
        for lvl in range(5):
            Pn, PnT = (d["Pa"], d["PaT"]) if lvl % 2 == 0 else (d["Pb"], d["PbT"])
            kPn, kPnT = (K(ci, "Pa"), K(ci, "PaT")) if lvl % 2 == 0 else (K(ci, "Pb"), K(ci, "PbT"))
            Zn, kZn = (d["Zb"], K(ci, "Zb")) if lvl % 2 == 0 else (d["Za"], K(ci, "Za"))
            Rp.pe(lambda e, Pc=Pc, PcT=PcT: e.matmul(q1, lhsT=Pc[:], rhs=PcT[:], start=True, stop=True),
                 reads=[kPc, kPcT], writes=[k1])
            Rp.act(lambda e, PnT=PnT: e.copy(out=PnT[:], in_=q1), reads=[k1], writes=[kPnT])
            if lvl < 4:
                Rp.pe(lambda e, Pc=Pc, PcT=PcT: e.matmul(q0, lhsT=PcT[:], rhs=Pc[:], start=True, stop=True),
                     reads=[kPc, kPcT], writes=[k0])
                Rp.act(lambda e, Pn=Pn: e.copy(out=Pn[:], in_=q0), reads=[k0], writes=[kPn])
            Rp.pe(lambda e, PnT=PnT, Zc=Zc: e.matmul(q2, lhsT=PnT[:], rhs=Zc[:], start=True, stop=True),
                 reads=[kPnT, kZc], writes=[k2])
            Rp.dve(lambda e, Zn=Zn, Zc=Zc: e.tensor_tensor(out=Zn[:], in0=q2, in1=Zc[:], op=ALU.add),
                  reads=[k2, kZc], writes=[kZn])
            Pc, PcT, kPc, kPcT = Pn, PnT, kPn, kPnT
            Zc, kZc = Zn, kZn
        Rp.pe(lambda e: e.matmul(q3, lhsT=Zc[:], rhs=d["vb"][:], start=True, stop=True), reads=[kZc, K(ci, "vb")], writes=[k3])
        Rp.act(lambda e: e.copy(out=d["u"][lb][:], in_=q3), reads=[k3], writes=[K(ci, "u" + str(lb))])
        Rp.pe(lambda e: e.matmul(q1, lhsT=d["kbg"][:], rhs=Zc[:], start=True, stop=True), reads=[kZc, K(ci, "kbg")], writes=[k1])
        Rp.act(lambda e: e.activation(out=d["nwT"][lb][:], in_=q1, func=AF.Copy, scale=-1.0), reads=[k1], writes=[K(ci, "nwT" + str(lb))])
        order = [0, 1] if dr == 0 else [1, 0]
        osb = d["osb"][lb]
        for cc in order:
            rs = slice(cc * 64, (cc + 1) * 64)
            Rs.pe(lambda e, rs=rs: e.matmul(q4[rs, :], lhsT=d["nwT"][lb][:, rs], rhs=d["Sbf"][:], start=True, stop=True),
                 reads=[K(ci, "nwT" + str(lb)), ("Sbf", ci)], writes=[k4])
            Rs.dve(lambda e, rs=rs: e.tensor_tensor(out=d["vn"][rs, :], in0=q4[rs, :], in1=d["u"][lb][rs, :], op=ALU.add),
                  reads=[k4, K(ci, "u" + str(lb))], writes=[K(ci, "vn")])

            def mmo(e, rs=rs):
                e.matmul(q5[rs, :], lhsT=d["qgT"][lb][:, rs], rhs=d["Sbf"][:], start=True, stop=False)
                return e.matmul(q5[rs, :], lhsT=d["qkT"][lb][rs, rs], rhs=d["vn"][rs, :], start=False, stop=True)
            Rs.pe(mmo, reads=[K(ci, "qgT" + str(lb)), K(ci, "qkT" + str(lb)), K(ci, "vn"), ("Sbf", ci)], writes=[k5])
            Rs.pe(lambda e, rs=rs: e.matmul(q7, lhsT=d["kd"][lb][rs, :], rhs=d["vn"][rs, :], start=True, stop=True),
                 reads=[K(ci, "kd" + str(lb)), K(ci, "vn")], writes=[k7])
            dcol = (cc * 64 + 63) if dr == 0 else (cc * 64)
            Rs.dve(lambda e, dcol=dcol: e.scalar_tensor_tensor(out=d["S32"][:], in0=d["S32"][:], scalar=d["erow"][lb][:, dcol:dcol + 1],
                                                              in1=q7, op0=ALU.mult, op1=ALU.add),
                  reads=[("S32", ci), K(ci, "erow" + str(lb)), k7], writes=[("S32", ci)])
            Rs.act(lambda e: e.copy(out=d["Sbf"][:], in_=d["S32"][:]), reads=[("S32", ci)], writes=[("Sbf", ci)])
            Rs.act(lambda e, rs=rs: e.copy(out=osb[rs, :], in_=q5[rs, :]), reads=[k5], writes=[K(ci, "osb" + str(lb))])
        Rs.dma(o_s.ap()[dr][h][t * 128:(t + 1) * 128, :], osb[:], reads=[K(ci, "osb" + str(lb))], writes=[("o", dr, h, t)])

    def phase2(hp):
        for ci in range(4):
            P.dve(lambda e, ci=ci: e.memset(st[ci]["S32"][:], 0.0), writes=[("S32", ci)])
            P.dve(lambda e, ci=ci: e.memset(st[ci]["Sbf"][:], 0.0), writes=[("Sbf", ci)])
        if 2 in phases:
            pres, scans = {}, {}
            for s in range(nsteps):
                pres[s] = [Lazy(P) for _ in range(4)]
                scans[s] = [Lazy(P) for _ in range(4)]
                for ci in range(4):
                    chain_tile(ci, s, pres[s][ci], scans[s][ci])
            interleave(pres[0])
            for s in range(nsteps):
                interleave(scans[s] + (pres[s + 1] if s + 1 < nsteps else []))

    if DBG.get("dump"):
        gd_ = P.dram("gates_o", [128, NTILE, 8], F32, "ExternalOutput")
        P.dma(gd_.ap(), gates[:], reads=[("gates", t_) for t_ in range(NTILE)])
    of = [P.sb([128, 2, 128], F32) for _ in range(2)]
    obk = [P.sb([128, 2, 128], F32) for _ in range(2)]
    zt = [P.sb([128, 256], BF16) for _ in range(2)]
    gz = P.sb([128, 256], F32, "gz")
    osum = P.sb([128, 2, 128], F32, "osum")
    jk = P.sb([128, 128], F32, "jk")
    s3 = P.sb([128, 8], F32, "s3")
    yo = [P.sb([128, 256], F32) for _ in range(2)]
    def phase3(hp):
        for t in (range(NTILE) if 3 in phases else []):
            b = t % 2
            for h in range(2):
                P.dma(of[b][:, h, :], o_s.ap()[0][h][t * 128:(t + 1) * 128, :], reads=[("o", 0, h, t)], writes=[("of", b)])
                P.dma(obk[b][:, h, :], o_s.ap()[1][h][t * 128:(t + 1) * 128, :], reads=[("o", 1, h, t)], writes=[("obk", b)])
            P.dma(zt[b][:], z_s.ap()[t * 128:(t + 1) * 128, :], reads=[("z", t)], writes=[("zt", b)])
            P.dve(lambda e, b=b: e.tensor_tensor(out=osum[:], in0=of[b][:], in1=obk[b][:], op=ALU.add),
                  reads=[("of", b), ("obk", b)], writes=["osum"])
            P.dve(lambda e, b=b: e.tensor_tensor(out=gz[:], in0=zt[b][:], in1=onorm[:].rearrange("p h d -> p (h d)"), op=ALU.mult),
                  reads=[("zt", b), "onorm"], writes=["gz"])
            for h in range(2):
                P.act(lambda e, h=h: e.activation(out=jk[:], in_=osum[:, h, :], func=AF.Square, accum_out=s3[:, h:h + 1]),
                      reads=["osum"], writes=["jk", "s3"])
            P.act(lambda e: e.activation(out=s3[:, 2:4], in_=s3[:, 0:2], func=AF.Sqrt, scale=1.0 / 128.0, bias=W["epsc"][:, 0:1]),
                  reads=["s3", "epsc"], writes=["s3"])
            P.dve(lambda e: e.reciprocal(out=s3[:, 4:6], in_=s3[:, 2:4]), reads=["s3"], writes=["s3"])
            for h in range(2):
                P.dve(lambda e, h=h, b=b: e.scalar_tensor_tensor(out=yo[b][:, h * 128:(h + 1) * 128], in0=osum[:, h, :],
                                                                 scalar=s3[:, 4 + h:5 + h], in1=gz[:, h * 128:(h + 1) * 128],
                                                                 op0=ALU.mult, op1=ALU.mult),
                      reads=["osum", "s3", "gz"], writes=[("yo", b)])
            if fusedm:
                P.dma(y_d.ap()[t * 128:(t + 1) * 128, hp * 256:(hp + 1) * 256], yo[b][:], reads=[("yo", b)], writes=[("gdn_o", t)])
            else:
                P.dma(y_d.ap()[t * 128:(t + 1) * 128, :], yo[b][:], reads=[("yo", b)])
    for hp in range(nhp):
        load_weights(hp)
        phase1(hp)
        phase2(hp)
        phase3(hp)
    if standalone:
        return P.build()
    P.pop_scope()


def gdn_masks():
    m = np.zeros((5, 128, 128), np.float32)
    kp = np.arange(128)[:, None]
    c = np.arange(128)[None, :]
    same = (kp // 64) == (c // 64)
    m[0] = (same & (kp <= c))
    m[1] = (same & (kp < c))
    m[2] = (same & (kp >= c))
    m[3] = (same & (kp > c))
    m[4] = np.eye(128)
    return m


def gdn_weights(hp, c_w_in, c_conv, a_log_f, dtb_f, a_log_b, dtb_b):
    w = c_w_in[0]
    hs = [2 * hp, 2 * hp + 1]
    cols = []
    for base in (0, 1024, 2048):
        for h in hs:
            cols.append(w[:, base + h * 128: base + (h + 1) * 128])
    wf = np.ascontiguousarray(np.concatenate(cols, axis=1))
    zc = [w[:, 3072 + h * 128:3072 + (h + 1) * 128] for h in hs]
    gb = 4096
    gcols = [w[:, gb + 0 + h:gb + 0 + h + 1] for h in hs] + [w[:, gb + 16 + h:gb + 16 + h + 1] for h in hs] + \
            [w[:, gb + 8 + h:gb + 8 + h + 1] for h in hs] + [w[:, gb + 24 + h:gb + 24 + h + 1] for h in hs]
    wt = np.ascontiguousarray(np.concatenate(zc + gcols, axis=1))
    cw = np.zeros((128, 6, 5), np.float32)
    bi = 0
    for base in (0, 1024, 2048):
        for h in hs:
            cw[:, bi, :] = c_conv[0][:, base + h * 128: base + (h + 1) * 128].T
            bi += 1
    gconst = np.array([dtb_f[0][hs[0]], dtb_f[0][hs[1]], dtb_b[0][hs[0]], dtb_b[0][hs[1]],
                       a_log_f[0][hs[0]], a_log_f[0][hs[1]], a_log_b[0][hs[0]], a_log_b[0][hs[1]]], np.float32)
    return wf, wt, cw, gconst


def gdn_inputs(x2, c_norm, c_w_in, c_conv, a_log_f, dtb_f, a_log_b, dtb_b, out_norm):
    import ml_dtypes
    w = c_w_in[0]
    ident = np.eye(128, dtype=np.float32).astype(ml_dtypes.bfloat16)
    masks = gdn_masks()
    maps = []
    for c in range(NCORES):
        b, hp = c // 4, c % 4
        hs = [2 * hp, 2 * hp + 1]
        cols = []
        for base in (0, 1024, 2048):
            for h in hs:
                cols.append(w[:, base + h * 128: base + (h + 1) * 128])
        wf = np.ascontiguousarray(np.concatenate(cols, axis=1))
        zc = [w[:, 3072 + h * 128:3072 + (h + 1) * 128] for h in hs]
        gb = 4096
        gcols = [w[:, gb + 0 + h:gb + 0 + h + 1] for h in hs] + [w[:, gb + 16 + h:gb + 16 + h + 1] for h in hs] + \
                [w[:, gb + 8 + h:gb + 8 + h + 1] for h in hs] + [w[:, gb + 24 + h:gb + 24 + h + 1] for h in hs]
        wt = np.ascontiguousarray(np.concatenate(zc + gcols, axis=1))
        cw = np.zeros((128, 6, 5), np.float32)
        bi = 0
        for base in (0, 1024, 2048):
            for h in hs:
                cw[:, bi, :] = c_conv[0][:, base + h * 128: base + (h + 1) * 128].T
                bi += 1
        gconst = np.array([[dtb_f[0][hs[0]], dtb_f[0][hs[1]], dtb_b[0][hs[0]], dtb_b[0][hs[1]],
                            a_log_f[0][hs[0]], a_log_f[0][hs[1]], a_log_b[0][hs[0]], a_log_b[0][hs[1]]]], np.float32)
        xp = np.zeros((SEQ + 128, DM), np.float32)
        xp[2:2 + SEQ] = x2[b]
        maps.append({"xp": xp, "g": c_norm[0:1], "wf": wf[None], "wt": wt[None], "cw": cw[None], "gconst": gconst,
                     "onorm": out_norm[0:1], "gmask": masks, "ident": ident})
    return maps


def build_l0():
    P = Prog()
    x1_s = P.dram("x1_s", [NTOK, DM], F32, "Internal")
    build_attn(P, "a_", x1_s)
    P.fence()
    build_ffn(1, False, P, "f_", x1_s)
    return P.build()


def build_fused():
    P = Prog()
    x1_s = P.dram("x1_s", [SEQ, DM], F32, "Internal")
    x2_s = P.dram("x2_s", [SEQ, DM], F32, "Internal")
    og_s = P.dram("og_s", [SEQ, DM], F32, "Internal")
    qoff = P.dram("m_sel", [1, 4], F32, "ExternalInput")
    build_attn(P, "a_", x1_s, nq=4)
    P.fence()
    H = ffn_handles(P, "f_", 1, False)
    for q in range(4):
        build_ffn(1, False, P, "f_", x_handle=x1_s, H=H, x_row0=q * NTOK, y_handle=x2_s, y_row0=q * NTOK)
    P.fence()
    build_gdn(P=P, pf="g_", x_handle=x2_s, o_handle=og_s, nhp=4)
    P.fence()
    build_ffn(8, True, P, "m_", x_handle=x2_s, o_handle=og_s, dyn=qoff)
    return P.build()


def kernel(x, ab_norm, ab_w_in, ab_q_norm_a, ab_k_norm_a, ab_sink_a, ab_q_norm_b, ab_k_norm_b,
                 ab_w_out, ffn_norm, ffn_w_gate, ffn_w_up, ffn_w_down, c_norm, c_w_in, c_conv,
                 c_a_log_fwd, c_dt_bias_fwd, c_a_log_bwd, c_dt_bias_bwd, c_out_norm, c_w_out,
                 moe_norm, moe_w_router, moe_w_gate, moe_w_up, moe_w_down):
    f = lambda a: np.ascontiguousarray(np.asarray(a, dtype=np.float32))
    x = f(x)
    cores = list(range(NCORES))
    ident = _ident_bf16()
    identf = np.eye(128, dtype=np.float32)
    amaps = attn_inputs(x, f(ab_norm), f(ab_w_in), f(ab_q_norm_a), f(ab_k_norm_a), f(ab_sink_a), f(ab_q_norm_b),
                        f(ab_k_norm_b), f(ab_w_out), fused=True)
    gw = [gdn_weights(hp, f(c_w_in), f(c_conv), f(c_a_log_fwd), f(c_dt_bias_fwd), f(c_a_log_bwd), f(c_dt_bias_bwd))
          for hp in range(4)]
    g_wf = np.ascontiguousarray(np.stack([w[0] for w in gw]))
    g_wt = np.ascontiguousarray(np.stack([w[1] for w in gw]))
    g_cw = np.ascontiguousarray(np.stack([w[2] for w in gw]))
    g_gc = np.ascontiguousarray(np.stack([w[3] for w in gw]))
    gmask = gdn_masks()
    maps = []
    for c in cores:
        m = {"a_" + k: v for k, v in amaps[c].items()}
        m.update({"f_g": f(ffn_norm)[0:1], "f_wg": f(ffn_w_gate), "f_wu": f(ffn_w_up), "f_wd": f(ffn_w_down), "f_ident": ident})
        m.update({"g_g": f(c_norm)[0:1], "g_wf": g_wf, "g_wt": g_wt, "g_cw": g_cw, "g_gconst": g_gc,
                  "g_onorm": f(c_out_norm)[0:1], "g_gmask": gmask, "g_ident": ident})
        m.update({"m_wo": f(c_w_out)[0], "m_g": f(moe_norm)[0:1], "m_wg": f(moe_w_gate)[0], "m_wu": f(moe_w_up)[0],
                  "m_wd": f(moe_w_down)[0], "m_wr": f(moe_w_router)[0], "m_ident": ident, "m_identf": identf,
                  "m_sel": np.eye(4, dtype=np.float32)[c % 4][None, :]})
        maps.append(m)
    res = run_bass_kernel_spmd(_prog("fused", build_fused), maps, core_ids=cores)
    y = np.concatenate([r["m_y"] for r in res.results], 0)
    return y.reshape(2, SEQ, DM).astype(np.float32)


_CACHE = {}
STAGES = {}


def _prog(name, fn):
    if name not in _CACHE:
        _CACHE[name] = fn()
    return _CACHE[name]


def _ident_bf16():
    import ml_dtypes
    return np.eye(128, dtype=np.float32).astype(ml_dtypes.bfloat16)


def kernel_unfused(x, ab_norm, ab_w_in, ab_q_norm_a, ab_k_norm_a, ab_sink_a, ab_q_norm_b, ab_k_norm_b,
           ab_w_out, ffn_norm, ffn_w_gate, ffn_w_up, ffn_w_down, c_norm, c_w_in, c_conv,
           c_a_log_fwd, c_dt_bias_fwd, c_a_log_bwd, c_dt_bias_bwd, c_out_norm, c_w_out,
           moe_norm, moe_w_router, moe_w_gate, moe_w_up, moe_w_down):
    f = lambda a: np.ascontiguousarray(np.asarray(a, dtype=np.float32))
    x = f(x)
    cores = list(range(NCORES))
    ident = _ident_bf16()
    identf = np.eye(128, dtype=np.float32)
    amaps = attn_inputs(x, f(ab_norm), f(ab_w_in), f(ab_q_norm_a), f(ab_k_norm_a), f(ab_sink_a), f(ab_q_norm_b),
                        f(ab_k_norm_b), f(ab_w_out))
    maps = []
    for c in cores:
        m = {"a_" + k: v for k, v in amaps[c].items()}
        m.update({"f_g": f(ffn_norm)[0:1], "f_wg": f(ffn_w_gate), "f_wu": f(ffn_w_up), "f_wd": f(ffn_w_down), "f_ident": ident})
        maps.append(m)
    res = run_bass_kernel_spmd(_prog("l0", build_l0), maps, core_ids=cores)
    x2 = np.concatenate([r["f_y"] for r in res.results], 0)
    STAGES["x2"] = x2
    maps = gdn_inputs(x2.reshape(2, SEQ, DM), f(c_norm), f(c_w_in), f(c_conv), f(c_a_log_fwd), f(c_dt_bias_fwd),
                      f(c_a_log_bwd), f(c_dt_bias_bwd), f(c_out_norm))
    res = run_bass_kernel_spmd(_prog("gdn", build_gdn), maps, core_ids=cores)
    o = np.zeros((2, SEQ, DM), np.float32)
    for c in cores:
        b, hp = c // 4, c % 4
        o[b][:, hp * 256:(hp + 1) * 256] = res.results[c]["y"]
    o = o.reshape(-1, DM)
    STAGES["o"] = o
    maps = [{"x": np.ascontiguousarray(x2[c * NTOK:(c + 1) * NTOK]), "o": np.ascontiguousarray(o[c * NTOK:(c + 1) * NTOK]),
             "wo": f(c_w_out)[0], "g": f(moe_norm)[0:1], "wg": f(moe_w_gate)[0], "wu": f(moe_w_up)[0], "wd": f(moe_w_down)[0],
             "wr": f(moe_w_router)[0], "ident": ident, "identf": identf} for c in cores]
    res = run_bass_kernel_spmd(_prog("moe", lambda: build_ffn(8, True)), maps, core_ids=cores)
    y = np.concatenate([r["y"] for r in res.results], 0)
    return y.reshape(2, SEQ, DM).astype(np.float32)
```

```python
import numpy as np
import concourse.bass as bass
import concourse.mybir as mybir
from concourse.bass_utils import run_bass_kernel_spmd
from contextlib import ExitStack

F32 = mybir.dt.float32
BF16 = mybir.dt.bfloat16
I32 = mybir.dt.int32
ALU = mybir.AluOpType
AF = mybir.ActivationFunctionType
AX = mybir.AxisListType

ENGS = ["tensor", "vector", "scalar", "gpsimd", "sync"]
NCORES = 8


class _Op:
    __slots__ = ("eng", "seq", "fn", "deps", "signal", "count", "dma", "dsem", "dcum", "snap")

    def __init__(self, eng, seq, fn, dma):
        self.eng = eng
        self.seq = seq
        self.fn = fn
        self.deps = []
        self.signal = False
        self.count = 0
        self.dma = dma
        self.dsem = -1
        self.dcum = 0
        self.snap = None


class Lazy:
    def __init__(self, P):
        self.P = P
        self.q = []
        self.grp = None

    def begin(self):
        self.grp = []

    def end(self):
        g, self.grp = self.grp, None
        self.q.append(lambda: [t() for t in g])

    def __getattr__(self, name):
        f = getattr(self.P, name)

        def rec(*a, **k):
            (self.q if self.grp is None else self.grp).append(lambda: f(*a, **k))
        return rec


def interleave(lazies):
    n = max(len(L.q) for L in lazies)
    for i in range(n):
        for L in lazies:
            if i < len(L.q):
                L.q[i]()


class Prog:
    DMA_RING = 12

    def __init__(self):
        self.nc = bass.Bass("TRN2", target_bir_lowering=False)
        self.es = ExitStack()
        self.ops = {e: [] for e in ENGS}
        self.lastw = {}
        self.readers = {}
        self.known = {e: {} for e in ENGS}
        self.knownd = {e: {} for e in ENGS}
        self.dmas = []
        self.ring_last = {}
        self.ring_cum = {}
        self.ring_next = {e: 0 for e in ENGS}
        self.n_sb = 0
        self.alias = {}
        self.scopes = []
        self.fence_t = self.es.enter_context(self.nc.sbuf_tensor("fence_t", [128, 64], BF16))
        self.nfence = 0

    def push_scope(self):
        self.scopes.append(self.es)
        self.es = ExitStack()

    def pop_scope(self):
        self.es.close()
        self.es = self.scopes.pop()
        self.alias = {}

    def fence(self):
        toks = []
        for e in ENGS:
            if self.ops[e] and not self.ops[e][-1].dma:
                toks.append(("e", e, self.ops[e][-1].seq))
        for key, idx in self.ring_last.items():
            toks.append(("d", idx))
        ft = self.fence_t
        n = self.nfence
        self.nfence += 1
        self.push_scope()
        pf = self.ps([128, 512], F32, f"fence_ps{n}")
        self.op("vector", lambda e: e.memset(ft[:, 0:16], 0.0), extra=toks)
        self.op("scalar", lambda e: e.copy(out=ft[:, 16:32], in_=ft[:, 16:32]), extra=toks)
        self.op("gpsimd", lambda e: e.memset(ft[:, 32:48], 0.0), extra=toks)
        self.op("tensor", lambda e: e.matmul(pf[0:1, 0:8], lhsT=ft[0:1, 48:49], rhs=ft[0:1, 48:56], start=True, stop=True),
                extra=toks)
        self.op("sync", lambda e: e.dma_start(out=ft[0:1, 56:60], in_=ft[0:1, 60:64]), extra=toks, dma=True)
        self.scopes_tmp = None
        self.es.close()
        self.es = self.scopes.pop()

    def bank(self, key, bank_id):
        self.alias[key] = ("BANK", bank_id)

    def sb(self, shape, dt, name=None):
        self.n_sb += 1
        return self.es.enter_context(self.nc.sbuf_tensor(f"sb{self.n_sb}_{name or ''}", list(shape), dt))

    def ps(self, shape, dt, name=None):
        self.n_sb += 1
        return self.es.enter_context(self.nc.psum_tensor(f"ps{self.n_sb}_{name or ''}", list(shape), dt))

    def dram(self, name, shape, dt, kind):
        return self.nc.dram_tensor(name, list(shape), dt, kind=kind)

    def _dep_tokens(self, reads, writes):
        deps = set()
        for k in reads:
            w = self.lastw.get(k)
            if w is not None:
                deps.add(w)
        for k in writes:
            w = self.lastw.get(k)
            if w is not None:
                deps.add(w)
            for r in self.readers.get(k, ()):
                deps.add(r)
        return deps

    def _need(self, eng, tok):
        if tok[0] == "e":
            _, se, sq = tok
            if se == eng and eng in ("tensor", "sync"):
                return False
            return self.known[eng].get(se, -1) < sq
        else:
            d = self.dmas[tok[1]]
            return self.knownd[eng].get(d.dsem, 0) < d.dcum

    def _learn(self, eng, tok):
        if tok[0] == "e":
            _, se, sq = tok
            src = self.ops[se][sq]
            src.signal = True
            k = self.known[eng]
            if k.get(se, -1) < sq:
                k[se] = sq
            if src.snap is not None:
                sk, sd = src.snap
                for a, b in sk.items():
                    if k.get(a, -1) < b:
                        k[a] = b
                kd = self.knownd[eng]
                for a, b in sd.items():
                    if kd.get(a, 0) < b:
                        kd[a] = b
        else:
            d = self.dmas[tok[1]]
            kd = self.knownd[eng]
            if kd.get(d.dsem, 0) < d.dcum:
                kd[d.dsem] = d.dcum

    def op(self, eng, fn, reads=(), writes=(), dma=False, extra=()):
        al = self.alias
        if al:
            excl = {al[k] for k in reads if k in al} | {al[k] for k in writes if k in al}
            reads = [k for k in reads if k not in al]
            writes = [k for k in writes if k not in al] + list(excl)
        deps = self._dep_tokens(reads, writes)
        deps.update(extra)
        seq = len(self.ops[eng])
        o = _Op(eng, seq, fn, dma)
        if dma:
            slot = self.ring_next[eng]
            self.ring_next[eng] = (slot + 1) % self.DMA_RING
            key = (eng, slot)
            prev = self.ring_last.get(key)
            if prev is not None:
                deps.add(("d", prev))
            o.dsem = key
            self.ring_cum[key] = self.ring_cum.get(key, 0) + 16
            o.dcum = self.ring_cum[key]
        for tok in sorted(deps, key=lambda t: (t[0], str(t[1]), t[2] if len(t) > 2 else 0)):
            if self._need(eng, tok):
                o.deps.append(tok)
                self._learn(eng, tok)
        o.snap = (dict(self.known[eng]), dict(self.knownd[eng]))
        self.ops[eng].append(o)
        if dma:
            idx = len(self.dmas)
            self.dmas.append(o)
            self.ring_last[o.dsem] = idx
            tok = ("d", idx)
        else:
            tok = ("e", eng, seq)
        for k in writes:
            self.lastw[k] = tok
            self.readers[k] = []
        for k in reads:
            if k not in writes:
                self.readers.setdefault(k, []).append(tok)
        return tok

    def pe(self, fn, reads=(), writes=()):
        return self.op("tensor", fn, reads, writes)

    def dve(self, fn, reads=(), writes=()):
        return self.op("vector", fn, reads, writes)

    def act(self, fn, reads=(), writes=()):
        return self.op("scalar", fn, reads, writes)

    def pool(self, fn, reads=(), writes=()):
        return self.op("gpsimd", fn, reads, writes)

    def dma(self, out, in_, reads=(), writes=(), q="sync", **kw):
        return self.op(q, lambda e: e.dma_start(out=out, in_=in_, **kw), reads, writes, dma=True)

    def build(self):
        nc = self.nc
        es = self.es
        esem = {e: es.enter_context(nc.semaphore(f"s_{e}")) for e in ENGS}
        dsem = {}
        for key in self.ring_cum:
            dsem[key] = es.enter_context(nc.semaphore(f"d_{key[0]}_{key[1]}"))
        for e in ENGS:
            c = 0
            for o in self.ops[e]:
                if o.signal and not o.dma:
                    c += 1
                o.count = c
        block = es.enter_context(nc.Block())
        ops = self.ops
        dmas = self.dmas
        ring_cum = self.ring_cum

        def emit(engname):
            def body(eng):
                for o in ops[engname]:
                    for tok in o.deps:
                        if tok[0] == "e":
                            eng.wait_ge(esem[tok[1]], ops[tok[1]][tok[2]].count)
                        else:
                            d = dmas[tok[1]]
                            eng.wait_ge(dsem[d.dsem], d.dcum)
                    ins = o.fn(eng)
                    if o.dma:
                        ins.then_inc(dsem[o.dsem], 16)
                    elif o.signal:
                        ins.then_inc(esem[engname], 1)
                for key, cum in ring_cum.items():
                    if key[0] == engname:
                        eng.wait_ge(dsem[key], cum)
            return body

        for e in ENGS:
            if not ops[e]:
                continue
            getattr(block, e)(emit(e))
        es.close()
        return nc


EPS = 1e-6


def bcast_rows(handle, ncols, nparts=128, offset=0):
    return bass.AP(handle, offset, [[0, nparts], [1, ncols]])


def emit_norm_T(P, x_ap, xkey, g_bc, dstT, dst_cols, dkey, W, uid):
    nc = P.nc
    xn = W["xn"][uid % 2]
    kxn = ("xn", uid % 2)
    psT = W["psT"]
    if g_bc is not None:
        junk = W["junk"]
        ss = W["ss"][uid % 2]
        kss = ("ss", uid % 2)
        P.act(lambda e: e.activation(out=junk[:], in_=x_ap, func=AF.Square, accum_out=ss[:, 0:1]),
              reads=[xkey], writes=[kss, "junk"])
        P.act(lambda e: e.activation(out=ss[:, 1:2], in_=ss[:, 0:1], func=AF.Sqrt,
                                     scale=1.0 / 1024.0, bias=W["epsc"][:, 0:1]),
              reads=[kss], writes=[kss])
        P.dve(lambda e: e.reciprocal(out=ss[:, 2:3], in_=ss[:, 1:2]), reads=[kss], writes=[kss])
        P.dve(lambda e: e.scalar_tensor_tensor(out=xn[:], in0=x_ap, scalar=ss[:, 2:3], in1=g_bc[:],
                                               op0=ALU.mult, op1=ALU.mult),
              reads=[xkey, kss, "gbc"], writes=[kxn])
    else:
        P.dve(lambda e: e.tensor_copy(out=xn[:], in_=x_ap), reads=[xkey], writes=[kxn])

    def tr(e):
        ins = None
        for kc in range(8):
            ins = e.transpose(out=psT[:, kc * 128:(kc + 1) * 128], in_=xn[:, kc * 128:(kc + 1) * 128],
                              identity=W["ident"][:])
        return ins
    P.pe(tr, reads=[kxn, "ident"], writes=["psT"])
    P.act(lambda e: e.copy(out=dstT[:, :, dst_cols], in_=psT[:].rearrange("p (k c) -> p k c", k=8)),
          reads=["psT"], writes=[dkey])


def alloc_norm_work(P):
    W = {}
    W["xn"] = [P.sb([128, 1024], BF16) for _ in range(2)]
    W["junk"] = P.sb([128, 1024], BF16)
    W["ss"] = [P.sb([128, 4], F32) for _ in range(2)]
    W["psT"] = P.ps([128, 1024], BF16)
    W["ident"] = P.sb([128, 128], BF16)
    W["epsc"] = P.sb([128, 1], F32)
    return W


NTOK = 2048
NT = NTOK // 128
DM = 1024
DFF = 3584
NF = DFF // 128
FR = 7


def ffn_handles(P, pf, n_exp, with_proj):
    H = {}
    H["g"] = P.dram(pf + "g", [1, DM], F32, "ExternalInput")
    H["wg"] = P.dram(pf + "wg", [n_exp, DM, DFF], F32, "ExternalInput")
    H["wu"] = P.dram(pf + "wu", [n_exp, DM, DFF], F32, "ExternalInput")
    H["wd"] = P.dram(pf + "wd", [n_exp, DFF, DM], F32, "ExternalInput")
    H["ident"] = P.dram(pf + "ident", [128, 128], BF16, "ExternalInput")
    if n_exp > 1:
        H["wr"] = P.dram(pf + "wr", [DM, n_exp], F32, "ExternalInput")
        H["identf"] = P.dram(pf + "identf", [128, 128], F32, "ExternalInput")
    if with_proj:
        H["wo"] = P.dram(pf + "wo", [DM, DM], F32, "ExternalInput")
    return H


def build_ffn(n_exp, with_proj, P=None, pf="", x_handle=None, o_handle=None, H=None, x_row0=0, y_handle=None, y_row0=0, dyn=None):
    standalone = P is None
    if standalone:
        P = Prog()
    nc = P.nc
    if not standalone:
        P.push_scope()
    x_d = x_handle if x_handle is not None else P.dram(pf + "x", [NTOK, DM], F32, "ExternalInput")
    if H is None:
        H = ffn_handles(P, pf, n_exp, with_proj)
    g_d, wg_d, wu_d, wd_d, id_d = H["g"], H["wg"], H["wu"], H["wd"], H["ident"]
    y_d = y_handle if y_handle is not None else P.dram(pf + "y", [NTOK, DM], F32, "ExternalOutput")
    if n_exp > 1:
        wr_d, idf_d = H["wr"], H["identf"]
    if with_proj:
        o_d = o_handle if o_handle is not None else P.dram(pf + "o", [NTOK, DM], F32, "ExternalInput")
        wo_d = H["wo"]
    W = alloc_norm_work(P)
    yacc = P.sb([128, NT, DM], F32, "yacc")
    xnT = P.sb([128, 8, NTOK], BF16, "xnT")
    gbc = P.sb([128, DM], F32, "gbc")
    actT = P.sb([128, FR, NTOK], BF16, "actT")
    wgc = [P.sb([128, 8, 128], BF16) for _ in range(3)]
    wuc = [P.sb([128, 8, 128], BF16) for _ in range(3)]
    wdc = [P.sb([128, DM], BF16) for _ in range(FR + 3)]
    sg = [P.sb([128, 512], F32) for _ in range(2)]
    psG = [P.ps([128, 512], F32) for _ in range(2)]
    psU = [P.ps([128, 512], F32) for _ in range(2)]
    psY = [P.ps([128, 512], F32) for _ in range(2)]
    W["psRL"] = P.ps([128, 512], F32)
    for i_, k_ in enumerate(["psT", "psRL", ("psG", 0), ("psG", 1), ("psU", 0), ("psU", 1), ("psY", 0), ("psY", 1)]):
        P.bank(k_, i_)

    P.dma(W["ident"][:], id_d.ap(), writes=["ident"])
    P.dma(gbc[:], bcast_rows(g_d, DM), writes=["gbc"])
    P.dve(lambda e: e.memset(W["epsc"][:], EPS), writes=["epsc"])
    if dyn is not None:
        sel = P.sb([128, 4], F32, "sel")
        actf = actT.bitcast(F32)
        selt = [actf[:, b_, :] for b_ in range(2)]
        seltk = [[("actT", b_, tg_) for tg_ in range(4)] for b_ in range(2)]
        P.dma(sel[:], bcast_rows(dyn, 4), writes=["sel"])
        seli = [0]

        def load_sel(handle, dst_ap, dkey, t):
            for q in range(4):
                b = seli[0] % 2
                seli[0] += 1
                P.dma(selt[b], handle.ap()[q * NTOK + t * 128:q * NTOK + (t + 1) * 128, :], writes=seltk[b])
                if q == 0:
                    P.dve(lambda e, b=b: e.tensor_scalar(out=dst_ap, in0=selt[b], scalar1=sel[:, 0:1], scalar2=None,
                                                        op0=ALU.mult), reads=seltk[b] + ["sel"], writes=[dkey])
                else:
                    P.dve(lambda e, b=b, q=q: e.scalar_tensor_tensor(out=dst_ap, in0=selt[b], scalar=sel[:, q:q + 1],
                                                                     in1=dst_ap, op0=ALU.mult, op1=ALU.add),
                          reads=seltk[b] + ["sel", dkey], writes=[dkey])
    for t in range(NT):
        if dyn is not None:
            load_sel(x_d, yacc[:, t, :], ("y", t), t)
        else:
            r0 = x_row0 + t * 128
            P.dma(yacc[:, t, :], x_d.ap()[r0:r0 + 128, :], reads=[("x1s", r0 // 128)], writes=[("y", t)])

    if with_proj:
        wo = P.sb([128, 8, DM], BF16, "wo")
        P.dma(wo[:], wo_d.ap().rearrange("(k p) c -> p k c", p=128), writes=["wo"], q="gpsimd")
        ot = [P.sb([128, DM], F32) for _ in range(2)]
        for t in range(NT):
            if dyn is not None:
                load_sel(o_d, ot[t % 2][:], ("ot", t % 2), t)
            else:
                r0 = x_row0 + t * 128
                P.dma(ot[t % 2][:], o_d.ap()[r0:r0 + 128, :], writes=[("ot", t % 2)])
            emit_norm_T(P, ot[t % 2][:], ("ot", t % 2), None, xnT, slice(t * 128, (t + 1) * 128), ("xnT", t), W, t)
            for h in range(2):
                def mm(e, t=t, h=h):
                    ins = None
                    for kc in range(8):
                        ins = e.matmul(psY[h][:], lhsT=xnT[:, kc, t * 128:(t + 1) * 128],
                                       rhs=wo[:, kc, h * 512:(h + 1) * 512], start=(kc == 0), stop=(kc == 7))
                    return ins
                P.pe(mm, reads=[("xnT", t), "wo"], writes=[("psY", h)])
                P.dve(lambda e, t=t, h=h: e.tensor_tensor(out=yacc[:, t, h * 512:(h + 1) * 512], in0=psY[h][:],
                                                          in1=yacc[:, t, h * 512:(h + 1) * 512], op=ALU.add),
                      reads=[("psY", h), ("y", t)], writes=[("y", t)])

    for t in range(NT):
        emit_norm_T(P, yacc[:, t, :], ("y", t), gbc, xnT, slice(t * 128, (t + 1) * 128), ("xnT", t), W, t)
    allx = [("xnT", t) for t in range(NT)]

    comb = None
    if n_exp > 1:
        W["psRA"], W["psRB"] = psG[0], psG[1]
        comb = emit_router_keys(P, yacc, gbc, W, wr_d, idf_d, n_exp)

    ci = 0
    di = 0
    for ex in range(n_exp):
        wgv = wg_d.ap()[ex].rearrange("(k p) c -> p k c", p=128)
        wuv = wu_d.ap()[ex].rearrange("(k p) c -> p k c", p=128)
        wdv = wd_d.ap()[ex].rearrange("(f p) c -> p f c", p=128)
        for r in range(NF // FR):
            dslots = []
            for fi in range(FR):
                f = r * FR + fi
                cs = ci % 3
                ci += 1
                P.dma(wgc[cs][:], wgv[:, :, f * 128:(f + 1) * 128], writes=[("wgc", cs)], q="gpsimd")
                P.dma(wuc[cs][:], wuv[:, :, f * 128:(f + 1) * 128], writes=[("wuc", cs)], q="gpsimd")
                ds = di % (FR + 3)
                di += 1
                dslots.append(ds)
                P.dma(wdc[ds][:], wdv[:, f, :], writes=[("wdc", ds)], q="gpsimd")
                for tg in range(4):
                    b = (fi * 4 + tg) % 2

                    def mmg(e, cs=cs, tg=tg, b=b):
                        ins = None
                        for kc in range(8):
                            ins = e.matmul(psG[b][:], lhsT=wgc[cs][:, kc, :], rhs=xnT[:, kc, tg * 512:(tg + 1) * 512],
                                           start=(kc == 0), stop=(kc == 7))
                        return ins

                    def mmu(e, cs=cs, tg=tg, b=b):
                        ins = None
                        for kc in range(8):
                            ins = e.matmul(psU[b][:], lhsT=wuc[cs][:, kc, :], rhs=xnT[:, kc, tg * 512:(tg + 1) * 512],
                                           start=(kc == 0), stop=(kc == 7))
                        return ins
                    xk = [("xnT", t) for t in range(tg * 4, tg * 4 + 4)]
                    P.pe(mmg, reads=[("wgc", cs)] + xk, writes=[("psG", b)])
                    P.pe(mmu, reads=[("wuc", cs)] + xk, writes=[("psU", b)])
                    P.act(lambda e, b=b: e.activation(out=sg[b][:], in_=psG[b][:], func=AF.Silu),
                          reads=[("psG", b)], writes=[("sg", b)])
                    P.dve(lambda e, b=b, fi=fi, tg=tg: e.tensor_tensor(out=actT[:, fi, tg * 512:(tg + 1) * 512],
                                                                       in0=sg[b][:], in1=psU[b][:], op=ALU.mult),
                          reads=[("sg", b), ("psU", b)], writes=[("actT", fi, tg)])
            for t in range(NT):
                for h in range(2):
                    def mmd(e, t=t, h=h, dslots=dslots):
                        ins = None
                        for fi in range(FR):
                            ins = e.matmul(psY[h][:], lhsT=actT[:, fi, t * 128:(t + 1) * 128],
                                           rhs=wdc[dslots[fi]][:, h * 512:(h + 1) * 512],
                                           start=(fi == 0), stop=(fi == FR - 1))
                        return ins
                    P.pe(mmd, reads=[("actT", fi, t // 4) for fi in range(FR)] + [("wdc", s) for s in dslots],
                         writes=[("psY", h)])
                    if comb is None:
                        P.dve(lambda e, t=t, h=h: e.tensor_tensor(out=yacc[:, t, h * 512:(h + 1) * 512], in0=psY[h][:],
                                                                  in1=yacc[:, t, h * 512:(h + 1) * 512], op=ALU.add),
                              reads=[("psY", h), ("y", t)], writes=[("y", t)])
                    else:
                        P.dve(lambda e, t=t, h=h, ex=ex: e.scalar_tensor_tensor(
                            out=yacc[:, t, h * 512:(h + 1) * 512], in0=psY[h][:],
                            scalar=comb[:, t, ex:ex + 1], in1=yacc[:, t, h * 512:(h + 1) * 512],
                            op0=ALU.mult, op1=ALU.add),
                            reads=[("psY", h), ("y", t), "comb"], writes=[("y", t)])
    yv = y_d.ap()[y_row0:y_row0 + NTOK, :].rearrange("(t p) d -> p t d", p=128)
    for t4 in range(4):
        P.dma(yv[:, t4 * 4:(t4 + 1) * 4, :], yacc[:, t4 * 4:(t4 + 1) * 4, :],
              reads=[("y", t) for t in range(t4 * 4, t4 * 4 + 4)],
              writes=[("yout", pf, (y_row0 // 128) + t) for t in range(t4 * 4, t4 * 4 + 4)])
    if standalone:
        return P.build()
    P.pop_scope()


def emit_router_keys(P, yacc, gbc, W, wr_d, idf_d, n_exp):
    identf = P.sb([128, 128], F32, "identf")
    wr = P.sb([128, 8, n_exp], F32, "wr")
    comb = P.sb([128, NT, n_exp], F32, "comb")
    x32 = P.sb([128, DM], F32, "rx32")
    xT32 = P.sb([128, 8, 128], F32, "rxT32")
    rs = P.sb([128, 16], F32, "rsmall")
    lg = P.sb([128, 8], F32, "rlg")
    mx = P.sb([128, 8], F32, "rmx")
    tmp = P.sb([128, 8], F32, "rtmp")
    psA = W["psRA"]
    psB = W["psRB"]
    psL = W["psRL"]
    P.dma(identf[:], idf_d.ap(), writes=["identf"])
    P.dma(wr[:], wr_d.ap().rearrange("(k p) e -> p k e", p=128), writes=["wr"])
    for t in range(NT):
        xt = yacc[:, t, :]
        P.act(lambda e, xt=xt: e.activation(out=W["junk"][:], in_=xt, func=AF.Square, accum_out=rs[:, 0:1]),
              reads=[("y", t)], writes=["junk", "rs"])
        P.act(lambda e: e.activation(out=rs[:, 1:2], in_=rs[:, 0:1], func=AF.Sqrt, scale=1.0 / 1024.0,
                                     bias=W["epsc"][:, 0:1]), reads=["rs"], writes=["rs"])
        P.dve(lambda e: e.reciprocal(out=rs[:, 2:3], in_=rs[:, 1:2]), reads=["rs"], writes=["rs"])
        P.dve(lambda e, xt=xt: e.scalar_tensor_tensor(out=x32[:], in0=xt, scalar=rs[:, 2:3], in1=gbc[:],
                                                      op0=ALU.mult, op1=ALU.mult),
              reads=[("y", t), "rs", "gbc"], writes=["rx32"])

        def tr(e):
            ins = None
            for kc in range(8):
                dst = (psA if kc < 4 else psB)[:, (kc % 4) * 128:(kc % 4 + 1) * 128]
                ins = e.transpose(out=dst, in_=x32[:, kc * 128:(kc + 1) * 128], identity=identf[:])
            return ins
        P.pe(tr, reads=["rx32", "identf"], writes=[("psG", 0), ("psG", 1)])
        P.act(lambda e: e.copy(out=xT32[:, 0:4, :], in_=psA[:].rearrange("p (k c) -> p k c", k=4)),
              reads=[("psG", 0)], writes=["rxTa"])
        P.act(lambda e: e.copy(out=xT32[:, 4:8, :], in_=psB[:].rearrange("p (k c) -> p k c", k=4)),
              reads=[("psG", 1)], writes=["rxTb"])

        def mm(e):
            ins = None
            for kc in range(8):
                ins = e.matmul(psL[:, 0:n_exp], lhsT=xT32[:, kc, :], rhs=wr[:, kc, :], start=(kc == 0), stop=(kc == 7))
            return ins
        P.pe(mm, reads=["rxTa", "rxTb", "wr"], writes=["psRL"])
        P.dve(lambda e: e.tensor_copy(out=lg[:], in_=psL[:, 0:n_exp]), reads=["psRL"], writes=["rlg"])
        P.dve(lambda e: e.max(out=mx[:], in_=lg[:]), reads=["rlg"], writes=["rmx"])
        P.dve(lambda e: e.tensor_tensor(out=rs[:, 4:5], in0=mx[:, 1:2], in1=mx[:, 0:1], op=ALU.subtract),
              reads=["rmx"], writes=["rs"])
        P.act(lambda e: e.activation(out=rs[:, 5:6], in_=rs[:, 4:5], func=AF.Exp), reads=["rs"], writes=["rs"])
        P.dve(lambda e: e.tensor_scalar(out=rs[:, 6:7], in0=rs[:, 5:6], scalar1=1.0, scalar2=None, op0=ALU.add),
              reads=["rs"], writes=["rs"])
        P.dve(lambda e: e.reciprocal(out=rs[:, 7:8], in_=rs[:, 6:7]), reads=["rs"], writes=["rs"])
        P.dve(lambda e: e.tensor_tensor(out=rs[:, 8:9], in0=rs[:, 5:6], in1=rs[:, 7:8], op=ALU.mult),
              reads=["rs"], writes=["rs"])
        P.dve(lambda e, t=t: e.tensor_scalar(out=comb[:, t, :], in0=lg[:], scalar1=mx[:, 0:1], scalar2=rs[:, 7:8],
                                             op0=ALU.is_equal, op1=ALU.mult),
              reads=["rlg", "rmx", "rs"], writes=["comb"])
        P.dve(lambda e: e.tensor_scalar(out=tmp[:], in0=lg[:], scalar1=mx[:, 1:2], scalar2=rs[:, 8:9],
                                        op0=ALU.is_equal, op1=ALU.mult),
              reads=["rlg", "rmx", "rs"], writes=["rtmp"])
        P.dve(lambda e, t=t: e.tensor_tensor(out=comb[:, t, :], in0=comb[:, t, :], in1=tmp[:], op=ALU.add),
              reads=["rtmp", "comb"], writes=["comb"])
    return comb


SEQ = 8192
NG = SEQ // 512
KA_GROUPS = {0: 0, 1: 1, 2: 2, 3: 3, 4: 4, 15: 5}


def build_attn(P=None, pf="", y_handle=None, nq=1):
    standalone = P is None
    if standalone:
        P = Prog()
    nc = P.nc
    if not standalone:
        P.push_scope()
    xr_d = P.dram(pf + "xr", [SEQ, DM], F32, "ExternalInput")
    g_d = P.dram(pf + "g", [1, DM], F32, "ExternalInput")
    wq_d = P.dram(pf + "wq", [DM, 1024], F32, "ExternalInput")
    wk_d = P.dram(pf + "wk", [DM, 256], F32, "ExternalInput")
    wv_d = P.dram(pf + "wv", [DM, 256], F32, "ExternalInput")
    wo_d = P.dram(pf + "wo", [64, 16, DM], F32, "ExternalInput")
    gains_d = P.dram(pf + "gains", [128, 4], F32, "ExternalInput")
    rope_d = P.dram(pf + "rope", [4, 128, SEQ], F32, "ExternalInput")
    rmat_d = P.dram(pf + "rmat", [3, 128, 128], BF16, "ExternalInput")
    masks_d = P.dram(pf + "masks", [4, 128, 512], BF16, "ExternalInput")
    sink_d = P.dram(pf + "sink", [1, 8], F32, "ExternalInput")
    id_d = P.dram(pf + "ident", [128, 128], BF16, "ExternalInput")
    y_d = y_handle if y_handle is not None else P.dram(pf + "y", [NTOK, DM], F32, "ExternalOutput")
    full = nq > 1
    ka_groups = {G_: G_ for G_ in range(NG)} if full else KA_GROUPS
    nka = 64 if full else 24

    W = alloc_norm_work(P)
    gbc = P.sb([128, DM], F32, "gbc")
    wq = P.sb([128, 8, 1024], BF16, "wq")
    wk = P.sb([128, 8, 256], BF16, "wk")
    wv = P.sb([128, 8, 256], BF16, "wv")
    wo = P.sb([64, 16, DM], BF16, "wo")
    gains = P.sb([128, 4], F32, "gains")
    rmat = P.sb([128, 3, 128], BF16, "rmat")
    masks = P.sb([128, 4, 512], BF16, "masks")
    KTa = P.sb([128, nka * 128], BF16, "KTa")
    KTb = P.sb([128, SEQ], BF16, "KTb")
    Va = P.sb([128, nka, 2, 65], BF16, "Va")
    Vb = P.sb([128, 64, 2, 65], BF16, "Vb")
    QTa = [P.sb([128, 4, 4, 128], BF16) for _ in range(2)]
    QTb = [P.sb([128, 4, 4, 128], BF16) for _ in range(2)]
    xt = [P.sb([128, DM], F32) for _ in range(2)]
    xng = [P.sb([128, 8, 512], BF16) for _ in range(1 if full else 2)]
    tab = [P.sb([128, 512], F32) for _ in range(4)]
    qg = P.sb([128, 512], BF16, "qg")
    sq = P.sb([128, 512], BF16, "sq")
    lnt = P.sb([128, 512], F32, "lnt")
    rstd = P.sb([128, 512], F32, "rstd")
    t1 = P.sb([128, 512], F32, "t1")
    t2 = P.sb([128, 512], F32, "t2")
    pT = [P.sb([128, 512], BF16) for _ in range(4)]
    den = P.sb([65, 512], F32, "den")
    nxng = len(xng)
    NTq = 16 * nq
    onesf = P.sb([65, 64], F32, "onesf")
    sink8 = P.sb([64, 8], F32, "sink8")
    lnr = P.sb([64, 512], F32, "lnr") if not full else None
    rec = P.sb([64, 512], F32, "rec") if not full else None
    OT = [P.sb([64, 16, 128], BF16) for _ in range(2)]
    xres = P.sb([128, DM], F32, "xres") if not full else None
    ysb = P.sb([128, DM], F32, "ysb")
    if full:
        lnr_ap, lnr_k, rec_ap, rec_k = lnt[0:64, :], "lnt", rstd[0:64, :], "rstd"
    else:
        lnr_ap, lnr_k, rec_ap, rec_k = lnr[:], "lnr", rec[:], "rec"
    b0 = W["psT"]
    bk = [None] + [P.ps([128, 512], F32) for _ in range(7)]

    def BK(i):
        return ("bank", i)
    P.bank("psT", 0)
    for i_ in range(1, 8):
        P.bank(BK(i_), i_)

    P.dma(W["ident"][:], id_d.ap(), writes=["ident"])
    P.dma(gbc[:], bcast_rows(g_d, DM), writes=["gbc"])
    P.dve(lambda e: e.memset(W["epsc"][:], EPS), writes=["epsc"])
    P.dma(gains[:], gains_d.ap(), writes=["gains"])
    P.dma(rmat[:], rmat_d.ap().rearrange("r p c -> p r c"), writes=["rmat"])
    P.dma(masks[:], masks_d.ap().rearrange("r p c -> p r c"), writes=["masks"])
    P.dma(wq[:], wq_d.ap().rearrange("(k p) c -> p k c", p=128), writes=["wq"], q="gpsimd")
    P.dma(wk[:], wk_d.ap().rearrange("(k p) c -> p k c", p=128), writes=["wk"], q="gpsimd")
    P.dma(wv[:], wv_d.ap().rearrange("(k p) c -> p k c", p=128), writes=["wv"], q="gpsimd")
    P.dma(wo[:], wo_d.ap(), writes=["wo"], q="gpsimd")
    P.dve(lambda e: e.memset(Va[:, :, :, 64:65], 1.0), writes=["Va1"])
    P.dve(lambda e: e.memset(Vb[:, :, :, 64:65], 1.0), writes=["Vb1"])
    P.dve(lambda e: e.memset(onesf[:], 1.0), writes=["onesf"])
    P.dma(sink8[:], bcast_rows(sink_d, 8, 64), writes=["sink8"])
    P.act(lambda e: e.activation(out=sink8[:], in_=sink8[:], func=AF.Exp), reads=["sink8"], writes=["sink8"])
    for gi in range(2):
        P.dve(lambda e, gi=gi: e.memset(QTa[gi][:], 0.0), writes=[("QT", "a", gi)])
        P.dve(lambda e, gi=gi: e.memset(QTb[gi][:], 0.0), writes=[("QT", "b", gi)])

    xrv = xr_d.ap().rearrange("(t p) d -> p t d", p=128)

    def load_group(G):
        pb = G % nxng
        for tl in range(4):
            tg = G * 4 + tl
            P.dma(xt[tg % 2][:], xrv[:, tg, :], writes=[("xt", tg % 2)])
            emit_norm_T(P, xt[tg % 2][:], ("xt", tg % 2), gbc, xng[pb], slice(tl * 128, (tl + 1) * 128),
                        ("xng", pb, tl), W, tg)
        for i in range(4):
            P.dma(tab[i][:], rope_d.ap()[i][:, G * 512:(G + 1) * 512], writes=[("tab", i)])
        return pb

    qkc = [0]

    def qk_block(pb, lhs_fn, gcol, ti, ri, dest3, dkey):
        b = 1 + (qkc[0] % 2)
        qkc[0] += 1
        xk = [("xng", pb, tl) for tl in range(4)]

        def mm(e):
            ins = None
            for kc in range(8):
                ins = e.matmul(bk[b][:], lhsT=lhs_fn(kc), rhs=xng[pb][:, kc, :], start=(kc == 0), stop=(kc == 7))
            return ins
        P.pe(mm, reads=xk + ["wq", "wk"], writes=[BK(b)])
        P.act(lambda e: e.activation(out=qg[:], in_=bk[b][:], func=AF.Copy, scale=gains[:, gcol:gcol + 1]),
              reads=[BK(b), "gains"], writes=["qg"])
        P.act(lambda e: e.activation(out=sq[:], in_=bk[b][:], func=AF.Square), reads=[BK(b)], writes=["sq"])
        P.pe(lambda e: e.matmul(bk[3][:], lhsT=rmat[:, ri, :], rhs=qg[:], start=True, stop=True),
             reads=["qg", "rmat"], writes=[BK(3)])
        P.pe(lambda e: e.matmul(bk[4][:], lhsT=rmat[:, 2, :], rhs=sq[:], start=True, stop=True),
             reads=["sq", "rmat"], writes=[BK(4)])
        P.act(lambda e: e.activation(out=lnt[:], in_=bk[4][:], func=AF.Ln, scale=1.0 / 64.0, bias=W["epsc"][:, 0:1]),
              reads=[BK(4), "epsc"], writes=["lnt"])
        P.act(lambda e: e.activation(out=rstd[:], in_=lnt[:], func=AF.Exp, scale=-0.5), reads=["lnt"], writes=["rstd"])
        P.dve(lambda e: e.tensor_tensor(out=t1[:], in0=qg[:], in1=tab[ti][:], op=ALU.mult),
              reads=["qg", ("tab", ti)], writes=["t1"])
        P.dve(lambda e: e.tensor_tensor(out=t2[:], in0=bk[3][:], in1=tab[ti + 1][:], op=ALU.mult),
              reads=[BK(3), ("tab", ti + 1)], writes=["t2"])
        P.dve(lambda e: e.tensor_tensor(out=t1[:], in0=t1[:], in1=t2[:], op=ALU.add), reads=["t1", "t2"], writes=["t1"])
        dests = dest3 if isinstance(dest3, list) else [(slice(0, 128), dest3, dkey)]
        for ps_, dap, dk in dests:
            P.dve(lambda e, ps_=ps_, dap=dap: e.tensor_tensor(out=dap, in0=t1[ps_, :].rearrange("p (a b) -> p a b", a=4),
                                                              in1=rstd[ps_, :].rearrange("p (a b) -> p a b", a=4), op=ALU.mult),
                  reads=["t1", "rstd"], writes=[dk])

    for G in range(NG):
        pb = load_group(G)
        qk_block(pb, lambda kc: wk[:, kc, 128:256], 3, 2, 1,
                 KTb[:, G * 512:(G + 1) * 512].rearrange("p (a b) -> p a b", a=4), ("KTb", G))
        sg = ka_groups.get(G)
        if sg is not None:
            qk_block(pb, lambda kc: wk[:, kc, 0:128], 1, 0, 0,
                     KTa[:, sg * 512:(sg + 1) * 512].rearrange("p (a b) -> p a b", a=4), ("KTa", sg))
        for tl in range(4):
            tg = G * 4 + tl

            def mmv(e, tl=tl, pb=pb):
                ins = None
                for kc in range(8):
                    ins = e.matmul(bk[5][:, 0:256], lhsT=xng[pb][:, kc, tl * 128:(tl + 1) * 128], rhs=wv[:, kc, :],
                                   start=(kc == 0), stop=(kc == 7))
                return ins
            P.pe(mmv, reads=[("xng", pb, tl), "wv"], writes=[BK(5)])
            P.act(lambda e, tg=tg: e.copy(out=Vb[:, tg, :, 0:64], in_=bk[5][:, 128:256].rearrange("p (h d) -> p h d", h=2)),
                  reads=[BK(5)], writes=[("Vb", tg)])
            if sg is not None:
                sl = sg * 4 + tl
                P.act(lambda e, sl=sl: e.copy(out=Va[:, sl, :, 0:64], in_=bk[5][:, 0:128].rearrange("p (h d) -> p h d", h=2)),
                      reads=[BK(5)], writes=[("Va", sl)])

    pcount = [0]
    ocount = [0]
    D = 3

    def attend(R, kind, tl, g, slots, mk, obuf, hbase):
        QT = (QTa if kind == "a" else QTb)[g]
        KT = KTa if kind == "a" else KTb
        V = Va if kind == "a" else Vb
        ob = 4 + (ocount[0] % 2)
        ocount[0] += 1
        n = len(slots)
        hist = []
        for i in range(n + D):
            if i < n:
                s = slots[i]
                sb_ = (1, 2, 3, 6)[pcount[0] % 4]
                pb_ = pcount[0] % 4
                pcount[0] += 1
                hist.append(pb_)
                kkey = ("KTa", s // 4) if kind == "a" else ("KTb", s // 4)
                R.pe(lambda e, s=s, sb_=sb_: e.matmul(bk[sb_][:], lhsT=KT[:, s * 128:(s + 1) * 128],
                                                       rhs=QT[:, tl, :, :], start=True, stop=True),
                     reads=[kkey, ("QT", kind, g)], writes=[BK(sb_)])
                R.act(lambda e, sb_=sb_, pb_=pb_: e.activation(out=pT[pb_][:], in_=bk[sb_][:], func=AF.Exp, scale=0.125),
                      reads=[BK(sb_)], writes=[("pT", pb_)])
                if mk[i] is not None:
                    R.dve(lambda e, pb_=pb_, m=mk[i]: e.tensor_tensor(out=pT[pb_][:], in0=pT[pb_][:], in1=masks[:, m, :],
                                                                      op=ALU.mult),
                          reads=[("pT", pb_), "masks"], writes=[("pT", pb_)])
            if i >= D:
                j = i - D
                s = slots[j]
                pb_ = hist[j]
                vkey = ("Va", s) if kind == "a" else ("Vb", s)
                R.pe(lambda e, s=s, pb_=pb_, j=j: e.matmul(bk[ob][0:65, :], lhsT=V[:, s, g, :], rhs=pT[pb_][:],
                                                            start=(j == 0), stop=(j == n - 1)),
                     reads=[vkey, ("pT", pb_), "Va1", "Vb1"], writes=[BK(ob)])
        R.dve(lambda e: e.tensor_copy(out=den[64:65, :], in_=bk[ob][64:65, :]), reads=[BK(ob)], writes=["den"])
        R.pe(lambda e: e.matmul(bk[6][0:64, :], lhsT=onesf[64:65, :], rhs=den[64:65, :], start=True, stop=True),
             reads=["den", "onesf"], writes=[BK(6)])
        if kind == "a":
            denb = t1[0:64, :]
            for hi in range(4):
                R.dve(lambda e, hi=hi: e.tensor_scalar(out=denb[:, hi * 128:(hi + 1) * 128], in0=bk[6][0:64, hi * 128:(hi + 1) * 128],
                                                       scalar1=sink8[:, g * 4 + hi:g * 4 + hi + 1], scalar2=None, op0=ALU.add),
                      reads=[BK(6), "sink8"], writes=["t1"])
            R.act(lambda e: e.activation(out=lnr_ap, in_=denb, func=AF.Ln), reads=["t1"], writes=[lnr_k])
        else:
            R.act(lambda e: e.activation(out=lnr_ap, in_=bk[6][0:64, :], func=AF.Ln), reads=[BK(6)], writes=[lnr_k])
        R.act(lambda e: e.activation(out=rec_ap, in_=lnr_ap, func=AF.Exp, scale=-1.0), reads=[lnr_k], writes=[rec_k])
        h0 = hbase + 4 * g
        R.dve(lambda e: e.tensor_tensor(out=OT[obuf][:, h0:h0 + 4, :],
                                        in0=bk[ob][0:64, :].rearrange("p (a b) -> p a b", a=4),
                                        in1=rec_ap.rearrange("p (a b) -> p a b", a=4), op=ALU.mult),
              reads=[BK(ob), rec_k], writes=[("OT", obuf, kind, g)])

    yv = y_d.ap().rearrange("(t p) d -> p t d", p=128)
    for G in range(4 * nq):
        pb = load_group(G)
        for j in range(4):
            qk_block(pb, lambda kc, j=j: wq[:, kc, j * 128:(j + 1) * 128], 0, 0, 0,
                     [(slice(0, 64), QTa[0][0:64, :, j, :], ("QT", "a", 0)), (slice(64, 128), QTa[1][64:128, :, j, :], ("QT", "a", 1))], None)
            qk_block(pb, lambda kc, j=j: wq[:, kc, 512 + j * 128:512 + (j + 1) * 128], 2, 2, 1,
                     [(slice(0, 64), QTb[0][0:64, :, j, :], ("QT", "b", 0)), (slice(64, 128), QTb[1][64:128, :, j, :], ("QT", "b", 1))], None)
        for tl in range(4):
            t = G * 4 + tl
            obuf = t % 2
            if full:
                left = t - 1 if t > 0 else 0
                right = t + 1 if t < NTq - 1 else NTq - 1
            else:
                left = t - 1 if t > 0 else 23
                right = t + 1
            for g in range(2):
                attend(P, "a", tl, g, [left, t, right], [0 if t == 0 else 1, None, 3 if t == NTq - 1 else 2], obuf, 0)
            for g in range(2):
                attend(P, "b", tl, g, list(range(64)), [None] * 64, obuf, 8)
            if full:
                xres, xres_k = xt[t % 2], ("xt", t % 2)
            else:
                xres_k = "xres"
            P.dma(xres[:], xrv[:, t, :], writes=[xres_k])
            okeys = [("OT", obuf, k, g) for k in "ab" for g in range(2)]
            for h2 in range(2):
                def mmo(e, h2=h2, obuf=obuf):
                    ins = None
                    for h in range(16):
                        ins = e.matmul(bk[7][:], lhsT=OT[obuf][:, h, :], rhs=wo[:, h, h2 * 512:(h2 + 1) * 512],
                                       start=(h == 0), stop=(h == 15))
                    return ins
                P.pe(mmo, reads=okeys + ["wo"], writes=[BK(7)])
                P.dve(lambda e, h2=h2, xres=xres: e.tensor_tensor(out=ysb[:, h2 * 512:(h2 + 1) * 512], in0=bk[7][:],
                                                                  in1=xres[:, h2 * 512:(h2 + 1) * 512], op=ALU.add),
                      reads=[BK(7), xres_k], writes=[("ysb", h2)])
            P.dma(yv[:, t, :], ysb[:], reads=[("ysb", 0), ("ysb", 1)], writes=[("x1s", t)])
    if standalone:
        return P.build()
    P.pop_scope()


def rope_consts(r):
    pos = (np.arange(SEQ) + 2048 * r) % SEQ
    posf = pos.astype(np.float32)
    inv32 = (np.float32(10000.0) ** (-np.arange(0, 64, 2, dtype=np.float32) / np.float32(64))).astype(np.float32)
    inv16 = (np.float32(10000.0) ** (-np.arange(0, 32, 2, dtype=np.float32) / np.float32(32))).astype(np.float32)
    ang_a = posf[:, None] * inv32[None, :]
    row = (pos // 64).astype(np.float32)
    col = (pos % 64).astype(np.float32)
    ang_r = row[:, None] * inv16[None, :]
    ang_c = col[:, None] * inv16[None, :]
    d = np.arange(128) % 64
    tabs = np.zeros((4, 128, SEQ), np.float32)
    tabs[0] = np.cos(ang_a).astype(np.float32)[:, d % 32].T
    tabs[1] = np.sin(ang_a).astype(np.float32)[:, d % 32].T
    ang_b = np.where((d < 32)[None, :], ang_r[:, d % 16], ang_c[:, d % 16])
    tabs[2] = np.cos(ang_b).astype(np.float32).T
    tabs[3] = np.sin(ang_b).astype(np.float32).T
    return tabs


def rot_consts():
    import ml_dtypes
    Ra = np.zeros((64, 64), np.float32)
    for i in range(64):
        if i < 32:
            Ra[i, i + 32] = -1.0
        else:
            Ra[i, i - 32] = 1.0
    Rb = np.zeros((64, 64), np.float32)
    for i in range(64):
        if (i % 32) < 16:
            Rb[i, i + 16] = -1.0
        else:
            Rb[i, i - 16] = 1.0
    out = np.zeros((3, 128, 128), np.float32)
    for blk in range(2):
        s = slice(blk * 64, (blk + 1) * 64)
        out[0, s, s] = Ra.T
        out[1, s, s] = Rb.T
        out[2, s, s] = 1.0
    return out.astype(ml_dtypes.bfloat16)


def mask_consts(r):
    import ml_dtypes
    k = np.arange(128)[:, None]
    q = np.arange(128)[None, :]
    mL = (k >= q).astype(np.float32)
    mR = (q >= k).astype(np.float32)
    m = np.zeros((4, 128, 512), np.float32)
    m[0] = np.tile(mL if (r is not None and r > 0) else 0 * mL, (1, 4))
    m[1] = np.tile(mL, (1, 4))
    m[2] = np.tile(mR, (1, 4))
    m[3] = np.tile(mR if (r is not None and r < 3) else 0 * mR, (1, 4))
    return m.astype(ml_dtypes.bfloat16)


def attn_inputs(x, ab_norm, ab_w_in, qn_a, kn_a, sink_a, qn_b, kn_b, ab_w_out, fused=False):
    import ml_dtypes
    w = ab_w_in[0]
    qa, ka, va, qb, kb, vb = (w[:, 0:512], w[:, 512:640], w[:, 640:768], w[:, 768:1280], w[:, 1280:1408], w[:, 1408:1536])

    def pair(wq_):
        cols = []
        for j in range(4):
            cols.append(wq_[:, j * 64:(j + 1) * 64])
            cols.append(wq_[:, (j + 4) * 64:(j + 5) * 64])
        return np.concatenate(cols, axis=1)
    wq = np.ascontiguousarray(np.concatenate([pair(qa), pair(qb)], axis=1))
    wk = np.ascontiguousarray(np.concatenate([ka, kb], axis=1))
    wv = np.ascontiguousarray(np.concatenate([va, vb], axis=1))
    wo = np.ascontiguousarray(ab_w_out[0].reshape(16, 64, DM).transpose(1, 0, 2))
    gains = np.ascontiguousarray(np.stack([np.tile(qn_a[0], 2), np.tile(kn_a[0], 2), np.tile(qn_b[0], 2), np.tile(kn_b[0], 2)], axis=1))
    ident = np.eye(128, dtype=np.float32).astype(ml_dtypes.bfloat16)
    rmat = rot_consts()
    maps = []
    if fused:
        rope0, masks0 = rope_consts(0), mask_consts(None)
    for c in range(NCORES):
        b, r = c // 4, c % 4
        if fused:
            maps.append({"xr": x[b], "g": ab_norm[0:1], "wq": wq, "wk": wk, "wv": wv, "wo": wo, "gains": gains,
                         "rope": rope0, "rmat": rmat, "masks": masks0, "sink": sink_a[0:1], "ident": ident})
            continue
        xr = np.ascontiguousarray(np.roll(x[b], -2048 * r, axis=0))
        maps.append({"xr": xr, "g": ab_norm[0:1], "wq": wq, "wk": wk, "wv": wv, "wo": wo, "gains": gains,
                     "rope": rope_consts(r), "rmat": rmat, "masks": mask_consts(r), "sink": sink_a[0:1], "ident": ident})
    return maps


NTILE = SEQ // 128
DBG = {}


def build_gdn(phases=(1, 2, 3), nsteps=NTILE, P=None, pf="", x_handle=None, o_handle=None, nhp=1):
    standalone = P is None
    if standalone:
        P = Prog()
    else:
        P.push_scope()
    nc = P.nc
    fusedm = x_handle is not None
    xp_d = x_handle if fusedm else P.dram(pf + "xp", [SEQ + 128, DM], F32, "ExternalInput")
    g_d = P.dram(pf + "g", [1, DM], F32, "ExternalInput")
    wf_d = P.dram(pf + "wf", [nhp, DM, 768], F32, "ExternalInput")
    wt_d = P.dram(pf + "wt", [nhp, DM, 264], F32, "ExternalInput")
    cw_d = P.dram(pf + "cw", [nhp, 128, 6, 5], F32, "ExternalInput")
    gc_d = P.dram(pf + "gconst", [nhp, 8], F32, "ExternalInput")
    on_d = P.dram(pf + "onorm", [1, 128], F32, "ExternalInput")
    mk_d = P.dram(pf + "gmask", [5, 128, 128], F32, "ExternalInput")
    id_d = P.dram(pf + "ident", [128, 128], BF16, "ExternalInput")
    y_d = o_handle if fusedm else P.dram(pf + "y", [SEQ, 256], F32, "ExternalOutput")
    SK = "ExternalOutput" if DBG.get("dump") else "Internal"
    qT_s = P.dram(pf + "qT_s", [2, 128, SEQ], BF16, SK)
    kT_s = P.dram(pf + "kT_s", [2, 128, SEQ], BF16, SK)
    k_s = P.dram(pf + "k_s", [2, SEQ, 128], BF16, SK)
    v_s = P.dram(pf + "v_s", [2, SEQ, 128], BF16, SK)
    z_s = P.dram(pf + "z_s", [SEQ, 256], BF16, SK)
    o_s = P.dram(pf + "o_s", [2, 2, SEQ, 128], F32, SK)

    W = alloc_norm_work(P)
    gbc = P.sb([128, DM], F32, "gbc")
    wf = P.sb([128, 8, 768], BF16, "wf")
    wt = P.sb([128, 8, 264], BF16, "wt")
    cw = P.sb([128, 6, 5], F32, "cw")
    gconst = P.sb([128, 8], F32, "gconst")
    onorm = P.sb([128, 2, 128], F32, "onorm")
    gmask = P.sb([128, 5, 128], F32, "gmask")
    onesb = P.sb([128, 128], BF16, "onesb")
    onesf = P.sb([128, 128], F32, "onesf")
    onec = P.sb([128, 1], F32, "onec")
    gates = P.sb([128, NTILE, 8], F32, "gates")
    xt = [P.sb([128, DM], F32) for _ in range(2)]
    xng = [P.sb([128, 8, 516], BF16) for _ in range(2)]
    pre = [P.sb([128, 516], F32) for _ in range(2)]
    cacc = [P.sb([128, 512], F32) for _ in range(2)]
    cblk = [P.sb([128, 512], F32) for _ in range(2)]
    sqb = [P.sb([128, 512], BF16) for _ in range(2)]
    lnt = [P.sb([128, 512], F32) for _ in range(2)]
    rst = [P.sb([128, 512], F32) for _ in range(2)]
    nT = [P.sb([128, 512], BF16) for _ in range(2)]
    tm = [P.sb([128, 4, 128], BF16) for _ in range(2)]
    zsb = [P.sb([128, 256], BF16) for _ in range(2)]
    gtmp = P.sb([128, 8], F32, "gtmp")
    banks = [P.ps([128, 512], F32) for _ in range(7)]
    bankT = W["psT"]

    def BQ(b, q):
        return ("bq", b, q)

    def BK(b):
        return [BQ(b, q) for q in range(4)]

    KT_ALL = [BQ(7, q) for q in range(4)]
    for b_ in range(8):
        for q_ in range(4):
            P.bank(BQ(b_, q_), b_)

    P.dma(W["ident"][:], id_d.ap(), writes=["ident"])
    P.dma(gbc[:], bcast_rows(g_d, DM), writes=["gbc"])
    P.dve(lambda e: e.memset(W["epsc"][:], EPS), writes=["epsc"])
    P.dma(onorm[:, 0, :], bcast_rows(on_d, 128), writes=["onorm"])
    P.dma(onorm[:, 1, :], bcast_rows(on_d, 128), writes=["onorm"])
    P.dma(gmask[:], mk_d.ap().rearrange("r p c -> p r c"), writes=["gmask"])
    P.dve(lambda e: e.memset(onesb[:], 1.0), writes=["onesb"])
    P.dve(lambda e: e.memset(onesf[:], 1.0), writes=["onesf"])
    P.dve(lambda e: e.memset(onec[:], 1.0), writes=["onec"])
    def load_weights(hp):
        P.dma(wf[:], wf_d.ap()[hp].rearrange("(k p) c -> p k c", p=128), writes=["wf"], q="gpsimd")
        P.dma(wt[:], wt_d.ap()[hp].rearrange("(k p) c -> p k c", p=128), writes=["wt"], q="gpsimd")
        P.dma(cw[:], cw_d.ap()[hp], writes=["cw"])
        P.dma(gconst[:], bcast_rows(gc_d, 8, 128, hp * 8), writes=["gconst"])
        P.act(lambda e: e.activation(out=gconst[:, 4:8], in_=gconst[:, 4:8], func=AF.Exp), reads=["gconst"], writes=["gconst"])
        P.dve(lambda e: e.tensor_scalar(out=gconst[:, 4:8], in0=gconst[:, 4:8], scalar1=-1.0, scalar2=None, op0=ALU.mult),
              reads=["gconst"], writes=["gconst"])

    xpv = xp_d.ap()
    uid = [0]

    def norm_rows(x_ap, xkey, rows, dst, dkey):
        u = uid[0]
        uid[0] += 1
        xn = W["xn"][u % 2]
        kxn = ("xn", u % 2)
        ss = W["ss"][u % 2]
        kss = ("ss", u % 2)
        psT = bankT
        P.act(lambda e: e.activation(out=W["junk"][0:rows, :], in_=x_ap, func=AF.Square, accum_out=ss[0:rows, 0:1]),
              reads=[xkey], writes=[kss, "junk"])
        P.act(lambda e: e.activation(out=ss[0:rows, 1:2], in_=ss[0:rows, 0:1], func=AF.Sqrt, scale=1.0 / 1024.0,
                                     bias=W["epsc"][0:rows, 0:1]), reads=[kss, "epsc"], writes=[kss])
        P.dve(lambda e: e.reciprocal(out=ss[0:rows, 2:3], in_=ss[0:rows, 1:2]), reads=[kss], writes=[kss])
        P.dve(lambda e: e.scalar_tensor_tensor(out=xn[0:rows, :], in0=x_ap, scalar=ss[0:rows, 2:3], in1=gbc[0:rows, :],
                                               op0=ALU.mult, op1=ALU.mult), reads=[xkey, kss, "gbc"], writes=[kxn])

        def tr(e):
            ins = None
            for kc in range(8):
                ins = e.transpose(out=psT[:, kc * 128:kc * 128 + rows], in_=xn[0:rows, kc * 128:(kc + 1) * 128],
                                  identity=W["ident"][0:rows, 0:rows])
            return ins
        P.pe(tr, reads=[kxn, "ident"], writes=KT_ALL)
        P.act(lambda e: e.copy(out=dst, in_=psT[:].rearrange("p (k c) -> p k c", k=8)[:, :, 0:rows]),
              reads=KT_ALL, writes=[dkey])

    def phase1(hp):
        for G in (range(DBG.get('ng', NG)) if 1 in phases else []):
            pb = G % 2
            LV = DBG.get('lv', 9)
            for tl in range(5):
                rows = 128 if tl < 4 else 4
                u = uid[0]
                xo = 0 if fusedm else 2
                if tl < 4:
                    r0 = G * 512 + xo + tl * 128
                    P.dma(xt[u % 2][0:rows, :], xpv[r0:r0 + rows, :], reads=[("yout", "f_", r0 // 128)], writes=[("xt", u % 2)])
                else:
                    if fusedm and (G == 0 or G == NG - 1):
                        P.dve(lambda e, u=u: e.memset(xt[u % 2][0:4, :], 0.0), writes=[("xt", u % 2)])
                    if not (fusedm and G == 0):
                        r0 = G * 512 + xo - 2
                        P.dma(xt[u % 2][0:2, :], xpv[r0:r0 + 2, :], reads=[("yout", "f_", r0 // 128)], writes=[("xt", u % 2)])
                    if not (fusedm and G == NG - 1):
                        r0 = G * 512 + xo + 512
                        P.dma(xt[u % 2][2:4, :], xpv[r0:r0 + 2, :], reads=[("yout", "f_", r0 // 128)], writes=[("xt", u % 2)])
                norm_rows(xt[u % 2][0:rows, :], ("xt", u % 2), rows, xng[pb][:, :, tl * 128:tl * 128 + rows], ("xng", pb, tl))
            xk = [("xng", pb, tl) for tl in range(5)]
            Ls = [Lazy(P), Lazy(P), Lazy(P)]
            for blk in (range(6) if LV >= 2 else []):
                R = Ls[blk % 2]
                pbk = 1 + (blk % 2)
                prb = blk % 2

                def mm_main(e, blk=blk, pbk=pbk, pb=pb):
                    ins = None
                    for kc in range(8):
                        ins = e.matmul(banks[pbk][:], lhsT=wf[:, kc, blk * 128:(blk + 1) * 128], rhs=xng[pb][:, kc, 0:512],
                                       start=(kc == 0), stop=(kc == 7))
                    return ins

                def mm_halo(e, blk=blk, pb=pb):
                    ins = None
                    for kc in range(8):
                        ins = e.matmul(banks[3][:, 0:4], lhsT=wf[:, kc, blk * 128:(blk + 1) * 128], rhs=xng[pb][:, kc, 512:516],
                                       start=(kc == 0), stop=(kc == 7))
                    return ins
                R.pe(mm_main, reads=xk + ["wf"], writes=BK(pbk))
                R.begin()
                R.pe(mm_halo, reads=xk + ["wf"], writes=BK(3))
                R.act(lambda e, prb=prb: e.copy(out=pre[prb][:, 0:2], in_=banks[3][:, 0:2]), reads=BK(3), writes=[("pre", prb)])
                R.act(lambda e, prb=prb: e.copy(out=pre[prb][:, 514:516], in_=banks[3][:, 2:4]), reads=BK(3), writes=[("pre", prb)])
                R.end()
                R.act(lambda e, pbk=pbk, prb=prb: e.copy(out=pre[prb][:, 2:514], in_=banks[pbk][:]), reads=BK(pbk), writes=[("pre", prb)])
                R.dve(lambda e, blk=blk, prb=prb: e.tensor_scalar(out=cacc[prb][:], in0=pre[prb][:, 0:512], scalar1=cw[:, blk, 0:1],
                                                                  scalar2=None, op0=ALU.mult),
                      reads=[("pre", prb), "cw"], writes=[("cacc", prb)])
                for tap in range(1, 5):
                    R.dve(lambda e, blk=blk, prb=prb, tap=tap: e.scalar_tensor_tensor(
                        out=cacc[prb][:], in0=pre[prb][:, tap:tap + 512], scalar=cw[:, blk, tap:tap + 1], in1=cacc[prb][:],
                        op0=ALU.mult, op1=ALU.add), reads=[("pre", prb), "cw", ("cacc", prb)], writes=[("cacc", prb)])
                nb = blk % 2
                h = blk % 2
                if LV < 3:
                    continue
                if blk < 4:
                    R.act(lambda e, prb=prb: e.activation(out=cblk[prb][:], in_=cacc[prb][:], func=AF.Silu), reads=[("cacc", prb)], writes=[("cblk", prb)])
                    R.act(lambda e, prb=prb: e.activation(out=sqb[prb][:], in_=cblk[prb][:], func=AF.Square), reads=[("cblk", prb)], writes=[("sqb", prb)])
                    R.begin()
                    R.pe(lambda e, prb=prb: e.matmul(banks[4][:], lhsT=onesb[:], rhs=sqb[prb][:], start=True, stop=True),
                         reads=[("sqb", prb), "onesb"], writes=BK(4))
                    R.act(lambda e, prb=prb: e.activation(out=lnt[prb][:], in_=banks[4][:], func=AF.Ln, bias=W["epsc"][:, 0:1]),
                          reads=BK(4) + ["epsc"], writes=[("lnt", prb)])
                    R.end()
                    R.act(lambda e, prb=prb: e.activation(out=rst[prb][:], in_=lnt[prb][:], func=AF.Exp, scale=-0.5), reads=[("lnt", prb)], writes=[("rst", prb)])
                    qs = (128.0 ** -0.5) if blk < 2 else 1.0
                    R.dve(lambda e, nb=nb, qs=qs, prb=prb: e.scalar_tensor_tensor(out=nT[nb][:], in0=cblk[prb][:], scalar=qs, in1=rst[prb][:],
                                                                         op0=ALU.mult, op1=ALU.mult),
                          reads=[("cblk", prb), ("rst", prb)], writes=[("nT", nb)])
                    dst = qT_s if blk < 2 else kT_s
                    nm = "qT" if blk < 2 else "kT"
                    if LV >= 4:
                      R.dma(dst.ap()[h][:, G * 512:(G + 1) * 512], nT[nb][:], reads=[("nT", nb)],
                          writes=[(nm, h, G * 4 + tl) for tl in range(4)])
                else:
                    R.act(lambda e, nb=nb, prb=prb: e.activation(out=nT[nb][:], in_=cacc[prb][:], func=AF.Silu), reads=[("cacc", prb)], writes=[("nT", nb)])
                if blk >= 2 and LV >= 5:
                    bT = bankT

                    def trk(e, nb=nb):
                        ins = None
                        for tl in range(4):
                            ins = e.transpose(out=bT[:, tl * 128:(tl + 1) * 128], in_=nT[nb][:, tl * 128:(tl + 1) * 128],
                                              identity=W["ident"][:])
                        return ins
                    R.begin()
                    R.pe(trk, reads=[("nT", nb), "ident"], writes=KT_ALL)
                    R.act(lambda e, nb=nb: e.copy(out=tm[nb][:], in_=bT[:, 0:512].rearrange("p (t c) -> p t c", t=4)),
                          reads=KT_ALL, writes=[("tm", nb)])
                    R.end()
                    dst = k_s if blk < 4 else v_s
                    nm = "k" if blk < 4 else "v"
                    R.dma(dst.ap()[h].rearrange("(t p) d -> p t d", p=128)[:, G * 4:(G + 1) * 4, :], tm[nb][:],
                          reads=[("tm", nb)], writes=[(nm, h, G * 4 + tl) for tl in range(4)])
            R = Ls[2]
            for tl in (range(4) if LV >= 6 else []):
                t = G * 4 + tl
                zb = t % 2

                def mmt(e, tl=tl, pb=pb):
                    ins = None
                    for kc in range(8):
                        ins = e.matmul(banks[5][:, 0:264], lhsT=xng[pb][:, kc, tl * 128:(tl + 1) * 128], rhs=wt[:, kc, :],
                                       start=(kc == 0), stop=(kc == 7))
                    return ins
                R.pe(mmt, reads=xk + ["wt"], writes=BK(5))
                R.act(lambda e, zb=zb: e.activation(out=zsb[zb][:], in_=banks[5][:, 0:256], func=AF.Silu), reads=BK(5), writes=[("zsb", zb)])
                R.dma(z_s.ap()[t * 128:(t + 1) * 128, :], zsb[zb][:], reads=[("zsb", zb)], writes=[("z", t)])
                if LV < 7:
                    continue
                R.dve(lambda e: e.tensor_tensor(out=gtmp[:, 0:4], in0=banks[5][:, 256:260], in1=gconst[:, 0:4], op=ALU.add),
                      reads=BK(5) + ["gconst"], writes=["gtmp"])
                SUB = DBG.get('sub', 9)
                if SUB >= 2:
                    R.act(lambda e: e.activation(out=gtmp[:, 0:4], in_=gtmp[:, 0:4], func=AF.Exp), reads=["gtmp"], writes=["gtmp"])
                if SUB >= 3:
                    R.act(lambda e: e.activation(out=gtmp[:, 0:4], in_=gtmp[:, 0:4], func=AF.Ln, bias=onec[:, 0:1]),
                          reads=["gtmp", "onec"], writes=["gtmp"])
                if SUB >= 4:
                    R.dve(lambda e, t=t: e.tensor_tensor(out=gates[:, t, 0:4], in0=gtmp[:, 0:4], in1=gconst[:, 4:8], op=ALU.mult),
                          reads=["gtmp", "gconst"], writes=[("gates", t)])
                if LV < 8:
                    continue
                R.act(lambda e: e.activation(out=gtmp[:, 4:8], in_=banks[5][:, 260:264], func=AF.Exp, scale=-1.0),
                      reads=BK(5), writes=["gtmp"])
                R.dve(lambda e: e.tensor_scalar(out=gtmp[:, 4:8], in0=gtmp[:, 4:8], scalar1=1.0, scalar2=None, op0=ALU.add),
                      reads=["gtmp"], writes=["gtmp"])
                R.dve(lambda e, t=t: e.reciprocal(out=gates[:, t, 4:8], in_=gtmp[:, 4:8]), reads=["gtmp"], writes=[("gates", t)])
            interleave(Ls)

    allb = [P.ps([128, 512], F32)] if False else None
    chains = [(h, d) for d in range(2) for h in range(2)]
    bank_ap = banks + [None]
    pbank8 = P.ps

    def Q(ci, q):
        b = 2 * ci + q // 4
        qq = q % 4
        if b == 7:
            ap = bankT.bitcast(F32)[:, qq * 128:(qq + 1) * 128]
        else:
            ap = banks[b][:, qq * 128:(qq + 1) * 128]
        return ap, BQ(b, qq)

    def Qb(ci, q):
        b = 2 * ci + q // 4
        qq = q % 4
        if b == 7:
            ap = bankT[:, qq * 256:qq * 256 + 128]
        else:
            ap = banks[b].bitcast(BF16)[:, qq * 256:qq * 256 + 128]
        return ap, BQ(b, qq)

    st = {}
    for ci in range(4):
        d = {}
        d["S32"] = P.sb([128, 128], F32)
        d["Sbf"] = P.sb([128, 128], BF16)
        for nm in ["qT", "kT", "k", "v"]:
            d[nm] = [P.sb([128, 128], BF16) for _ in range(2)]
        for nm in ["gcol", "cols"]:
            d[nm] = P.sb([128, 8], F32)
        for nm in ["TG", "IB", "Dm", "E", "Bm", "Eb"]:
            d[nm] = P.sb([128, 128], F32)
        for nm in ["erow", "u"]:
            d[nm] = [P.sb([128, 128], F32) for _ in range(2)]
        for nm in ["X", "XT", "Pa", "PaT", "Pb", "PbT", "Za", "Zb", "vb", "kbg", "vn"]:
            d[nm] = P.sb([128, 128], BF16)
        for nm in ["kd", "qgT", "nwT", "qkT"]:
            d[nm] = [P.sb([128, 128], BF16) for _ in range(2)]
        d["osb"] = [P.sb([128, 128], F32) for _ in range(2)]
        st[ci] = d

    def K(ci, nm):
        return (nm, "c", ci)

    def chain_tile(ci, s, Rp, Rs):
        h, dr = chains[ci]
        d = st[ci]
        t = s if dr == 0 else NTILE - 1 - s
        lb = s % 2
        incl = gmask[:, 0 + 2 * dr, :]
        strict = gmask[:, 1 + 2 * dr, :]
        identf = gmask[:, 4, :]
        gcolumn = gates[:, t, 2 * dr + h:2 * dr + h + 1]
        bcolumn = gates[:, t, 4 + 2 * dr + h:4 + 2 * dr + h + 1]
        for nm, src in (("qT", qT_s), ("kT", kT_s)):
            Rp.dma(d[nm][lb][:], src.ap()[h][:, t * 128:(t + 1) * 128], reads=[(nm, h, t)], writes=[K(ci, nm + str(lb))])
        for nm, src in (("k", k_s), ("v", v_s)):
            Rp.dma(d[nm][lb][:], src.ap()[h][t * 128:(t + 1) * 128, :], reads=[(nm, h, t)], writes=[K(ci, nm + str(lb))])
        qT, kT, kk, vv = d["qT"][lb], d["kT"][lb], d["k"][lb], d["v"][lb]
        kqT, kkT, kkk, kvv = K(ci, "qT" + str(lb)), K(ci, "kT" + str(lb)), K(ci, "k" + str(lb)), K(ci, "v" + str(lb))
        q0, k0 = Q(ci, 0)
        q1, k1 = Q(ci, 1)
        q2, k2 = Q(ci, 2)
        q3, k3 = Q(ci, 3)
        q4, k4 = Q(ci, 4)
        q5, k5 = Q(ci, 5)
        q6, k6 = Q(ci, 6)
        q7, k7 = Q(ci, 7)
        Rp.pe(lambda e: e.matmul(q0, lhsT=kT[:], rhs=kT[:], start=True, stop=True), reads=[kkT], writes=[k0])
        Rp.pe(lambda e: e.matmul(q1, lhsT=kT[:], rhs=qT[:], start=True, stop=True), reads=[kkT, kqT], writes=[k1])
        Rp.dve(lambda e: e.tensor_scalar(out=d["TG"][:], in0=incl, scalar1=gcolumn, scalar2=None, op0=ALU.mult),
              reads=["gmask", ("gates", t)], writes=[K(ci, "TG")])
        Rp.dve(lambda e: e.tensor_scalar(out=d["IB"][:], in0=identf, scalar1=bcolumn, scalar2=None, op0=ALU.mult),
              reads=["gmask", ("gates", t)], writes=[K(ci, "IB")])
        Rp.pe(lambda e: e.matmul(q2, lhsT=onesf[:], rhs=d["TG"][:], start=True, stop=True),
             reads=[K(ci, "TG"), "onesf"], writes=[k2])
        Rp.pe(lambda e: e.matmul(q3[:, 0:2], lhsT=d["TG"][:], rhs=onesf[:, 0:2], start=True, stop=True),
             reads=[K(ci, "TG"), "onesf"], writes=[k3])
        Rp.dve(lambda e: e.tensor_copy(out=d["gcol"][:, 0:1], in_=q3[:, 0:1]), reads=[k3], writes=[K(ci, "gcol")])
        Rp.pe(lambda e: e.matmul(q3, lhsT=onesf[:], rhs=d["IB"][:], start=True, stop=True),
             reads=[K(ci, "IB"), "onesf", K(ci, "gcol")], writes=[k3])
        Rp.dve(lambda e: e.tensor_scalar(out=d["Dm"][:], in0=q2, scalar1=d["gcol"][:, 0:1], scalar2=0.0,
                                        op0=ALU.subtract, op1=ALU.min), reads=[k2, K(ci, "gcol")], writes=[K(ci, "Dm")])
        Rp.act(lambda e: e.activation(out=d["E"][:], in_=d["Dm"][:], func=AF.Exp), reads=[K(ci, "Dm")], writes=[K(ci, "E")])
        Rp.act(lambda e: e.activation(out=d["erow"][lb][:], in_=q2, func=AF.Exp), reads=[k2], writes=[K(ci, "erow" + str(lb))])
        Rp.dve(lambda e: e.tensor_tensor(out=d["E"][:], in0=d["E"][:], in1=incl, op=ALU.mult),
              reads=[K(ci, "E"), "gmask"], writes=[K(ci, "E")])
        Rp.dve(lambda e: e.tensor_tensor(out=d["Bm"][:], in0=q3, in1=strict, op=ALU.mult), reads=[k3, "gmask"], writes=[K(ci, "Bm")])
        Rp.dve(lambda e: e.tensor_tensor(out=d["Eb"][:], in0=d["E"][:], in1=d["Bm"][:], op=ALU.mult),
              reads=[K(ci, "E"), K(ci, "Bm")], writes=[K(ci, "Eb")])
        Rp.dve(lambda e: e.scalar_tensor_tensor(out=d["X"][:], in0=q0, scalar=-1.0, in1=d["Eb"][:], op0=ALU.mult, op1=ALU.mult),
              reads=[k0, K(ci, "Eb")], writes=[K(ci, "X")])
        Rp.dve(lambda e: e.tensor_tensor(out=d["qkT"][lb][:], in0=q1, in1=d["E"][:], op=ALU.mult),
              reads=[k1, K(ci, "E")], writes=[K(ci, "qkT" + str(lb))])
        cols = d["cols"]
        Rp.act(lambda e: e.activation(out=cols[:, 0:1], in_=d["gcol"][:, 0:1], func=AF.Exp), reads=[K(ci, "gcol")], writes=[K(ci, "cols")])
        Rp.dve(lambda e: e.tensor_tensor(out=cols[:, 1:2], in0=cols[:, 0:1], in1=bcolumn, op=ALU.mult),
              reads=[K(ci, "cols"), ("gates", t)], writes=[K(ci, "cols")])
        for cc in range(2):
            col = (cc * 64 + 63) if dr == 0 else (cc * 64)
            Rp.dve(lambda e, cc=cc, col=col: e.tensor_copy(out=cols[cc * 64:(cc + 1) * 64, 3:4], in_=q2[cc * 64:(cc + 1) * 64, col:col + 1]),
                  reads=[k2], writes=[K(ci, "cols")])
        Rp.dve(lambda e: e.tensor_tensor(out=cols[:, 4:5], in0=cols[:, 3:4], in1=d["gcol"][:, 0:1], op=ALU.subtract),
              reads=[K(ci, "cols"), K(ci, "gcol")], writes=[K(ci, "cols")])
        Rp.act(lambda e: e.activation(out=cols[:, 2:3], in_=cols[:, 4:5], func=AF.Exp), reads=[K(ci, "cols")], writes=[K(ci, "cols")])
        Rp.dve(lambda e: e.tensor_scalar(out=d["vb"][:], in0=vv[:], scalar1=bcolumn, scalar2=None, op0=ALU.mult),
              reads=[kvv, ("gates", t)], writes=[K(ci, "vb")])
        Rp.dve(lambda e: e.tensor_scalar(out=d["kbg"][:], in0=kk[:], scalar1=cols[:, 1:2], scalar2=None, op0=ALU.mult),
              reads=[kkk, K(ci, "cols")], writes=[K(ci, "kbg")])
        Rp.dve(lambda e: e.tensor_scalar(out=d["kd"][lb][:], in0=kk[:], scalar1=cols[:, 2:3], scalar2=None, op0=ALU.mult),
              reads=[kkk, K(ci, "cols")], writes=[K(ci, "kd" + str(lb))])
        Rp.dve(lambda e: e.tensor_tensor(out=d["qgT"][lb][:], in0=qT[:], in1=d["erow"][lb][:], op=ALU.mult),
              reads=[kqT, K(ci, "erow" + str(lb))], writes=[K(ci, "qgT" + str(lb))])
        qb4, _ = Qb(ci, 0)
        Rp.pe(lambda e: e.transpose(out=qb4, in_=d["X"][:], identity=W["ident"][:]), reads=[K(ci, "X"), "ident"], writes=[k0])
        Rp.act(lambda e: e.copy(out=d["XT"][:], in_=qb4), reads=[k0], writes=[K(ci, "XT")])
        Rp.dve(lambda e: e.tensor_tensor(out=d["Za"][:], in0=d["X"][:], in1=identf, op=ALU.add),
              reads=[K(ci, "X"), "gmask"], writes=[K(ci, "Za")])
        Pc, PcT, kPc, kPcT = d["X"], d["XT"], K(ci, "X"), K(ci, "XT")
        Zc, kZc = d["Za"], K(ci, "Za")
        for lvl in range(5):
            Pn, PnT = (d["Pa"], d["PaT"]) if lvl % 2 == 0 else (d["Pb"], d["PbT"])
            kPn, kPnT = (K(ci, "Pa"), K(ci, "PaT")) if lvl % 2 == 0 else (K(ci, "Pb"), K(ci, "PbT"))
            Zn, kZn = (d["Zb"], K(ci, "Zb")) if lvl % 2 == 0 else (d["Za"], K(ci, "Za"))
            Rp.pe(lambda e, Pc=Pc, PcT=PcT: e.matmul(q1, lhsT=Pc[:], rhs=PcT[:], start=True, stop=True),
                 reads=[kPc, kPcT], writes=[k1])
            Rp.act(lambda e, PnT=PnT: e.copy(out=PnT[:], in_=q1), reads=[k1], writes=[kPnT])
            if lvl < 4:
                Rp.pe(lambda e, Pc=Pc, PcT=PcT: e.matmul(q0, lhsT=PcT[:], rhs=Pc[:], start=True, stop=True),
                     reads=[kPc, kPcT], writes=[k0])
                Rp.act(lambda e, Pn=Pn: e.copy(out=Pn[:], in_=q0), reads=[k0], writes=[kPn])
            Rp.pe(lambda e, PnT=PnT, Zc=Zc: e.matmul(q2, lhsT=PnT[:], rhs=Zc[:], start=True, stop=True),
                 reads=[kPnT, kZc], writes=[k2])
            Rp.dve(lambda e, Zn=Zn, Zc=Zc: e.tensor_tensor(out=Zn[:], in0=q2, in1=Zc[:], op=ALU.add),
                  reads=[k2, kZc], writes=[kZn])
            Pc, PcT, kPc, kPcT = Pn, PnT, kPn, kPnT
            Zc, kZc = Zn, kZn
        Rp.pe(lambda e: e.matmul(q3, lhsT=Zc[:], rhs=d["vb"][:], start=True, stop=True), reads=[kZc, K(ci, "vb")], writes=[k3])
        Rp.act(lambda e: e.copy(out=d["u"][lb][:], in_=q3), reads=[k3], writes=[K(ci, "u" + str(lb))])
        Rp.pe(lambda e: e.matmul(q1, lhsT=d["kbg"][:], rhs=Zc[:], start=True, stop=True), reads=[kZc, K(ci, "kbg")], writes=[k1])
        Rp.act(lambda e: e.activation(out=d["nwT"][lb][:], in_=q1, func=AF.Copy, scale=-1.0), reads=[k1], writes=[K(ci, "nwT" + str(lb))])
        order = [0, 1] if dr == 0 else [1, 0]
        osb = d["osb"][lb]
        for cc in order:
            rs = slice(cc * 64, (cc + 1) * 64)
            Rs.pe(lambda e, rs=rs: e.matmul(q4[rs, :], lhsT=d["nwT"][lb][:, rs], rhs=d["Sbf"][:], start=True, stop=True),
                 reads=[K(ci, "nwT" + str(lb)), ("Sbf", ci)], writes=[k4])
            Rs.dve(lambda e, rs=rs: e.tensor_tensor(out=d["vn"][rs, :], in0=q4[rs, :], in1=d["u"][lb][rs, :], op=ALU.add),
                  reads=[k4, K(ci, "u" + str(lb))], writes=[K(ci, "vn")])

            def mmo(e, rs=rs):
                e.matmul(q5[rs, :], lhsT=d["qgT"][lb][:, rs], rhs=d["Sbf"][:], start=True, stop=False)
                return e.matmul(q5[rs, :], lhsT=d["qkT"][lb][rs, rs], rhs=d["vn"][rs, :], start=False, stop=True)
            Rs.pe(mmo, reads=[K(ci, "qgT" + str(lb)), K(ci, "qkT" + str(lb)), K(ci, "vn"), ("Sbf", ci)], writes=[k5])
            Rs.pe(lambda e, rs=rs: e.matmul(q7, lhsT=d["kd"][lb][rs, :], rhs=d["vn"][rs, :], start=True, stop=True),
                 reads=[K(ci, "kd" + str(lb)), K(ci, "vn")], writes=[k7])
            dcol = (cc * 64 + 63) if dr == 0 else (cc * 64)
            Rs.dve(lambda e, dcol=dcol: e.scalar_tensor_tensor(out=d["S32"][:], in0=d["S32"][:], scalar=d["erow"][lb][:, dcol:dcol + 1],
                                                              in1=q7, op0=ALU.mult, op1=ALU.add),
                  reads=[("S32", ci), K(ci, "erow" + str(lb)), k7], writes=[("S32", ci)])
            Rs.act(lambda e: e.copy(out=d["Sbf"][:], in_=d["S32"][:]), reads=[("S32", ci)], writes=[("Sbf", ci)])
            Rs.act(lambda e, rs=rs: e.copy(out=osb[rs, :], in_=q5[rs, :]), reads=[k5], writes=[K(ci, "osb" + str(lb))])
        Rs.dma(o_s.ap()[dr][h][t * 128:(t + 1) * 128, :], osb[:], reads=[K(ci, "osb" + str(lb))], writes=[("o", dr, h, t)])

    def phase2(hp):
        for ci in range(4):
            P.dve(lambda e, ci=ci: e.memset(st[ci]["S32"][:], 0.0), writes=[("S32", ci)])
            P.dve(lambda e, ci=ci: e.memset(st[ci]["Sbf"][:], 0.0), writes=[("Sbf", ci)])
        if 2 in phases:
            pres, scans = {}, {}
            for s in range(nsteps):
                pres[s] = [Lazy(P) for _ in range(4)]
                scans[s] = [Lazy(P) for _ in range(4)]
                for ci in range(4):
                    chain_tile(ci, s, pres[s][ci], scans[s][ci])
            interleave(pres[0])
            for s in range(nsteps):
                interleave(scans[s] + (pres[s + 1] if s + 1 < nsteps else []))

    if DBG.get("dump"):
        gd_ = P.dram("gates_o", [128, NTILE, 8], F32, "ExternalOutput")
        P.dma(gd_.ap(), gates[:], reads=[("gates", t_) for t_ in range(NTILE)])
    of = [P.sb([128, 2, 128], F32) for _ in range(2)]
    obk = [P.sb([128, 2, 128], F32) for _ in range(2)]
    zt = [P.sb([128, 256], BF16) for _ in range(2)]
    gz = P.sb([128, 256], F32, "gz")
    osum = P.sb([128, 2, 128], F32, "osum")
    jk = P.sb([128, 128], F32, "jk")
    s3 = P.sb([128, 8], F32, "s3")
    yo = [P.sb([128, 256], F32) for _ in range(2)]
    def phase3(hp):
        for t in (range(NTILE) if 3 in phases else []):
            b = t % 2
            for h in range(2):
                P.dma(of[b][:, h, :], o_s.ap()[0][h][t * 128:(t + 1) * 128, :], reads=[("o", 0, h, t)], writes=[("of", b)])
                P.dma(obk[b][:, h, :], o_s.ap()[1][h][t * 128:(t + 1) * 128, :], reads=[("o", 1, h, t)], writes=[("obk", b)])
            P.dma(zt[b][:], z_s.ap()[t * 128:(t + 1) * 128, :], reads=[("z", t)], writes=[("zt", b)])
            P.dve(lambda e, b=b: e.tensor_tensor(out=osum[:], in0=of[b][:], in1=obk[b][:], op=ALU.add),
                  reads=[("of", b), ("obk", b)], writes=["osum"])
            P.dve(lambda e, b=b: e.tensor_tensor(out=gz[:], in0=zt[b][:], in1=onorm[:].rearrange("p h d -> p (h d)"), op=ALU.mult),
                  reads=[("zt", b), "onorm"], writes=["gz"])
            for h in range(2):
                P.act(lambda e, h=h: e.activation(out=jk[:], in_=osum[:, h, :], func=AF.Square, accum_out=s3[:, h:h + 1]),
                      reads=["osum"], writes=["jk", "s3"])
            P.act(lambda e: e.activation(out=s3[:, 2:4], in_=s3[:, 0:2], func=AF.Sqrt, scale=1.0 / 128.0, bias=W["epsc"][:, 0:1]),
                  reads=["s3", "epsc"], writes=["s3"])
            P.dve(lambda e: e.reciprocal(out=s3[:, 4:6], in_=s3[:, 2:4]), reads=["s3"], writes=["s3"])
            for h in range(2):
                P.dve(lambda e, h=h, b=b: e.scalar_tensor_tensor(out=yo[b][:, h * 128:(h + 1) * 128], in0=osum[:, h, :],
                                                                 scalar=s3[:, 4 + h:5 + h], in1=gz[:, h * 128:(h + 1) * 128],
                                                                 op0=ALU.mult, op1=ALU.mult),
                      reads=["osum", "s3", "gz"], writes=[("yo", b)])
            if fusedm:
                P.dma(y_d.ap()[t * 128:(t + 1) * 128, hp * 256:(hp + 1) * 256], yo[b][:], reads=[("yo", b)], writes=[("gdn_o", t)])
            else:
                P.dma(y_d.ap()[t * 128:(t + 1) * 128, :], yo[b][:], reads=[("yo", b)])
    for hp in range(nhp):
        load_weights(hp)
        phase1(hp)
        phase2(hp)
        phase3(hp)
    if standalone:
        return P.build()
    P.pop_scope()


def gdn_masks():
    m = np.zeros((5, 128, 128), np.float32)
    kp = np.arange(128)[:, None]
    c = np.arange(128)[None, :]
    same = (kp // 64) == (c // 64)
    m[0] = (same & (kp <= c))
    m[1] = (same & (kp < c))
    m[2] = (same & (kp >= c))
    m[3] = (same & (kp > c))
    m[4] = np.eye(128)
    return m


def gdn_weights(hp, c_w_in, c_conv, a_log_f, dtb_f, a_log_b, dtb_b):
    w = c_w_in[0]
    hs = [2 * hp, 2 * hp + 1]
    cols = []
    for base in (0, 1024, 2048):
        for h in hs:
            cols.append(w[:, base + h * 128: base + (h + 1) * 128])
    wf = np.ascontiguousarray(np.concatenate(cols, axis=1))
    zc = [w[:, 3072 + h * 128:3072 + (h + 1) * 128] for h in hs]
    gb = 4096
    gcols = [w[:, gb + 0 + h:gb + 0 + h + 1] for h in hs] + [w[:, gb + 16 + h:gb + 16 + h + 1] for h in hs] + \
            [w[:, gb + 8 + h:gb + 8 + h + 1] for h in hs] + [w[:, gb + 24 + h:gb + 24 + h + 1] for h in hs]
    wt = np.ascontiguousarray(np.concatenate(zc + gcols, axis=1))
    cw = np.zeros((128, 6, 5), np.float32)
    bi = 0
    for base in (0, 1024, 2048):
        for h in hs:
            cw[:, bi, :] = c_conv[0][:, base + h * 128: base + (h + 1) * 128].T
            bi += 1
    gconst = np.array([dtb_f[0][hs[0]], dtb_f[0][hs[1]], dtb_b[0][hs[0]], dtb_b[0][hs[1]],
                       a_log_f[0][hs[0]], a_log_f[0][hs[1]], a_log_b[0][hs[0]], a_log_b[0][hs[1]]], np.float32)
    return wf, wt, cw, gconst


def gdn_inputs(x2, c_norm, c_w_in, c_conv, a_log_f, dtb_f, a_log_b, dtb_b, out_norm):
    import ml_dtypes
    w = c_w_in[0]
    ident = np.eye(128, dtype=np.float32).astype(ml_dtypes.bfloat16)
    masks = gdn_masks()
    maps = []
    for c in range(NCORES):
        b, hp = c // 4, c % 4
        hs = [2 * hp, 2 * hp + 1]
        cols = []
        for base in (0, 1024, 2048):
            for h in hs:
                cols.append(w[:, base + h * 128: base + (h + 1) * 128])
        wf = np.ascontiguousarray(np.concatenate(cols, axis=1))
        zc = [w[:, 3072 + h * 128:3072 + (h + 1) * 128] for h in hs]
        gb = 4096
        gcols = [w[:, gb + 0 + h:gb + 0 + h + 1] for h in hs] + [w[:, gb + 16 + h:gb + 16 + h + 1] for h in hs] + \
                [w[:, gb + 8 + h:gb + 8 + h + 1] for h in hs] + [w[:, gb + 24 + h:gb + 24 + h + 1] for h in hs]
        wt = np.ascontiguousarray(np.concatenate(zc + gcols, axis=1))
        cw = np.zeros((128, 6, 5), np.float32)
        bi = 0
        for base in (0, 1024, 2048):
            for h in hs:
                cw[:, bi, :] = c_conv[0][:, base + h * 128: base + (h + 1) * 128].T
                bi += 1
        gconst = np.array([[dtb_f[0][hs[0]], dtb_f[0][hs[1]], dtb_b[0][hs[0]], dtb_b[0][hs[1]],
                            a_log_f[0][hs[0]], a_log_f[0][hs[1]], a_log_b[0][hs[0]], a_log_b[0][hs[1]]]], np.float32)
        xp = np.zeros((SEQ + 128, DM), np.float32)
        xp[2:2 + SEQ] = x2[b]
        maps.append({"xp": xp, "g": c_norm[0:1], "wf": wf[None], "wt": wt[None], "cw": cw[None], "gconst": gconst,
                     "onorm": out_norm[0:1], "gmask": masks, "ident": ident})
    return maps


def build_l0():
    P = Prog()
    x1_s = P.dram("x1_s", [NTOK, DM], F32, "Internal")
    build_attn(P, "a_", x1_s)
    P.fence()
    build_ffn(1, False, P, "f_", x1_s)
    return P.build()


def build_fused():
    P = Prog()
    x1_s = P.dram("x1_s", [SEQ, DM], F32, "Internal")
    x2_s = P.dram("x2_s", [SEQ, DM], F32, "Internal")
    og_s = P.dram("og_s", [SEQ, DM], F32, "Internal")
    qoff = P.dram("m_sel", [1, 4], F32, "ExternalInput")
    build_attn(P, "a_", x1_s, nq=4)
    P.fence()
    H = ffn_handles(P, "f_", 1, False)
    for q in range(4):
        build_ffn(1, False, P, "f_", x_handle=x1_s, H=H, x_row0=q * NTOK, y_handle=x2_s, y_row0=q * NTOK)
    P.fence()
    build_gdn(P=P, pf="g_", x_handle=x2_s, o_handle=og_s, nhp=4)
    P.fence()
    build_ffn(8, True, P, "m_", x_handle=x2_s, o_handle=og_s, dyn=qoff)
    return P.build()


def kernel(x, ab_norm, ab_w_in, ab_q_norm_a, ab_k_norm_a, ab_sink_a, ab_q_norm_b, ab_k_norm_b,
                 ab_w_out, ffn_norm, ffn_w_gate, ffn_w_up, ffn_w_down, c_norm, c_w_in, c_conv,
                 c_a_log_fwd, c_dt_bias_fwd, c_a_log_bwd, c_dt_bias_bwd, c_out_norm, c_w_out,
                 moe_norm, moe_w_router, moe_w_gate, moe_w_up, moe_w_down):
    f = lambda a: np.ascontiguousarray(np.asarray(a, dtype=np.float32))
    x = f(x)
    cores = list(range(NCORES))
    ident = _ident_bf16()
    identf = np.eye(128, dtype=np.float32)
    amaps = attn_inputs(x, f(ab_norm), f(ab_w_in), f(ab_q_norm_a), f(ab_k_norm_a), f(ab_sink_a), f(ab_q_norm_b),
                        f(ab_k_norm_b), f(ab_w_out), fused=True)
    gw = [gdn_weights(hp, f(c_w_in), f(c_conv), f(c_a_log_fwd), f(c_dt_bias_fwd), f(c_a_log_bwd), f(c_dt_bias_bwd))
          for hp in range(4)]
    g_wf = np.ascontiguousarray(np.stack([w[0] for w in gw]))
    g_wt = np.ascontiguousarray(np.stack([w[1] for w in gw]))
    g_cw = np.ascontiguousarray(np.stack([w[2] for w in gw]))
    g_gc = np.ascontiguousarray(np.stack([w[3] for w in gw]))
    gmask = gdn_masks()
    maps = []
    for c in cores:
        m = {"a_" + k: v for k, v in amaps[c].items()}
        m.update({"f_g": f(ffn_norm)[0:1], "f_wg": f(ffn_w_gate), "f_wu": f(ffn_w_up), "f_wd": f(ffn_w_down), "f_ident": ident})
        m.update({"g_g": f(c_norm)[0:1], "g_wf": g_wf, "g_wt": g_wt, "g_cw": g_cw, "g_gconst": g_gc,
                  "g_onorm": f(c_out_norm)[0:1], "g_gmask": gmask, "g_ident": ident})
        m.update({"m_wo": f(c_w_out)[0], "m_g": f(moe_norm)[0:1], "m_wg": f(moe_w_gate)[0], "m_wu": f(moe_w_up)[0],
                  "m_wd": f(moe_w_down)[0], "m_wr": f(moe_w_router)[0], "m_ident": ident, "m_identf": identf,
                  "m_sel": np.eye(4, dtype=np.float32)[c % 4][None, :]})
        maps.append(m)
    res = run_bass_kernel_spmd(_prog("fused", build_fused), maps, core_ids=cores)
    y = np.concatenate([r["m_y"] for r in res.results], 0)
    return y.reshape(2, SEQ, DM).astype(np.float32)


_CACHE = {}
STAGES = {}


def _prog(name, fn):
    if name not in _CACHE:
        _CACHE[name] = fn()
    return _CACHE[name]


def _ident_bf16():
    import ml_dtypes
    return np.eye(128, dtype=np.float32).astype(ml_dtypes.bfloat16)


def kernel_unfused(x, ab_norm, ab_w_in, ab_q_norm_a, ab_k_norm_a, ab_sink_a, ab_q_norm_b, ab_k_norm_b,
           ab_w_out, ffn_norm, ffn_w_gate, ffn_w_up, ffn_w_down, c_norm, c_w_in, c_conv,
           c_a_log_fwd, c_dt_bias_fwd, c_a_log_bwd, c_dt_bias_bwd, c_out_norm, c_w_out,
           moe_norm, moe_w_router, moe_w_gate, moe_w_up, moe_w_down):
    f = lambda a: np.ascontiguousarray(np.asarray(a, dtype=np.float32))
    x = f(x)
    cores = list(range(NCORES))
    ident = _ident_bf16()
    identf = np.eye(128, dtype=np.float32)
    amaps = attn_inputs(x, f(ab_norm), f(ab_w_in), f(ab_q_norm_a), f(ab_k_norm_a), f(ab_sink_a), f(ab_q_norm_b),
                        f(ab_k_norm_b), f(ab_w_out))
    maps = []
    for c in cores:
        m = {"a_" + k: v for k, v in amaps[c].items()}
        m.update({"f_g": f(ffn_norm)[0:1], "f_wg": f(ffn_w_gate), "f_wu": f(ffn_w_up), "f_wd": f(ffn_w_down), "f_ident": ident})
        maps.append(m)
    res = run_bass_kernel_spmd(_prog("l0", build_l0), maps, core_ids=cores)
    x2 = np.concatenate([r["f_y"] for r in res.results], 0)
    STAGES["x2"] = x2
    maps = gdn_inputs(x2.reshape(2, SEQ, DM), f(c_norm), f(c_w_in), f(c_conv), f(c_a_log_fwd), f(c_dt_bias_fwd),
                      f(c_a_log_bwd), f(c_dt_bias_bwd), f(c_out_norm))
    res = run_bass_kernel_spmd(_prog("gdn", build_gdn), maps, core_ids=cores)
    o = np.zeros((2, SEQ, DM), np.float32)
    for c in cores:
        b, hp = c // 4, c % 4
        o[b][:, hp * 256:(hp + 1) * 256] = res.results[c]["y"]
    o = o.reshape(-1, DM)
    STAGES["o"] = o
    maps = [{"x": np.ascontiguousarray(x2[c * NTOK:(c + 1) * NTOK]), "o": np.ascontiguousarray(o[c * NTOK:(c + 1) * NTOK]),
             "wo": f(c_w_out)[0], "g": f(moe_norm)[0:1], "wg": f(moe_w_gate)[0], "wu": f(moe_w_up)[0], "wd": f(moe_w_down)[0],
             "wr": f(moe_w_router)[0], "ident": ident, "identf": identf} for c in cores]
    res = run_bass_kernel_spmd(_prog("moe", lambda: build_ffn(8, True)), maps, core_ids=cores)
    y = np.concatenate([r["y"] for r in res.results], 0)
    return y.reshape(2, SEQ, DM).astype(np.float32)
```

```python
import numpy as np
import concourse.bass as bass
import concourse.mybir as mybir
from concourse.bass_utils import run_bass_kernel_spmd
from contextlib import ExitStack

F32 = mybir.dt.float32
BF16 = mybir.dt.bfloat16
I32 = mybir.dt.int32
ALU = mybir.AluOpType
AF = mybir.ActivationFunctionType
AX = mybir.AxisListType

ENGS = ["tensor", "vector", "scalar", "gpsimd", "sync"]
NCORES = 8


class _Op:
    __slots__ = ("eng", "seq", "fn", "deps", "signal", "count", "dma", "dsem", "dcum", "snap")

    def __init__(self, eng, seq, fn, dma):
        self.eng = eng
        self.seq = seq
        self.fn = fn
        self.deps = []
        self.signal = False
        self.count = 0
        self.dma = dma
        self.dsem = -1
        self.dcum = 0
        self.snap = None


class Lazy:
    def __init__(self, P):
        self.P = P
        self.q = []
        self.grp = None

    def begin(self):
        self.grp = []

    def end(self):
        g, self.grp = self.grp, None
        self.q.append(lambda: [t() for t in g])

    def __getattr__(self, name):
        f = getattr(self.P, name)

        def rec(*a, **k):
            (self.q if self.grp is None else self.grp).append(lambda: f(*a, **k))
        return rec


def interleave(lazies):
    n = max(len(L.q) for L in lazies)
    for i in range(n):
        for L in lazies:
            if i < len(L.q):
                L.q[i]()


class Prog:
    DMA_RING = 12

    def __init__(self):
        self.nc = bass.Bass("TRN2", target_bir_lowering=False)
        self.es = ExitStack()
        self.ops = {e: [] for e in ENGS}
        self.lastw = {}
        self.readers = {}
        self.known = {e: {} for e in ENGS}
        self.knownd = {e: {} for e in ENGS}
        self.dmas = []
        self.ring_last = {}
        self.ring_cum = {}
        self.ring_next = {e: 0 for e in ENGS}
        self.n_sb = 0
        self.alias = {}
        self.scopes = []
        self.fence_t = self.es.enter_context(self.nc.sbuf_tensor("fence_t", [128, 64], BF16))
        self.nfence = 0

    def push_scope(self):
        self.scopes.append(self.es)
        self.es = ExitStack()

    def pop_scope(self):
        self.es.close()
        self.es = self.scopes.pop()
        self.alias = {}

    def fence(self):
        toks = []
        for e in ENGS:
            if self.ops[e] and not self.ops[e][-1].dma:
                toks.append(("e", e, self.ops[e][-1].seq))
        for key, idx in self.ring_last.items():
            toks.append(("d", idx))
        ft = self.fence_t
        n = self.nfence
        self.nfence += 1
        self.push_scope()
        pf = self.ps([128, 512], F32, f"fence_ps{n}")
        self.op("vector", lambda e: e.memset(ft[:, 0:16], 0.0), extra=toks)
        self.op("scalar", lambda e: e.copy(out=ft[:, 16:32], in_=ft[:, 16:32]), extra=toks)
        self.op("gpsimd", lambda e: e.memset(ft[:, 32:48], 0.0), extra=toks)
        self.op("tensor", lambda e: e.matmul(pf[0:1, 0:8], lhsT=ft[0:1, 48:49], rhs=ft[0:1, 48:56], start=True, stop=True),
                extra=toks)
        self.op("sync", lambda e: e.dma_start(out=ft[0:1, 56:60], in_=ft[0:1, 60:64]), extra=toks, dma=True)
        self.scopes_tmp = None
        self.es.close()
        self.es = self.scopes.pop()

    def bank(self, key, bank_id):
        self.alias[key] = ("BANK", bank_id)

    def sb(self, shape, dt, name=None):
        self.n_sb += 1
        return self.es.enter_context(self.nc.sbuf_tensor(f"sb{self.n_sb}_{name or ''}", list(shape), dt))

    def ps(self, shape, dt, name=None):
        self.n_sb += 1
        return self.es.enter_context(self.nc.psum_tensor(f"ps{self.n_sb}_{name or ''}", list(shape), dt))

    def dram(self, name, shape, dt, kind):
        return self.nc.dram_tensor(name, list(shape), dt, kind=kind)

    def _dep_tokens(self, reads, writes):
        deps = set()
        for k in reads:
            w = self.lastw.get(k)
            if w is not None:
                deps.add(w)
        for k in writes:
            w = self.lastw.get(k)
            if w is not None:
                deps.add(w)
            for r in self.readers.get(k, ()):
                deps.add(r)
        return deps

    def _need(self, eng, tok):
        if tok[0] == "e":
            _, se, sq = tok
            if se == eng and eng in ("tensor", "sync"):
                return False
            return self.known[eng].get(se, -1) < sq
        else:
            d = self.dmas[tok[1]]
            return self.knownd[eng].get(d.dsem, 0) < d.dcum

    def _learn(self, eng, tok):
        if tok[0] == "e":
            _, se, sq = tok
            src = self.ops[se][sq]
            src.signal = True
            k = self.known[eng]
            if k.get(se, -1) < sq:
                k[se] = sq
            if src.snap is not None:
                sk, sd = src.snap
                for a, b in sk.items():
                    if k.get(a, -1) < b:
                        k[a] = b
                kd = self.knownd[eng]
                for a, b in sd.items():
                    if kd.get(a, 0) < b:
                        kd[a] = b
        else:
            d = self.dmas[tok[1]]
            kd = self.knownd[eng]
            if kd.get(d.dsem, 0) < d.dcum:
                kd[d.dsem] = d.dcum

    def op(self, eng, fn, reads=(), writes=(), dma=False, extra=()):
        al = self.alias
        if al:
            excl = {al[k] for k in reads if k in al} | {al[k] for k in writes if k in al}
            reads = [k for k in reads if k not in al]
            writes = [k for k in writes if k not in al] + list(excl)
        deps = self._dep_tokens(reads, writes)
        deps.update(extra)
        seq = len(self.ops[eng])
        o = _Op(eng, seq, fn, dma)
        if dma:
            slot = self.ring_next[eng]
            self.ring_next[eng] = (slot + 1) % self.DMA_RING
            key = (eng, slot)
            prev = self.ring_last.get(key)
            if prev is not None:
                deps.add(("d", prev))
            o.dsem = key
            self.ring_cum[key] = self.ring_cum.get(key, 0) + 16
            o.dcum = self.ring_cum[key]
        for tok in sorted(deps, key=lambda t: (t[0], str(t[1]), t[2] if len(t) > 2 else 0)):
            if self._need(eng, tok):
                o.deps.append(tok)
                self._learn(eng, tok)
        o.snap = (dict(self.known[eng]), dict(self.knownd[eng]))
        self.ops[eng].append(o)
        if dma:
            idx = len(self.dmas)
            self.dmas.append(o)
            self.ring_last[o.dsem] = idx
            tok = ("d", idx)
        else:
            tok = ("e", eng, seq)
        for k in writes:
            self.lastw[k] = tok
            self.readers[k] = []
        for k in reads:
            if k not in writes:
                self.readers.setdefault(k, []).append(tok)
        return tok

    def pe(self, fn, reads=(), writes=()):
        return self.op("tensor", fn, reads, writes)

    def dve(self, fn, reads=(), writes=()):
        return self.op("vector", fn, reads, writes)

    def act(self, fn, reads=(), writes=()):
        return self.op("scalar", fn, reads, writes)

    def pool(self, fn, reads=(), writes=()):
        return self.op("gpsimd", fn, reads, writes)

    def dma(self, out, in_, reads=(), writes=(), q="sync", **kw):
        return self.op(q, lambda e: e.dma_start(out=out, in_=in_, **kw), reads, writes, dma=True)

    def build(self):
        nc = self.nc
        es = self.es
        esem = {e: es.enter_context(nc.semaphore(f"s_{e}")) for e in ENGS}
        dsem = {}
        for key in self.ring_cum:
            dsem[key] = es.enter_context(nc.semaphore(f"d_{key[0]}_{key[1]}"))
        for e in ENGS:
            c = 0
            for o in self.ops[e]:
                if o.signal and not o.dma:
                    c += 1
                o.count = c
        block = es.enter_context(nc.Block())
        ops = self.ops
        dmas = self.dmas
        ring_cum = self.ring_cum

        def emit(engname):
            def body(eng):
                for o in ops[engname]:
                    for tok in o.deps:
                        if tok[0] == "e":
                            eng.wait_ge(esem[tok[1]], ops[tok[1]][tok[2]].count)
                        else:
                            d = dmas[tok[1]]
                            eng.wait_ge(dsem[d.dsem], d.dcum)
                    ins = o.fn(eng)
                    if o.dma:
                        ins.then_inc(dsem[o.dsem], 16)
                    elif o.signal:
                        ins.then_inc(esem[engname], 1)
                for key, cum in ring_cum.items():
                    if key[0] == engname:
                        eng.wait_ge(dsem[key], cum)
            return body

        for e in ENGS:
            if not ops[e]:
                continue
            getattr(block, e)(emit(e))
        es.close()
        return nc


EPS = 1e-6


def bcast_rows(handle, ncols, nparts=128, offset=0):
    return bass.AP(handle, offset, [[0, nparts], [1, ncols]])


def emit_norm_T(P, x_ap, xkey, g_bc, dstT, dst_cols, dkey, W, uid):
    nc = P.nc
    xn = W["xn"][uid % 2]
    kxn = ("xn", uid % 2)
    psT = W["psT"]
    if g_bc is not None:
        junk = W["junk"]
        ss = W["ss"][uid % 2]
        kss = ("ss", uid % 2)
        P.act(lambda e: e.activation(out=junk[:], in_=x_ap, func=AF.Square, accum_out=ss[:, 0:1]),
              reads=[xkey], writes=[kss, "junk"])
        P.act(lambda e: e.activation(out=ss[:, 1:2], in_=ss[:, 0:1], func=AF.Sqrt,
                                     scale=1.0 / 1024.0, bias=W["epsc"][:, 0:1]),
              reads=[kss], writes=[kss])
        P.dve(lambda e: e.reciprocal(out=ss[:, 2:3], in_=ss[:, 1:2]), reads=[kss], writes=[kss])
        P.dve(lambda e: e.scalar_tensor_tensor(out=xn[:], in0=x_ap, scalar=ss[:, 2:3], in1=g_bc[:],
                                               op0=ALU.mult, op1=ALU.mult),
              reads=[xkey, kss, "gbc"], writes=[kxn])
    else:
        P.dve(lambda e: e.tensor_copy(out=xn[:], in_=x_ap), reads=[xkey], writes=[kxn])

    def tr(e):
        ins = None
        for kc in range(8):
            ins = e.transpose(out=psT[:, kc * 128:(kc + 1) * 128], in_=xn[:, kc * 128:(kc + 1) * 128],
                              identity=W["ident"][:])
        return ins
    P.pe(tr, reads=[kxn, "ident"], writes=["psT"])
    P.act(lambda e: e.copy(out=dstT[:, :, dst_cols], in_=psT[:].rearrange("p (k c) -> p k c", k=8)),
          reads=["psT"], writes=[dkey])


def alloc_norm_work(P):
    W = {}
    W["xn"] = [P.sb([128, 1024], BF16) for _ in range(2)]
    W["junk"] = P.sb([128, 1024], BF16)
    W["ss"] = [P.sb([128, 4], F32) for _ in range(2)]
    W["psT"] = P.ps([128, 1024], BF16)
    W["ident"] = P.sb([128, 128], BF16)
    W["epsc"] = P.sb([128, 1], F32)
    return W


NTOK = 2048
NT = NTOK // 128
DM = 1024
DFF = 3584
NF = DFF // 128
FR = 7


def ffn_handles(P, pf, n_exp, with_proj):
    H = {}
    H["g"] = P.dram(pf + "g", [1, DM], F32, "ExternalInput")
    H["wg"] = P.dram(pf + "wg", [n_exp, DM, DFF], F32, "ExternalInput")
    H["wu"] = P.dram(pf + "wu", [n_exp, DM, DFF], F32, "ExternalInput")
    H["wd"] = P.dram(pf + "wd", [n_exp, DFF, DM], F32, "ExternalInput")
    H["ident"] = P.dram(pf + "ident", [128, 128], BF16, "ExternalInput")
    if n_exp > 1:
        H["wr"] = P.dram(pf + "wr", [DM, n_exp], F32, "ExternalInput")
        H["identf"] = P.dram(pf + "identf", [128, 128], F32, "ExternalInput")
    if with_proj:
        H["wo"] = P.dram(pf + "wo", [DM, DM], F32, "ExternalInput")
    return H


def build_ffn(n_exp, with_proj, P=None, pf="", x_handle=None, o_handle=None, H=None, x_row0=0, y_handle=None, y_row0=0, dyn=None):
    standalone = P is None
    if standalone:
        P = Prog()
    nc = P.nc
    if not standalone:
        P.push_scope()
    x_d = x_handle if x_handle is not None else P.dram(pf + "x", [NTOK, DM], F32, "ExternalInput")
    if H is None:
        H = ffn_handles(P, pf, n_exp, with_proj)
    g_d, wg_d, wu_d, wd_d, id_d = H["g"], H["wg"], H["wu"], H["wd"], H["ident"]
    y_d = y_handle if y_handle is not None else P.dram(pf + "y", [NTOK, DM], F32, "ExternalOutput")
    if n_exp > 1:
        wr_d, idf_d = H["wr"], H["identf"]
    if with_proj:
        o_d = o_handle if o_handle is not None else P.dram(pf + "o", [NTOK, DM], F32, "ExternalInput")
        wo_d = H["wo"]
    W = alloc_norm_work(P)
    yacc = P.sb([128, NT, DM], F32, "yacc")
    xnT = P.sb([128, 8, NTOK], BF16, "xnT")
    gbc = P.sb([128, DM], F32, "gbc")
    actT = P.sb([128, FR, NTOK], BF16, "actT")
    wgc = [P.sb([128, 8, 128], BF16) for _ in range(3)]
    wuc = [P.sb([128, 8, 128], BF16) for _ in range(3)]
    wdc = [P.sb([128, DM], BF16) for _ in range(FR + 3)]
    sg = [P.sb([128, 512], F32) for _ in range(2)]
    psG = [P.ps([128, 512], F32) for _ in range(2)]
    psU = [P.ps([128, 512], F32) for _ in range(2)]
    psY = [P.ps([128, 512], F32) for _ in range(2)]
    W["psRL"] = P.ps([128, 512], F32)
    for i_, k_ in enumerate(["psT", "psRL", ("psG", 0), ("psG", 1), ("psU", 0), ("psU", 1), ("psY", 0), ("psY", 1)]):
        P.bank(k_, i_)

    P.dma(W["ident"][:], id_d.ap(), writes=["ident"])
    P.dma(gbc[:], bcast_rows(g_d, DM), writes=["gbc"])
    P.dve(lambda e: e.memset(W["epsc"][:], EPS), writes=["epsc"])
    if dyn is not None:
        sel = P.sb([128, 4], F32, "sel")
        actf = actT.bitcast(F32)
        selt = [actf[:, b_, :] for b_ in range(2)]
        seltk = [[("actT", b_, tg_) for tg_ in range(4)] for b_ in range(2)]
        P.dma(sel[:], bcast_rows(dyn, 4), writes=["sel"])
        seli = [0]

        def load_sel(handle, dst_ap, dkey, t):
            for q in range(4):
                b = seli[0] % 2
                seli[0] += 1
                P.dma(selt[b], handle.ap()[q * NTOK + t * 128:q * NTOK + (t + 1) * 128, :], writes=seltk[b])
                if q == 0:
                    P.dve(lambda e, b=b: e.tensor_scalar(out=dst_ap, in0=selt[b], scalar1=sel[:, 0:1], scalar2=None,
                                                        op0=ALU.mult), reads=seltk[b] + ["sel"], writes=[dkey])
                else:
                    P.dve(lambda e, b=b, q=q: e.scalar_tensor_tensor(out=dst_ap, in0=selt[b], scalar=sel[:, q:q + 1],
                                                                     in1=dst_ap, op0=ALU.mult, op1=ALU.add),
                          reads=seltk[b] + ["sel", dkey], writes=[dkey])
    for t in range(NT):
        if dyn is not None:
            load_sel(x_d, yacc[:, t, :], ("y", t), t)
        else:
            r0 = x_row0 + t * 128
            P.dma(yacc[:, t, :], x_d.ap()[r0:r0 + 128, :], reads=[("x1s", r0 // 128)], writes=[("y", t)])

    if with_proj:
        wo = P.sb([128, 8, DM], BF16, "wo")
        P.dma(wo[:], wo_d.ap().rearrange("(k p) c -> p k c", p=128), writes=["wo"], q="gpsimd")
        ot = [P.sb([128, DM], F32) for _ in range(2)]
        for t in range(NT):
            if dyn is not None:
                load_sel(o_d, ot[t % 2][:], ("ot", t % 2), t)
            else:
                r0 = x_row0 + t * 128
                P.dma(ot[t % 2][:], o_d.ap()[r0:r0 + 128, :], writes=[("ot", t % 2)])
            emit_norm_T(P, ot[t % 2][:], ("ot", t % 2), None, xnT, slice(t * 128, (t + 1) * 128), ("xnT", t), W, t)
            for h in range(2):
                def mm(e, t=t, h=h):
                    ins = None
                    for kc in range(8):
                        ins = e.matmul(psY[h][:], lhsT=xnT[:, kc, t * 128:(t + 1) * 128],
                                       rhs=wo[:, kc, h * 512:(h + 1) * 512], start=(kc == 0), stop=(kc == 7))
                    return ins
                P.pe(mm, reads=[("xnT", t), "wo"], writes=[("psY", h)])
                P.dve(lambda e, t=t, h=h: e.tensor_tensor(out=yacc[:, t, h * 512:(h + 1) * 512], in0=psY[h][:],
                                                          in1=yacc[:, t, h * 512:(h + 1) * 512], op=ALU.add),
                      reads=[("psY", h), ("y", t)], writes=[("y", t)])

    for t in range(NT):
        emit_norm_T(P, yacc[:, t, :], ("y", t), gbc, xnT, slice(t * 128, (t + 1) * 128), ("xnT", t), W, t)
    allx = [("xnT", t) for t in range(NT)]

    comb = None
    if n_exp > 1:
        W["psRA"], W["psRB"] = psG[0], psG[1]
        comb = emit_router_keys(P, yacc, gbc, W, wr_d, idf_d, n_exp)

    ci = 0
    di = 0
    for ex in range(n_exp):
        wgv = wg_d.ap()[ex].rearrange("(k p) c -> p k c", p=128)
        wuv = wu_d.ap()[ex].rearrange("(k p) c -> p k c", p=128)
        wdv = wd_d.ap()[ex].rearrange("(f p) c -> p f c", p=128)
        for r in range(NF // FR):
            dslots = []
            for fi in range(FR):
                f = r * FR + fi
                cs = ci % 3
                ci += 1
                P.dma(wgc[cs][:], wgv[:, :, f * 128:(f + 1) * 128], writes=[("wgc", cs)], q="gpsimd")
                P.dma(wuc[cs][:], wuv[:, :, f * 128:(f + 1) * 128], writes=[("wuc", cs)], q="gpsimd")
                ds = di % (FR + 3)
                di += 1
                dslots.append(ds)
                P.dma(wdc[ds][:], wdv[:, f, :], writes=[("wdc", ds)], q="gpsimd")
                for tg in range(4):
                    b = (fi * 4 + tg) % 2

                    def mmg(e, cs=cs, tg=tg, b=b):
                        ins = None
                        for kc in range(8):
                            ins = e.matmul(psG[b][:], lhsT=wgc[cs][:, kc, :], rhs=xnT[:, kc, tg * 512:(tg + 1) * 512],
                                           start=(kc == 0), stop=(kc == 7))
                        return ins

                    def mmu(e, cs=cs, tg=tg, b=b):
                        ins = None
                        for kc in range(8):
                            ins = e.matmul(psU[b][:], lhsT=wuc[cs][:, kc, :], rhs=xnT[:, kc, tg * 512:(tg + 1) * 512],
                                           start=(kc == 0), stop=(kc == 7))
                        return ins
                    xk = [("xnT", t) for t in range(tg * 4, tg * 4 + 4)]
                    P.pe(mmg, reads=[("wgc", cs)] + xk, writes=[("psG", b)])
                    P.pe(mmu, reads=[("wuc", cs)] + xk, writes=[("psU", b)])
                    P.act(lambda e, b=b: e.activation(out=sg[b][:], in_=psG[b][:], func=AF.Silu),
                          reads=[("psG", b)], writes=[("sg", b)])
                    P.dve(lambda e, b=b, fi=fi, tg=tg: e.tensor_tensor(out=actT[:, fi, tg * 512:(tg + 1) * 512],
                                                                       in0=sg[b][:], in1=psU[b][:], op=ALU.mult),
                          reads=[("sg", b), ("psU", b)], writes=[("actT", fi, tg)])
            for t in range(NT):
                for h in range(2):
                    def mmd(e, t=t, h=h, dslots=dslots):
                        ins = None
                        for fi in range(FR):
                            ins = e.matmul(psY[h][:], lhsT=actT[:, fi, t * 128:(t + 1) * 128],
                                           rhs=wdc[dslots[fi]][:, h * 512:(h + 1) * 512],
                                           start=(fi == 0), stop=(fi == FR - 1))
                        return ins
                    P.pe(mmd, reads=[("actT", fi, t // 4) for fi in range(FR)] + [("wdc", s) for s in dslots],
                         writes=[("psY", h)])
                    if comb is None:
                        P.dve(lambda e, t=t, h=h: e.tensor_tensor(out=yacc[:, t, h * 512:(h + 1) * 512], in0=psY[h][:],
                                                                  in1=yacc[:, t, h * 512:(h + 1) * 512], op=ALU.add),
                              reads=[("psY", h), ("y", t)], writes=[("y", t)])
                    else:
                        P.dve(lambda e, t=t, h=h, ex=ex: e.scalar_tensor_tensor(
                            out=yacc[:, t, h * 512:(h + 1) * 512], in0=psY[h][:],
                            scalar=comb[:, t, ex:ex + 1], in1=yacc[:, t, h * 512:(h + 1) * 512],
                            op0=ALU.mult, op1=ALU.add),
                            reads=[("psY", h), ("y", t), "comb"], writes=[("y", t)])
    yv = y_d.ap()[y_row0:y_row0 + NTOK, :].rearrange("(t p) d -> p t d", p=128)
    for t4 in range(4):
        P.dma(yv[:, t4 * 4:(t4 + 1) * 4, :], yacc[:, t4 * 4:(t4 + 1) * 4, :],
              reads=[("y", t) for t in range(t4 * 4, t4 * 4 + 4)],
              writes=[("yout", pf, (y_row0 // 128) + t) for t in range(t4 * 4, t4 * 4 + 4)])
    if standalone:
        return P.build()
    P.pop_scope()


def emit_router_keys(P, yacc, gbc, W, wr_d, idf_d, n_exp):
    identf = P.sb([128, 128], F32, "identf")
    wr = P.sb([128, 8, n_exp], F32, "wr")
    comb = P.sb([128, NT, n_exp], F32, "comb")
    x32 = P.sb([128, DM], F32, "rx32")
    xT32 = P.sb([128, 8, 128], F32, "rxT32")
    rs = P.sb([128, 16], F32, "rsmall")
    lg = P.sb([128, 8], F32, "rlg")
    mx = P.sb([128, 8], F32, "rmx")
    tmp = P.sb([128, 8], F32, "rtmp")
    psA = W["psRA"]
    psB = W["psRB"]
    psL = W["psRL"]
    P.dma(identf[:], idf_d.ap(), writes=["identf"])
    P.dma(wr[:], wr_d.ap().rearrange("(k p) e -> p k e", p=128), writes=["wr"])
    for t in range(NT):
        xt = yacc[:, t, :]
        P.act(lambda e, xt=xt: e.activation(out=W["junk"][:], in_=xt, func=AF.Square, accum_out=rs[:, 0:1]),
              reads=[("y", t)], writes=["junk", "rs"])
        P.act(lambda e: e.activation(out=rs[:, 1:2], in_=rs[:, 0:1], func=AF.Sqrt, scale=1.0 / 1024.0,
                                     bias=W["epsc"][:, 0:1]), reads=["rs"], writes=["rs"])
        P.dve(lambda e: e.reciprocal(out=rs[:, 2:3], in_=rs[:, 1:2]), reads=["rs"], writes=["rs"])
        P.dve(lambda e, xt=xt: e.scalar_tensor_tensor(out=x32[:], in0=xt, scalar=rs[:, 2:3], in1=gbc[:],
                                                      op0=ALU.mult, op1=ALU.mult),
              reads=[("y", t), "rs", "gbc"], writes=["rx32"])

        def tr(e):
            ins = None
            for kc in range(8):
                dst = (psA if kc < 4 else psB)[:, (kc % 4) * 128:(kc % 4 + 1) * 128]
                ins = e.transpose(out=dst, in_=x32[:, kc * 128:(kc + 1) * 128], identity=identf[:])
            return ins
        P.pe(tr, reads=["rx32", "identf"], writes=[("psG", 0), ("psG", 1)])
        P.act(lambda e: e.copy(out=xT32[:, 0:4, :], in_=psA[:].rearrange("p (k c) -> p k c", k=4)),
              reads=[("psG", 0)], writes=["rxTa"])
        P.act(lambda e: e.copy(out=xT32[:, 4:8, :], in_=psB[:].rearrange("p (k c) -> p k c", k=4)),
              reads=[("psG", 1)], writes=["rxTb"])

        def mm(e):
            ins = None
            for kc in range(8):
                ins = e.matmul(psL[:, 0:n_exp], lhsT=xT32[:, kc, :], rhs=wr[:, kc, :], start=(kc == 0), stop=(kc == 7))
            return ins
        P.pe(mm, reads=["rxTa", "rxTb", "wr"], writes=["psRL"])
        P.dve(lambda e: e.tensor_copy(out=lg[:], in_=psL[:, 0:n_exp]), reads=["psRL"], writes=["rlg"])
        P.dve(lambda e: e.max(out=mx[:], in_=lg[:]), reads=["rlg"], writes=["rmx"])
        P.dve(lambda e: e.tensor_tensor(out=rs[:, 4:5], in0=mx[:, 1:2], in1=mx[:, 0:1], op=ALU.subtract),
              reads=["rmx"], writes=["rs"])
        P.act(lambda e: e.activation(out=rs[:, 5:6], in_=rs[:, 4:5], func=AF.Exp), reads=["rs"], writes=["rs"])
        P.dve(lambda e: e.tensor_scalar(out=rs[:, 6:7], in0=rs[:, 5:6], scalar1=1.0, scalar2=None, op0=ALU.add),
              reads=["rs"], writes=["rs"])
        P.dve(lambda e: e.reciprocal(out=rs[:, 7:8], in_=rs[:, 6:7]), reads=["rs"], writes=["rs"])
        P.dve(lambda e: e.tensor_tensor(out=rs[:, 8:9], in0=rs[:, 5:6], in1=rs[:, 7:8], op=ALU.mult),
              reads=["rs"], writes=["rs"])
        P.dve(lambda e, t=t: e.tensor_scalar(out=comb[:, t, :], in0=lg[:], scalar1=mx[:, 0:1], scalar2=rs[:, 7:8],
                                             op0=ALU.is_equal, op1=ALU.mult),
              reads=["rlg", "rmx", "rs"], writes=["comb"])
        P.dve(lambda e: e.tensor_scalar(out=tmp[:], in0=lg[:], scalar1=mx[:, 1:2], scalar2=rs[:, 8:9],
                                        op0=ALU.is_equal, op1=ALU.mult),
              reads=["rlg", "rmx", "rs"], writes=["rtmp"])
        P.dve(lambda e, t=t: e.tensor_tensor(out=comb[:, t, :], in0=comb[:, t, :], in1=tmp[:], op=ALU.add),
              reads=["rtmp", "comb"], writes=["comb"])
    return comb


SEQ = 8192
NG = SEQ // 512
KA_GROUPS = {0: 0, 1: 1, 2: 2, 3: 3, 4: 4, 15: 5}


def build_attn(P=None, pf="", y_handle=None, nq=1):
    standalone = P is None
    if standalone:
        P = Prog()
    nc = P.nc
    if not standalone:
        P.push_scope()
    xr_d = P.dram(pf + "xr", [SEQ, DM], F32, "ExternalInput")
    g_d = P.dram(pf + "g", [1, DM], F32, "ExternalInput")
    wq_d = P.dram(pf + "wq", [DM, 1024], F32, "ExternalInput")
    wk_d = P.dram(pf + "wk", [DM, 256], F32, "ExternalInput")
    wv_d = P.dram(pf + "wv", [DM, 256], F32, "ExternalInput")
    wo_d = P.dram(pf + "wo", [64, 16, DM], F32, "ExternalInput")
    gains_d = P.dram(pf + "gains", [128, 4], F32, "ExternalInput")
    rope_d = P.dram(pf + "rope", [4, 128, SEQ], F32, "ExternalInput")
    rmat_d = P.dram(pf + "rmat", [3, 128, 128], BF16, "ExternalInput")
    masks_d = P.dram(pf + "masks", [4, 128, 512], BF16, "ExternalInput")
    sink_d = P.dram(pf + "sink", [1, 8], F32, "ExternalInput")
    id_d = P.dram(pf + "ident", [128, 128], BF16, "ExternalInput")
    y_d = y_handle if y_handle is not None else P.dram(pf + "y", [NTOK, DM], F32, "ExternalOutput")
    full = nq > 1
    ka_groups = {G_: G_ for G_ in range(NG)} if full else KA_GROUPS
    nka = 64 if full else 24

    W = alloc_norm_work(P)
    gbc = P.sb([128, DM], F32, "gbc")
    wq = P.sb([128, 8, 1024], BF16, "wq")
    wk = P.sb([128, 8, 256], BF16, "wk")
    wv = P.sb([128, 8, 256], BF16, "wv")
    wo = P.sb([64, 16, DM], BF16, "wo")
    gains = P.sb([128, 4], F32, "gains")
    rmat = P.sb([128, 3, 128], BF16, "rmat")
    masks = P.sb([128, 4, 512], BF16, "masks")
    KTa = P.sb([128, nka * 128], BF16, "KTa")
    KTb = P.sb([128, SEQ], BF16, "KTb")
    Va = P.sb([128, nka, 2, 65], BF16, "Va")
    Vb = P.sb([128, 64, 2, 65], BF16, "Vb")
    QTa = [P.sb([128, 4, 4, 128], BF16) for _ in range(2)]
    QTb = [P.sb([128, 4, 4, 128], BF16) for _ in range(2)]
    xt = [P.sb([128, DM], F32) for _ in range(2)]
    xng = [P.sb([128, 8, 512], BF16) for _ in range(1 if full else 2)]
    tab = [P.sb([128, 512], F32) for _ in range(4)]
    qg = P.sb([128, 512], BF16, "qg")
    sq = P.sb([128, 512], BF16, "sq")
    lnt = P.sb([128, 512], F32, "lnt")
    rstd = P.sb([128, 512], F32, "rstd")
    t1 = P.sb([128, 512], F32, "t1")
    t2 = P.sb([128, 512], F32, "t2")
    pT = [P.sb([128, 512], BF16) for _ in range(4)]
    den = P.sb([65, 512], F32, "den")
    nxng = len(xng)
    NTq = 16 * nq
    onesf = P.sb([65, 64], F32, "onesf")
    sink8 = P.sb([64, 8], F32, "sink8")
    lnr = P.sb([64, 512], F32, "lnr") if not full else None
    rec = P.sb([64, 512], F32, "rec") if not full else None
    OT = [P.sb([64, 16, 128], BF16) for _ in range(2)]
    xres = P.sb([128, DM], F32, "xres") if not full else None
    ysb = P.sb([128, DM], F32, "ysb")
    if full:
        lnr_ap, lnr_k, rec_ap, rec_k = lnt[0:64, :], "lnt", rstd[0:64, :], "rstd"
    else:
        lnr_ap, lnr_k, rec_ap, rec_k = lnr[:], "lnr", rec[:], "rec"
    b0 = W["psT"]
    bk = [None] + [P.ps([128, 512], F32) for _ in range(7)]

    def BK(i):
        return ("bank", i)
    P.bank("psT", 0)
    for i_ in range(1, 8):
        P.bank(BK(i_), i_)

    P.dma(W["ident"][:], id_d.ap(), writes=["ident"])
    P.dma(gbc[:], bcast_rows(g_d, DM), writes=["gbc"])
    P.dve(lambda e: e.memset(W["epsc"][:], EPS), writes=["epsc"])
    P.dma(gains[:], gains_d.ap(), writes=["gains"])
    P.dma(rmat[:], rmat_d.ap().rearrange("r p c -> p r c"), writes=["rmat"])
    P.dma(masks[:], masks_d.ap().rearrange("r p c -> p r c"), writes=["masks"])
    P.dma(wq[:], wq_d.ap().rearrange("(k p) c -> p k c", p=128), writes=["wq"], q="gpsimd")
    P.dma(wk[:], wk_d.ap().rearrange("(k p) c -> p k c", p=128), writes=["wk"], q="gpsimd")
    P.dma(wv[:], wv_d.ap().rearrange("(k p) c -> p k c", p=128), writes=["wv"], q="gpsimd")
    P.dma(wo[:], wo_d.ap(), writes=["wo"], q="gpsimd")
    P.dve(lambda e: e.memset(Va[:, :, :, 64:65], 1.0), writes=["Va1"])
    P.dve(lambda e: e.memset(Vb[:, :, :, 64:65], 1.0), writes=["Vb1"])
    P.dve(lambda e: e.memset(onesf[:], 1.0), writes=["onesf"])
    P.dma(sink8[:], bcast_rows(sink_d, 8, 64), writes=["sink8"])
    P.act(lambda e: e.activation(out=sink8[:], in_=sink8[:], func=AF.Exp), reads=["sink8"], writes=["sink8"])
    for gi in range(2):
        P.dve(lambda e, gi=gi: e.memset(QTa[gi][:], 0.0), writes=[("QT", "a", gi)])
        P.dve(lambda e, gi=gi: e.memset(QTb[gi][:], 0.0), writes=[("QT", "b", gi)])

    xrv = xr_d.ap().rearrange("(t p) d -> p t d", p=128)

    def load_group(G):
        pb = G % nxng
        for tl in range(4):
            tg = G * 4 + tl
            P.dma(xt[tg % 2][:], xrv[:, tg, :], writes=[("xt", tg % 2)])
            emit_norm_T(P, xt[tg % 2][:], ("xt", tg % 2), gbc, xng[pb], slice(tl * 128, (tl + 1) * 128),
                        ("xng", pb, tl), W, tg)
        for i in range(4):
            P.dma(tab[i][:], rope_d.ap()[i][:, G * 512:(G + 1) * 512], writes=[("tab", i)])
        return pb

    qkc = [0]

    def qk_block(pb, lhs_fn, gcol, ti, ri, dest3, dkey):
        b = 1 + (qkc[0] % 2)
        qkc[0] += 1
        xk = [("xng", pb, tl) for tl in range(4)]

        def mm(e):
            ins = None
            for kc in range(8):
                ins = e.matmul(bk[b][:], lhsT=lhs_fn(kc), rhs=xng[pb][:, kc, :], start=(kc == 0), stop=(kc == 7))
            return ins
        P.pe(mm, reads=xk + ["wq", "wk"], writes=[BK(b)])
        P.act(lambda e: e.activation(out=qg[:], in_=bk[b][:], func=AF.Copy, scale=gains[:, gcol:gcol + 1]),
              reads=[BK(b), "gains"], writes=["qg"])
        P.act(lambda e: e.activation(out=sq[:], in_=bk[b][:], func=AF.Square), reads=[BK(b)], writes=["sq"])
        P.pe(lambda e: e.matmul(bk[3][:], lhsT=rmat[:, ri, :], rhs=qg[:], start=True, stop=True),
             reads=["qg", "rmat"], writes=[BK(3)])
        P.pe(lambda e: e.matmul(bk[4][:], lhsT=rmat[:, 2, :], rhs=sq[:], start=True, stop=True),
             reads=["sq", "rmat"], writes=[BK(4)])
        P.act(lambda e: e.activation(out=lnt[:], in_=bk[4][:], func=AF.Ln, scale=1.0 / 64.0, bias=W["epsc"][:, 0:1]),
              reads=[BK(4), "epsc"], writes=["lnt"])
        P.act(lambda e: e.activation(out=rstd[:], in_=lnt[:], func=AF.Exp, scale=-0.5), reads=["lnt"], writes=["rstd"])
        P.dve(lambda e: e.tensor_tensor(out=t1[:], in0=qg[:], in1=tab[ti][:], op=ALU.mult),
              reads=["qg", ("tab", ti)], writes=["t1"])
        P.dve(lambda e: e.tensor_tensor(out=t2[:], in0=bk[3][:], in1=tab[ti + 1][:], op=ALU.mult),
              reads=[BK(3), ("tab", ti + 1)], writes=["t2"])
        P.dve(lambda e: e.tensor_tensor(out=t1[:], in0=t1[:], in1=t2[:], op=ALU.add), reads=["t1", "t2"], writes=["t1"])
        dests = dest3 if isinstance(dest3, list) else [(slice(0, 128), dest3, dkey)]
        for ps_, dap, dk in dests:
            P.dve(lambda e, ps_=ps_, dap=dap: e.tensor_tensor(out=dap, in0=t1[ps_, :].rearrange("p (a b) -> p a b", a=4),
                                                              in1=rstd[ps_, :].rearrange("p (a b) -> p a b", a=4), op=ALU.mult),
                  reads=["t1", "rstd"], writes=[dk])

    for G in range(NG):
        pb = load_group(G)
        qk_block(pb, lambda kc: wk[:, kc, 128:256], 3, 2, 1,
                 KTb[:, G * 512:(G + 1) * 512].rearrange("p (a b) -> p a b", a=4), ("KTb", G))
        sg = ka_groups.get(G)
        if sg is not None:
            qk_block(pb, lambda kc: wk[:, kc, 0:128], 1, 0, 0,
                     KTa[:, sg * 512:(sg + 1) * 512].rearrange("p (a b) -> p a b", a=4), ("KTa", sg))
        for tl in range(4):
            tg = G * 4 + tl

            def mmv(e, tl=tl, pb=pb):
                ins = None
                for kc in range(8):
                    ins = e.matmul(bk[5][:, 0:256], lhsT=xng[pb][:, kc, tl * 128:(tl + 1) * 128], rhs=wv[:, kc, :],
                                   start=(kc == 0), stop=(kc == 7))
                return ins
            P.pe(mmv, reads=[("xng", pb, tl), "wv"], writes=[BK(5)])
            P.act(lambda e, tg=tg: e.copy(out=Vb[:, tg, :, 0:64], in_=bk[5][:, 128:256].rearrange("p (h d) -> p h d", h=2)),
                  reads=[BK(5)], writes=[("Vb", tg)])
            if sg is not None:
                sl = sg * 4 + tl
                P.act(lambda e, sl=sl: e.copy(out=Va[:, sl, :, 0:64], in_=bk[5][:, 0:128].rearrange("p (h d) -> p h d", h=2)),
                      reads=[BK(5)], writes=[("Va", sl)])

    pcount = [0]
    ocount = [0]
    D = 3

    def attend(R, kind, tl, g, slots, mk, obuf, hbase):
        QT = (QTa if kind == "a" else QTb)[g]
        KT = KTa if kind == "a" else KTb
        V = Va if kind == "a" else Vb
        ob = 4 + (ocount[0] % 2)
        ocount[0] += 1
        n = len(slots)
        hist = []
        for i in range(n + D):
            if i < n:
                s = slots[i]
                sb_ = (1, 2, 3, 6)[pcount[0] % 4]
                pb_ = pcount[0] % 4
                pcount[0] += 1
                hist.append(pb_)
                kkey = ("KTa", s // 4) if kind == "a" else ("KTb", s // 4)
                R.pe(lambda e, s=s, sb_=sb_: e.matmul(bk[sb_][:], lhsT=KT[:, s * 128:(s + 1) * 128],
                                                       rhs=QT[:, tl, :, :], start=True, stop=True),
                     reads=[kkey, ("QT", kind, g)], writes=[BK(sb_)])
                R.act(lambda e, sb_=sb_, pb_=pb_: e.activation(out=pT[pb_][:], in_=bk[sb_][:], func=AF.Exp, scale=0.125),
                      reads=[BK(sb_)], writes=[("pT", pb_)])
                if mk[i] is not None:
                    R.dve(lambda e, pb_=pb_, m=mk[i]: e.tensor_tensor(out=pT[pb_][:], in0=pT[pb_][:], in1=masks[:, m, :],
                                                                      op=ALU.mult),
                          reads=[("pT", pb_), "masks"], writes=[("pT", pb_)])
            if i >= D:
                j = i - D
                s = slots[j]
                pb_ = hist[j]
                vkey = ("Va", s) if kind == "a" else ("Vb", s)
                R.pe(lambda e, s=s, pb_=pb_, j=j: e.matmul(bk[ob][0:65, :], lhsT=V[:, s, g, :], rhs=pT[pb_][:],
                                                            start=(j == 0), stop=(j == n - 1)),
                     reads=[vkey, ("pT", pb_), "Va1", "Vb1"], writes=[BK(ob)])
        R.dve(lambda e: e.tensor_copy(out=den[64:65, :], in_=bk[ob][64:65, :]), reads=[BK(ob)], writes=["den"])
        R.pe(lambda e: e.matmul(bk[6][0:64, :], lhsT=onesf[64:65, :], rhs=den[64:65, :], start=True, stop=True),
             reads=["den", "onesf"], writes=[BK(6)])
        if kind == "a":
            denb = t1[0:64, :]
            for hi in range(4):
                R.dve(lambda e, hi=hi: e.tensor_scalar(out=denb[:, hi * 128:(hi + 1) * 128], in0=bk[6][0:64, hi * 128:(hi + 1) * 128],
                                                       scalar1=sink8[:, g * 4 + hi:g * 4 + hi + 1], scalar2=None, op0=ALU.add),
                      reads=[BK(6), "sink8"], writes=["t1"])
            R.act(lambda e: e.activation(out=lnr_ap, in_=denb, func=AF.Ln), reads=["t1"], writes=[lnr_k])
        else:
            R.act(lambda e: e.activation(out=lnr_ap, in_=bk[6][0:64, :], func=AF.Ln), reads=[BK(6)], writes=[lnr_k])
        R.act(lambda e: e.activation(out=rec_ap, in_=lnr_ap, func=AF.Exp, scale=-1.0), reads=[lnr_k], writes=[rec_k])
        h0 = hbase + 4 * g
        R.dve(lambda e: e.tensor_tensor(out=OT[obuf][:, h0:h0 + 4, :],
                                        in0=bk[ob][0:64, :].rearrange("p (a b) -> p a b", a=4),
                                        in1=rec_ap.rearrange("p (a b) -> p a b", a=4), op=ALU.mult),
              reads=[BK(ob), rec_k], writes=[("OT", obuf, kind, g)])

    yv = y_d.ap().rearrange("(t p) d -> p t d", p=128)
    for G in range(4 * nq):
        pb = load_group(G)
        for j in range(4):
            qk_block(pb, lambda kc, j=j: wq[:, kc, j * 128:(j + 1) * 128], 0, 0, 0,
                     [(slice(0, 64), QTa[0][0:64, :, j, :], ("QT", "a", 0)), (slice(64, 128), QTa[1][64:128, :, j, :], ("QT", "a", 1))], None)
            qk_block(pb, lambda kc, j=j: wq[:, kc, 512 + j * 128:512 + (j + 1) * 128], 2, 2, 1,
                     [(slice(0, 64), QTb[0][0:64, :, j, :], ("QT", "b", 0)), (slice(64, 128), QTb[1][64:128, :, j, :], ("QT", "b", 1))], None)
        for tl in range(4):
            t = G * 4 + tl
            obuf = t % 2
            if full:
                left = t - 1 if t > 0 else 0
                right = t + 1 if t < NTq - 1 else NTq - 1
            else:
                left = t - 1 if t > 0 else 23
                right = t + 1
            for g in range(2):
                attend(P, "a", tl, g, [left, t, right], [0 if t == 0 else 1, None, 3 if t == NTq - 1 else 2], obuf, 0)
            for g in range(2):
                attend(P, "b", tl, g, list(range(64)), [None] * 64, obuf, 8)
            if full:
                xres, xres_k = xt[t % 2], ("xt", t % 2)
            else:
                xres_k = "xres"
            P.dma(xres[:], xrv[:, t, :], writes=[xres_k])
            okeys = [("OT", obuf, k, g) for k in "ab" for g in range(2)]
            for h2 in range(2):
                def mmo(e, h2=h2, obuf=obuf):
                    ins = None
                    for h in range(16):
                        ins = e.matmul(bk[7][:], lhsT=OT[obuf][:, h, :], rhs=wo[:, h, h2 * 512:(h2 + 1) * 512],
                                       start=(h == 0), stop=(h == 15))
                    return ins
                P.pe(mmo, reads=okeys + ["wo"], writes=[BK(7)])
                P.dve(lambda e, h2=h2, xres=xres: e.tensor_tensor(out=ysb[:, h2 * 512:(h2 + 1) * 512], in0=bk[7][:],
                                                                  in1=xres[:, h2 * 512:(h2 + 1) * 512], op=ALU.add),
                      reads=[BK(7), xres_k], writes=[("ysb", h2)])
            P.dma(yv[:, t, :], ysb[:], reads=[("ysb", 0), ("ysb", 1)], writes=[("x1s", t)])
    if standalone:
        return P.build()
    P.pop_scope()


def rope_consts(r):
    pos = (np.arange(SEQ) + 2048 * r) % SEQ
    posf = pos.astype(np.float32)
    inv32 = (np.float32(10000.0) ** (-np.arange(0, 64, 2, dtype=np.float32) / np.float32(64))).astype(np.float32)
    inv16 = (np.float32(10000.0) ** (-np.arange(0, 32, 2, dtype=np.float32) / np.float32(32))).astype(np.float32)
    ang_a = posf[:, None] * inv32[None, :]
    row = (pos // 64).astype(np.float32)
    col = (pos % 64).astype(np.float32)
    ang_r = row[:, None] * inv16[None, :]
    ang_c = col[:, None] * inv16[None, :]
    d = np.arange(128) % 64
    tabs = np.zeros((4, 128, SEQ), np.float32)
    tabs[0] = np.cos(ang_a).astype(np.float32)[:, d % 32].T
    tabs[1] = np.sin(ang_a).astype(np.float32)[:, d % 32].T
    ang_b = np.where((d < 32)[None, :], ang_r[:, d % 16], ang_c[:, d % 16])
    tabs[2] = np.cos(ang_b).astype(np.float32).T
    tabs[3] = np.sin(ang_b).astype(np.float32).T
    return tabs


def rot_consts():
    import ml_dtypes
    Ra = np.zeros((64, 64), np.float32)
    for i in range(64):
        if i < 32:
            Ra[i, i + 32] = -1.0
        else:
            Ra[i, i - 32] = 1.0
    Rb = np.zeros((64, 64), np.float32)
    for i in range(64):
        if (i % 32) < 16:
            Rb[i, i + 16] = -1.0
        else:
            Rb[i, i - 16] = 1.0
    out = np.zeros((3, 128, 128), np.float32)
    for blk in range(2):
        s = slice(blk * 64, (blk + 1) * 64)
        out[0, s, s] = Ra.T
        out[1, s, s] = Rb.T
        out[2, s, s] = 1.0
    return out.astype(ml_dtypes.bfloat16)


def mask_consts(r):
    import ml_dtypes
    k = np.arange(128)[:, None]
    q = np.arange(128)[None, :]
    mL = (k >= q).astype(np.float32)
    mR = (q >= k).astype(np.float32)
    m = np.zeros((4, 128, 512), np.float32)
    m[0] = np.tile(mL if (r is not None and r > 0) else 0 * mL, (1, 4))
    m[1] = np.tile(mL, (1, 4))
    m[2] = np.tile(mR, (1, 4))
    m[3] = np.tile(mR if (r is not None and r < 3) else 0 * mR, (1, 4))
    return m.astype(ml_dtypes.bfloat16)


def attn_inputs(x, ab_norm, ab_w_in, qn_a, kn_a, sink_a, qn_b, kn_b, ab_w_out, fused=False):
    import ml_dtypes
    w = ab_w_in[0]
    qa, ka, va, qb, kb, vb = (w[:, 0:512], w[:, 512:640], w[:, 640:768], w[:, 768:1280], w[:, 1280:1408], w[:, 1408:1536])

    def pair(wq_):
        cols = []
        for j in range(4):
            cols.append(wq_[:, j * 64:(j + 1) * 64])
            cols.append(wq_[:, (j + 4) * 64:(j + 5) * 64])
        return np.concatenate(cols, axis=1)
    wq = np.ascontiguousarray(np.concatenate([pair(qa), pair(qb)], axis=1))
    wk = np.ascontiguousarray(np.concatenate([ka, kb], axis=1))
    wv = np.ascontiguousarray(np.concatenate([va, vb], axis=1))
    wo = np.ascontiguousarray(ab_w_out[0].reshape(16, 64, DM).transpose(1, 0, 2))
    gains = np.ascontiguousarray(np.stack([np.tile(qn_a[0], 2), np.tile(kn_a[0], 2), np.tile(qn_b[0], 2), np.tile(kn_b[0], 2)], axis=1))
    ident = np.eye(128, dtype=np.float32).astype(ml_dtypes.bfloat16)
    rmat = rot_consts()
    maps = []
    if fused:
        rope0, masks0 = rope_consts(0), mask_consts(None)
    for c in range(NCORES):
        b, r = c // 4, c % 4
        if fused:
            maps.append({"xr": x[b], "g": ab_norm[0:1], "wq": wq, "wk": wk, "wv": wv, "wo": wo, "gains": gains,
                         "rope": rope0, "rmat": rmat, "masks": masks0, "sink": sink_a[0:1], "ident": ident})
            continue
        xr = np.ascontiguousarray(np.roll(x[b], -2048 * r, axis=0))
        maps.append({"xr": xr, "g": ab_norm[0:1], "wq": wq, "wk": wk, "wv": wv, "wo": wo, "gains": gains,
                     "rope": rope_consts(r), "rmat": rmat, "masks": mask_consts(r), "sink": sink_a[0:1], "ident": ident})
    return maps


NTILE = SEQ // 128
DBG = {}


def build_gdn(phases=(1, 2, 3), nsteps=NTILE, P=None, pf="", x_handle=None, o_handle=None, nhp=1):
    standalone = P is None
    if standalone:
        P = Prog()
    else:
        P.push_scope()
    nc = P.nc
    fusedm = x_handle is not None
    xp_d = x_handle if fusedm else P.dram(pf + "xp", [SEQ + 128, DM], F32, "ExternalInput")
    g_d = P.dram(pf + "g", [1, DM], F32, "ExternalInput")
    wf_d = P.dram(pf + "wf", [nhp, DM, 768], F32, "ExternalInput")
    wt_d = P.dram(pf + "wt", [nhp, DM, 264], F32, "ExternalInput")
    cw_d = P.dram(pf + "cw", [nhp, 128, 6, 5], F32, "ExternalInput")
    gc_d = P.dram(pf + "gconst", [nhp, 8], F32, "ExternalInput")
    on_d = P.dram(pf + "onorm", [1, 128], F32, "ExternalInput")
    mk_d = P.dram(pf + "gmask", [5, 128, 128], F32, "ExternalInput")
    id_d = P.dram(pf + "ident", [128, 128], BF16, "ExternalInput")
    y_d = o_handle if fusedm else P.dram(pf + "y", [SEQ, 256], F32, "ExternalOutput")
    SK = "ExternalOutput" if DBG.get("dump") else "Internal"
    qT_s = P.dram(pf + "qT_s", [2, 128, SEQ], BF16, SK)
    kT_s = P.dram(pf + "kT_s", [2, 128, SEQ], BF16, SK)
    k_s = P.dram(pf + "k_s", [2, SEQ, 128], BF16, SK)
    v_s = P.dram(pf + "v_s", [2, SEQ, 128], BF16, SK)
    z_s = P.dram(pf + "z_s", [SEQ, 256], BF16, SK)
    o_s = P.dram(pf + "o_s", [2, 2, SEQ, 128], F32, SK)

    W = alloc_norm_work(P)
    gbc = P.sb([128, DM], F32, "gbc")
    wf = P.sb([128, 8, 768], BF16, "wf")
    wt = P.sb([128, 8, 264], BF16, "wt")
    cw = P.sb([128, 6, 5], F32, "cw")
    gconst = P.sb([128, 8], F32, "gconst")
    onorm = P.sb([128, 2, 128], F32, "onorm")
    gmask = P.sb([128, 5, 128], F32, "gmask")
    onesb = P.sb([128, 128], BF16, "onesb")
    onesf = P.sb([128, 128], F32, "onesf")
    onec = P.sb([128, 1], F32, "onec")
    gates = P.sb([128, NTILE, 8], F32, "gates")
    xt = [P.sb([128, DM], F32) for _ in range(2)]
    xng = [P.sb([128, 8, 516], BF16) for _ in range(2)]
    pre = [P.sb([128, 516], F32) for _ in range(2)]
    cacc = [P.sb([128, 512], F32) for _ in range(2)]
    cblk = [P.sb([128, 512], F32) for _ in range(2)]
    sqb = [P.sb([128, 512], BF16) for _ in range(2)]
    lnt = [P.sb([128, 512], F32) for _ in range(2)]
    rst = [P.sb([128, 512], F32) for _ in range(2)]
    nT = [P.sb([128, 512], BF16) for _ in range(2)]
    tm = [P.sb([128, 4, 128], BF16) for _ in range(2)]
    zsb = [P.sb([128, 256], BF16) for _ in range(2)]
    gtmp = P.sb([128, 8], F32, "gtmp")
    banks = [P.ps([128, 512], F32) for _ in range(7)]
    bankT = W["psT"]

    def BQ(b, q):
        return ("bq", b, q)

    def BK(b):
        return [BQ(b, q) for q in range(4)]

    KT_ALL = [BQ(7, q) for q in range(4)]
    for b_ in range(8):
        for q_ in range(4):
            P.bank(BQ(b_, q_), b_)

    P.dma(W["ident"][:], id_d.ap(), writes=["ident"])
    P.dma(gbc[:], bcast_rows(g_d, DM), writes=["gbc"])
    P.dve(lambda e: e.memset(W["epsc"][:], EPS), writes=["epsc"])
    P.dma(onorm[:, 0, :], bcast_rows(on_d, 128), writes=["onorm"])
    P.dma(onorm[:, 1, :], bcast_rows(on_d, 128), writes=["onorm"])
    P.dma(gmask[:], mk_d.ap().rearrange("r p c -> p r c"), writes=["gmask"])
    P.dve(lambda e: e.memset(onesb[:], 1.0), writes=["onesb"])
    P.dve(lambda e: e.memset(onesf[:], 1.0), writes=["onesf"])
    P.dve(lambda e: e.memset(onec[:], 1.0), writes=["onec"])
    def load_weights(hp):
        P.dma(wf[:], wf_d.ap()[hp].rearrange("(k p) c -> p k c", p=128), writes=["wf"], q="gpsimd")
        P.dma(wt[:], wt_d.ap()[hp].rearrange("(k p) c -> p k c", p=128), writes=["wt"], q="gpsimd")
        P.dma(cw[:], cw_d.ap()[hp], writes=["cw"])
        P.dma(gconst[:], bcast_rows(gc_d, 8, 128, hp * 8), writes=["gconst"])
        P.act(lambda e: e.activation(out=gconst[:, 4:8], in_=gconst[:, 4:8], func=AF.Exp), reads=["gconst"], writes=["gconst"])
        P.dve(lambda e: e.tensor_scalar(out=gconst[:, 4:8], in0=gconst[:, 4:8], scalar1=-1.0, scalar2=None, op0=ALU.mult),
              reads=["gconst"], writes=["gconst"])

    xpv = xp_d.ap()
    uid = [0]

    def norm_rows(x_ap, xkey, rows, dst, dkey):
        u = uid[0]
        uid[0] += 1
        xn = W["xn"][u % 2]
        kxn = ("xn", u % 2)
        ss = W["ss"][u % 2]
        kss = ("ss", u % 2)
        psT = bankT
        P.act(lambda e: e.activation(out=W["junk"][0:rows, :], in_=x_ap, func=AF.Square, accum_out=ss[0:rows, 0:1]),
              reads=[xkey], writes=[kss, "junk"])
        P.act(lambda e: e.activation(out=ss[0:rows, 1:2], in_=ss[0:rows, 0:1], func=AF.Sqrt, scale=1.0 / 1024.0,
                                     bias=W["epsc"][0:rows, 0:1]), reads=[kss, "epsc"], writes=[kss])
        P.dve(lambda e: e.reciprocal(out=ss[0:rows, 2:3], in_=ss[0:rows, 1:2]), reads=[kss], writes=[kss])
        P.dve(lambda e: e.scalar_tensor_tensor(out=xn[0:rows, :], in0=x_ap, scalar=ss[0:rows, 2:3], in1=gbc[0:rows, :],
                                               op0=ALU.mult, op1=ALU.mult), reads=[xkey, kss, "gbc"], writes=[kxn])

        def tr(e):
            ins = None
            for kc in range(8):
                ins = e.transpose(out=psT[:, kc * 128:kc * 128 + rows], in_=xn[0:rows, kc * 128:(kc + 1) * 128],
                                  identity=W["ident"][0:rows, 0:rows])
            return ins
        P.pe(tr, reads=[kxn, "ident"], writes=KT_ALL)
        P.act(lambda e: e.copy(out=dst, in_=psT[:].rearrange("p (k c) -> p k c", k=8)[:, :, 0:rows]),
              reads=KT_ALL, writes=[dkey])

    def phase1(hp):
        for G in (range(DBG.get('ng', NG)) if 1 in phases else []):
            pb = G % 2
            LV = DBG.get('lv', 9)
            for tl in range(5):
                rows = 128 if tl < 4 else 4
                u = uid[0]
                xo = 0 if fusedm else 2
                if tl < 4:
                    r0 = G * 512 + xo + tl * 128
                    P.dma(xt[u % 2][0:rows, :], xpv[r0:r0 + rows, :], reads=[("yout", "f_", r0 // 128)], writes=[("xt", u % 2)])
                else:
                    if fusedm and (G == 0 or G == NG - 1):
                        P.dve(lambda e, u=u: e.memset(xt[u % 2][0:4, :], 0.0), writes=[("xt", u % 2)])
                    if not (fusedm and G == 0):
                        r0 = G * 512 + xo - 2
                        P.dma(xt[u % 2][0:2, :], xpv[r0:r0 + 2, :], reads=[("yout", "f_", r0 // 128)], writes=[("xt", u % 2)])
                    if not (fusedm and G == NG - 1):
                        r0 = G * 512 + xo + 512
                        P.dma(xt[u % 2][2:4, :], xpv[r0:r0 + 2, :], reads=[("yout", "f_", r0 // 128)], writes=[("xt", u % 2)])
                norm_rows(xt[u % 2][0:rows, :], ("xt", u % 2), rows, xng[pb][:, :, tl * 128:tl * 128 + rows], ("xng", pb, tl))
            xk = [("xng", pb, tl) for tl in range(5)]
            Ls = [Lazy(P), Lazy(P), Lazy(P)]
            for blk in (range(6) if LV >= 2 else []):
                R = Ls[blk % 2]
                pbk = 1 + (blk % 2)
                prb = blk % 2

                def mm_main(e, blk=blk, pbk=pbk, pb=pb):
                    ins = None
                    for kc in range(8):
                        ins = e.matmul(banks[pbk][:], lhsT=wf[:, kc, blk * 128:(blk + 1) * 128], rhs=xng[pb][:, kc, 0:512],
                                       start=(kc == 0), stop=(kc == 7))
                    return ins

                def mm_halo(e, blk=blk, pb=pb):
                    ins = None
                    for kc in range(8):
                        ins = e.matmul(banks[3][:, 0:4], lhsT=wf[:, kc, blk * 128:(blk + 1) * 128], rhs=xng[pb][:, kc, 512:516],
                                       start=(kc == 0), stop=(kc == 7))
                    return ins
                R.pe(mm_main, reads=xk + ["wf"], writes=BK(pbk))
                R.begin()
                R.pe(mm_halo, reads=xk + ["wf"], writes=BK(3))
                R.act(lambda e, prb=prb: e.copy(out=pre[prb][:, 0:2], in_=banks[3][:, 0:2]), reads=BK(3), writes=[("pre", prb)])
                R.act(lambda e, prb=prb: e.copy(out=pre[prb][:, 514:516], in_=banks[3][:, 2:4]), reads=BK(3), writes=[("pre", prb)])
                R.end()
                R.act(lambda e, pbk=pbk, prb=prb: e.copy(out=pre[prb][:, 2:514], in_=banks[pbk][:]), reads=BK(pbk), writes=[("pre", prb)])
                R.dve(lambda e, blk=blk, prb=prb: e.tensor_scalar(out=cacc[prb][:], in0=pre[prb][:, 0:512], scalar1=cw[:, blk, 0:1],
                                                                  scalar2=None, op0=ALU.mult),
                      reads=[("pre", prb), "cw"], writes=[("cacc", prb)])
                for tap in range(1, 5):
                    R.dve(lambda e, blk=blk, prb=prb, tap=tap: e.scalar_tensor_tensor(
                        out=cacc[prb][:], in0=pre[prb][:, tap:tap + 512], scalar=cw[:, blk, tap:tap + 1], in1=cacc[prb][:],
                        op0=ALU.mult, op1=ALU.add), reads=[("pre", prb), "cw", ("cacc", prb)], writes=[("cacc", prb)])
                nb = blk % 2
                h = blk % 2
                if LV < 3:
                    continue
                if blk < 4:
                    R.act(lambda e, prb=prb: e.activation(out=cblk[prb][:], in_=cacc[prb][:], func=AF.Silu), reads=[("cacc", prb)], writes=[("cblk", prb)])
                    R.act(lambda e, prb=prb: e.activation(out=sqb[prb][:], in_=cblk[prb][:], func=AF.Square), reads=[("cblk", prb)], writes=[("sqb", prb)])
                    R.begin()
                    R.pe(lambda e, prb=prb: e.matmul(banks[4][:], lhsT=onesb[:], rhs=sqb[prb][:], start=True, stop=True),
                         reads=[("sqb", prb), "onesb"], writes=BK(4))
                    R.act(lambda e, prb=prb: e.activation(out=lnt[prb][:], in_=banks[4][:], func=AF.Ln, bias=W["epsc"][:, 0:1]),
                          reads=BK(4) + ["epsc"], writes=[("lnt", prb)])
                    R.end()
                    R.act(lambda e, prb=prb: e.activation(out=rst[prb][:], in_=lnt[prb][:], func=AF.Exp, scale=-0.5), reads=[("lnt", prb)], writes=[("rst", prb)])
                    qs = (128.0 ** -0.5) if blk < 2 else 1.0
                    R.dve(lambda e, nb=nb, qs=qs, prb=prb: e.scalar_tensor_tensor(out=nT[nb][:], in0=cblk[prb][:], scalar=qs, in1=rst[prb][:],
                                                                         op0=ALU.mult, op1=ALU.mult),
                          reads=[("cblk", prb), ("rst", prb)], writes=[("nT", nb)])
                    dst = qT_s if blk < 2 else kT_s
                    nm = "qT" if blk < 2 else "kT"
                    if LV >= 4:
                      R.dma(dst.ap()[h][:, G * 512:(G + 1) * 512], nT[nb][:], reads=[("nT", nb)],
                          writes=[(nm, h, G * 4 + tl) for tl in range(4)])
                else:
                    R.act(lambda e, nb=nb, prb=prb: e.activation(out=nT[nb][:], in_=cacc[prb][:], func=AF.Silu), reads=[("cacc", prb)], writes=[("nT", nb)])
                if blk >= 2 and LV >= 5:
                    bT = bankT

                    def trk(e, nb=nb):
                        ins = None
                        for tl in range(4):
                            ins = e.transpose(out=bT[:, tl * 128:(tl + 1) * 128], in_=nT[nb][:, tl * 128:(tl + 1) * 128],
                                              identity=W["ident"][:])
                        return ins
                    R.begin()
                    R.pe(trk, reads=[("nT", nb), "ident"], writes=KT_ALL)
                    R.act(lambda e, nb=nb: e.copy(out=tm[nb][:], in_=bT[:, 0:512].rearrange("p (t c) -> p t c", t=4)),
                          reads=KT_ALL, writes=[("tm", nb)])
                    R.end()
                    dst = k_s if blk < 4 else v_s
                    nm = "k" if blk < 4 else "v"
                    R.dma(dst.ap()[h].rearrange("(t p) d -> p t d", p=128)[:, G * 4:(G + 1) * 4, :], tm[nb][:],
                          reads=[("tm", nb)], writes=[(nm, h, G * 4 + tl) for tl in range(4)])
            R = Ls[2]
            for tl in (range(4) if LV >= 6 else []):
                t = G * 4 + tl
                zb = t % 2

                def mmt(e, tl=tl, pb=pb):
                    ins = None
                    for kc in range(8):
                        ins = e.matmul(banks[5][:, 0:264], lhsT=xng[pb][:, kc, tl * 128:(tl + 1) * 128], rhs=wt[:, kc, :],
                                       start=(kc == 0), stop=(kc == 7))
                    return ins
                R.pe(mmt, reads=xk + ["wt"], writes=BK(5))
                R.act(lambda e, zb=zb: e.activation(out=zsb[zb][:], in_=banks[5][:, 0:256], func=AF.Silu), reads=BK(5), writes=[("zsb", zb)])
                R.dma(z_s.ap()[t * 128:(t + 1) * 128, :], zsb[zb][:], reads=[("zsb", zb)], writes=[("z", t)])
                if LV < 7:
                    continue
                R.dve(lambda e: e.tensor_tensor(out=gtmp[:, 0:4], in0=banks[5][:, 256:260], in1=gconst[:, 0:4], op=ALU.add),
                      reads=BK(5) + ["gconst"], writes=["gtmp"])
                SUB = DBG.get('sub', 9)
                if SUB >= 2:
                    R.act(lambda e: e.activation(out=gtmp[:, 0:4], in_=gtmp[:, 0:4], func=AF.Exp), reads=["gtmp"], writes=["gtmp"])
                if SUB >= 3:
                    R.act(lambda e: e.activation(out=gtmp[:, 0:4], in_=gtmp[:, 0:4], func=AF.Ln, bias=onec[:, 0:1]),
                          reads=["gtmp", "onec"], writes=["gtmp"])
                if SUB >= 4:
                    R.dve(lambda e, t=t: e.tensor_tensor(out=gates[:, t, 0:4], in0=gtmp[:, 0:4], in1=gconst[:, 4:8], op=ALU.mult),
                          reads=["gtmp", "gconst"], writes=[("gates", t)])
                if LV < 8:
                    continue
                R.act(lambda e: e.activation(out=gtmp[:, 4:8], in_=banks[5][:, 260:264], func=AF.Exp, scale=-1.0),
                      reads=BK(5), writes=["gtmp"])
                R.dve(lambda e: e.tensor_scalar(out=gtmp[:, 4:8], in0=gtmp[:, 4:8], scalar1=1.0, scalar2=None, op0=ALU.add),
                      reads=["gtmp"], writes=["gtmp"])
                R.dve(lambda e, t=t: e.reciprocal(out=gates[:, t, 4:8], in_=gtmp[:, 4:8]), reads=["gtmp"], writes=[("gates", t)])
            interleave(Ls)

    allb = [P.ps([128, 512], F32)] if False else None
    chains = [(h, d) for d in range(2) for h in range(2)]
    bank_ap = banks + [None]
    pbank8 = P.ps

    def Q(ci, q):
        b = 2 * ci + q // 4
        qq = q % 4
        if b == 7:
            ap = bankT.bitcast(F32)[:, qq * 128:(qq + 1) * 128]
        else:
            ap = banks[b][:, qq * 128:(qq + 1) * 128]
        return ap, BQ(b, qq)

    def Qb(ci, q):
        b = 2 * ci + q // 4
        qq = q % 4
        if b == 7:
            ap = bankT[:, qq * 256:qq * 256 + 128]
        else:
            ap = banks[b].bitcast(BF16)[:, qq * 256:qq * 256 + 128]
        return ap, BQ(b, qq)

    st = {}
    for ci in range(4):
        d = {}
        d["S32"] = P.sb([128, 128], F32)
        d["Sbf"] = P.sb([128, 128], BF16)
        for nm in ["qT", "kT", "k", "v"]:
            d[nm] = [P.sb([128, 128], BF16) for _ in range(2)]
        for nm in ["gcol", "cols"]:
            d[nm] = P.sb([128, 8], F32)
        for nm in ["TG", "IB", "Dm", "E", "Bm", "Eb"]:
            d[nm] = P.sb([128, 128], F32)
        for nm in ["erow", "u"]:
            d[nm] = [P.sb([128, 128], F32) for _ in range(2)]
        for nm in ["X", "XT", "Pa", "PaT", "Pb", "PbT", "Za", "Zb", "vb", "kbg", "vn"]:
            d[nm] = P.sb([128, 128], BF16)
        for nm in ["qgT", "nwT", "qkT"]:
            d[nm] = [P.sb([128, 128], BF16) for _ in range(2)]
        d["kd"] = [[P.sb([128, 128], BF16) for _ in range(2)] for _ in range(2)]
        d["osb"] = [P.sb([128, 128], F32) for _ in range(2)]
        st[ci] = d

    def K(ci, nm):
        return (nm, "c", ci)

    def chain_tile(ci, s, Rp, Rs):
        h, dr = chains[ci]
        d = st[ci]
        t = s if dr == 0 else NTILE - 1 - s
        lb = s % 2
        incl = gmask[:, 0 + 2 * dr, :]
        strict = gmask[:, 1 + 2 * dr, :]
        identf = gmask[:, 4, :]
        gcolumn = gates[:, t, 2 * dr + h:2 * dr + h + 1]
        bcolumn = gates[:, t, 4 + 2 * dr + h:4 + 2 * dr + h + 1]
        for nm, src in (("qT", qT_s), ("kT", kT_s)):
            Rp.dma(d[nm][lb][:], src.ap()[h][:, t * 128:(t + 1) * 128], reads=[(nm, h, t)], writes=[K(ci, nm + str(lb))])
        for nm, src in (("k", k_s), ("v", v_s)):
            Rp.dma(d[nm][lb][:], src.ap()[h][t * 128:(t + 1) * 128, :], reads=[(nm, h, t)], writes=[K(ci, nm + str(lb))])
        qT, kT, kk, vv = d["qT"][lb], d["kT"][lb], d["k"][lb], d["v"][lb]
        kqT, kkT, kkk, kvv = K(ci, "qT" + str(lb)), K(ci, "kT" + str(lb)), K(ci, "k" + str(lb)), K(ci, "v" + str(lb))
        q0, k0 = Q(ci, 0)
        q1, k1 = Q(ci, 1)
        q2, k2 = Q(ci, 2)
        q3, k3 = Q(ci, 3)
        q4, k4 = Q(ci, 4)
        q5, k5 = Q(ci, 5)
        q6, k6 = Q(ci, 6)
        q7, k7 = Q(ci, 7)
        Rp.pe(lambda e: e.matmul(q0, lhsT=kT[:], rhs=kT[:], start=True, stop=True), reads=[kkT], writes=[k0])
        Rp.pe(lambda e: e.matmul(q1, lhsT=kT[:], rhs=qT[:], start=True, stop=True), reads=[kkT, kqT], writes=[k1])
        Rp.dve(lambda e: e.tensor_scalar(out=d["TG"][:], in0=incl, scalar1=gcolumn, scalar2=None, op0=ALU.mult),
              reads=["gmask", ("gates", t)], writes=[K(ci, "TG")])
        Rp.dve(lambda e: e.tensor_scalar(out=d["IB"][:], in0=identf, scalar1=bcolumn, scalar2=None, op0=ALU.mult),
              reads=["gmask", ("gates", t)], writes=[K(ci, "IB")])
        Rp.pe(lambda e: e.matmul(q2, lhsT=onesf[:], rhs=d["TG"][:], start=True, stop=True),
             reads=[K(ci, "TG"), "onesf"], writes=[k2])
        Rp.pe(lambda e: e.matmul(q3[:, 0:2], lhsT=d["TG"][:], rhs=onesf[:, 0:2], start=True, stop=True),
             reads=[K(ci, "TG"), "onesf"], writes=[k3])
        Rp.dve(lambda e: e.tensor_copy(out=d["gcol"][:, 0:1], in_=q3[:, 0:1]), reads=[k3], writes=[K(ci, "gcol")])
        Rp.pe(lambda e: e.matmul(q3, lhsT=onesf[:], rhs=d["IB"][:], start=True, stop=True),
             reads=[K(ci, "IB"), "onesf", K(ci, "gcol")], writes=[k3])
        Rp.dve(lambda e: e.tensor_scalar(out=d["Dm"][:], in0=q2, scalar1=d["gcol"][:, 0:1], scalar2=0.0,
                                        op0=ALU.subtract, op1=ALU.min), reads=[k2, K(ci, "gcol")], writes=[K(ci, "Dm")])
        Rp.act(lambda e: e.activation(out=d["E"][:], in_=d["Dm"][:], func=AF.Exp), reads=[K(ci, "Dm")], writes=[K(ci, "E")])
        Rp.act(lambda e: e.activation(out=d["erow"][lb][:], in_=q2, func=AF.Exp), reads=[k2], writes=[K(ci, "erow" + str(lb))])
        Rp.dve(lambda e: e.tensor_tensor(out=d["E"][:], in0=d["E"][:], in1=incl, op=ALU.mult),
              reads=[K(ci, "E"), "gmask"], writes=[K(ci, "E")])
        Rp.dve(lambda e: e.tensor_tensor(out=d["Bm"][:], in0=q3, in1=strict, op=ALU.mult), reads=[k3, "gmask"], writes=[K(ci, "Bm")])
        Rp.dve(lambda e: e.tensor_tensor(out=d["Eb"][:], in0=d["E"][:], in1=d["Bm"][:], op=ALU.mult),
              reads=[K(ci, "E"), K(ci, "Bm")], writes=[K(ci, "Eb")])
        Rp.dve(lambda e: e.scalar_tensor_tensor(out=d["X"][:], in0=q0, scalar=-1.0, in1=d["Eb"][:], op0=ALU.mult, op1=ALU.mult),
              reads=[k0, K(ci, "Eb")], writes=[K(ci, "X")])
        Rp.dve(lambda e: e.tensor_tensor(out=d["qkT"][lb][:], in0=q1, in1=d["E"][:], op=ALU.mult),
              reads=[k1, K(ci, "E")], writes=[K(ci, "qkT" + str(lb))])
        cols = d["cols"]
        Rp.act(lambda e: e.activation(out=cols[:, 0:1], in_=d["gcol"][:, 0:1], func=AF.Exp), reads=[K(ci, "gcol")], writes=[K(ci, "cols")])
        Rp.dve(lambda e: e.tensor_tensor(out=cols[:, 1:2], in0=cols[:, 0:1], in1=bcolumn, op=ALU.mult),
              reads=[K(ci, "cols"), ("gates", t)], writes=[K(ci, "cols")])
        for cc in range(2):
            col = (cc * 64 + 63) if dr == 0 else (cc * 64)
            Rp.dve(lambda e, cc=cc, col=col: e.tensor_copy(out=cols[cc * 64:(cc + 1) * 64, 3:4], in_=q2[cc * 64:(cc + 1) * 64, col:col + 1]),
                  reads=[k2], writes=[K(ci, "cols")])
        Rp.dve(lambda e: e.tensor_tensor(out=cols[:, 4:5], in0=cols[:, 3:4], in1=d["gcol"][:, 0:1], op=ALU.subtract),
              reads=[K(ci, "cols"), K(ci, "gcol")], writes=[K(ci, "cols")])
        Rp.act(lambda e: e.activation(out=cols[:, 2:3], in_=cols[:, 4:5], func=AF.Exp), reads=[K(ci, "cols")], writes=[K(ci, "cols")])
        Rp.dve(lambda e: e.tensor_scalar(out=d["vb"][:], in0=vv[:], scalar1=bcolumn, scalar2=None, op0=ALU.mult),
              reads=[kvv, ("gates", t)], writes=[K(ci, "vb")])
        Rp.dve(lambda e: e.tensor_scalar(out=d["kbg"][:], in0=kk[:], scalar1=cols[:, 1:2], scalar2=None, op0=ALU.mult),
              reads=[kkk, K(ci, "cols")], writes=[K(ci, "kbg")])
        for cc_ in range(2):
            Rp.dve(lambda e, cc_=cc_: e.tensor_scalar(out=d["kd"][lb][cc_][:], in0=kk[:], scalar1=cols[:, 2:3],
                                                      scalar2=gmask[:, 0, cc_ * 64 + 63:cc_ * 64 + 64], op0=ALU.mult, op1=ALU.mult),
                   reads=[kkk, K(ci, "cols"), "gmask"], writes=[K(ci, "kd" + str(lb))])
        Rp.dve(lambda e: e.tensor_tensor(out=d["qgT"][lb][:], in0=qT[:], in1=d["erow"][lb][:], op=ALU.mult),
              reads=[kqT, K(ci, "erow" + str(lb))], writes=[K(ci, "qgT" + str(lb))])
        qb4, _ = Qb(ci, 0)
        Rp.pe(lambda e: e.transpose(out=qb4, in_=d["X"][:], identity=W["ident"][:]), reads=[K(ci, "X"), "ident"], writes=[k0])
        Rp.act(lambda e: e.copy(out=d["XT"][:], in_=qb4), reads=[k0], writes=[K(ci, "XT")])
        Rp.dve(lambda e: e.tensor_tensor(out=d["Za"][:], in0=d["X"][:], in1=identf, op=ALU.add),
              reads=[K(ci, "X"), "gmask"], writes=[K(ci, "Za")])
        Pc, PcT, kPc, kPcT = d["X"], d["XT"], K(ci, "X"), K(ci, "XT")
        Zc, kZc = d["Za"], K(ci, "Za")
        for lvl in range(5):
            Pn, PnT = (d["Pa"], d["PaT"]) if lvl % 2 == 0 else (d["Pb"], d["PbT"])
            kPn, kPnT = (K(ci, "Pa"), K(ci, "PaT")) if lvl % 2 == 0 else (K(ci, "Pb"), K(ci, "PbT"))
            Zn, kZn = (d["Zb"], K(ci, "Zb")) if lvl % 2 == 0 else (d["Za"], K(ci, "Za"))
            Rp.pe(lambda e, Pc=Pc, PcT=PcT: e.matmul(q1, lhsT=Pc[:], rhs=PcT[:], start=True, stop=True),
                 reads=[kPc, kPcT], writes=[k1])
            Rp.act(lambda e, PnT=PnT: e.copy(out=PnT[:], in_=q1), reads=[k1], writes=[kPnT])
            if lvl < 4:
                Rp.pe(lambda e, Pc=Pc, PcT=PcT: e.matmul(q0, lhsT=PcT[:], rhs=Pc[:], start=True, stop=True),
                     reads=[kPc, kPcT], writes=[k0])
                Rp.act(lambda e, Pn=Pn: e.copy(out=Pn[:], in_=q0), reads=[k0], writes=[kPn])
            Rp.pe(lambda e, PnT=PnT, Zc=Zc: e.matmul(q2, lhsT=PnT[:], rhs=Zc[:], start=True, stop=True),
                 reads=[kPnT, kZc], writes=[k2])
            Rp.dve(lambda e, Zn=Zn, Zc=Zc: e.tensor_tensor(out=Zn[:], in0=q2, in1=Zc[:], op=ALU.add),
                  reads=[k2, kZc], writes=[kZn])
            Pc, PcT, kPc, kPcT = Pn, PnT, kPn, kPnT
            Zc, kZc = Zn, kZn
        Rp.pe(lambda e: e.matmul(q3, lhsT=Zc[:], rhs=d["vb"][:], start=True, stop=True), reads=[kZc, K(ci, "vb")], writes=[k3])
        Rp.act(lambda e: e.copy(out=d["u"][lb][:], in_=q3), reads=[k3], writes=[K(ci, "u" + str(lb))])
        Rp.pe(lambda e: e.matmul(q1, lhsT=d["kbg"][:], rhs=Zc[:], start=True, stop=True), reads=[kZc, K(ci, "kbg")], writes=[k1])
        Rp.act(lambda e: e.activation(out=d["nwT"][lb][:], in_=q1, func=AF.Copy, scale=-1.0), reads=[k1], writes=[K(ci, "nwT" + str(lb))])
        order = [0, 1] if dr == 0 else [1, 0]
        osb = d["osb"][lb]
        for cc in order:
            rs = slice(cc * 64, (cc + 1) * 64)
            Rs.pe(lambda e, rs=rs: e.matmul(q4, lhsT=d["nwT"][lb][:], rhs=d["Sbf"][:], start=True, stop=True),
                 reads=[K(ci, "nwT" + str(lb)), ("Sbf", ci)], writes=[k4])
            Rs.dve(lambda e, rs=rs: e.tensor_tensor(out=d["vn"][rs, :], in0=q4[rs, :], in1=d["u"][lb][rs, :], op=ALU.add),
                  reads=[k4, K(ci, "u" + str(lb))], writes=[K(ci, "vn")])

            def mmo(e, rs=rs):
                e.matmul(q5, lhsT=d["qgT"][lb][:], rhs=d["Sbf"][:], start=True, stop=False)
                return e.matmul(q5, lhsT=d["qkT"][lb][:], rhs=d["vn"][:], start=False, stop=True)
            Rs.pe(mmo, reads=[K(ci, "qgT" + str(lb)), K(ci, "qkT" + str(lb)), K(ci, "vn"), ("Sbf", ci)], writes=[k5])
            Rs.pe(lambda e, cc=cc: e.matmul(q7, lhsT=d["kd"][lb][cc][:], rhs=d["vn"][:], start=True, stop=True),
                 reads=[K(ci, "kd" + str(lb)), K(ci, "vn")], writes=[k7])
            dcol = (cc * 64 + 63) if dr == 0 else (cc * 64)
            Rs.dve(lambda e, dcol=dcol: e.scalar_tensor_tensor(out=d["S32"][:], in0=d["S32"][:], scalar=d["erow"][lb][:, dcol:dcol + 1],
                                                              in1=q7, op0=ALU.mult, op1=ALU.add),
                  reads=[("S32", ci), K(ci, "erow" + str(lb)), k7], writes=[("S32", ci)])
            Rs.act(lambda e: e.copy(out=d["Sbf"][:], in_=d["S32"][:]), reads=[("S32", ci)], writes=[("Sbf", ci)])
            Rs.act(lambda e, rs=rs: e.copy(out=osb[rs, :], in_=q5[rs, :]), reads=[k5], writes=[K(ci, "osb" + str(lb))])
        Rs.dma(o_s.ap()[dr][h][t * 128:(t + 1) * 128, :], osb[:], reads=[K(ci, "osb" + str(lb))], writes=[("o", dr, h, t)])

    def phase2(hp):
        for ci in range(4):
            P.dve(lambda e, ci=ci: e.memset(st[ci]["S32"][:], 0.0), writes=[("S32", ci)])
            P.dve(lambda e, ci=ci: e.memset(st[ci]["Sbf"][:], 0.0), writes=[("Sbf", ci)])
            P.dve(lambda e, ci=ci: e.memset(st[ci]["vn"][:], 0.0), writes=[("vn", "c", ci)])
        if 2 in phases:
            pres, scans = {}, {}
            for s in range(nsteps):
                pres[s] = [Lazy(P) for _ in range(4)]
                scans[s] = [Lazy(P) for _ in range(4)]
                for ci in range(4):
                    chain_tile(ci, s, pres[s][ci], scans[s][ci])
            interleave(pres[0])
            for s in range(nsteps):
                interleave(scans[s] + (pres[s + 1] if s + 1 < nsteps else []))

    if DBG.get("dump"):
        gd_ = P.dram("gates_o", [128, NTILE, 8], F32, "ExternalOutput")
        P.dma(gd_.ap(), gates[:], reads=[("gates", t_) for t_ in range(NTILE)])
    NS3 = 4
    of = [P.sb([128, 2, 128], F32) for _ in range(NS3)]
    obk = [P.sb([128, 2, 128], F32) for _ in range(NS3)]
    zt = [P.sb([128, 256], BF16) for _ in range(NS3)]
    gz = [P.sb([128, 256], F32) for _ in range(NS3)]
    osum = [P.sb([128, 2, 128], F32) for _ in range(NS3)]
    jk = [P.sb([128, 128], F32) for _ in range(NS3)]
    s3 = [P.sb([128, 8], F32) for _ in range(NS3)]
    yo = [P.sb([128, 256], F32) for _ in range(NS3)]

    def phase3_tile(R, hp, t, b):
        for h in range(2):
            R.dma(of[b][:, h, :], o_s.ap()[0][h][t * 128:(t + 1) * 128, :], reads=[("o", 0, h, t)], writes=[("of", b)])
            R.dma(obk[b][:, h, :], o_s.ap()[1][h][t * 128:(t + 1) * 128, :], reads=[("o", 1, h, t)], writes=[("obk", b)])
        R.dma(zt[b][:], z_s.ap()[t * 128:(t + 1) * 128, :], reads=[("z", t)], writes=[("zt", b)])
        R.dve(lambda e: e.tensor_tensor(out=osum[b][:], in0=of[b][:], in1=obk[b][:], op=ALU.add),
              reads=[("of", b), ("obk", b)], writes=[("osum", b)])
        R.dve(lambda e: e.tensor_tensor(out=gz[b][:], in0=zt[b][:], in1=onorm[:].rearrange("p h d -> p (h d)"), op=ALU.mult),
              reads=[("zt", b), "onorm"], writes=[("gz", b)])
        for h in range(2):
            R.act(lambda e, h=h: e.activation(out=jk[b][:], in_=osum[b][:, h, :], func=AF.Square, accum_out=s3[b][:, h:h + 1]),
                  reads=[("osum", b)], writes=[("jk", b), ("s3", b)])
        R.act(lambda e: e.activation(out=s3[b][:, 2:4], in_=s3[b][:, 0:2], func=AF.Sqrt, scale=1.0 / 128.0, bias=W["epsc"][:, 0:1]),
              reads=[("s3", b), "epsc"], writes=[("s3", b)])
        R.dve(lambda e: e.reciprocal(out=s3[b][:, 4:6], in_=s3[b][:, 2:4]), reads=[("s3", b)], writes=[("s3", b)])
        for h in range(2):
            R.dve(lambda e, h=h: e.scalar_tensor_tensor(out=yo[b][:, h * 128:(h + 1) * 128], in0=osum[b][:, h, :],
                                                        scalar=s3[b][:, 4 + h:5 + h], in1=gz[b][:, h * 128:(h + 1) * 128],
                                                        op0=ALU.mult, op1=ALU.mult),
                  reads=[("osum", b), ("s3", b), ("gz", b)], writes=[("yo", b)])
        if fusedm:
            R.dma(y_d.ap()[t * 128:(t + 1) * 128, hp * 256:(hp + 1) * 256], yo[b][:], reads=[("yo", b)], writes=[("gdn_o", t)])
        else:
            R.dma(y_d.ap()[t * 128:(t + 1) * 128, :], yo[b][:], reads=[("yo", b)])

    def phase3(hp):
        if 3 not in phases:
            return
        for t0 in range(0, NTILE, NS3):
            Ls = [Lazy(P) for _ in range(NS3)]
            for i in range(NS3):
                phase3_tile(Ls[i], hp, t0 + i, i)
            interleave(Ls)

    for hp in range(nhp):
        load_weights(hp)
        phase1(hp)
        phase2(hp)
        phase3(hp)
    if standalone:
        return P.build()
    P.pop_scope()


def gdn_masks():
    m = np.zeros((5, 128, 128), np.float32)
    kp = np.arange(128)[:, None]
    c = np.arange(128)[None, :]
    same = (kp // 64) == (c // 64)
    m[0] = (same & (kp <= c))
    m[1] = (same & (kp < c))
    m[2] = (same & (kp >= c))
    m[3] = (same & (kp > c))
    m[4] = np.eye(128)
    return m


def gdn_weights(hp, c_w_in, c_conv, a_log_f, dtb_f, a_log_b, dtb_b):
    w = c_w_in[0]
    hs = [2 * hp, 2 * hp + 1]
    cols = []
    for base in (0, 1024, 2048):
        for h in hs:
            cols.append(w[:, base + h * 128: base + (h + 1) * 128])
    wf = np.ascontiguousarray(np.concatenate(cols, axis=1))
    zc = [w[:, 3072 + h * 128:3072 + (h + 1) * 128] for h in hs]
    gb = 4096
    gcols = [w[:, gb + 0 + h:gb + 0 + h + 1] for h in hs] + [w[:, gb + 16 + h:gb + 16 + h + 1] for h in hs] + \
            [w[:, gb + 8 + h:gb + 8 + h + 1] for h in hs] + [w[:, gb + 24 + h:gb + 24 + h + 1] for h in hs]
    wt = np.ascontiguousarray(np.concatenate(zc + gcols, axis=1))
    cw = np.zeros((128, 6, 5), np.float32)
    bi = 0
    for base in (0, 1024, 2048):
        for h in hs:
            cw[:, bi, :] = c_conv[0][:, base + h * 128: base + (h + 1) * 128].T
            bi += 1
    gconst = np.array([dtb_f[0][hs[0]], dtb_f[0][hs[1]], dtb_b[0][hs[0]], dtb_b[0][hs[1]],
                       a_log_f[0][hs[0]], a_log_f[0][hs[1]], a_log_b[0][hs[0]], a_log_b[0][hs[1]]], np.float32)
    return wf, wt, cw, gconst


def gdn_inputs(x2, c_norm, c_w_in, c_conv, a_log_f, dtb_f, a_log_b, dtb_b, out_norm):
    import ml_dtypes
    w = c_w_in[0]
    ident = np.eye(128, dtype=np.float32).astype(ml_dtypes.bfloat16)
    masks = gdn_masks()
    maps = []
    for c in range(NCORES):
        b, hp = c // 4, c % 4
        hs = [2 * hp, 2 * hp + 1]
        cols = []
        for base in (0, 1024, 2048):
            for h in hs:
                cols.append(w[:, base + h * 128: base + (h + 1) * 128])
        wf = np.ascontiguousarray(np.concatenate(cols, axis=1))
        zc = [w[:, 3072 + h * 128:3072 + (h + 1) * 128] for h in hs]
        gb = 4096
        gcols = [w[:, gb + 0 + h:gb + 0 + h + 1] for h in hs] + [w[:, gb + 16 + h:gb + 16 + h + 1] for h in hs] + \
                [w[:, gb + 8 + h:gb + 8 + h + 1] for h in hs] + [w[:, gb + 24 + h:gb + 24 + h + 1] for h in hs]
        wt = np.ascontiguousarray(np.concatenate(zc + gcols, axis=1))
        cw = np.zeros((128, 6, 5), np.float32)
        bi = 0
        for base in (0, 1024, 2048):
            for h in hs:
                cw[:, bi, :] = c_conv[0][:, base + h * 128: base + (h + 1) * 128].T
                bi += 1
        gconst = np.array([[dtb_f[0][hs[0]], dtb_f[0][hs[1]], dtb_b[0][hs[0]], dtb_b[0][hs[1]],
                            a_log_f[0][hs[0]], a_log_f[0][hs[1]], a_log_b[0][hs[0]], a_log_b[0][hs[1]]]], np.float32)
        xp = np.zeros((SEQ + 128, DM), np.float32)
        xp[2:2 + SEQ] = x2[b]
        maps.append({"xp": xp, "g": c_norm[0:1], "wf": wf[None], "wt": wt[None], "cw": cw[None], "gconst": gconst,
                     "onorm": out_norm[0:1], "gmask": masks, "ident": ident})
    return maps


def build_l0():
    P = Prog()
    x1_s = P.dram("x1_s", [NTOK, DM], F32, "Internal")
    build_attn(P, "a_", x1_s)
    P.fence()
    build_ffn(1, False, P, "f_", x1_s)
    return P.build()


def build_fused():
    P = Prog()
    x1_s = P.dram("x1_s", [SEQ, DM], F32, "Internal")
    x2_s = P.dram("x2_s", [SEQ, DM], F32, "Internal")
    og_s = P.dram("og_s", [SEQ, DM], F32, "Internal")
    qoff = P.dram("m_sel", [1, 4], F32, "ExternalInput")
    build_attn(P, "a_", x1_s, nq=4)
    P.fence()
    H = ffn_handles(P, "f_", 1, False)
    for q in range(4):
        build_ffn(1, False, P, "f_", x_handle=x1_s, H=H, x_row0=q * NTOK, y_handle=x2_s, y_row0=q * NTOK)
    P.fence()
    build_gdn(P=P, pf="g_", x_handle=x2_s, o_handle=og_s, nhp=4)
    P.fence()
    build_ffn(8, True, P, "m_", x_handle=x2_s, o_handle=og_s, dyn=qoff)
    return P.build()


def kernel_fused(x, ab_norm, ab_w_in, ab_q_norm_a, ab_k_norm_a, ab_sink_a, ab_q_norm_b, ab_k_norm_b,
                 ab_w_out, ffn_norm, ffn_w_gate, ffn_w_up, ffn_w_down, c_norm, c_w_in, c_conv,
                 c_a_log_fwd, c_dt_bias_fwd, c_a_log_bwd, c_dt_bias_bwd, c_out_norm, c_w_out,
                 moe_norm, moe_w_router, moe_w_gate, moe_w_up, moe_w_down):
    f = lambda a: np.ascontiguousarray(np.asarray(a, dtype=np.float32))
    x = f(x)
    cores = list(range(NCORES))
    ident = _ident_bf16()
    identf = np.eye(128, dtype=np.float32)
    amaps = attn_inputs(x, f(ab_norm), f(ab_w_in), f(ab_q_norm_a), f(ab_k_norm_a), f(ab_sink_a), f(ab_q_norm_b),
                        f(ab_k_norm_b), f(ab_w_out), fused=True)
    gw = [gdn_weights(hp, f(c_w_in), f(c_conv), f(c_a_log_fwd), f(c_dt_bias_fwd), f(c_a_log_bwd), f(c_dt_bias_bwd))
          for hp in range(4)]
    g_wf = np.ascontiguousarray(np.stack([w[0] for w in gw]))
    g_wt = np.ascontiguousarray(np.stack([w[1] for w in gw]))
    g_cw = np.ascontiguousarray(np.stack([w[2] for w in gw]))
    g_gc = np.ascontiguousarray(np.stack([w[3] for w in gw]))
    gmask = gdn_masks()
    maps = []
    for c in cores:
        m = {"a_" + k: v for k, v in amaps[c].items()}
        m.update({"f_g": f(ffn_norm)[0:1], "f_wg": f(ffn_w_gate), "f_wu": f(ffn_w_up), "f_wd": f(ffn_w_down), "f_ident": ident})
        m.update({"g_g": f(c_norm)[0:1], "g_wf": g_wf, "g_wt": g_wt, "g_cw": g_cw, "g_gconst": g_gc,
                  "g_onorm": f(c_out_norm)[0:1], "g_gmask": gmask, "g_ident": ident})
        m.update({"m_wo": f(c_w_out)[0], "m_g": f(moe_norm)[0:1], "m_wg": f(moe_w_gate)[0], "m_wu": f(moe_w_up)[0],
                  "m_wd": f(moe_w_down)[0], "m_wr": f(moe_w_router)[0], "m_ident": ident, "m_identf": identf,
                  "m_sel": np.eye(4, dtype=np.float32)[c % 4][None, :]})
        maps.append(m)
    res = run_bass_kernel_spmd(_prog("fused", build_fused), maps, core_ids=cores)
    y = np.concatenate([r["m_y"] for r in res.results], 0)
    return y.reshape(2, SEQ, DM).astype(np.float32)


_CACHE = {}
STAGES = {}


def _prog(name, fn):
    if name not in _CACHE:
        _CACHE[name] = fn()
    return _CACHE[name]


def _ident_bf16():
    import ml_dtypes
    return np.eye(128, dtype=np.float32).astype(ml_dtypes.bfloat16)


def kernel(x, ab_norm, ab_w_in, ab_q_norm_a, ab_k_norm_a, ab_sink_a, ab_q_norm_b, ab_k_norm_b,
           ab_w_out, ffn_norm, ffn_w_gate, ffn_w_up, ffn_w_down, c_norm, c_w_in, c_conv,
           c_a_log_fwd, c_dt_bias_fwd, c_a_log_bwd, c_dt_bias_bwd, c_out_norm, c_w_out,
           moe_norm, moe_w_router, moe_w_gate, moe_w_up, moe_w_down):
    f = lambda a: np.ascontiguousarray(np.asarray(a, dtype=np.float32))
    x = f(x)
    cores = list(range(NCORES))
    ident = _ident_bf16()
    identf = np.eye(128, dtype=np.float32)
    amaps = attn_inputs(x, f(ab_norm), f(ab_w_in), f(ab_q_norm_a), f(ab_k_norm_a), f(ab_sink_a), f(ab_q_norm_b),
                        f(ab_k_norm_b), f(ab_w_out))
    maps = []
    for c in cores:
        m = {"a_" + k: v for k, v in amaps[c].items()}
        m.update({"f_g": f(ffn_norm)[0:1], "f_wg": f(ffn_w_gate), "f_wu": f(ffn_w_up), "f_wd": f(ffn_w_down), "f_ident": ident})
        maps.append(m)
    res = run_bass_kernel_spmd(_prog("l0", build_l0), maps, core_ids=cores)
    x2 = np.concatenate([r["f_y"] for r in res.results], 0)
    STAGES["x2"] = x2
    maps = gdn_inputs(x2.reshape(2, SEQ, DM), f(c_norm), f(c_w_in), f(c_conv), f(c_a_log_fwd), f(c_dt_bias_fwd),
                      f(c_a_log_bwd), f(c_dt_bias_bwd), f(c_out_norm))
    res = run_bass_kernel_spmd(_prog("gdn", build_gdn), maps, core_ids=cores)
    o = np.zeros((2, SEQ, DM), np.float32)
    for c in cores:
        b, hp = c // 4, c % 4
        o[b][:, hp * 256:(hp + 1) * 256] = res.results[c]["y"]
    o = o.reshape(-1, DM)
    STAGES["o"] = o
    maps = [{"x": np.ascontiguousarray(x2[c * NTOK:(c + 1) * NTOK]), "o": np.ascontiguousarray(o[c * NTOK:(c + 1) * NTOK]),
             "wo": f(c_w_out)[0], "g": f(moe_norm)[0:1], "wg": f(moe_w_gate)[0], "wu": f(moe_w_up)[0], "wd": f(moe_w_down)[0],
             "wr": f(moe_w_router)[0], "ident": ident, "identf": identf} for c in cores]
    res = run_bass_kernel_spmd(_prog("moe", lambda: build_ffn(8, True)), maps, core_ids=cores)
    y = np.concatenate([r["y"] for r in res.results], 0)
    return y.reshape(2, SEQ, DM).astype(np.float32)
```
